# Optimizing a Trainium2 kernel written in Bass

```python
import jax
import jax.numpy as jnp
from jax import lax
import numpy as np

D_MODEL = 1024
BATCH = 8
SEQ = 8192
DEPTH = 1

ROPE_THETA = 500000.0
NORM_EPS = 1e-6
MLA_HEADS = 8
MLA_NOPE = 64
MLA_ROPE = 32
MLA_V = 64
KV_LORA = 4 * MLA_V
Q_LORA = 3 * KV_LORA
ATTN_Q_BLOCK = 128
MOBA_HEADS = 8
MOBA_HD = 64
MOBA_ROT = MOBA_HD // 4
MOBA_BLOCK = 256
MOBA_TOPK = 3
MOBA_Q_CHUNK = 32
N_GROUPS = 4
EXPERTS_PER_GROUP = 8
N_EXPERTS = N_GROUPS * EXPERTS_PER_GROUP
EXPERT_TOPK = 2
EXPERT_FF = 256
EXPERT_BLOCK = 256
IN_SPLITS = (Q_LORA, KV_LORA, MLA_ROPE,
             MOBA_HEADS * MOBA_HD, MOBA_HEADS * MOBA_HD, MOBA_HEADS * MOBA_HD,
             D_MODEL, D_MODEL)
IN_WIDTH = sum(IN_SPLITS)

kernel_name = 'hybrid_mla_moba_hmoe_block'


def rmsnorm(x, g):
    xf = x.astype(jnp.float32)
    y = xf * lax.rsqrt(jnp.mean(xf * xf, axis=-1, keepdims=True) + NORM_EPS)
    return (y * g.astype(jnp.float32)).astype(x.dtype)


def rope_tables(seq, rot_dim):
    half = rot_dim // 2
    inv_freq = jnp.power(ROPE_THETA, -jnp.arange(half, dtype=jnp.float32) / half)
    ang = jnp.arange(seq, dtype=jnp.float32)[:, None] * inv_freq[None, :]
    return jnp.cos(ang), jnp.sin(ang)


def apply_rope(x, cos, sin):
    half = x.shape[-1] // 2
    xf = x.astype(jnp.float32)
    x1, x2 = xf[..., :half], xf[..., half:]
    c = cos[None, :, None, :]
    s = sin[None, :, None, :]
    return jnp.concatenate([x1 * c - x2 * s, x2 * c + x1 * s], axis=-1).astype(x.dtype)


def causal_dense_attention(q, k, v):
    B, S, H, Dq = q.shape
    Dv = v.shape[-1]
    scale = Dq ** -0.5
    nqb = S // ATTN_Q_BLOCK
    q_blocks = q.reshape(B, nqb, ATTN_Q_BLOCK, H, Dq).transpose(1, 0, 2, 3, 4)
    kpos = jnp.arange(S)

    def block(args):
        qi, i = args
        s = jnp.einsum('bqhd,bkhd->bhqk', qi, k).astype(jnp.float32) * scale
        qpos = i * ATTN_Q_BLOCK + jnp.arange(ATTN_Q_BLOCK)
        s = jnp.where(kpos[None, :] <= qpos[:, None], s, -jnp.inf)
        p = jax.nn.softmax(s, axis=-1).astype(v.dtype)
        return jnp.einsum('bhqk,bkhd->bqhd', p, v)

    o = lax.map(block, (q_blocks, jnp.arange(nqb)))
    return o.transpose(1, 0, 2, 3, 4).reshape(B, S, H * Dv)


def mla_attention(c_q, c_kv, k_r, q_norm, w_uq, kv_norm, w_ukv, cos, sin):
    B, S, _ = c_q.shape
    q = (rmsnorm(c_q, q_norm) @ w_uq).reshape(B, S, MLA_HEADS, MLA_NOPE + MLA_ROPE)
    q = jnp.concatenate([q[..., :MLA_NOPE], apply_rope(q[..., MLA_NOPE:], cos, sin)], axis=-1)
    kv = (rmsnorm(c_kv, kv_norm) @ w_ukv).reshape(B, S, MLA_HEADS, MLA_NOPE + MLA_V)
    k_nope, v = kv[..., :MLA_NOPE], kv[..., MLA_NOPE:]
    k_rope = apply_rope(k_r[:, :, None, :], cos, sin)
    k = jnp.concatenate([k_nope, jnp.broadcast_to(k_rope, (B, S, MLA_HEADS, MLA_ROPE))], axis=-1)
    return causal_dense_attention(q, k, v)


def moba_attention(q, k, v, cos, sin):
    B, S, H, dh = q.shape
    scale = dh ** -0.5
    q = jnp.concatenate([apply_rope(q[..., :MOBA_ROT], cos, sin), q[..., MOBA_ROT:]], axis=-1)
    k = jnp.concatenate([apply_rope(k[..., :MOBA_ROT], cos, sin), k[..., MOBA_ROT:]], axis=-1)
    nb = -(-S // MOBA_BLOCK)
    pad = nb * MOBA_BLOCK - S
    kp = jnp.pad(k, ((0, 0), (0, pad), (0, 0), (0, 0)))
    vp = jnp.pad(v, ((0, 0), (0, pad), (0, 0), (0, 0)))
    k_blocks = kp.reshape(B, nb, MOBA_BLOCK, H, dh).transpose(0, 3, 1, 2, 4)
    v_blocks = vp.reshape(B, nb, MOBA_BLOCK, H, dh).transpose(0, 3, 1, 2, 4)
    k_mean = jnp.mean(k_blocks.astype(jnp.float32), axis=3)
    n_sel = min(MOBA_TOPK, nb)
    gather = jax.vmap(jax.vmap(lambda blocks, idx: blocks[idx]))
    nqc = S // MOBA_Q_CHUNK
    q_chunks = q.reshape(B, nqc, MOBA_Q_CHUNK, H, dh).transpose(1, 0, 3, 2, 4)
    blk_ids = jnp.arange(nb)
    sel_ids = jnp.arange(n_sel)

    def chunk(args):
        qc, c = args
        q0 = c * MOBA_Q_CHUNK
        own = q0 // MOBA_BLOCK
        qpos = q0 + jnp.arange(MOBA_Q_CHUNK)
        gate = jnp.einsum('bhqd,bhnd->bhqn', qc.astype(jnp.float32), k_mean)
        gate = jnp.where(blk_ids < own, gate, -jnp.inf)
        _, idx = lax.top_k(gate, n_sel)
        valid = sel_ids < own
        k_sel = gather(k_blocks, idx)
        v_sel = gather(v_blocks, idx)
        s_sel = jnp.einsum('bhqd,bhqjkd->bhqjk', qc, k_sel).astype(jnp.float32) * scale
        s_sel = jnp.where(valid[:, None], s_sel, -jnp.inf).reshape(B, H, MOBA_Q_CHUNK, n_sel * MOBA_BLOCK)
        k_own = lax.dynamic_index_in_dim(k_blocks, own, axis=2, keepdims=False)
        v_own = lax.dynamic_index_in_dim(v_blocks, own, axis=2, keepdims=False)
        kpos = own * MOBA_BLOCK + jnp.arange(MOBA_BLOCK)
        s_own = jnp.einsum('bhqd,bhkd->bhqk', qc, k_own).astype(jnp.float32) * scale
        s_own = jnp.where(kpos[None, :] <= qpos[:, None], s_own, -jnp.inf)
        p = jax.nn.softmax(jnp.concatenate([s_own, s_sel], axis=-1), axis=-1).astype(v.dtype)
        p_own = p[..., :MOBA_BLOCK]
        p_sel = p[..., MOBA_BLOCK:].reshape(B, H, MOBA_Q_CHUNK, n_sel, MOBA_BLOCK)
        return (jnp.einsum('bhqk,bhkd->bhqd', p_own, v_own)
                + jnp.einsum('bhqjk,bhqjkd->bhqd', p_sel, v_sel))

    o = lax.map(chunk, (q_chunks, jnp.arange(nqc)))
    return o.transpose(1, 0, 3, 2, 4).reshape(B, S, H * dh)


def hierarchical_moe(h, w_rg, b_rg, w_re, b_re, w_g, w_u, w_d):
    B, S, D = h.shape
    T = B * S
    hf = h.reshape(T, D)
    g_logits = (hf @ w_rg + b_rg).astype(jnp.float32)
    g_sel = jnp.argmax(g_logits, axis=-1).astype(jnp.int32)
    p_group = jnp.take_along_axis(jax.nn.softmax(g_logits, axis=-1), g_sel[:, None], axis=1)
    e_logits = (jnp.einsum('td,gde->tge', hf, w_re) + b_re).astype(jnp.float32)
    e_in = jnp.take_along_axis(e_logits, g_sel[:, None, None], axis=1)[:, 0]
    top_vals, top_idx = lax.top_k(e_in, EXPERT_TOPK)
    weight = p_group * jax.nn.softmax(top_vals, axis=-1)
    expert_id = g_sel[:, None] * EXPERTS_PER_GROUP + top_idx.astype(jnp.int32)

    A = T * EXPERT_TOPK
    eid = expert_id.reshape(A)
    w_flat = weight.reshape(A)
    tok = jnp.repeat(jnp.arange(T, dtype=jnp.int32), EXPERT_TOPK)
    order = jnp.argsort(eid, stable=True)
    se = eid[order]
    counts = jnp.bincount(eid, length=N_EXPERTS).astype(jnp.int32)
    padded = (counts + EXPERT_BLOCK - 1) // EXPERT_BLOCK * EXPERT_BLOCK
    start = jnp.cumsum(counts) - counts
    pend = jnp.cumsum(padded)
    pstart = pend - padded
    dest = pstart[se] + jnp.arange(A, dtype=jnp.int32) - start[se]
    n_slots = (-(-A // EXPERT_BLOCK) + N_EXPERTS) * EXPERT_BLOCK
    slot_tok = jnp.zeros((n_slots,), jnp.int32).at[dest].set(tok[order])
    slot_w = jnp.zeros((n_slots,), h.dtype).at[dest].set(w_flat[order].astype(h.dtype))
    n_blocks = n_slots // EXPERT_BLOCK
    blk_e = jnp.minimum(jnp.searchsorted(pend, jnp.arange(n_blocks, dtype=jnp.int32) * EXPERT_BLOCK,
                                         side='right'), N_EXPERTS - 1)
    xs = hf[slot_tok].reshape(n_blocks, EXPERT_BLOCK, D)

    def expert_block(args):
        xb, e = args
        return (jax.nn.silu(xb @ w_g[e]) * (xb @ w_u[e])) @ w_d[e]

    ys = lax.map(expert_block, (xs, blk_e)).reshape(n_slots, D)
    out = jnp.zeros((T, D), h.dtype).at[slot_tok].add(ys * slot_w[:, None])
    return out.reshape(B, S, D)


def setup_inputs(seed: int = 0) -> dict:
    key = jax.random.key(seed)
    ks = jax.random.split(key, 20)
    f32 = jnp.float32
    L = DEPTH

    def dense(k, shape, fan_in):
        return jax.random.normal(k, shape, f32) * fan_in ** -0.5

    def gain(k, shape):
        return 1.0 + 0.02 * jax.random.normal(k, shape, f32)

    return {
        'x': jax.random.normal(ks[0], (BATCH, SEQ, D_MODEL), f32),
        'attn_norm': gain(ks[1], (L, D_MODEL)),
        'w_in': dense(ks[2], (L, D_MODEL, IN_WIDTH), D_MODEL),
        'q_norm': gain(ks[3], (L, Q_LORA)),
        'w_uq': dense(ks[4], (L, Q_LORA, MLA_HEADS * (MLA_NOPE + MLA_ROPE)), Q_LORA),
        'kv_norm': gain(ks[5], (L, KV_LORA)),
        'w_ukv': dense(ks[6], (L, KV_LORA, MLA_HEADS * (MLA_NOPE + MLA_V)), KV_LORA),
        'w_o_mla': dense(ks[7], (L, MLA_HEADS * MLA_V, D_MODEL), MLA_HEADS * MLA_V),
        'w_o_moba': dense(ks[8], (L, MOBA_HEADS * MOBA_HD, D_MODEL), MOBA_HEADS * MOBA_HD),
        'w_out': dense(ks[9], (L, D_MODEL, D_MODEL), D_MODEL),
        'ffn_norm': gain(ks[10], (L, D_MODEL)),
        'w_router_group': dense(ks[11], (L, D_MODEL, N_GROUPS), D_MODEL),
        'b_router_group': 0.01 * jax.random.normal(ks[12], (L, N_GROUPS), f32),
        'w_router_expert': dense(ks[13], (L, N_GROUPS, D_MODEL, EXPERTS_PER_GROUP), D_MODEL),
        'b_router_expert': 0.01 * jax.random.normal(ks[14], (L, N_GROUPS, EXPERTS_PER_GROUP), f32),
        'w_exp_gate': dense(ks[15], (L, N_EXPERTS, D_MODEL, EXPERT_FF), D_MODEL),
        'w_exp_up': dense(ks[16], (L, N_EXPERTS, D_MODEL, EXPERT_FF), D_MODEL),
        'w_exp_down': dense(ks[17], (L, N_EXPERTS, EXPERT_FF, D_MODEL), EXPERT_FF),
        'final_norm': gain(ks[18], (D_MODEL,)),
    }


def reference(x, attn_norm, w_in, q_norm, w_uq, kv_norm, w_ukv, w_o_mla, w_o_moba, w_out,
              ffn_norm, w_router_group, b_router_group, w_router_expert, b_router_expert,
              w_exp_gate, w_exp_up, w_exp_down, final_norm):
    B, S, _ = x.shape
    cos_mla, sin_mla = rope_tables(S, MLA_ROPE)
    cos_moba, sin_moba = rope_tables(S, MOBA_ROT)
    split_at = [int(i) for i in np.cumsum(IN_SPLITS)[:-1]]
    for l in range(DEPTH):
        h = rmsnorm(x, attn_norm[l])
        proj = h @ w_in[l]
        c_q, c_kv, k_r, q_b, k_b, v_b, gate_a, gate_b = jnp.split(proj, split_at, axis=-1)
        o_a = mla_attention(c_q, c_kv, k_r, q_norm[l], w_uq[l], kv_norm[l], w_ukv[l], cos_mla, sin_mla)
        o_b = moba_attention(q_b.reshape(B, S, MOBA_HEADS, MOBA_HD),
                             k_b.reshape(B, S, MOBA_HEADS, MOBA_HD),
                             v_b.reshape(B, S, MOBA_HEADS, MOBA_HD), cos_moba, sin_moba)
        mixed = (jax.nn.sigmoid(gate_a) * (o_a @ w_o_mla[l])
                 + jax.nn.sigmoid(gate_b) * (o_b @ w_o_moba[l]))
        x = x + mixed @ w_out[l]
        x = x + hierarchical_moe(rmsnorm(x, ffn_norm[l]), w_router_group[l], b_router_group[l],
                                 w_router_expert[l], b_router_expert[l],
                                 w_exp_gate[l], w_exp_up[l], w_exp_down[l])
    return rmsnorm(x, final_norm)
```

```python
import numpy as np
from contextlib import ExitStack
import ml_dtypes
import concourse.bass as bass
import concourse.mybir as mybir
from concourse.bass_utils import run_bass_kernel_spmd

F32 = mybir.dt.float32
BF16 = mybir.dt.bfloat16
I32 = mybir.dt.int32
AF = mybir.ActivationFunctionType
ALU = mybir.AluOpType
AX = mybir.AxisListType

SEM_LIMIT = 30000
D = 1024
NE = 32
EPS = 1e-6
ROPE_THETA = 500000.0


class H:
    __slots__ = ("w", "r")

    def __init__(self):
        self.w = None
        self.r = {}


class Q:
    def __init__(self, name, sems):
        self.name = name
        self.sems = sems
        self.epoch = 0
        self.count = 0
        self.ops = []
        self.waited = {}


class Sched:
    def __init__(self, nc, stack, n_dma_sems=72):
        self.nc = nc
        self.semobj = {}
        self.q = {}
        for name, nep in (("pe", 5), ("act", 3), ("dve", 3), ("pool", 3), ("sp", 1)):
            sems = []
            for e in range(nep):
                s = stack.enter_context(nc.semaphore(f"s_{name}{e}"))
                key = f"{name}{e}"
                self.semobj[key] = s
                sems.append(key)
            self.q[name] = Q(name, sems)
        self.dma_sems = []
        for i in range(n_dma_sems):
            s = stack.enter_context(nc.semaphore(f"s_dma{i}"))
            key = f"dma{i}"
            self.semobj[key] = s
            self.dma_sems.append([key, 0])
        self.dma_rr = 0
        self.dma_rr_q = {}
        self.n_ops = 0

    def _deps(self, reads, writes):
        deps = {}

        def add(k, v):
            if v > deps.get(k, -1):
                deps[k] = v
        for h in reads:
            if h.w is not None:
                add(*h.w)
        for h in writes:
            if h.w is not None:
                add(*h.w)
            for k, v in h.r.items():
                add(k, v)
        return deps

    def op(self, qname, fn, reads=(), writes=(), dma=False):
        q = self.q[qname]
        deps = self._deps(reads, writes)
        if dma:
            third = len(self.dma_sems) // 3
            base = {"sp": 0, "pool": third, "act": 2 * third}[qname]
            rr = self.dma_rr_q.get(qname, 0)
            slot = self.dma_sems[base + rr]
            self.dma_rr_q[qname] = (rr + 1) % third
            if slot[1] > 0 and slot[1] > deps.get(slot[0], -1):
                deps[slot[0]] = slot[1]
            assert slot[1] + 16 < 60000
            slot[1] += 16
            comp = (slot[0], slot[1])
            inc = 16
        else:
            if q.count + 1 > SEM_LIMIT:
                q.epoch += 1
                q.count = 0
            q.count += 1
            comp = (q.sems[q.epoch], q.count)
            inc = 1
        waits = []
        for k, v in deps.items():
            if qname == "pe" and k.startswith("pe"):
                continue
            if q.waited.get(k, -1) >= v:
                continue
            q.waited[k] = v
            waits.append((self.semobj[k], v))
        csem = self.semobj[comp[0]]

        def emit(eng, waits=waits, fn=fn, csem=csem, inc=inc):
            for s, v in waits:
                eng.wait_ge(s, v)
            fn(eng).then_inc(csem, inc)
        q.ops.append(emit)
        for h in writes:
            h.w = comp
            h.r = {}
        for h in reads:
            if comp[1] > h.r.get(comp[0], -1):
                h.r[comp[0]] = comp[1]
        self.n_ops += 1
        return comp

    def barrier(self):
        deps = {}
        for q in self.q.values():
            for e in range(q.epoch + 1):
                cnt = q.count if e == q.epoch else SEM_LIMIT
                if cnt > 0:
                    deps[q.sems[e]] = cnt
        for k, v in self.dma_sems:
            if v > 0:
                deps[k] = v
        for q in self.q.values():
            waits = []
            for k, v in deps.items():
                if q.waited.get(k, -1) >= v:
                    continue
                q.waited[k] = v
                waits.append((self.semobj[k], v))

            def emit(eng, waits=waits):
                for s, v in waits:
                    eng.wait_ge(s, v)
            q.ops.append(emit)

    def emit_all(self):
        nc = self.nc
        with nc.Block() as block:
            @block.tensor
            def _(e):
                for f in self.q["pe"].ops:
                    f(e)

            @block.scalar
            def _(e):
                for f in self.q["act"].ops:
                    f(e)

            @block.vector
            def _(e):
                for f in self.q["dve"].ops:
                    f(e)

            @block.gpsimd
            def _(e):
                for f in self.q["pool"].ops:
                    f(e)

            @block.sync
            def _(e):
                for f in self.q["sp"].ops:
                    f(e)


class T:
    def __init__(self, t, n=1):
        self.t = t
        self.h = H()
        self.hs = [H() for _ in range(n)]


def build(S=8192, C=768, stop_after=99, debug=False):
    NG = S // 512
    NT = S // 128
    NSLOT = NE * C
    nc = bass.Bass("TRN2", target_bir_lowering=False)

    def din(name, shape, dt=F32):
        return nc.dram_tensor(name, shape, dt, kind="ExternalInput").ap()

    def dscr(name, shape, dt):
        return nc.dram_tensor(name, shape, dt, kind=("ExternalOutput" if debug else "Internal")).ap()

    x_d = din("x", [S, D])
    xT_d = din("xT", [D, S])
    attn_norm_d = din("attn_norm", [128, 8])
    w_in_d = din("w_in", [D, 4640])
    q_norm_d = din("q_norm", [128, 6])
    w_uq_d = din("w_uq", [768, 768])
    kv_norm_d = din("kv_norm", [128, 2])
    w_ukv_d = din("w_ukv_kv", [256, 1024])
    w_oa_d = din("w_o_mla", [512, D])
    w_ob_d = din("w_o_moba", [512, D])
    w_out_d = din("w_out", [D, D])
    ffn_norm_d = din("ffn_norm", [128, 8])
    w_r_d = din("w_r", [D, 36])
    b_r_d = din("b_r", [1, 36])
    w_g_d = din("w_exp_gate", [NE, D, 256])
    w_u_d = din("w_exp_up", [NE, D, 256])
    w_d_d = din("w_exp_down", [NE, 256, D])
    fnorm_d = din("final_norm", [1, D])
    c_identb = din("c_identb", [128, 128], BF16)
    c_identf = din("c_identf", [128, 128])
    c_onesb = din("c_onesb", [128, 128], BF16)
    c_onesf = din("c_onesf", [128, 128])
    c_perma = din("c_perma", [96, 96], BF16)
    c_permb = din("c_permb", [128, 128], BF16)
    c_tac = din("c_tac", [96, S])
    c_tas = din("c_tas", [96, S])
    c_tbc = din("c_tbc", [128, S])
    c_tbs = din("c_tbs", [128, S])
    c_mask = din("c_mask", [128, 4, 512], BF16)
    c_oh = din("c_oh", [32, S], BF16)
    c_wneg = din("c_wneg", [1, 64], BF16)
    c_offs = din("c_offs", [128, 32])
    c_triu = din("c_triu", [128, 128], BF16)

    out_d = nc.dram_tensor("out", [S, D], F32, kind="ExternalOutput").ap()

    WIN = dscr("WIN", [D, 4640], BF16)
    WUQ = dscr("WUQ", [768, 768], BF16)
    WUKV = dscr("WUKV", [256, 1024], BF16)
    WOA = dscr("WOA", [512, D], BF16)
    WOB = dscr("WOB", [512, D], BF16)
    WOUT = dscr("WOUT", [D, D], BF16)
    HT = dscr("HT", [D, S], BF16)
    QA = dscr("QA", [768, S], BF16)
    KNA = dscr("KNA", [512, S], BF16)
    KRA = dscr("KRA", [32, S], BF16)
    VA = dscr("VA", [S, 512], BF16)
    QB = dscr("QB", [512, S], BF16)
    KB = dscr("KB", [512, S], BF16)
    VB = dscr("VB", [S, 512], BF16)
    MB = dscr("MB", [256, S], BF16)
    OA = dscr("OA", [512, S], BF16)
    OB = dscr("OB", [512, S], BF16)
    X1 = dscr("X1", [S, D], F32)
    XS = dscr("XS", [NSLOT, D], BF16)
    YS = dscr("YS", [NSLOT, D], BF16)

    with ExitStack() as top:
        Sx = Sched(nc, top)
        op = Sx.op

        def mk(st):
            def sb(name, shape, dt, n=1):
                return T(st.enter_context(nc.sbuf_tensor(name, shape, dt)), n)

            def ps(name, shape, dt):
                return T(st.enter_context(nc.psum_tensor(name, shape, dt)))
            return sb, ps

        def load(dst_ap, src_ap, wr, rd=()):
            op("sp", lambda e: e.dma_start(out=dst_ap, in_=src_ap), reads=rd, writes=wr, dma=True)

        store_q = ["pool"]

        def store(dst_ap, src_ap, rd, wr):
            op(store_q[0], lambda e: e.dma_start(out=dst_ap, in_=src_ap), reads=rd, writes=wr, dma=True)

        def mm(out_ap, lhsT, rhs, start, stop, rd, wr):
            op("pe", lambda e: e.matmul(out_ap, lhsT, rhs, start=start, stop=stop), reads=rd, writes=wr)

        hd = {}

        def dh(name):
            if name not in hd:
                hd[name] = H()
            return hd[name]

        sbp, _ = mk(top)
        slot_i = sbp("slot_i", [128, NT, 2], I32, 4)
        wts = sbp("wts", [128, NT, 2], F32, 4)

        with ExitStack() as st:
            sb, ps = mk(st)
            stg = [sb(f"stg{i}", [128, 8, 512], F32) for i in range(2)]
            obf = [sb(f"obf{i}", [128, 8, 512], BF16) for i in range(2)]
            gains = sb("gains", [128, 3, 8], F32)
            load(gains.t[:, 0, :], attn_norm_d, [gains.hs[0]])
            load(gains.t[:, 1, 0:6], q_norm_d, [gains.hs[0]])
            load(gains.t[:, 2, 0:2], kv_norm_d, [gains.hs[0]])
            cnt = [0]

            def conv(src, dst, dname, nk, ncols, gi, cstart=0):
                srcv = src.rearrange("(k p) c -> p k c", p=128)
                dstv = dst.rearrange("(k p) c -> p k c", p=128)
                for c0 in range(cstart, ncols, 512):
                    cw = min(512, ncols - c0)
                    i = cnt[0] % 2
                    cnt[0] += 1
                    a, b = stg[i], obf[i]
                    load(a.t[:, 0:nk, 0:cw], srcv[:, :, c0:c0 + cw], [a.h])
                    for k in range(nk):
                        if gi is None:
                            if k % 2 == 0:
                                op("act", lambda e, a=a, b=b, k=k, cw=cw: e.copy(out=b.t[:, k, 0:cw], in_=a.t[:, k, 0:cw]),
                                   reads=[a.h], writes=[b.hs[0]] if False else [b.h])
                            else:
                                op("dve", lambda e, a=a, b=b, k=k, cw=cw: e.tensor_copy(out=b.t[:, k, 0:cw], in_=a.t[:, k, 0:cw]),
                                   reads=[a.h], writes=[b.h])
                        else:
                            if k % 2 == 0:
                                op("act", lambda e, a=a, b=b, k=k, cw=cw, gi=gi: e.activation(out=b.t[:, k, 0:cw], in_=a.t[:, k, 0:cw], func=AF.Copy, scale=gains.t[:, gi, k:k + 1]),
                                   reads=[a.h, gains.hs[0]], writes=[b.h])
                            else:
                                op("dve", lambda e, a=a, b=b, k=k, cw=cw, gi=gi: e.tensor_scalar_mul(out=b.t[:, k, 0:cw], in0=a.t[:, k, 0:cw], scalar1=gains.t[:, gi, k:k + 1]),
                                   reads=[a.h, gains.hs[0]], writes=[b.h])
                    store(dstv[:, :, c0:c0 + cw], b.t[:, 0:nk, 0:cw], [b.h], [dh(dname)])

            conv(w_in_d, WIN, "WIN", 8, 2592, 0)
            conv(w_uq_d, WUQ, "WUQ", 6, 768, 1)
            conv(w_ukv_d, WUKV, "WUKV", 2, 1024, 2)
            Sx.barrier()
        if stop_after <= 0:
            return _finish(nc, Sx, out_d, S)

        with ExitStack() as st:
            sb, ps = mk(st)
            onesb = sb("onesb", [128, 128], BF16)
            identb = sb("identb", [128, 128], BF16)
            perma = sb("perma", [96, 96], BF16)
            permb = sb("permb", [128, 128], BF16)
            wneg = sb("wneg", [1, 64], BF16)
            load(onesb.t[:], c_onesb, [onesb.h])
            load(identb.t[:], c_identb, [identb.h])
            load(perma.t[:], c_perma, [perma.h])
            load(permb.t[:], c_permb, [permb.h])
            load(wneg.t[:], c_wneg, [wneg.h])
            store_q[0] = "sp"
            NCW = 2592
            win = sb("win", [128, 8, NCW], BF16)
            wuq = sb("wuq", [128, 6, 768], BF16)
            wukv = sb("wukv", [128, 2, 1024], BF16)
            winv = WIN.rearrange("(k p) c -> p k c", p=128)
            for c0 in range(0, NCW, 648):
                load(win.t[:, :, c0:c0 + 648], winv[:, :, c0:c0 + 648], [win.h], [dh("WIN")])
            load(wuq.t[:], WUQ.rearrange("(k p) c -> p k c", p=128), [wuq.h], [dh("WUQ")])
            load(wukv.t[:], WUKV.rearrange("(k p) c -> p k c", p=128), [wukv.h], [dh("WUKV")])
            km = sb("km", [128, 4, 32], BF16)
            op("dve", lambda e: e.memset(km.t[:], 0.0), writes=[km.h])

            xt = [sb(f"xt{i}", [128, 8, 512], F32) for i in range(2)]
            xsq = sb("xsq", [128, 8, 512], BF16)
            hTs = [sb(f"hT{i}", [128, 8, 512], BF16, 8) for i in range(2)]
            rss = [sb(f"rs{i}", [128, 512], F32) for i in range(2)]
            tac = sb("tac", [96, 512], F32)
            tas = sb("tas", [96, 512], F32)
            tbc = sb("tbc", [128, 512], F32)
            tbs = sb("tbs", [128, 512], F32)
            tac2 = sb("tac2", [96, 512], F32)
            tas2 = sb("tas2", [96, 512], F32)
            tbc2 = sb("tbc2", [128, 512], F32)
            tbs2 = sb("tbs2", [128, 512], F32)
            cq = sb("cq", [128, 6, 512], BF16, 6)
            cqsq = sb("cqsq", [128, 6, 512], BF16, 6)
            cqn = sb("cqn", [128, 6, 512], BF16, 6)
            ckv = sb("ckv", [128, 2, 512], BF16, 2)
            ckvsq = sb("ckvsq", [128, 2, 512], BF16, 2)
            ckvn = sb("ckvn", [128, 2, 512], BF16, 2)
            rsq = sb("rsq", [128, 512], F32)
            rskv = sb("rskv", [128, 512], F32)
            NR = 3
            rsb = [sb(f"rsb{i}", [128, 512], BF16) for i in range(NR)]
            t1 = [sb(f"t1_{i}", [128, 512], F32) for i in range(NR)]
            t2 = [sb(f"t2_{i}", [128, 512], F32) for i in range(NR)]
            ro = [sb(f"ro{i}", [128, 512], BF16) for i in range(NR)]
            qbo = [sb(f"qbo{i}", [128, 512], BF16) for i in range(4)]
            evo = [sb(f"evo{i}", [128, 512], BF16) for i in range(3)]
            kms = sb("kms", [128, 2], F32)
            gsbs = [sb(f"gsb{i}", [128, 256], F32) for i in range(4)]
            t8s = [sb(f"t8_{i}", [128, 8, 8], F32, 8) for i in range(4)]
            thrs = [sb(f"thr{i}", [128, 8], F32) for i in range(4)]
            mkfs = [sb(f"mk_f{i}", [128, 256], F32, 8) for i in range(4)]
            mkbs = [sb(f"mkb{i}", [128, 256], BF16) for i in range(4)]
            mT = sb("mT", [128, 2, 512], BF16, 2)
            pm = [ps(f"pm{i}", [128, 512], F32) for i in range(3)]
            pp = [ps(f"pp{i}", [128, 512], F32) for i in range(1)]
            pstat = ps("pstat", [128, 512], F32)
            pgs = [ps(f"pg{i}", [128, 512], F32) for i in range(2)]
            ptr = ps("ptr", [128, 1024], BF16)
            ctr = {"pm": 0, "pp": 0, "r": 0, "ev": 0}

            def nxt(key, n):
                v = ctr[key] % n
                ctr[key] += 1
                return v

            def rope(src_ps, rows, perm, tc_, ts_, out_t=None, after=None):
                i = nxt("r", NR)
                a, b1, b2, o = rsb[i], t1[i], t2[i], (out_t or ro[i])
                op("act", lambda e: e.copy(out=a.t[0:rows, :], in_=src_ps.t[0:rows, :]), reads=[src_ps.h], writes=[a.h])

                def fin():
                    p2 = pp[nxt("pp", 1)]
                    mm(p2.t[0:rows, :], perm.t[0:rows, 0:rows], a.t[0:rows, :], True, True, [perm.h, a.h], [p2.h])
                    op("dve", lambda e: e.tensor_tensor(out=b1.t[0:rows, :], in0=p2.t[0:rows, :], in1=ts_.t[0:rows, :], op=ALU.mult),
                       reads=[p2.h, ts_.h], writes=[b1.h])
                    op("pool", lambda e: e.tensor_tensor(out=b2.t[0:rows, :], in0=a.t[0:rows, :], in1=tc_.t[0:rows, :], op=ALU.mult),
                       reads=[a.h, tc_.h], writes=[b2.h])
                    op("dve", lambda e: e.tensor_tensor(out=o.t[0:rows, :], in0=b1.t[0:rows, :], in1=b2.t[0:rows, :], op=ALU.add),
                       reads=[b1.h, b2.h], writes=[o.h])
                    if after is not None:
                        after(o)
                return fin

            def rstd_from(pst, dst, n):
                op("act", lambda e: e.activation(out=dst.t[:], in_=pst.t[:], func=AF.Ln, scale=1.0 / n, bias=EPS),
                   reads=[pst.h], writes=[dst.h])
                op("act", lambda e: e.activation(out=dst.t[:], in_=dst.t[:], func=AF.Exp, scale=-0.5), reads=[dst.h], writes=[dst.h])

            xTv = xT_d.rearrange("(k p) s -> p k s", p=128)
            HTv = HT.rearrange("(k p) s -> p k s", p=128)
            dq = []

            def defer(fn):
                dq.append(fn)
                while len(dq) > 1:
                    dq.pop(0)()

            def flush():
                while dq:
                    dq.pop(0)()

            gate_pend = []
            tabs = [(tac, tas, tbc, tbs), (tac2, tas2, tbc2, tbs2)]

            sq_done = {}

            def S1a(g):
                x_ = xt[g % 2]
                sq_done[g] = True
                op("act", lambda e: e.activation(out=xsq.t[:], in_=x_.t[:], func=AF.Square), reads=[x_.h], writes=[xsq.h])

            def S1(g):
                tok = slice(g * 512, (g + 1) * 512)
                x_, hT, rs = xt[g % 2], hTs[g % 2], rss[g % 2]
                ta_c, ta_s, tb_c, tb_s = tabs[g % 2]
                if not sq_done.get(g):
                    S1a(g)
                for k in range(8):
                    mm(pstat.t[:], onesb.t[:], xsq.t[:, k, :], k == 0, k == 7, [onesb.h, xsq.h], [pstat.h])
                rstd_from(pstat, rs, 1024.0)
                for k in range(8):
                    op("dve", lambda e, k=k: e.tensor_tensor(out=hT.t[:, k, :], in0=x_.t[:, k, :], in1=rs.t[:], op=ALU.mult),
                       reads=[x_.h, rs.h], writes=[hT.hs[k]])
                store(HTv[:, :, tok], hT.t[:], list(hT.hs), [dh("HT")])

            def S1_load(g):
                tok = slice(g * 512, (g + 1) * 512)
                x_ = xt[g % 2]
                ta_c, ta_s, tb_c, tb_s = tabs[g % 2]
                for dst, src in ((x_, xTv[:, :, tok]), (ta_c, c_tac[:, tok]), (ta_s, c_tas[:, tok]), (tb_c, c_tbc[:, tok]), (tb_s, c_tbs[:, tok])):
                    op("act", lambda e, dst=dst, src=src: e.dma_start(out=dst.t[:], in_=src), writes=[dst.h], dma=True)

            S1_load(0)
            S1(0)
            for g in range(NG):
                tok = slice(g * 512, (g + 1) * 512)
                if g + 1 < NG:
                    S1_load(g + 1)
                hT = hTs[g % 2]
                tac, tas, tbc, tbs = tabs[g % 2]
                for (dst, dsq, nchunk, col0) in ((cq, cqsq, 6, 0), (ckv, ckvsq, 2, 768)):
                    for c in range(nchunk):
                        p = pm[nxt("pm", 3)]
                        for k in range(8):
                            mm(p.t[:], win.t[:, k, col0 + c * 128: col0 + (c + 1) * 128], hT.t[:, k, :], k == 0, k == 7, [win.h, hT.hs[k]], [p.h])
                        op("act", lambda e, p=p, dst=dst, c=c: e.copy(out=dst.t[:, c, :], in_=p.t[:]), reads=[p.h], writes=[dst.hs[c]])
                        op("dve", lambda e, dst=dst, dsq=dsq, c=c: e.tensor_tensor(out=dsq.t[:, c, :], in0=dst.t[:, c, :], in1=dst.t[:, c, :], op=ALU.mult),
                           reads=[dst.hs[c]], writes=[dsq.hs[c]])

                def after_q(j):
                    def f(o):
                        store(QB[j * 128:(j + 1) * 128, tok], o.t[:], [o.h], [dh("QB")])
                    return f

                def after_k(j, g=g):
                    def f(o):
                        store(KB[j * 128:(j + 1) * 128, tok], o.t[:], [o.h], [dh("KB")])
                        op("dve", lambda e: e.tensor_reduce(out=kms.t[:], in_=o.t[:].rearrange("p (b t) -> p b t", t=256), axis=AX.X, op=ALU.add),
                           reads=[o.h], writes=[kms.h])
                        op("act", lambda e: e.activation(out=km.t[:, j, 2 * g:2 * g + 2], in_=kms.t[:], func=AF.Copy, scale=1.0 / 256),
                           reads=[kms.h], writes=[km.h])
                    return f

                for which, col0 in (("q", 1056), ("k", 1568)):
                    for j in range(4):
                        p = pm[nxt("pm", 3)]
                        for k in range(8):
                            mm(p.t[:], win.t[:, k, col0 + j * 128: col0 + (j + 1) * 128], hT.t[:, k, :], k == 0, k == 7, [win.h, hT.hs[k]], [p.h])
                        if which == "q":
                            defer(rope(p, 128, permb, tbc, tbs, out_t=qbo[j], after=after_q(j)))
                        else:
                            defer(rope(p, 128, permb, tbc, tbs, after=after_k(j)))
                while gate_pend:
                    gate_pend.pop(0)()
                if g + 1 < NG:
                    S1a(g + 1)
                for (dst, dsq, dn, nchunk, rr, nn) in ((cq, cqsq, cqn, 6, rsq, 768.0), (ckv, ckvsq, ckvn, 2, rskv, 256.0)):
                    for c in range(nchunk):
                        mm(pstat.t[:], onesb.t[:], dsq.t[:, c, :], c == 0, c == nchunk - 1, [onesb.h, dsq.hs[c]], [pstat.h])
                    rstd_from(pstat, rr, nn)
                    for c in range(nchunk):
                        op("dve", lambda e, dst=dst, dn=dn, c=c, rr=rr: e.tensor_tensor(out=dn.t[:, c, :], in0=dst.t[:, c, :], in1=rr.t[:], op=ALU.mult),
                           reads=[dst.hs[c], rr.h], writes=[dn.hs[c]])
                p = pm[nxt("pm", 3)]
                for k in range(8):
                    mm(p.t[0:96, :], win.t[:, k, 960:1056], hT.t[:, k, :], k == 0, k == 7, [win.h, hT.hs[k]], [p.h])
                defer(rope(p, 96, perma, tac, tas, after=lambda o: store(KRA[:, tok], o.t[64:96, :], [o.h], [dh("KRA")])))
                for tt in range(4):
                    p = pm[nxt("pm", 3)]
                    for k in range(8):
                        mm(p.t[:], hT.t[:, k, tt * 128:(tt + 1) * 128], win.t[:, k, 2080:2592], k == 0, k == 7, [win.h, hT.hs[k]], [p.h])
                    o = evo[nxt("ev", 3)]
                    op("act", lambda e, p=p, o=o: e.copy(out=o.t[:], in_=p.t[:]), reads=[p.h], writes=[o.h])
                    store(VB[g * 512 + tt * 128: g * 512 + (tt + 1) * 128, :], o.t[:], [o.h], [dh("VB")])
                for h in range(8):
                    p = pm[nxt("pm", 3)]
                    for c in range(6):
                        mm(p.t[0:96, :], wuq.t[:, c, h * 96:(h + 1) * 96], cqn.t[:, c, :], c == 0, c == 5, [wuq.h, cqn.hs[c]], [p.h])
                    defer(rope(p, 96, perma, tac, tas, after=(lambda h: (lambda o: store(QA[h * 96:(h + 1) * 96, tok], o.t[0:96, :], [o.h], [dh("QA")])))(h)))
                for j in range(4):
                    p = pm[nxt("pm", 3)]
                    for c in range(2):
                        mm(p.t[:], wukv.t[:, c, j * 128:(j + 1) * 128], ckvn.t[:, c, :], c == 0, c == 1, [wukv.h, ckvn.hs[c]], [p.h])
                    o = evo[nxt("ev", 3)]
                    op("act", lambda e, p=p, o=o: e.copy(out=o.t[:], in_=p.t[:]), reads=[p.h], writes=[o.h])
                    store(KNA[j * 128:(j + 1) * 128, tok], o.t[:], [o.h], [dh("KNA")])
                for tt in range(4):
                    p = pm[nxt("pm", 3)]
                    for c in range(2):
                        mm(p.t[:], ckvn.t[:, c, tt * 128:(tt + 1) * 128], wukv.t[:, c, 512:1024], c == 0, c == 1, [wukv.h, ckvn.hs[c]], [p.h])
                    o = evo[nxt("ev", 3)]
                    op("act", lambda e, p=p, o=o: e.copy(out=o.t[:], in_=p.t[:]), reads=[p.h], writes=[o.h])
                    store(VA[g * 512 + tt * 128: g * 512 + (tt + 1) * 128, :], o.t[:], [o.h], [dh("VA")])
                if g + 1 < NG:
                    S1(g + 1)
                flush()
                for tt in range(4):
                    qt = g * 4 + tt
                    own = qt // 2
                    pg_ = pgs[tt % 2]
                    gsb_ = gsbs[tt]
                    for h in range(8):
                        j, r0 = h // 2, (h % 2) * 64
                        mm(pg_.t[:, h * 32:(h + 1) * 32], qbo[j].t[r0:r0 + 64, tt * 128:(tt + 1) * 128], km.t[r0:r0 + 64, j, :], True, False,
                           [qbo[j].h, km.h], [pg_.h])
                        mm(pg_.t[:, h * 32:(h + 1) * 32], onesb.t[0:1, :], wneg.t[0:1, 32 - own:64 - own], False, True,
                           [onesb.h, wneg.h], [pg_.h])
                    op("act", lambda e, pg_=pg_, gsb_=gsb_: e.copy(out=gsb_.t[:], in_=pg_.t[:, 0:256]), reads=[pg_.h], writes=[gsb_.h])
                for h in range(8):
                    for tt in range(4):
                        gsb_, t8_ = gsbs[tt], t8s[tt]
                        op("dve", lambda e, h=h, gsb_=gsb_, t8_=t8_: e.max(out=t8_.t[:, h, :], in_=gsb_.t[:, h * 32:(h + 1) * 32]), reads=[gsb_.h], writes=[t8_.hs[h]])
                for tt in range(4):
                    t8_, thr_ = t8s[tt], thrs[tt]
                    op("dve", lambda e, t8_=t8_, thr_=thr_: e.tensor_scalar_max(out=thr_.t[:], in0=t8_.t[:, :, 3], scalar1=-1e29), reads=list(t8_.hs), writes=[thr_.h])
                for h in range(8):
                    for tt in range(4):
                        gsb_, thr_, mkf_ = gsbs[tt], thrs[tt], mkfs[tt]
                        op("dve", lambda e, h=h, gsb_=gsb_, thr_=thr_, mkf_=mkf_: e.tensor_scalar(out=mkf_.t[:, h * 32:(h + 1) * 32], in0=gsb_.t[:, h * 32:(h + 1) * 32],
                                                                 scalar1=thr_.t[:, h:h + 1], scalar2=30000.0, op0=ALU.is_ge, op1=ALU.mult),
                           reads=[gsb_.h, thr_.h], writes=[mkf_.hs[h]])
                for tt in range(4):
                    mkf_, mkb_ = mkfs[tt], mkbs[tt]
                    op("dve", lambda e, mkf_=mkf_, mkb_=mkb_: e.tensor_scalar_add(out=mkb_.t[:], in0=mkf_.t[:], scalar1=-30000.0), reads=list(mkf_.hs), writes=[mkb_.h])

                def gate_fin(g=g, tok=tok):
                    for tt in range(4):
                        mkb_ = mkbs[tt]
                        for half in range(2):
                            op("pe", lambda e, half=half, mkb_=mkb_: e.transpose(out=ptr.t[:, half * 128:(half + 1) * 128], in_=mkb_.t[:, half * 128:(half + 1) * 128], identity=identb.t[:]),
                               reads=[mkb_.h, identb.h], writes=[ptr.h])
                        op("act", lambda e, tt=tt: e.copy(out=mT.t[:, :, tt * 128:(tt + 1) * 128], in_=ptr.t[:, 0:256].rearrange("p (a b) -> p a b", a=2)),
                           reads=[ptr.h], writes=[mT.h])
                    for half in range(2):
                        store(MB[half * 128:(half + 1) * 128, tok], mT.t[:, half, :], [mT.h], [dh("MB")])
                gate_pend.append(gate_fin)
            while gate_pend:
                gate_pend.pop(0)()
            store_q[0] = "pool"
            Sx.barrier()
        if stop_after <= 1:
            return _finish(nc, Sx, out_d, S)

        with ExitStack() as st:
            sb, ps = mk(st)
            maskc = sb("maskc", [128, 2, 1024], BF16)
            load(maskc.t[:], c_mask.rearrange("p (a b) c -> p a (b c)", a=2), [maskc.h])
            QT = [sb(f"QT{i}", [96, S], BF16) for i in range(2)]
            KT = [sb(f"KT{i}", [96, S], BF16) for i in range(2)]
            VV = [sb(f"VV{i}", [128, NT, 128], BF16) for i in range(2)]
            for v in VV:
                op("dve", lambda e, v=v: e.memset(v.t[:, :, 64:128], 1.0), writes=[v.h])
            NP = 4
            PT = [sb(f"PT{i}", [128, 1024], BF16) for i in range(NP)]
            rcp = [sb(f"rcp{i}", [128, 512], F32) for i in range(2)]
            rc0 = [sb(f"rc0{i}", [64, 512], F32) for i in range(2)]
            onb = [sb(f"onb{i}", [64, 512], BF16) for i in range(2)]
            psc = [ps(f"psc{i}", [128, 1024], F32) for i in range(3)]
            pov = [ps(f"pov{i}", [128, 512], F32) for i in range(2)]
            VAv = VA.rearrange("(n p) f -> p n f", p=128)
            VBv = VB.rearrange("(n p) f -> p n f", p=128)
            stg3 = [sb(f"stg3{i}", [128, 8, 512], F32) for i in range(2)]
            obf3 = [sb(f"obf3{i}", [128, 8, 512], BF16) for i in range(2)]
            gain3 = sb("gain3", [128, 8], F32)
            load(gain3.t[:], attn_norm_d, [gain3.h])
            cnt3 = [0]
            c3q = []

            def conv3(src, dst, dname, nk, ncols, use_gain, cstart=0):
                srcv = src.rearrange("(k p) c -> p k c", p=128)
                dstv = dst.rearrange("(k p) c -> p k c", p=128)
                for c0 in range(cstart, ncols, 512):
                    def chunk(c0=c0):
                        cw = min(512, ncols - c0)
                        i = cnt3[0] % 2
                        cnt3[0] += 1
                        a_, b_ = stg3[i], obf3[i]
                        load(a_.t[:, 0:nk, 0:cw], srcv[:, :, c0:c0 + cw], [a_.h])
                        for k in range(nk):
                            en = "pool"
                            if use_gain:
                                op(en, lambda e, k=k: e.tensor_scalar_mul(out=b_.t[:, k, 0:cw], in0=a_.t[:, k, 0:cw], scalar1=gain3.t[:, k:k + 1]),
                                   reads=[a_.h, gain3.h], writes=[b_.h])
                            else:
                                op(en, lambda e, k=k: e.tensor_copy(out=b_.t[:, k, 0:cw], in_=a_.t[:, k, 0:cw]),
                                   reads=[a_.h], writes=[b_.h])
                        store(dstv[:, :, c0:c0 + cw], b_.t[:, 0:nk, 0:cw], [b_.h], [dh(dname)])
                    c3q.append(chunk)

            LA = 2
            pend = []
            state = {"ui": 0, "gi": 0}

            def emit_pair(q_, k_, v_, sc, g, kp, nkp, odst, on, h):
                ui = state["ui"]
                state["ui"] += 1
                pscore = psc[ui % 3]
                pt = PT[ui % NP]
                for j in range(2):
                    kt = 2 * kp + j
                    mm(pscore.t[:, j * 512:(j + 1) * 512], k_.t[0:96, kt * 128:(kt + 1) * 128], q_.t[0:96, g * 512:(g + 1) * 512], True, True,
                       [k_.h, q_.h], [pscore.h])
                op("act", lambda e: e.activation(out=pt.t[:], in_=pscore.t[:], func=AF.Exp, scale=sc), reads=[pscore.h], writes=[pt.h])
                r = 2 * kp - 4 * g
                if r >= 0:
                    for j in range(2):
                        wd = (r + j + 1) * 128
                        op("dve", lambda e, j=j, wd=wd: e.tensor_tensor(out=pt.t[:, j * 512:j * 512 + wd], in0=pt.t[:, j * 512:j * 512 + wd],
                                                                      in1=maskc.t[:, r // 2, j * 512:j * 512 + wd], op=ALU.mult),
                           reads=[pt.h, maskc.h], writes=[pt.h])
                first, last = (kp == 0), (kp == nkp - 1)
                if first:
                    state["gi"] += 1
                gi = state["gi"]
                po = pov[gi % 2]

                def pv():
                    for j in range(2):
                        kt = 2 * kp + j
                        mm(po.t[:], v_.t[:, kt, :], pt.t[:, j * 512:(j + 1) * 512], first and j == 0, last and j == 1, [v_.h, pt.h], [po.h])
                    if last:
                        rc, r0, onb_ = rcp[gi % 2], rc0[gi % 2], onb[gi % 2]
                        op("dve", lambda e: e.reciprocal(out=rc.t[64:128, :], in_=po.t[64:128, :]), reads=[po.h], writes=[rc.h])
                        op("dve", lambda e: e.tensor_copy(out=r0.t[0:64, :], in_=rc.t[64:128, :]), reads=[rc.h], writes=[r0.h])
                        op("dve", lambda e: e.tensor_tensor(out=onb_.t[:], in0=po.t[0:64, :], in1=r0.t[0:64, :], op=ALU.mult),
                           reads=[po.h, r0.h], writes=[onb_.h])
                        store(odst[h * 64:(h + 1) * 64, g * 512:(g + 1) * 512], onb_.t[:], [onb_.h], [dh(on)])
                return pv

            for hp in range(16):
                typ, h = hp // 8, hp % 8
                q_, k_, v_ = QT[hp % 2], KT[hp % 2], VV[hp % 2]
                if typ == 0:
                    load(q_.t[0:96, :], QA[h * 96:(h + 1) * 96, :], [q_.h], [dh("QA")])
                    load(k_.t[0:64, :], KNA[h * 64:(h + 1) * 64, :], [k_.h], [dh("KNA")])
                    load(k_.t[64:96, :], KRA[:, :], [k_.h], [dh("KRA")])
                    vsrc, vn, sc, odst, on = VAv, "VA", 96.0 ** -0.5, OA, "OA"
                else:
                    load(q_.t[0:64, :], QB[h * 64:(h + 1) * 64, :], [q_.h], [dh("QB")])
                    load(q_.t[64:96, :], MB[h * 32:(h + 1) * 32, :], [q_.h], [dh("MB")])
                    load(k_.t[0:64, :], KB[h * 64:(h + 1) * 64, :], [k_.h], [dh("KB")])
                    load(k_.t[64:96, :], c_oh[:, :], [k_.h])
                    vsrc, vn, sc, odst, on = VBv, "VB", 64.0 ** -0.5, OB, "OB"
                vstep = max(1, NT // 4)
                for n0 in range(0, NT, vstep):
                    load(v_.t[:, n0:n0 + vstep, 0:64], vsrc[:, n0:n0 + vstep, h * 64:(h + 1) * 64], [v_.h], [dh(vn)])
                if hp == 3:
                    zt = sb("zt", [128, 4, D], BF16)
                    op("pool", lambda e: e.memset(zt.t[:], 0.0), writes=[zt.h])
                    XSv = XS.rearrange("(n p) d -> p n d", p=128)
                    zlist = list(range(0, NSLOT // 128, 4))
                if hp >= 3:
                    nz = -(-len(zlist) // 12) if hp < 15 else len(zlist)
                    for _ in range(min(nz, len(zlist))):
                        n0 = zlist.pop(0)
                        store(XSv[:, n0:n0 + 4, :], zt.t[:], [zt.h], [dh("XS")])
                if hp == 2:
                    conv3(w_in_d, WIN, "WIN", 8, 4640, True, cstart=2592)
                    conv3(w_oa_d, WOA, "WOA", 4, D, False)
                    conv3(w_ob_d, WOB, "WOB", 4, D, False)
                    conv3(w_out_d, WOUT, "WOUT", 8, D, False)
                if hp >= 2 and c3q:
                    c3q.pop(0)()
                for g in range(NG):
                    nkp = 2 * g + 2
                    for kp in range(nkp):
                        pend.append(emit_pair(q_, k_, v_, sc, g, kp, nkp, odst, on, h))
                        if len(pend) > LA:
                            pend.pop(0)()
            while pend:
                pend.pop(0)()
            while c3q:
                c3q.pop(0)()
            Sx.barrier()
        if stop_after <= 3:
            return _finish(nc, Sx, out_d, S)

        with ExitStack() as st:
            sb, ps = mk(st)
            identf = sb("identf", [128, 128], F32)
            onesf = sb("onesf4", [128, 128], F32)
            onesb = sb("onesb4", [128, 128], BF16)
            triu = sb("triu", [128, 128], BF16)
            offs = sb("offs", [128, 32], F32)
            load(identf.t[:], c_identf, [identf.h])
            load(onesf.t[:], c_onesf, [onesf.h])
            load(onesb.t[:], c_onesb, [onesb.h])
            load(triu.t[:], c_triu, [triu.h])
            load(offs.t[:], c_offs, [offs.h])
            wg = sb("wgate", [128, 8, 2048], BF16)
            woa = sb("woa", [128, 4, D], BF16)
            wob = sb("wob", [128, 4, D], BF16)
            wout = sb("wout", [128, 8, D], BF16)
            winv = WIN.rearrange("(k p) c -> p k c", p=128)
            for c0 in range(0, 2048, 512):
                load(wg.t[:, :, c0:c0 + 512], winv[:, :, 2592 + c0:2592 + c0 + 512], [wg.h], [dh("WIN")])
            load(woa.t[:], WOA.rearrange("(k p) c -> p k c", p=128), [woa.h], [dh("WOA")])
            load(wob.t[:], WOB.rearrange("(k p) c -> p k c", p=128), [wob.h], [dh("WOB")])
            for c0 in range(0, D, 512):
                load(wout.t[:, :, c0:c0 + 512], WOUT.rearrange("(k p) c -> p k c", p=128)[:, :, c0:c0 + 512], [wout.h], [dh("WOUT")])
            gf = sb("gf", [128, 8], F32)
            wr_s = sb("wr_s", [128, 8, 36], F32)
            wr = sb("wr", [128, 8, 36], F32)
            br = sb("br", [1, 36], F32)
            load(gf.t[:], ffn_norm_d, [gf.h])
            load(wr_s.t[:], w_r_d.rearrange("(k p) c -> p k c", p=128), [wr_s.h])
            load(br.t[:], b_r_d, [br.h])
            for k in range(8):
                op("dve", lambda e, k=k: e.tensor_scalar_mul(out=wr.t[:, k, :], in0=wr_s.t[:, k, :], scalar1=gf.t[:, k:k + 1]),
                   reads=[wr_s.h, gf.h], writes=[wr.h])
            run = sb("run", [128, 32], F32)
            op("dve", lambda e: e.memset(run.t[:], 0.0), writes=[run.h])

            hTg = sb("hTg", [128, 8, 512], BF16)
            oag = sb("oag", [128, 4, 512], BF16)
            obg = sb("obg", [128, 4, 512], BF16)
            sig = [sb(f"sig{i}", [128, 512], F32) for i in range(2)]
            m1 = [sb(f"m1_{i}", [128, 512], F32) for i in range(2)]
            mixT = sb("mixT", [128, 8, 512], BF16, 8)
            xtok = [sb(f"xtok{i}", [128, D], F32) for i in range(2)]
            x1 = [sb(f"x1_{i}", [128, D], F32) for i in range(2)]
            junk = sb("junk", [128, D], F32)
            pA = [ps(f"pA{i}", [128, 512], F32) for i in range(2)]
            pG = [ps(f"pG{i}", [128, 512], F32) for i in range(2)]
            pY = [ps(f"pY{i}", [128, 512], F32) for i in range(2)]
            pTr = ps("pTr", [128, 512], F32)
            pL = ps("pL", [128, 512], F32)
            hpC = H()
            HTv = HT.rearrange("(k p) s -> p k s", p=128)
            OAv = OA.rearrange("(k p) s -> p k s", p=128)
            OBv = OB.rearrange("(k p) s -> p k s", p=128)
            RS = []
            for i in range(4):
                RS.append(dict(
                    L=sb(f"L{i}", [128, 36], F32), gmax=sb(f"gmax{i}", [128, 1], F32), ngmax=sb(f"ngmax{i}", [128, 1], F32),
                    sume=sb(f"sume{i}", [128, 1], F32), pgrp=sb(f"pgrp{i}", [128, 1], F32), mx1=sb(f"mx1{i}", [128, 1], F32),
                    mx2=sb(f"mx2{i}", [128, 1], F32), dd=sb(f"dd{i}", [128, 1], F32), sg1=sb(f"sg1{i}", [128, 1], F32),
                    gone=sb(f"gone{i}", [128, 4], F32),
                    ein=sb(f"ein{i}", [128, 8], F32), ein2=sb(f"ein2{i}", [128, 8], F32), one1=sb(f"one1{i}", [128, 8], F32),
                    one2=sb(f"one2{i}", [128, 8], F32), ex4=sb(f"ex4{i}", [128, 4], F32), E1=sb(f"E1{i}", [128, 32], F32),
                    E2=sb(f"E2{i}", [128, 32], F32), Ab=sb(f"Ab{i}", [128, 32], BF16), pos=sb(f"pos{i}", [128, 32], F32),
                    tmpa=sb(f"tmpa{i}", [128, 32], F32), tmpb=sb(f"tmpb{i}", [128, 32], F32), slf=sb(f"slf{i}", [128, 2], F32),
                    ss1=sb(f"ss1{i}", [128, 2], F32), hnT=sb(f"hnT{i}", [128, 8, 128], F32)))
            hn4 = [sb(f"hn4{i}", [128, D], F32) for i in range(4)]
            hnb8 = [sb(f"hnb8{i}", [128, D], BF16) for i in range(8)]

            bgq = []

            def bg_run(n):
                for _ in range(n):
                    if bgq:
                        bgq.pop(0)()

            def stageA(ti, tt):
                rows = slice(ti * 128, (ti + 1) * 128)
                xk, x1_, hn_, hnb_ = xtok[ti % 2], x1[ti % 2], hn4[ti % 4], hnb8[ti % 8]
                ss1_ = RS[ti % 4]["ss1"]
                load(xk.t[:], x_d[rows, :], [xk.h])
                for half in range(2):
                    py = pY[half]
                    for k in range(8):
                        mm(py.t[:], mixT.t[:, k, tt * 128:(tt + 1) * 128], wout.t[:, k, half * 512:(half + 1) * 512], k == 0, k == 7,
                           [mixT.hs[k], wout.h], [py.h])
                    op("dve", lambda e, py=py, half=half: e.tensor_tensor(out=x1_.t[:, half * 512:(half + 1) * 512], in0=xk.t[:, half * 512:(half + 1) * 512], in1=py.t[:], op=ALU.add),
                       reads=[py.h, xk.h], writes=[x1_.h])
                op("act", lambda e: e.dma_start(out=X1[rows, :], in_=x1_.t[:]), reads=[x1_.h], writes=[dh("X1")], dma=True)
                op("act", lambda e: e.activation(out=junk.t[:], in_=x1_.t[:], func=AF.Square, accum_out=ss1_.t[:, 0:1]),
                   reads=[x1_.h], writes=[junk.h, ss1_.h])
                op("act", lambda e: e.activation(out=ss1_.t[:, 1:2], in_=ss1_.t[:, 0:1], func=AF.Sqrt, scale=1.0 / D, bias=EPS), reads=[ss1_.h], writes=[ss1_.h])
                op("dve", lambda e: e.reciprocal(out=ss1_.t[:, 1:2], in_=ss1_.t[:, 1:2]), reads=[ss1_.h], writes=[ss1_.h])
                op("dve", lambda e: e.tensor_scalar_mul(out=hn_.t[:], in0=x1_.t[:], scalar1=ss1_.t[:, 1:2]), reads=[x1_.h, ss1_.h], writes=[hn_.h])
                op("act", lambda e: e.copy(out=hnb_.t[:], in_=hn_.t[:]), reads=[hn_.h], writes=[hnb_.h])

            def stageB4(g):
                tis = [g * 4 + tt for tt in range(4)]
                for ti in tis:
                    R = RS[ti % 4]
                    hn_, hnT_, L = hn4[ti % 4], R["hnT"], R["L"]
                    for k in range(8):
                        op("pe", lambda e, k=k, hn_=hn_: e.transpose(out=pTr.t[:, (k % 4) * 128:(k % 4 + 1) * 128], in_=hn_.t[:, k * 128:(k + 1) * 128], identity=identf.t[:]),
                           reads=[hn_.h, identf.h], writes=[pTr.h])
                        if k % 4 == 3:
                            kk = k // 4
                            op("act", lambda e, kk=kk, hnT_=hnT_: e.copy(out=hnT_.t[:, kk * 4:(kk + 1) * 4, :], in_=pTr.t[:].rearrange("p (a b) -> p a b", a=4)),
                               reads=[pTr.h], writes=[hnT_.h])
                for ti in tis:
                    R = RS[ti % 4]
                    hnT_, L = R["hnT"], R["L"]
                    c0 = (ti % 4) * 64
                    for k in range(8):
                        mm(pL.t[:, c0:c0 + 36], hnT_.t[:, k, :], wr.t[:, k, :], k == 0, False, [hnT_.h, wr.h], [pL.h])
                    mm(pL.t[:, c0:c0 + 36], onesf.t[0:1, :], br.t[0:1, :], False, True, [onesf.h, br.h], [pL.h])
                for ti in tis:
                    R = RS[ti % 4]
                    c0 = (ti % 4) * 64
                    op("act", lambda e, R=R, c0=c0: e.copy(out=R["L"].t[:], in_=pL.t[:, c0:c0 + 36]), reads=[pL.h], writes=[R["L"].h])

                def each(fn):
                    def step():
                        for ti in tis:
                            fn(ti, RS[ti % 4])
                    bgq.append(step)
                each(lambda ti, R: op("dve", lambda e: e.tensor_reduce(out=R["gmax"].t[:], in_=R["L"].t[:, 0:4], axis=AX.X, op=ALU.max), reads=[R["L"].h], writes=[R["gmax"].h]))
                each(lambda ti, R: op("dve", lambda e: e.tensor_scalar(out=R["gone"].t[:], in0=R["L"].t[:, 0:4], scalar1=R["gmax"].t[:, 0:1], scalar2=None, op0=ALU.is_equal), reads=[R["L"].h, R["gmax"].h], writes=[R["gone"].h]))
                each(lambda ti, R: op("dve", lambda e: e.tensor_scalar_mul(out=R["ngmax"].t[:], in0=R["gmax"].t[:], scalar1=-1.0), reads=[R["gmax"].h], writes=[R["ngmax"].h]))
                each(lambda ti, R: op("dve", lambda e: e.tensor_scalar_mul(out=R["ein"].t[:], in0=R["L"].t[:, 4:12], scalar1=R["gone"].t[:, 0:1]), reads=[R["L"].h, R["gone"].h], writes=[R["ein"].h]))
                each(lambda ti, R: op("act", lambda e: e.activation(out=R["ex4"].t[:], in_=R["L"].t[:, 0:4], func=AF.Exp, bias=R["ngmax"].t[:, 0:1], accum_out=R["sume"].t[:, 0:1]),
                                     reads=[R["L"].h, R["ngmax"].h], writes=[R["ex4"].h, R["sume"].h]))
                for gg in range(1, 4):
                    each(lambda ti, R, gg=gg: op("dve", lambda e: e.scalar_tensor_tensor(out=R["ein"].t[:], in0=R["L"].t[:, 4 + 8 * gg:12 + 8 * gg], scalar=R["gone"].t[:, gg:gg + 1], in1=R["ein"].t[:], op0=ALU.mult, op1=ALU.add),
                                                 reads=[R["L"].h, R["gone"].h, R["ein"].h], writes=[R["ein"].h]))
                each(lambda ti, R: op("dve", lambda e: e.tensor_reduce(out=R["mx1"].t[:], in_=R["ein"].t[:], axis=AX.X, op=ALU.max), reads=[R["ein"].h], writes=[R["mx1"].h]))
                each(lambda ti, R: op("dve", lambda e: e.tensor_scalar(out=R["one1"].t[:], in0=R["ein"].t[:], scalar1=R["mx1"].t[:, 0:1], scalar2=None, op0=ALU.is_equal), reads=[R["ein"].h, R["mx1"].h], writes=[R["one1"].h]))
                each(lambda ti, R: op("dve", lambda e: e.scalar_tensor_tensor(out=R["ein2"].t[:], in0=R["one1"].t[:], scalar=-1e30, in1=R["ein"].t[:], op0=ALU.mult, op1=ALU.add), reads=[R["one1"].h, R["ein"].h], writes=[R["ein2"].h]))
                each(lambda ti, R: op("dve", lambda e: e.tensor_reduce(out=R["mx2"].t[:], in_=R["ein2"].t[:], axis=AX.X, op=ALU.max), reads=[R["ein2"].h], writes=[R["mx2"].h]))
                each(lambda ti, R: op("dve", lambda e: e.tensor_scalar(out=R["one2"].t[:], in0=R["ein2"].t[:], scalar1=R["mx2"].t[:, 0:1], scalar2=None, op0=ALU.is_equal), reads=[R["ein2"].h, R["mx2"].h], writes=[R["one2"].h]))
                each(lambda ti, R: op("dve", lambda e: e.tensor_tensor(out=R["dd"].t[:], in0=R["mx1"].t[:], in1=R["mx2"].t[:], op=ALU.subtract), reads=[R["mx1"].h, R["mx2"].h], writes=[R["dd"].h]))
                each(lambda ti, R: op("dve", lambda e: e.reciprocal(out=R["pgrp"].t[:], in_=R["sume"].t[:]), reads=[R["sume"].h], writes=[R["pgrp"].h]))
                each(lambda ti, R: op("act", lambda e: e.activation(out=R["sg1"].t[:], in_=R["dd"].t[:], func=AF.Sigmoid), reads=[R["dd"].h], writes=[R["sg1"].h]))
                for gg in range(4):
                    each(lambda ti, R, gg=gg: op("dve", lambda e: e.tensor_scalar_mul(out=R["E1"].t[:, gg * 8:(gg + 1) * 8], in0=R["one1"].t[:], scalar1=R["gone"].t[:, gg:gg + 1]), reads=[R["one1"].h, R["gone"].h], writes=[R["E1"].h]))
                    each(lambda ti, R, gg=gg: op("dve", lambda e: e.tensor_scalar_mul(out=R["E2"].t[:, gg * 8:(gg + 1) * 8], in0=R["one2"].t[:], scalar1=R["gone"].t[:, gg:gg + 1]), reads=[R["one2"].h, R["gone"].h], writes=[R["E2"].h]))
                each(lambda ti, R: op("dve", lambda e: e.tensor_tensor(out=R["Ab"].t[:], in0=R["E1"].t[:], in1=R["E2"].t[:], op=ALU.add), reads=[R["E1"].h, R["E2"].h], writes=[R["Ab"].h]))
                each(lambda ti, R: op("dve", lambda e: e.tensor_tensor(out=wts.t[:, ti, 0:1], in0=R["sg1"].t[:], in1=R["pgrp"].t[:], op=ALU.mult), reads=[R["sg1"].h, R["pgrp"].h], writes=[wts.hs[ti % 4]]))
                each(lambda ti, R: op("dve", lambda e: e.tensor_tensor(out=wts.t[:, ti, 1:2], in0=R["pgrp"].t[:], in1=wts.t[:, ti, 0:1], op=ALU.subtract), reads=[R["pgrp"].h, wts.hs[ti % 4]], writes=[wts.hs[ti % 4]]))

            def stageC4(g):
                tis = [g * 4 + tt for tt in range(4)]
                for ti in tis:
                    R = RS[ti % 4]
                    c0 = (ti % 4) * 64
                    mm(pL.t[:, 256 + c0:256 + c0 + 32], triu.t[:], R["Ab"].t[:], True, True, [triu.h, R["Ab"].h], [hpC])
                    mm(pL.t[:, 256 + c0 + 32:256 + c0 + 64], onesb.t[:], R["Ab"].t[:], True, True, [onesb.h, R["Ab"].h], [hpC])
                for ti in tis:
                    R = RS[ti % 4]
                    c0 = (ti % 4) * 64
                    op("dve", lambda e, R=R, c0=c0: e.tensor_tensor(out=R["pos"].t[:], in0=pL.t[:, 256 + c0:256 + c0 + 32], in1=run.t[:], op=ALU.add), reads=[hpC, run.h], writes=[R["pos"].h])
                    op("dve", lambda e, c0=c0: e.tensor_tensor(out=run.t[:], in0=pL.t[:, 256 + c0 + 32:256 + c0 + 64], in1=run.t[:], op=ALU.add), reads=[hpC, run.h], writes=[run.h])

                def each(fn):
                    for ti in tis:
                        fn(ti, RS[ti % 4])
                each(lambda ti, R: op("dve", lambda e: e.tensor_tensor(out=R["pos"].t[:], in0=R["pos"].t[:], in1=offs.t[:], op=ALU.add), reads=[R["pos"].h, offs.h], writes=[R["pos"].h]))
                each(lambda ti, R: op("dve", lambda e: e.tensor_tensor(out=R["tmpa"].t[:], in0=R["pos"].t[:], in1=R["E1"].t[:], op=ALU.mult), reads=[R["pos"].h, R["E1"].h], writes=[R["tmpa"].h]))
                each(lambda ti, R: op("dve", lambda e: e.tensor_tensor(out=R["tmpb"].t[:], in0=R["pos"].t[:], in1=R["E2"].t[:], op=ALU.mult), reads=[R["pos"].h, R["E2"].h], writes=[R["tmpb"].h]))
                each(lambda ti, R: op("dve", lambda e: e.tensor_reduce(out=R["slf"].t[:, 0:1], in_=R["tmpa"].t[:], axis=AX.X, op=ALU.add), reads=[R["tmpa"].h], writes=[R["slf"].h]))
                each(lambda ti, R: op("dve", lambda e: e.tensor_reduce(out=R["slf"].t[:, 1:2], in_=R["tmpb"].t[:], axis=AX.X, op=ALU.add), reads=[R["tmpb"].h, R["slf"].h], writes=[R["slf"].h]))
                each(lambda ti, R: op("dve", lambda e: e.tensor_copy(out=slot_i.t[:, ti, :], in_=R["slf"].t[:]), reads=[R["slf"].h], writes=[slot_i.hs[ti % 4]]))
                for ti in tis:
                    hnb_ = hnb8[ti % 8]
                    for j in range(2):
                        op("pool", lambda e, j=j, ti=ti, hnb_=hnb_: e.indirect_dma_start(
                            out=XS, out_offset=bass.IndirectOffsetOnAxis(ap=slot_i.t[:, ti, j:j + 1], axis=0),
                            in_=hnb_.t[:], in_offset=None), reads=[hnb_.h, slot_i.hs[ti % 4]], writes=[dh("XS")], dma=True)

            ci = 0
            for g in range(NG):
                tok = slice(g * 512, (g + 1) * 512)
                load(hTg.t[:], HTv[:, :, tok], [hTg.h], [dh("HT")])
                load(oag.t[:], OAv[:, :, tok], [oag.h], [dh("OA")])
                load(obg.t[:], OBv[:, :, tok], [obg.h], [dh("OB")])
                it = 0
                for c in range(8):
                    for br_i, (og, wo, gc0) in enumerate(((oag, woa, 0), (obg, wob, 1024))):
                        if it == 3 and g >= 1:
                            stageB4(g - 1)
                        it += 1
                        pa, pg_ = pA[ci % 2], pG[ci % 2]
                        sg, mm1 = sig[ci % 2], m1[ci % 2]
                        ci += 1
                        for k in range(4):
                            mm(pa.t[:], wo.t[:, k, c * 128:(c + 1) * 128], og.t[:, k, :], k == 0, k == 3, [wo.h, og.h], [pa.h])
                        for k in range(8):
                            mm(pg_.t[:], wg.t[:, k, gc0 + c * 128: gc0 + (c + 1) * 128], hTg.t[:, k, :], k == 0, k == 7, [wg.h, hTg.h], [pg_.h])
                        op("act", lambda e, pg_=pg_, sg=sg: e.activation(out=sg.t[:], in_=pg_.t[:], func=AF.Sigmoid), reads=[pg_.h], writes=[sg.h])
                        if br_i == 0:
                            op("dve", lambda e, pa=pa, sg=sg, mm1=mm1: e.tensor_tensor(out=mm1.t[:], in0=sg.t[:], in1=pa.t[:], op=ALU.mult),
                               reads=[sg.h, pa.h], writes=[mm1.h])
                            prev = mm1
                        else:
                            op("dve", lambda e, pa=pa, sg=sg: e.tensor_tensor(out=sg.t[:], in0=sg.t[:], in1=pa.t[:], op=ALU.mult),
                               reads=[sg.h, pa.h], writes=[sg.h])
                            op("dve", lambda e, sg=sg, prev=prev, c=c: e.tensor_tensor(out=mixT.t[:, c, :], in0=sg.t[:], in1=prev.t[:], op=ALU.add),
                               reads=[sg.h, prev.h], writes=[mixT.hs[c]])
                        bg_run(3)
                bg_run(10 ** 6)
                if g >= 1:
                    stageC4(g - 1)
                for tt in range(4):
                    stageA(g * 4 + tt, tt)
            stageB4(NG - 1)
            bg_run(10 ** 6)
            stageC4(NG - 1)
            Sx.barrier()
        if stop_after <= 4:
            return _finish(nc, Sx, out_d, S)

        with ExitStack() as st:
            sb, ps = mk(st)
            identb5x = sb("identb5", [128, 128], BF16)
            load(identb5x.t[:], c_identb, [identb5x.h])
            gf5v = sb("gf5", [128, 8], F32)
            load(gf5v.t[:], ffn_norm_d, [gf5v.h])
            NST = C // 128
            HALF = C // 2
            NSH = HALF // 128
            wgs = [sb(f"wgs{i}", [128, 8, 256], F32) for i in range(2)]
            wus = [sb(f"wus{i}", [128, 8, 256], F32) for i in range(2)]
            wds = [sb(f"wds{i}", [128, 2, D], F32) for i in range(2)]
            wgb = [sb(f"wgb{i}", [128, 8, 256], BF16) for i in range(2)]
            wub = [sb(f"wub{i}", [128, 8, 256], BF16) for i in range(2)]
            wdb = [sb(f"wdb{i}", [128, 2, D], BF16) for i in range(2)]
            xs = [sb(f"xs{i}", [128, D], BF16) for i in range(6)]
            xeT = [sb(f"xeT{i}", [128, 8, C], BF16) for i in range(2)]
            sil = [sb(f"sil{i}", [128, HALF], F32) for i in range(2)]
            actT = [sb(f"actT{i}", [128, 2, HALF], BF16, 2) for i in range(2)]
            ysb = [sb(f"ysb{i}", [128, D], BF16) for i in range(6)]
            ptx = [ps(f"ptx{i}", [128, 1024], BF16) for i in range(2)]
            pgu = [ps(f"pgu{i}", [128, 512], F32) for i in range(4)]
            pyy = [ps(f"pyy{i}", [128, 512], F32) for i in range(2)]
            xi = [0]
            yi = [0]

            def stage_load(ex):
                b = ex % 2
                load(wgs[b].t[:], w_g_d[ex].rearrange("(k p) f -> p k f", p=128), [wgs[b].h])
                load(wus[b].t[:], w_u_d[ex].rearrange("(k p) f -> p k f", p=128), [wus[b].h])
                load(wds[b].t[:], w_d_d[ex].rearrange("(k p) f -> p k f", p=128), [wds[b].h])

            def conv_part(ex, part):
                b = ex % 2
                for k in (2 * part, 2 * part + 1):
                    op("act", lambda e, k=k, b=b: e.activation(out=wgb[b].t[:, k, :], in_=wgs[b].t[:, k, :], func=AF.Copy, scale=gf5v.t[:, k:k + 1]),
                       reads=[wgs[b].h, gf5v.h], writes=[wgb[b].h])
                    op("dve", lambda e, k=k, b=b: e.tensor_scalar_mul(out=wub[b].t[:, k, :], in0=wus[b].t[:, k, :], scalar1=gf5v.t[:, k:k + 1]),
                       reads=[wus[b].h, gf5v.h], writes=[wub[b].h])
                if part == 3:
                    op("dve", lambda e, b=b: e.tensor_copy(out=wdb[b].t[:, 0, :], in_=wds[b].t[:, 0, :]), reads=[wds[b].h], writes=[wdb[b].h])
                    op("dve", lambda e, b=b: e.tensor_copy(out=wdb[b].t[:, 1, :], in_=wds[b].t[:, 1, :]), reads=[wds[b].h], writes=[wdb[b].h])

            def stage_a(ex):
                b = ex % 2
                xe = xeT[b]
                for stl in range(NST):
                    row0 = ex * C + stl * 128
                    xs_ = xs[xi[0] % 6]
                    px = ptx[xi[0] % 2]
                    xi[0] += 1
                    load(xs_.t[:], XS[row0:row0 + 128, :], [xs_.h], [dh("XS")])
                    for k in range(8):
                        op("pe", lambda e, k=k, xs_=xs_, px=px: e.transpose(out=px.t[:, k * 128:(k + 1) * 128], in_=xs_.t[:, k * 128:(k + 1) * 128], identity=identb5x.t[:]),
                           reads=[xs_.h, identb5x.h], writes=[px.h])
                    op("act", lambda e, px=px, xe=xe, stl=stl: e.copy(out=xe.t[:, :, stl * 128:(stl + 1) * 128], in_=px.t[:].rearrange("p (a b) -> p a b", a=8)),
                       reads=[px.h], writes=[xe.h])

            def gu(ex, hf, fc):
                b = ex % 2
                xe = xeT[b]
                cs = slice(hf * HALF, (hf + 1) * HALF)
                at = actT[hf]
                pgt, put = pgu[fc * 2], pgu[fc * 2 + 1]
                for k in range(8):
                    mm(pgt.t[:, 0:HALF], wgb[b].t[:, k, fc * 128:(fc + 1) * 128], xe.t[:, k, cs], k == 0, k == 7, [wgb[b].h, xe.h], [pgt.h])
                for k in range(8):
                    mm(put.t[:, 0:HALF], wub[b].t[:, k, fc * 128:(fc + 1) * 128], xe.t[:, k, cs], k == 0, k == 7, [wub[b].h, xe.h], [put.h])
                sl = sil[fc]
                op("act", lambda e: e.activation(out=sl.t[:], in_=pgt.t[:, 0:HALF], func=AF.Silu), reads=[pgt.h], writes=[sl.h])
                op("dve", lambda e: e.tensor_tensor(out=at.t[:, fc, :], in0=sl.t[:], in1=put.t[:, 0:HALF], op=ALU.mult),
                   reads=[sl.h, put.h], writes=[at.hs[fc]])

            def down(ex, hf):
                b = ex % 2
                at = actT[hf]
                for stl in range(NSH):
                    ys_ = ysb[yi[0] % 6]
                    yi[0] += 1
                    for half in range(2):
                        py = pyy[half]
                        for fc in range(2):
                            mm(py.t[:], at.t[:, fc, stl * 128:(stl + 1) * 128], wdb[b].t[:, fc, half * 512:(half + 1) * 512], fc == 0, fc == 1,
                               [at.hs[fc], wdb[b].h], [py.h])
                        if half == 0:
                            op("act", lambda e, py=py, ys_=ys_: e.copy(out=ys_.t[:, 0:512], in_=py.t[:]), reads=[py.h], writes=[ys_.h])
                        else:
                            op("dve", lambda e, py=py, ys_=ys_: e.tensor_copy(out=ys_.t[:, 512:1024], in_=py.t[:]), reads=[py.h], writes=[ys_.h])
                    row0 = ex * C + hf * HALF + stl * 128
                    store(YS[row0:row0 + 128, :], ys_.t[:], [ys_.h], [dh("YS")])

            stage_load(0)
            stage_load(1)
            for part in range(4):
                conv_part(0, part)
            stage_a(0)
            for ex in range(NE):
                nxt_ex = ex + 1 < NE
                if nxt_ex:
                    stage_a(ex + 1)
                if ex + 2 < NE:
                    stage_load(ex + 2)
                gu(ex, 0, 0)
                if nxt_ex:
                    conv_part(ex + 1, 0)
                gu(ex, 0, 1)
                if nxt_ex:
                    conv_part(ex + 1, 1)
                gu(ex, 1, 0)
                if nxt_ex:
                    conv_part(ex + 1, 2)
                gu(ex, 1, 1)
                if nxt_ex:
                    conv_part(ex + 1, 3)
                down(ex, 0)
                down(ex, 1)
            Sx.barrier()
        if stop_after <= 5:
            return _finish(nc, Sx, out_d, S)

        with ExitStack() as st:
            sb, ps = mk(st)
            onesf6v = sb("onesf6", [128, 128], F32)
            fn_row = sb("fn_row", [1, D], F32)
            GF = sb("GF", [128, D], F32)
            load(onesf6v.t[:], c_onesf, [onesf6v.h])
            load(fn_row.t[:], fnorm_d, [fn_row.h])
            pb = [ps(f"pb{i}", [128, 512], F32) for i in range(2)]
            for half in range(2):
                mm(pb[half].t[:], onesf6v.t[0:1, :], fn_row.t[0:1, half * 512:(half + 1) * 512], True, True, [onesf6v.h, fn_row.h], [pb[half].h])
                op("act", lambda e, half=half: e.copy(out=GF.t[:, half * 512:(half + 1) * 512], in_=pb[half].t[:]), reads=[pb[half].h], writes=[GF.h])
            NB6 = 3
            x1t = [sb(f"x1t{i}", [128, D], F32) for i in range(NB6)]
            y1 = [sb(f"y1_{i}", [128, D], BF16) for i in range(NB6)]
            y2 = [sb(f"y2_{i}", [128, D], BF16) for i in range(NB6)]
            junk6v = sb("junk6", [128, D], F32)
            ssf = [sb(f"ssf{i}", [128, 2], F32) for i in range(2)]
            ot = [sb(f"ot{i}", [128, D], F32) for i in range(2)]
            hout = H()

            def fetch(ti):
                b = ti % NB6
                rows = slice(ti * 128, (ti + 1) * 128)
                load(x1t[b].t[:], X1[rows, :], [x1t[b].h], [dh("X1")])
                for j, yy in enumerate((y1[b], y2[b])):
                    op("pool", lambda e, j=j, yy=yy: e.indirect_dma_start(
                        out=yy.t[:], out_offset=None, in_=YS,
                        in_offset=bass.IndirectOffsetOnAxis(ap=slot_i.t[:, ti, j:j + 1], axis=0)),
                       reads=[dh("YS"), slot_i.h], writes=[yy.h], dma=True)

            def compute(ti):
                b = ti % NB6
                rows = slice(ti * 128, (ti + 1) * 128)
                xx, ya, yb_ = x1t[b], y1[b], y2[b]
                op("dve", lambda e: e.scalar_tensor_tensor(out=xx.t[:], in0=ya.t[:], scalar=wts.t[:, ti, 0:1], in1=xx.t[:], op0=ALU.mult, op1=ALU.add),
                   reads=[ya.h, wts.h, xx.h], writes=[xx.h])
                op("dve", lambda e: e.scalar_tensor_tensor(out=xx.t[:], in0=yb_.t[:], scalar=wts.t[:, ti, 1:2], in1=xx.t[:], op0=ALU.mult, op1=ALU.add),
                   reads=[yb_.h, wts.h, xx.h], writes=[xx.h])
                sf = ssf[ti % 2]
                op("act", lambda e: e.activation(out=junk6v.t[:], in_=xx.t[:], func=AF.Square, accum_out=sf.t[:, 0:1]), reads=[xx.h], writes=[junk6v.h, sf.h])
                op("act", lambda e: e.activation(out=sf.t[:, 1:2], in_=sf.t[:, 0:1], func=AF.Sqrt, scale=1.0 / D, bias=EPS), reads=[sf.h], writes=[sf.h])
                op("dve", lambda e: e.reciprocal(out=sf.t[:, 1:2], in_=sf.t[:, 1:2]), reads=[sf.h], writes=[sf.h])
                o_ = ot[ti % 2]
                op("dve", lambda e: e.scalar_tensor_tensor(out=o_.t[:], in0=xx.t[:], scalar=sf.t[:, 1:2], in1=GF.t[:], op0=ALU.mult, op1=ALU.mult),
                   reads=[xx.h, sf.h, GF.h], writes=[o_.h])
                op("sp", lambda e: e.dma_start(out=out_d[rows, :], in_=o_.t[:]), reads=[o_.h], writes=[hout], dma=True)

            fetch(0)
            if NT > 1:
                fetch(1)
            for ti in range(NT):
                if ti + 2 < NT:
                    fetch(ti + 2)
                compute(ti)
            Sx.barrier()
        return _finish(nc, Sx, out_d, S)


def _finish(nc, Sx, out_d, S):
    Sx.barrier()
    Sx.emit_all()
    return nc


def _consts(S, C):
    bf = ml_dtypes.bfloat16
    c = {}
    c["c_identb"] = np.eye(128, dtype=np.float32).astype(bf)
    c["c_identf"] = np.eye(128, dtype=np.float32)
    c["c_onesb"] = np.ones((128, 128), np.float32).astype(bf)
    c["c_onesf"] = np.ones((128, 128), np.float32)
    pa = np.zeros((96, 96), np.float32)
    for m in range(96):
        if m < 64:
            k = m
        elif m < 80:
            k = m + 16
        else:
            k = m - 16
        pa[k, m] = 1.0
    c["c_perma"] = pa.astype(bf)
    pb = np.zeros((128, 128), np.float32)
    for m in range(128):
        j = m % 64
        if j < 8:
            k = m + 8
        elif j < 16:
            k = m - 8
        else:
            k = m
        pb[k, m] = 1.0
    c["c_permb"] = pb.astype(bf)
    pos = np.arange(S, dtype=np.float64)
    inv = ROPE_THETA ** (-np.arange(16, dtype=np.float64) / 16)
    ang = pos[None, :] * inv[:, None]
    tac = np.ones((96, S), np.float64)
    tas = np.zeros((96, S), np.float64)
    tac[64:80] = np.cos(ang)
    tac[80:96] = np.cos(ang)
    tas[64:80] = -np.sin(ang)
    tas[80:96] = np.sin(ang)
    c["c_tac"] = tac.astype(np.float32)
    c["c_tas"] = tas.astype(np.float32)
    inv = ROPE_THETA ** (-np.arange(8, dtype=np.float64) / 8)
    ang = pos[None, :] * inv[:, None]
    tbc = np.ones((128, S), np.float64)
    tbs = np.zeros((128, S), np.float64)
    for b0 in (0, 64):
        tbc[b0:b0 + 8] = np.cos(ang)
        tbc[b0 + 8:b0 + 16] = np.cos(ang)
        tbs[b0:b0 + 8] = -np.sin(ang)
        tbs[b0 + 8:b0 + 16] = np.sin(ang)
    c["c_tbc"] = tbc.astype(np.float32)
    c["c_tbs"] = tbs.astype(np.float32)
    m = np.zeros((128, 4, 512), np.float32)
    p = np.arange(128)[:, None]
    j = np.arange(512)[None, :]
    for r in range(4):
        m[:, r, :] = (r * 128 + p <= j)
    c["c_mask"] = m.astype(bf)
    oh = np.zeros((32, S), np.float32)
    for n in range(32):
        oh[n, n * 256:(n + 1) * 256] = 1.0
    c["c_oh"] = oh.astype(bf)
    c["c_wneg"] = np.concatenate([np.zeros((1, 32), np.float32), np.full((1, 1), 1e30, np.float32), np.full((1, 31), -1e30, np.float32)], axis=1).astype(bf)
    c["c_offs"] = np.tile((np.arange(32, dtype=np.float32) * C - 1.0)[None, :], (128, 1)).astype(np.float32)
    kk = np.arange(128)[:, None]
    mm_ = np.arange(128)[None, :]
    c["c_triu"] = (kk <= mm_).astype(np.float32).astype(bf)
    return c


def _prep_inputs(inputs, S, C):
    f = lambda a: np.ascontiguousarray(np.asarray(a, dtype=np.float32))
    w_ukv = f(inputs["w_ukv"])[0].reshape(256, 8, 128)
    w_ukv_kv = np.concatenate([w_ukv[:, :, :64].reshape(256, 512), w_ukv[:, :, 64:].reshape(256, 512)], axis=1)
    w_rg = f(inputs["w_router_group"])[0]
    w_re = f(inputs["w_router_expert"])[0]
    w_r = np.concatenate([w_rg] + [w_re[g] for g in range(4)], axis=1)
    b_r = np.concatenate([f(inputs["b_router_group"])[0], f(inputs["b_router_expert"])[0].reshape(32)])[None, :]
    shared = {
        "attn_norm": np.ascontiguousarray(f(inputs["attn_norm"])[0].reshape(8, 128).T),
        "w_in": f(inputs["w_in"])[0],
        "q_norm": np.ascontiguousarray(f(inputs["q_norm"])[0].reshape(6, 128).T),
        "w_uq": f(inputs["w_uq"])[0],
        "kv_norm": np.ascontiguousarray(f(inputs["kv_norm"])[0].reshape(2, 128).T),
        "w_ukv_kv": np.ascontiguousarray(w_ukv_kv),
        "w_o_mla": f(inputs["w_o_mla"])[0],
        "w_o_moba": f(inputs["w_o_moba"])[0],
        "w_out": f(inputs["w_out"])[0],
        "ffn_norm": np.ascontiguousarray(f(inputs["ffn_norm"])[0].reshape(8, 128).T),
        "w_r": np.ascontiguousarray(w_r),
        "b_r": np.ascontiguousarray(b_r),
        "w_exp_gate": f(inputs["w_exp_gate"])[0],
        "w_exp_up": f(inputs["w_exp_up"])[0],
        "w_exp_down": f(inputs["w_exp_down"])[0],
        "final_norm": f(inputs["final_norm"]).reshape(1, D),
    }
    shared.update(_consts(S, C))
    return shared


def kernel(**inputs):
    x = np.asarray(inputs["x"], dtype=np.float32)
    B, S, _ = x.shape
    C = 768 if S >= 8192 else max(256, (S * 2 // 32) * 3 // 128 * 128 + 128)
    nc = build(S=S, C=C)
    shared = _prep_inputs(inputs, S, C)
    in_maps = []
    for b in range(B):
        m = dict(shared)
        m["x"] = np.ascontiguousarray(x[b])
        m["xT"] = np.ascontiguousarray(x[b].T)
        in_maps.append(m)
    res = run_bass_kernel_spmd(nc, in_maps, core_ids=list(range(B)))
    return np.stack([r["out"] for r in res.results], axis=0)
```

```python
import numpy as np
from contextlib import ExitStack
import ml_dtypes
import concourse.bass as bass
import concourse.mybir as mybir
from concourse.bass_utils import run_bass_kernel_spmd

F32 = mybir.dt.float32
BF16 = mybir.dt.bfloat16
I32 = mybir.dt.int32
AF = mybir.ActivationFunctionType
ALU = mybir.AluOpType
AX = mybir.AxisListType

SEM_LIMIT = 30000
D = 1024
NE = 32
EPS = 1e-6
ROPE_THETA = 500000.0


class H:
    __slots__ = ("w", "r")

    def __init__(self):
        self.w = None
        self.r = {}


class Q:
    def __init__(self, name, sems):
        self.name = name
        self.sems = sems
        self.epoch = 0
        self.count = 0
        self.ops = []
        self.waited = {}


class Sched:
    def __init__(self, nc, stack, n_dma_sems=72):
        self.nc = nc
        self.semobj = {}
        self.q = {}
        for name, nep in (("pe", 5), ("act", 3), ("dve", 3), ("pool", 3), ("sp", 1)):
            sems = []
            for e in range(nep):
                s = stack.enter_context(nc.semaphore(f"s_{name}{e}"))
                key = f"{name}{e}"
                self.semobj[key] = s
                sems.append(key)
            self.q[name] = Q(name, sems)
        self.dma_sems = []
        for i in range(n_dma_sems):
            s = stack.enter_context(nc.semaphore(f"s_dma{i}"))
            key = f"dma{i}"
            self.semobj[key] = s
            self.dma_sems.append([key, 0])
        self.dma_rr = 0
        self.dma_rr_q = {}
        self.n_ops = 0

    def _deps(self, reads, writes):
        deps = {}

        def add(k, v):
            if v > deps.get(k, -1):
                deps[k] = v
        for h in reads:
            if h.w is not None:
                add(*h.w)
        for h in writes:
            if h.w is not None:
                add(*h.w)
            for k, v in h.r.items():
                add(k, v)
        return deps

    def op(self, qname, fn, reads=(), writes=(), dma=False):
        q = self.q[qname]
        deps = self._deps(reads, writes)
        if dma:
            third = len(self.dma_sems) // 3
            base = {"sp": 0, "pool": third, "act": 2 * third}[qname]
            rr = self.dma_rr_q.get(qname, 0)
            slot = self.dma_sems[base + rr]
            self.dma_rr_q[qname] = (rr + 1) % third
            if slot[1] > 0 and slot[1] > deps.get(slot[0], -1):
                deps[slot[0]] = slot[1]
            assert slot[1] + 16 < 60000
            slot[1] += 16
            comp = (slot[0], slot[1])
            inc = 16
        else:
            if q.count + 1 > SEM_LIMIT:
                q.epoch += 1
                q.count = 0
            q.count += 1
            comp = (q.sems[q.epoch], q.count)
            inc = 1
        waits = []
        for k, v in deps.items():
            if qname == "pe" and k.startswith("pe"):
                continue
            if q.waited.get(k, -1) >= v:
                continue
            q.waited[k] = v
            waits.append((self.semobj[k], v))
        csem = self.semobj[comp[0]]

        def emit(eng, waits=waits, fn=fn, csem=csem, inc=inc):
            for s, v in waits:
                eng.wait_ge(s, v)
            fn(eng).then_inc(csem, inc)
        q.ops.append(emit)
        for h in writes:
            h.w = comp
            h.r = {}
        for h in reads:
            if comp[1] > h.r.get(comp[0], -1):
                h.r[comp[0]] = comp[1]
        self.n_ops += 1
        return comp

    def barrier(self):
        deps = {}
        for q in self.q.values():
            for e in range(q.epoch + 1):
                cnt = q.count if e == q.epoch else SEM_LIMIT
                if cnt > 0:
                    deps[q.sems[e]] = cnt
        for k, v in self.dma_sems:
            if v > 0:
                deps[k] = v
        for q in self.q.values():
            waits = []
            for k, v in deps.items():
                if q.waited.get(k, -1) >= v:
                    continue
                q.waited[k] = v
                waits.append((self.semobj[k], v))

            def emit(eng, waits=waits):
                for s, v in waits:
                    eng.wait_ge(s, v)
            q.ops.append(emit)

    def emit_all(self):
        nc = self.nc
        with nc.Block() as block:
            @block.tensor
            def _(e):
                for f in self.q["pe"].ops:
                    f(e)

            @block.scalar
            def _(e):
                for f in self.q["act"].ops:
                    f(e)

            @block.vector
            def _(e):
                for f in self.q["dve"].ops:
                    f(e)

            @block.gpsimd
            def _(e):
                for f in self.q["pool"].ops:
                    f(e)

            @block.sync
            def _(e):
                for f in self.q["sp"].ops:
                    f(e)


class T:
    def __init__(self, t, n=1):
        self.t = t
        self.h = H()
        self.hs = [H() for _ in range(n)]


def build(S=8192, C=768, stop_after=99, debug=False):
    NG = S // 512
    NT = S // 128
    NSLOT = NE * C
    nc = bass.Bass("TRN2", target_bir_lowering=False)

    def din(name, shape, dt=F32):
        return nc.dram_tensor(name, shape, dt, kind="ExternalInput").ap()

    def dscr(name, shape, dt):
        return nc.dram_tensor(name, shape, dt, kind=("ExternalOutput" if debug else "Internal")).ap()

    x_d = din("x", [S, D])
    xT_d = din("xT", [D, S])
    attn_norm_d = din("attn_norm", [128, 8])
    w_in_d = din("w_in", [D, 4640])
    q_norm_d = din("q_norm", [128, 6])
    w_uq_d = din("w_uq", [768, 768])
    kv_norm_d = din("kv_norm", [128, 2])
    w_ukv_d = din("w_ukv_kv", [256, 1024])
    w_oa_d = din("w_o_mla", [512, D])
    w_ob_d = din("w_o_moba", [512, D])
    w_out_d = din("w_out", [D, D])
    ffn_norm_d = din("ffn_norm", [128, 8])
    w_r_d = din("w_r", [D, 36])
    b_r_d = din("b_r", [1, 36])
    w_g_d = din("w_exp_gate", [NE, D, 256])
    w_u_d = din("w_exp_up", [NE, D, 256])
    w_d_d = din("w_exp_down", [NE, 256, D])
    fnorm_d = din("final_norm", [1, D])
    c_identb = din("c_identb", [128, 128], BF16)
    c_identf = din("c_identf", [128, 128])
    c_onesb = din("c_onesb", [128, 128], BF16)
    c_onesf = din("c_onesf", [128, 128])
    c_perma = din("c_perma", [96, 96], BF16)
    c_permb = din("c_permb", [128, 128], BF16)
    c_tac = din("c_tac", [96, S])
    c_tas = din("c_tas", [96, S])
    c_tbc = din("c_tbc", [128, S])
    c_tbs = din("c_tbs", [128, S])
    c_mask = din("c_mask", [128, 4, 512], BF16)
    c_oh = din("c_oh", [32, S], BF16)
    c_wneg = din("c_wneg", [1, 64], BF16)
    c_offs = din("c_offs", [128, 32])
    c_triu = din("c_triu", [128, 128], BF16)

    out_d = nc.dram_tensor("out", [S, D], F32, kind="ExternalOutput").ap()

    WIN = dscr("WIN", [D, 4640], BF16)
    WUQ = dscr("WUQ", [768, 768], BF16)
    WUKV = dscr("WUKV", [256, 1024], BF16)
    WOA = dscr("WOA", [512, D], BF16)
    WOB = dscr("WOB", [512, D], BF16)
    WOUT = dscr("WOUT", [D, D], BF16)
    HT = dscr("HT", [D, S], BF16)
    QA = dscr("QA", [768, S], BF16)
    KNA = dscr("KNA", [512, S], BF16)
    KRA = dscr("KRA", [32, S], BF16)
    VA = dscr("VA", [S, 512], BF16)
    QB = dscr("QB", [512, S], BF16)
    KB = dscr("KB", [512, S], BF16)
    VB = dscr("VB", [S, 512], BF16)
    MB = dscr("MB", [256, S], BF16)
    OA = dscr("OA", [512, S], BF16)
    OB = dscr("OB", [512, S], BF16)
    X1 = dscr("X1", [S, D], F32)
    XS = dscr("XS", [NSLOT, D], BF16)
    YS = dscr("YS", [NSLOT, D], BF16)

    with ExitStack() as top:
        Sx = Sched(nc, top)
        op = Sx.op

        def mk(st):
            def sb(name, shape, dt, n=1):
                return T(st.enter_context(nc.sbuf_tensor(name, shape, dt)), n)

            def ps(name, shape, dt):
                return T(st.enter_context(nc.psum_tensor(name, shape, dt)))
            return sb, ps

        def load(dst_ap, src_ap, wr, rd=()):
            op("sp", lambda e: e.dma_start(out=dst_ap, in_=src_ap), reads=rd, writes=wr, dma=True)

        store_q = ["pool"]

        def store(dst_ap, src_ap, rd, wr):
            op(store_q[0], lambda e: e.dma_start(out=dst_ap, in_=src_ap), reads=rd, writes=wr, dma=True)

        def mm(out_ap, lhsT, rhs, start, stop, rd, wr):
            op("pe", lambda e: e.matmul(out_ap, lhsT, rhs, start=start, stop=stop), reads=rd, writes=wr)

        hd = {}

        def dh(name):
            if name not in hd:
                hd[name] = H()
            return hd[name]

        sbp, _ = mk(top)
        slot_i = sbp("slot_i", [128, NT, 2], I32, 4)
        wts = sbp("wts", [128, NT, 2], F32, 4)

        with ExitStack() as st:
            sb, ps = mk(st)
            stg = [sb(f"stg{i}", [128, 8, 512], F32) for i in range(2)]
            obf = [sb(f"obf{i}", [128, 8, 512], BF16) for i in range(2)]
            gains = sb("gains", [128, 3, 8], F32)
            load(gains.t[:, 0, :], attn_norm_d, [gains.hs[0]])
            load(gains.t[:, 1, 0:6], q_norm_d, [gains.hs[0]])
            load(gains.t[:, 2, 0:2], kv_norm_d, [gains.hs[0]])
            cnt = [0]

            def conv(src, dst, dname, nk, ncols, gi, cstart=0):
                srcv = src.rearrange("(k p) c -> p k c", p=128)
                dstv = dst.rearrange("(k p) c -> p k c", p=128)
                for c0 in range(cstart, ncols, 512):
                    cw = min(512, ncols - c0)
                    i = cnt[0] % 2
                    cnt[0] += 1
                    a, b = stg[i], obf[i]
                    load(a.t[:, 0:nk, 0:cw], srcv[:, :, c0:c0 + cw], [a.h])
                    for k in range(nk):
                        if gi is None:
                            if k % 2 == 0:
                                op("act", lambda e, a=a, b=b, k=k, cw=cw: e.copy(out=b.t[:, k, 0:cw], in_=a.t[:, k, 0:cw]),
                                   reads=[a.h], writes=[b.hs[0]] if False else [b.h])
                            else:
                                op("dve", lambda e, a=a, b=b, k=k, cw=cw: e.tensor_copy(out=b.t[:, k, 0:cw], in_=a.t[:, k, 0:cw]),
                                   reads=[a.h], writes=[b.h])
                        else:
                            if k % 2 == 0:
                                op("act", lambda e, a=a, b=b, k=k, cw=cw, gi=gi: e.activation(out=b.t[:, k, 0:cw], in_=a.t[:, k, 0:cw], func=AF.Copy, scale=gains.t[:, gi, k:k + 1]),
                                   reads=[a.h, gains.hs[0]], writes=[b.h])
                            else:
                                op("dve", lambda e, a=a, b=b, k=k, cw=cw, gi=gi: e.tensor_scalar_mul(out=b.t[:, k, 0:cw], in0=a.t[:, k, 0:cw], scalar1=gains.t[:, gi, k:k + 1]),
                                   reads=[a.h, gains.hs[0]], writes=[b.h])
                    store(dstv[:, :, c0:c0 + cw], b.t[:, 0:nk, 0:cw], [b.h], [dh(dname)])

            conv(w_in_d, WIN, "WIN", 8, 2592, 0)
            conv(w_uq_d, WUQ, "WUQ", 6, 768, 1)
            conv(w_ukv_d, WUKV, "WUKV", 2, 1024, 2)
            Sx.barrier()
        if stop_after <= 0:
            return _finish(nc, Sx, out_d, S)

        with ExitStack() as st:
            sb, ps = mk(st)
            onesb = sb("onesb", [128, 128], BF16)
            identb = sb("identb", [128, 128], BF16)
            perma = sb("perma", [96, 96], BF16)
            permb = sb("permb", [128, 128], BF16)
            wneg = sb("wneg", [1, 64], BF16)
            load(onesb.t[:], c_onesb, [onesb.h])
            load(identb.t[:], c_identb, [identb.h])
            load(perma.t[:], c_perma, [perma.h])
            load(permb.t[:], c_permb, [permb.h])
            load(wneg.t[:], c_wneg, [wneg.h])
            store_q[0] = "sp"
            NCW = 2592
            win = sb("win", [128, 8, NCW], BF16)
            wuq = sb("wuq", [128, 6, 768], BF16)
            wukv = sb("wukv", [128, 2, 1024], BF16)
            winv = WIN.rearrange("(k p) c -> p k c", p=128)
            for c0 in range(0, NCW, 648):
                load(win.t[:, :, c0:c0 + 648], winv[:, :, c0:c0 + 648], [win.h], [dh("WIN")])
            load(wuq.t[:], WUQ.rearrange("(k p) c -> p k c", p=128), [wuq.h], [dh("WUQ")])
            load(wukv.t[:], WUKV.rearrange("(k p) c -> p k c", p=128), [wukv.h], [dh("WUKV")])
            km = sb("km", [128, 4, 32], BF16)
            op("dve", lambda e: e.memset(km.t[:], 0.0), writes=[km.h])

            xt = [sb(f"xt{i}", [128, 8, 512], F32) for i in range(2)]
            xsq = sb("xsq", [128, 8, 512], BF16)
            hTs = [sb(f"hT{i}", [128, 8, 512], BF16, 8) for i in range(2)]
            rss = [sb(f"rs{i}", [128, 512], F32) for i in range(2)]
            tac = sb("tac", [96, 512], F32)
            tas = sb("tas", [96, 512], F32)
            tbc = sb("tbc", [128, 512], F32)
            tbs = sb("tbs", [128, 512], F32)
            tac2 = sb("tac2", [96, 512], F32)
            tas2 = sb("tas2", [96, 512], F32)
            tbc2 = sb("tbc2", [128, 512], F32)
            tbs2 = sb("tbs2", [128, 512], F32)
            cq = sb("cq", [128, 6, 512], BF16, 6)
            cqsq = sb("cqsq", [128, 6, 512], BF16, 6)
            cqn = sb("cqn", [128, 6, 512], BF16, 6)
            ckv = sb("ckv", [128, 2, 512], BF16, 2)
            ckvsq = sb("ckvsq", [128, 2, 512], BF16, 2)
            ckvn = sb("ckvn", [128, 2, 512], BF16, 2)
            rsq = sb("rsq", [128, 512], F32)
            rskv = sb("rskv", [128, 512], F32)
            NR = 3
            rsb = [sb(f"rsb{i}", [128, 512], BF16) for i in range(NR)]
            t1 = [sb(f"t1_{i}", [128, 512], F32) for i in range(NR)]
            t2 = [sb(f"t2_{i}", [128, 512], F32) for i in range(NR)]
            ro = [sb(f"ro{i}", [128, 512], BF16) for i in range(NR)]
            qbo = [sb(f"qbo{i}", [128, 512], BF16) for i in range(4)]
            evo = [sb(f"evo{i}", [128, 512], BF16) for i in range(6)]
            kms = sb("kms", [128, 2], F32)
            gsbs = [sb(f"gsb{i}", [128, 256], F32) for i in range(4)]
            t8s = [sb(f"t8_{i}", [128, 8, 8], F32, 8) for i in range(4)]
            thrs = [sb(f"thr{i}", [128, 8], F32) for i in range(4)]
            mkfs = [sb(f"mk_f{i}", [128, 256], F32, 8) for i in range(4)]
            mkbs = [sb(f"mkb{i}", [128, 256], BF16) for i in range(4)]
            mT = sb("mT", [128, 2, 512], BF16, 2)
            pm = [ps(f"pm{i}", [128, 512], F32) for i in range(3)]
            pp = [ps(f"pp{i}", [128, 512], F32) for i in range(1)]
            pstat = ps("pstat", [128, 512], F32)
            pgs = [ps(f"pg{i}", [128, 512], F32) for i in range(2)]
            ptr = ps("ptr", [128, 1024], BF16)
            ctr = {"pm": 0, "pp": 0, "r": 0, "ev": 0}

            def nxt(key, n):
                v = ctr[key] % n
                ctr[key] += 1
                return v

            def rope(src_ps, rows, perm, tc_, ts_, out_t=None, after=None):
                i = nxt("r", NR)
                a, b1, b2, o = rsb[i], t1[i], t2[i], (out_t or ro[i])
                op("act", lambda e: e.copy(out=a.t[0:rows, :], in_=src_ps.t[0:rows, :]), reads=[src_ps.h], writes=[a.h])

                def fin():
                    p2 = pp[nxt("pp", 1)]
                    mm(p2.t[0:rows, :], perm.t[0:rows, 0:rows], a.t[0:rows, :], True, True, [perm.h, a.h], [p2.h])
                    op("dve", lambda e: e.tensor_tensor(out=b1.t[0:rows, :], in0=p2.t[0:rows, :], in1=ts_.t[0:rows, :], op=ALU.mult),
                       reads=[p2.h, ts_.h], writes=[b1.h])
                    op("pool", lambda e: e.tensor_tensor(out=b2.t[0:rows, :], in0=a.t[0:rows, :], in1=tc_.t[0:rows, :], op=ALU.mult),
                       reads=[a.h, tc_.h], writes=[b2.h])
                    op("dve", lambda e: e.tensor_tensor(out=o.t[0:rows, :], in0=b1.t[0:rows, :], in1=b2.t[0:rows, :], op=ALU.add),
                       reads=[b1.h, b2.h], writes=[o.h])
                    if after is not None:
                        after(o)
                return fin

            def rstd_from(pst, dst, n):
                op("act", lambda e: e.activation(out=dst.t[:], in_=pst.t[:], func=AF.Ln, scale=1.0 / n, bias=EPS),
                   reads=[pst.h], writes=[dst.h])
                op("act", lambda e: e.activation(out=dst.t[:], in_=dst.t[:], func=AF.Exp, scale=-0.5), reads=[dst.h], writes=[dst.h])

            xTv = xT_d.rearrange("(k p) s -> p k s", p=128)
            HTv = HT.rearrange("(k p) s -> p k s", p=128)
            dq = []

            def defer(fn):
                dq.append(fn)
                while len(dq) > 1:
                    dq.pop(0)()

            def flush():
                while dq:
                    dq.pop(0)()

            gate_pend = []
            tabs = [(tac, tas, tbc, tbs), (tac2, tas2, tbc2, tbs2)]

            sq_done = {}

            def S1a(g):
                x_ = xt[g % 2]
                sq_done[g] = True
                op("act", lambda e: e.activation(out=xsq.t[:], in_=x_.t[:], func=AF.Square), reads=[x_.h], writes=[xsq.h])

            def S1(g):
                tok = slice(g * 512, (g + 1) * 512)
                x_, hT, rs = xt[g % 2], hTs[g % 2], rss[g % 2]
                ta_c, ta_s, tb_c, tb_s = tabs[g % 2]
                if not sq_done.get(g):
                    S1a(g)
                for k in range(8):
                    mm(pstat.t[:], onesb.t[:], xsq.t[:, k, :], k == 0, k == 7, [onesb.h, xsq.h], [pstat.h])
                rstd_from(pstat, rs, 1024.0)
                for k in range(8):
                    op("dve", lambda e, k=k: e.tensor_tensor(out=hT.t[:, k, :], in0=x_.t[:, k, :], in1=rs.t[:], op=ALU.mult),
                       reads=[x_.h, rs.h], writes=[hT.hs[k]])
                store(HTv[:, :, tok], hT.t[:], list(hT.hs), [dh("HT")])

            def S1_load(g):
                tok = slice(g * 512, (g + 1) * 512)
                x_ = xt[g % 2]
                ta_c, ta_s, tb_c, tb_s = tabs[g % 2]
                for dst, src in ((x_, xTv[:, :, tok]), (ta_c, c_tac[:, tok]), (ta_s, c_tas[:, tok]), (tb_c, c_tbc[:, tok]), (tb_s, c_tbs[:, tok])):
                    op("act", lambda e, dst=dst, src=src: e.dma_start(out=dst.t[:], in_=src), writes=[dst.h], dma=True)

            S1_load(0)
            S1(0)
            for g in range(NG):
                tok = slice(g * 512, (g + 1) * 512)
                if g + 1 < NG:
                    S1_load(g + 1)
                hT = hTs[g % 2]
                tac, tas, tbc, tbs = tabs[g % 2]
                for (dst, dsq, nchunk, col0) in ((cq, cqsq, 6, 0), (ckv, ckvsq, 2, 768)):
                    for c in range(nchunk):
                        p = pm[nxt("pm", 3)]
                        for k in range(8):
                            mm(p.t[:], win.t[:, k, col0 + c * 128: col0 + (c + 1) * 128], hT.t[:, k, :], k == 0, k == 7, [win.h, hT.hs[k]], [p.h])
                        op("act", lambda e, p=p, dst=dst, c=c: e.copy(out=dst.t[:, c, :], in_=p.t[:]), reads=[p.h], writes=[dst.hs[c]])
                        op("dve", lambda e, dst=dst, dsq=dsq, c=c: e.tensor_tensor(out=dsq.t[:, c, :], in0=dst.t[:, c, :], in1=dst.t[:, c, :], op=ALU.mult),
                           reads=[dst.hs[c]], writes=[dsq.hs[c]])

                def after_q(j):
                    def f(o):
                        store(QB[j * 128:(j + 1) * 128, tok], o.t[:], [o.h], [dh("QB")])
                    return f

                def after_k(j, g=g):
                    def f(o):
                        store(KB[j * 128:(j + 1) * 128, tok], o.t[:], [o.h], [dh("KB")])
                        op("dve", lambda e: e.tensor_reduce(out=kms.t[:], in_=o.t[:].rearrange("p (b t) -> p b t", t=256), axis=AX.X, op=ALU.add),
                           reads=[o.h], writes=[kms.h])
                        op("act", lambda e: e.activation(out=km.t[:, j, 2 * g:2 * g + 2], in_=kms.t[:], func=AF.Copy, scale=1.0 / 256),
                           reads=[kms.h], writes=[km.h])
                    return f

                for which, col0 in (("q", 1056), ("k", 1568)):
                    for j in range(4):
                        p = pm[nxt("pm", 3)]
                        for k in range(8):
                            mm(p.t[:], win.t[:, k, col0 + j * 128: col0 + (j + 1) * 128], hT.t[:, k, :], k == 0, k == 7, [win.h, hT.hs[k]], [p.h])
                        if which == "q":
                            defer(rope(p, 128, permb, tbc, tbs, out_t=qbo[j], after=after_q(j)))
                        else:
                            defer(rope(p, 128, permb, tbc, tbs, after=after_k(j)))
                while gate_pend:
                    gate_pend.pop(0)()
                if g + 1 < NG:
                    S1a(g + 1)
                for (dst, dsq, dn, nchunk, rr, nn) in ((cq, cqsq, cqn, 6, rsq, 768.0), (ckv, ckvsq, ckvn, 2, rskv, 256.0)):
                    for c in range(nchunk):
                        mm(pstat.t[:], onesb.t[:], dsq.t[:, c, :], c == 0, c == nchunk - 1, [onesb.h, dsq.hs[c]], [pstat.h])
                    rstd_from(pstat, rr, nn)
                    for c in range(nchunk):
                        op("dve", lambda e, dst=dst, dn=dn, c=c, rr=rr: e.tensor_tensor(out=dn.t[:, c, :], in0=dst.t[:, c, :], in1=rr.t[:], op=ALU.mult),
                           reads=[dst.hs[c], rr.h], writes=[dn.hs[c]])
                p = pm[nxt("pm", 3)]
                for k in range(8):
                    mm(p.t[0:96, :], win.t[:, k, 960:1056], hT.t[:, k, :], k == 0, k == 7, [win.h, hT.hs[k]], [p.h])
                defer(rope(p, 96, perma, tac, tas, after=lambda o: store(KRA[:, tok], o.t[64:96, :], [o.h], [dh("KRA")])))
                for tt in range(4):
                    p = pm[nxt("pm", 3)]
                    for k in range(8):
                        mm(p.t[:], hT.t[:, k, tt * 128:(tt + 1) * 128], win.t[:, k, 2080:2592], k == 0, k == 7, [win.h, hT.hs[k]], [p.h])
                    o = evo[nxt("ev", 6)]
                    op("act", lambda e, p=p, o=o: e.copy(out=o.t[:], in_=p.t[:]), reads=[p.h], writes=[o.h])
                    store(VB[g * 512 + tt * 128: g * 512 + (tt + 1) * 128, :], o.t[:], [o.h], [dh("VB")])
                for h in range(8):
                    p = pm[nxt("pm", 3)]
                    for c in range(6):
                        mm(p.t[0:96, :], wuq.t[:, c, h * 96:(h + 1) * 96], cqn.t[:, c, :], c == 0, c == 5, [wuq.h, cqn.hs[c]], [p.h])
                    defer(rope(p, 96, perma, tac, tas, after=(lambda h: (lambda o: store(QA[h * 96:(h + 1) * 96, tok], o.t[0:96, :], [o.h], [dh("QA")])))(h)))
                for j in range(4):
                    p = pm[nxt("pm", 3)]
                    for c in range(2):
                        mm(p.t[:], wukv.t[:, c, j * 128:(j + 1) * 128], ckvn.t[:, c, :], c == 0, c == 1, [wukv.h, ckvn.hs[c]], [p.h])
                    o = evo[nxt("ev", 6)]
                    op("act", lambda e, p=p, o=o: e.copy(out=o.t[:], in_=p.t[:]), reads=[p.h], writes=[o.h])
                    store(KNA[j * 128:(j + 1) * 128, tok], o.t[:], [o.h], [dh("KNA")])
                for tt in range(4):
                    p = pm[nxt("pm", 3)]
                    for c in range(2):
                        mm(p.t[:], ckvn.t[:, c, tt * 128:(tt + 1) * 128], wukv.t[:, c, 512:1024], c == 0, c == 1, [wukv.h, ckvn.hs[c]], [p.h])
                    o = evo[nxt("ev", 6)]
                    op("act", lambda e, p=p, o=o: e.copy(out=o.t[:], in_=p.t[:]), reads=[p.h], writes=[o.h])
                    store(VA[g * 512 + tt * 128: g * 512 + (tt + 1) * 128, :], o.t[:], [o.h], [dh("VA")])
                if g + 1 < NG:
                    S1(g + 1)
                flush()
                for tt in range(4):
                    qt = g * 4 + tt
                    own = qt // 2
                    pg_ = pgs[tt % 2]
                    gsb_ = gsbs[tt]
                    for h in range(8):
                        j, r0 = h // 2, (h % 2) * 64
                        mm(pg_.t[:, h * 32:(h + 1) * 32], qbo[j].t[r0:r0 + 64, tt * 128:(tt + 1) * 128], km.t[r0:r0 + 64, j, :], True, False,
                           [qbo[j].h, km.h], [pg_.h])
                        mm(pg_.t[:, h * 32:(h + 1) * 32], onesb.t[0:1, :], wneg.t[0:1, 32 - own:64 - own], False, True,
                           [onesb.h, wneg.h], [pg_.h])
                    op("act", lambda e, pg_=pg_, gsb_=gsb_: e.copy(out=gsb_.t[:], in_=pg_.t[:, 0:256]), reads=[pg_.h], writes=[gsb_.h])
                for h in range(8):
                    for tt in range(4):
                        gsb_, t8_ = gsbs[tt], t8s[tt]
                        op("dve", lambda e, h=h, gsb_=gsb_, t8_=t8_: e.max(out=t8_.t[:, h, :], in_=gsb_.t[:, h * 32:(h + 1) * 32]), reads=[gsb_.h], writes=[t8_.hs[h]])
                for tt in range(4):
                    t8_, thr_ = t8s[tt], thrs[tt]
                    op("dve", lambda e, t8_=t8_, thr_=thr_: e.tensor_scalar_max(out=thr_.t[:], in0=t8_.t[:, :, 3], scalar1=-1e29), reads=list(t8_.hs), writes=[thr_.h])
                for h in range(8):
                    for tt in range(4):
                        gsb_, thr_, mkf_ = gsbs[tt], thrs[tt], mkfs[tt]
                        op("dve", lambda e, h=h, gsb_=gsb_, thr_=thr_, mkf_=mkf_: e.tensor_scalar(out=mkf_.t[:, h * 32:(h + 1) * 32], in0=gsb_.t[:, h * 32:(h + 1) * 32],
                                                                 scalar1=thr_.t[:, h:h + 1], scalar2=30000.0, op0=ALU.is_ge, op1=ALU.mult),
                           reads=[gsb_.h, thr_.h], writes=[mkf_.hs[h]])
                for tt in range(4):
                    mkf_, mkb_ = mkfs[tt], mkbs[tt]
                    op("dve", lambda e, mkf_=mkf_, mkb_=mkb_: e.tensor_scalar_add(out=mkb_.t[:], in0=mkf_.t[:], scalar1=-30000.0), reads=list(mkf_.hs), writes=[mkb_.h])

                def gate_fin(g=g, tok=tok):
                    for tt in range(4):
                        mkb_ = mkbs[tt]
                        for half in range(2):
                            op("pe", lambda e, half=half, mkb_=mkb_: e.transpose(out=ptr.t[:, half * 128:(half + 1) * 128], in_=mkb_.t[:, half * 128:(half + 1) * 128], identity=identb.t[:]),
                               reads=[mkb_.h, identb.h], writes=[ptr.h])
                        op("act", lambda e, tt=tt: e.copy(out=mT.t[:, :, tt * 128:(tt + 1) * 128], in_=ptr.t[:, 0:256].rearrange("p (a b) -> p a b", a=2)),
                           reads=[ptr.h], writes=[mT.h])
                    for half in range(2):
                        store(MB[half * 128:(half + 1) * 128, tok], mT.t[:, half, :], [mT.h], [dh("MB")])
                gate_pend.append(gate_fin)
            while gate_pend:
                gate_pend.pop(0)()
            store_q[0] = "pool"
            Sx.barrier()
        if stop_after <= 1:
            return _finish(nc, Sx, out_d, S)

        with ExitStack() as st:
            sb, ps = mk(st)
            maskc = sb("maskc", [128, 2, 1024], BF16)
            load(maskc.t[:], c_mask.rearrange("p (a b) c -> p a (b c)", a=2), [maskc.h])
            QT = [sb(f"QT{i}", [96, S], BF16) for i in range(2)]
            KT = [sb(f"KT{i}", [96, S], BF16) for i in range(2)]
            VV = [sb(f"VV{i}", [128, NT, 128], BF16) for i in range(2)]
            for v in VV:
                op("dve", lambda e, v=v: e.memset(v.t[:, :, 64:128], 1.0), writes=[v.h])
            NP = 4
            PT = [sb(f"PT{i}", [128, 1024], BF16) for i in range(NP)]
            rcp = [sb(f"rcp{i}", [128, 512], F32) for i in range(2)]
            rc0 = [sb(f"rc0{i}", [64, 512], F32) for i in range(2)]
            onb = [sb(f"onb{i}", [64, 512], BF16) for i in range(2)]
            psc = [ps(f"psc{i}", [128, 1024], F32) for i in range(3)]
            pov = [ps(f"pov{i}", [128, 512], F32) for i in range(2)]
            VAv = VA.rearrange("(n p) f -> p n f", p=128)
            VBv = VB.rearrange("(n p) f -> p n f", p=128)
            stg3 = [sb(f"stg3{i}", [128, 8, 512], F32) for i in range(2)]
            obf3 = [sb(f"obf3{i}", [128, 8, 512], BF16) for i in range(2)]
            gain3 = sb("gain3", [128, 8], F32)
            load(gain3.t[:], attn_norm_d, [gain3.h])
            cnt3 = [0]
            c3q = []

            def conv3(src, dst, dname, nk, ncols, use_gain, cstart=0):
                srcv = src.rearrange("(k p) c -> p k c", p=128)
                dstv = dst.rearrange("(k p) c -> p k c", p=128)
                for c0 in range(cstart, ncols, 512):
                    cw = min(512, ncols - c0)
                    i = cnt3[0] % 2
                    cnt3[0] += 1
                    a_, b_ = stg3[i], obf3[i]
                    c3q.append(lambda a_=a_, c0=c0, cw=cw: load(a_.t[:, 0:nk, 0:cw], srcv[:, :, c0:c0 + cw], [a_.h]))
                    for k in range(nk):
                        if use_gain:
                            c3q.append(lambda a_=a_, b_=b_, k=k, cw=cw: op("pool", lambda e: e.tensor_scalar_mul(out=b_.t[:, k, 0:cw], in0=a_.t[:, k, 0:cw], scalar1=gain3.t[:, k:k + 1]),
                                                                      reads=[a_.h, gain3.h], writes=[b_.h]))
                        else:
                            c3q.append(lambda a_=a_, b_=b_, k=k, cw=cw: op("pool", lambda e: e.tensor_copy(out=b_.t[:, k, 0:cw], in_=a_.t[:, k, 0:cw]),
                                                                      reads=[a_.h], writes=[b_.h]))
                    c3q.append(lambda b_=b_, c0=c0, cw=cw: store(dstv[:, :, c0:c0 + cw], b_.t[:, 0:nk, 0:cw], [b_.h], [dh(dname)]))

            LA = 2
            pend = []
            state = {"ui": 0, "gi": 0}

            def emit_pair(q_, k_, v_, sc, g, kp, nkp, odst, on, h):
                ui = state["ui"]
                state["ui"] += 1
                pscore = psc[ui % 3]
                pt = PT[ui % NP]
                for j in range(2):
                    kt = 2 * kp + j
                    mm(pscore.t[:, j * 512:(j + 1) * 512], k_.t[0:96, kt * 128:(kt + 1) * 128], q_.t[0:96, g * 512:(g + 1) * 512], True, True,
                       [k_.h, q_.h], [pscore.h])
                op("act", lambda e: e.activation(out=pt.t[:], in_=pscore.t[:], func=AF.Exp, scale=sc), reads=[pscore.h], writes=[pt.h])
                r = 2 * kp - 4 * g
                if r >= 0:
                    for j in range(2):
                        wd = (r + j + 1) * 128
                        op("dve", lambda e, j=j, wd=wd: e.tensor_tensor(out=pt.t[:, j * 512:j * 512 + wd], in0=pt.t[:, j * 512:j * 512 + wd],
                                                                      in1=maskc.t[:, r // 2, j * 512:j * 512 + wd], op=ALU.mult),
                           reads=[pt.h, maskc.h], writes=[pt.h])
                first, last = (kp == 0), (kp == nkp - 1)
                if first:
                    state["gi"] += 1
                gi = state["gi"]
                po = pov[gi % 2]

                def pv():
                    for j in range(2):
                        kt = 2 * kp + j
                        mm(po.t[:], v_.t[:, kt, :], pt.t[:, j * 512:(j + 1) * 512], first and j == 0, last and j == 1, [v_.h, pt.h], [po.h])
                    if last:
                        rc, r0, onb_ = rcp[gi % 2], rc0[gi % 2], onb[gi % 2]
                        op("dve", lambda e: e.reciprocal(out=rc.t[64:128, :], in_=po.t[64:128, :]), reads=[po.h], writes=[rc.h])
                        op("dve", lambda e: e.tensor_copy(out=r0.t[0:64, :], in_=rc.t[64:128, :]), reads=[rc.h], writes=[r0.h])
                        op("dve", lambda e: e.tensor_tensor(out=onb_.t[:], in0=po.t[0:64, :], in1=r0.t[0:64, :], op=ALU.mult),
                           reads=[po.h, r0.h], writes=[onb_.h])
                        store(odst[h * 64:(h + 1) * 64, g * 512:(g + 1) * 512], onb_.t[:], [onb_.h], [dh(on)])
                return pv

            for hp in range(16):
                typ, h = hp // 8, hp % 8
                q_, k_, v_ = QT[hp % 2], KT[hp % 2], VV[hp % 2]
                if typ == 0:
                    load(q_.t[0:96, :], QA[h * 96:(h + 1) * 96, :], [q_.h], [dh("QA")])
                    load(k_.t[0:64, :], KNA[h * 64:(h + 1) * 64, :], [k_.h], [dh("KNA")])
                    load(k_.t[64:96, :], KRA[:, :], [k_.h], [dh("KRA")])
                    vsrc, vn, sc, odst, on = VAv, "VA", 96.0 ** -0.5, OA, "OA"
                else:
                    load(q_.t[0:64, :], QB[h * 64:(h + 1) * 64, :], [q_.h], [dh("QB")])
                    load(q_.t[64:96, :], MB[h * 32:(h + 1) * 32, :], [q_.h], [dh("MB")])
                    load(k_.t[0:64, :], KB[h * 64:(h + 1) * 64, :], [k_.h], [dh("KB")])
                    load(k_.t[64:96, :], c_oh[:, :], [k_.h])
                    vsrc, vn, sc, odst, on = VBv, "VB", 64.0 ** -0.5, OB, "OB"
                vstep = max(1, NT // 4)
                for n0 in range(0, NT, vstep):
                    load(v_.t[:, n0:n0 + vstep, 0:64], vsrc[:, n0:n0 + vstep, h * 64:(h + 1) * 64], [v_.h], [dh(vn)])
                if hp == 3:
                    zt = sb("zt", [128, 4, D], BF16)
                    op("pool", lambda e: e.memset(zt.t[:], 0.0), writes=[zt.h])
                    XSv = XS.rearrange("(n p) d -> p n d", p=128)
                    zlist = list(range(0, NSLOT // 128, 4))
                if hp >= 3:
                    nz = -(-len(zlist) // 12) if hp < 15 else len(zlist)
                    for _ in range(min(nz, len(zlist))):
                        n0 = zlist.pop(0)
                        store(XSv[:, n0:n0 + 4, :], zt.t[:], [zt.h], [dh("XS")])
                if hp == 2:
                    conv3(w_in_d, WIN, "WIN", 8, 4640, True, cstart=2592)
                    conv3(w_oa_d, WOA, "WOA", 4, D, False)
                    conv3(w_ob_d, WOB, "WOB", 4, D, False)
                    conv3(w_out_d, WOUT, "WOUT", 8, D, False)
                for g in range(NG):
                    nkp = 2 * g + 2
                    if hp >= 2 and c3q and g >= 4:
                        c3q.pop(0)()
                    for kp in range(nkp):
                        pend.append(emit_pair(q_, k_, v_, sc, g, kp, nkp, odst, on, h))
                        if len(pend) > LA:
                            pend.pop(0)()
            while pend:
                pend.pop(0)()
            while c3q:
                c3q.pop(0)()
            Sx.barrier()
        if stop_after <= 3:
            return _finish(nc, Sx, out_d, S)

        with ExitStack() as st:
            sb, ps = mk(st)
            identf = sb("identf", [128, 128], F32)
            onesf = sb("onesf4", [128, 128], F32)
            onesb = sb("onesb4", [128, 128], BF16)
            triu = sb("triu", [128, 128], BF16)
            offs = sb("offs", [128, 32], F32)
            load(identf.t[:], c_identf, [identf.h])
            load(onesf.t[:], c_onesf, [onesf.h])
            load(onesb.t[:], c_onesb, [onesb.h])
            load(triu.t[:], c_triu, [triu.h])
            load(offs.t[:], c_offs, [offs.h])
            wg = sb("wgate", [128, 8, 2048], BF16)
            woa = sb("woa", [128, 4, D], BF16)
            wob = sb("wob", [128, 4, D], BF16)
            wout = sb("wout", [128, 8, D], BF16)
            winv = WIN.rearrange("(k p) c -> p k c", p=128)
            for c0 in range(0, 2048, 512):
                load(wg.t[:, :, c0:c0 + 512], winv[:, :, 2592 + c0:2592 + c0 + 512], [wg.h], [dh("WIN")])
            load(woa.t[:], WOA.rearrange("(k p) c -> p k c", p=128), [woa.h], [dh("WOA")])
            load(wob.t[:], WOB.rearrange("(k p) c -> p k c", p=128), [wob.h], [dh("WOB")])
            for c0 in range(0, D, 512):
                load(wout.t[:, :, c0:c0 + 512], WOUT.rearrange("(k p) c -> p k c", p=128)[:, :, c0:c0 + 512], [wout.h], [dh("WOUT")])
            gf = sb("gf", [128, 8], F32)
            wr_s = sb("wr_s", [128, 8, 36], F32)
            wr = sb("wr", [128, 8, 36], F32)
            br = sb("br", [1, 36], F32)
            load(gf.t[:], ffn_norm_d, [gf.h])
            load(wr_s.t[:], w_r_d.rearrange("(k p) c -> p k c", p=128), [wr_s.h])
            load(br.t[:], b_r_d, [br.h])
            for k in range(8):
                op("dve", lambda e, k=k: e.tensor_scalar_mul(out=wr.t[:, k, :], in0=wr_s.t[:, k, :], scalar1=gf.t[:, k:k + 1]),
                   reads=[wr_s.h, gf.h], writes=[wr.h])
            run = sb("run", [128, 32], F32)
            op("dve", lambda e: e.memset(run.t[:], 0.0), writes=[run.h])

            hTg = sb("hTg", [128, 8, 512], BF16)
            oag = sb("oag", [128, 4, 512], BF16)
            obg = sb("obg", [128, 4, 512], BF16)
            sig = [sb(f"sig{i}", [128, 512], F32) for i in range(2)]
            m1 = [sb(f"m1_{i}", [128, 512], F32) for i in range(2)]
            mixT = sb("mixT", [128, 8, 512], BF16, 8)
            xtok = [sb(f"xtok{i}", [128, D], F32) for i in range(2)]
            x1 = [sb(f"x1_{i}", [128, D], F32) for i in range(2)]
            junk = sb("junk", [128, D], F32)
            pA = [ps(f"pA{i}", [128, 512], F32) for i in range(2)]
            pG = [ps(f"pG{i}", [128, 512], F32) for i in range(2)]
            pY = [ps(f"pY{i}", [128, 512], F32) for i in range(2)]
            pTr = ps("pTr", [128, 512], F32)
            pL = ps("pL", [128, 512], F32)
            hpC = H()
            HTv = HT.rearrange("(k p) s -> p k s", p=128)
            OAv = OA.rearrange("(k p) s -> p k s", p=128)
            OBv = OB.rearrange("(k p) s -> p k s", p=128)
            RS = []
            for i in range(4):
                RS.append(dict(
                    L=sb(f"L{i}", [128, 36], F32), gmax=sb(f"gmax{i}", [128, 1], F32), ngmax=sb(f"ngmax{i}", [128, 1], F32),
                    sume=sb(f"sume{i}", [128, 1], F32), pgrp=sb(f"pgrp{i}", [128, 1], F32), mx1=sb(f"mx1{i}", [128, 1], F32),
                    mx2=sb(f"mx2{i}", [128, 1], F32), dd=sb(f"dd{i}", [128, 1], F32), sg1=sb(f"sg1{i}", [128, 1], F32),
                    gone=sb(f"gone{i}", [128, 4], F32),
                    ein=sb(f"ein{i}", [128, 8], F32), ein2=sb(f"ein2{i}", [128, 8], F32), one1=sb(f"one1{i}", [128, 8], F32),
                    one2=sb(f"one2{i}", [128, 8], F32), ex4=sb(f"ex4{i}", [128, 4], F32), E1=sb(f"E1{i}", [128, 32], F32),
                    E2=sb(f"E2{i}", [128, 32], F32), Ab=sb(f"Ab{i}", [128, 32], BF16), pos=sb(f"pos{i}", [128, 32], F32),
                    tmpa=sb(f"tmpa{i}", [128, 32], F32), tmpb=sb(f"tmpb{i}", [128, 32], F32), slf=sb(f"slf{i}", [128, 2], F32),
                    ss1=sb(f"ss1{i}", [128, 2], F32), hnT=sb(f"hnT{i}", [128, 8, 128], F32)))
            hn4 = [sb(f"hn4{i}", [128, D], F32) for i in range(4)]
            hnb8 = [sb(f"hnb8{i}", [128, D], BF16) for i in range(8)]

            bgq = []

            def bg_run(n):
                for _ in range(n):
                    if bgq:
                        bgq.pop(0)()

            def stageA(ti, tt):
                rows = slice(ti * 128, (ti + 1) * 128)
                xk, x1_, hn_, hnb_ = xtok[ti % 2], x1[ti % 2], hn4[ti % 4], hnb8[ti % 8]
                ss1_ = RS[ti % 4]["ss1"]
                load(xk.t[:], x_d[rows, :], [xk.h])
                for half in range(2):
                    py = pY[half]
                    for k in range(8):
                        mm(py.t[:], mixT.t[:, k, tt * 128:(tt + 1) * 128], wout.t[:, k, half * 512:(half + 1) * 512], k == 0, k == 7,
                           [mixT.hs[k], wout.h], [py.h])
                    op("dve", lambda e, py=py, half=half: e.tensor_tensor(out=x1_.t[:, half * 512:(half + 1) * 512], in0=xk.t[:, half * 512:(half + 1) * 512], in1=py.t[:], op=ALU.add),
                       reads=[py.h, xk.h], writes=[x1_.h])
                op("act", lambda e: e.dma_start(out=X1[rows, :], in_=x1_.t[:]), reads=[x1_.h], writes=[dh("X1")], dma=True)
                op("act", lambda e: e.activation(out=junk.t[:], in_=x1_.t[:], func=AF.Square, accum_out=ss1_.t[:, 0:1]),
                   reads=[x1_.h], writes=[junk.h, ss1_.h])
                op("act", lambda e: e.activation(out=ss1_.t[:, 1:2], in_=ss1_.t[:, 0:1], func=AF.Sqrt, scale=1.0 / D, bias=EPS), reads=[ss1_.h], writes=[ss1_.h])
                op("dve", lambda e: e.reciprocal(out=ss1_.t[:, 1:2], in_=ss1_.t[:, 1:2]), reads=[ss1_.h], writes=[ss1_.h])
                op("dve", lambda e: e.tensor_scalar_mul(out=hn_.t[:], in0=x1_.t[:], scalar1=ss1_.t[:, 1:2]), reads=[x1_.h, ss1_.h], writes=[hn_.h])
                op("act", lambda e: e.copy(out=hnb_.t[:], in_=hn_.t[:]), reads=[hn_.h], writes=[hnb_.h])

            def stageB4(g):
                tis = [g * 4 + tt for tt in range(4)]
                for ti in tis:
                    R = RS[ti % 4]
                    hn_, hnT_, L = hn4[ti % 4], R["hnT"], R["L"]
                    for k in range(8):
                        op("pe", lambda e, k=k, hn_=hn_: e.transpose(out=pTr.t[:, (k % 4) * 128:(k % 4 + 1) * 128], in_=hn_.t[:, k * 128:(k + 1) * 128], identity=identf.t[:]),
                           reads=[hn_.h, identf.h], writes=[pTr.h])
                        if k % 4 == 3:
                            kk = k // 4
                            op("act", lambda e, kk=kk, hnT_=hnT_: e.copy(out=hnT_.t[:, kk * 4:(kk + 1) * 4, :], in_=pTr.t[:].rearrange("p (a b) -> p a b", a=4)),
                               reads=[pTr.h], writes=[hnT_.h])
                for ti in tis:
                    R = RS[ti % 4]
                    hnT_, L = R["hnT"], R["L"]
                    c0 = (ti % 4) * 64
                    for k in range(8):
                        mm(pL.t[:, c0:c0 + 36], hnT_.t[:, k, :], wr.t[:, k, :], k == 0, False, [hnT_.h, wr.h], [pL.h])
                    mm(pL.t[:, c0:c0 + 36], onesf.t[0:1, :], br.t[0:1, :], False, True, [onesf.h, br.h], [pL.h])
                for ti in tis:
                    R = RS[ti % 4]
                    c0 = (ti % 4) * 64
                    op("act", lambda e, R=R, c0=c0: e.copy(out=R["L"].t[:], in_=pL.t[:, c0:c0 + 36]), reads=[pL.h], writes=[R["L"].h])

                def each(fn):
                    def step():
                        for ti in tis:
                            fn(ti, RS[ti % 4])
                    bgq.append(step)
                each(lambda ti, R: op("dve", lambda e: e.tensor_reduce(out=R["gmax"].t[:], in_=R["L"].t[:, 0:4], axis=AX.X, op=ALU.max), reads=[R["L"].h], writes=[R["gmax"].h]))
                each(lambda ti, R: op("dve", lambda e: e.tensor_scalar(out=R["gone"].t[:], in0=R["L"].t[:, 0:4], scalar1=R["gmax"].t[:, 0:1], scalar2=None, op0=ALU.is_equal), reads=[R["L"].h, R["gmax"].h], writes=[R["gone"].h]))
                each(lambda ti, R: op("dve", lambda e: e.tensor_scalar_mul(out=R["ngmax"].t[:], in0=R["gmax"].t[:], scalar1=-1.0), reads=[R["gmax"].h], writes=[R["ngmax"].h]))
                each(lambda ti, R: op("dve", lambda e: e.tensor_scalar_mul(out=R["ein"].t[:], in0=R["L"].t[:, 4:12], scalar1=R["gone"].t[:, 0:1]), reads=[R["L"].h, R["gone"].h], writes=[R["ein"].h]))
                each(lambda ti, R: op("act", lambda e: e.activation(out=R["ex4"].t[:], in_=R["L"].t[:, 0:4], func=AF.Exp, bias=R["ngmax"].t[:, 0:1], accum_out=R["sume"].t[:, 0:1]),
                                     reads=[R["L"].h, R["ngmax"].h], writes=[R["ex4"].h, R["sume"].h]))
                for gg in range(1, 4):
                    each(lambda ti, R, gg=gg: op("dve", lambda e: e.scalar_tensor_tensor(out=R["ein"].t[:], in0=R["L"].t[:, 4 + 8 * gg:12 + 8 * gg], scalar=R["gone"].t[:, gg:gg + 1], in1=R["ein"].t[:], op0=ALU.mult, op1=ALU.add),
                                                 reads=[R["L"].h, R["gone"].h, R["ein"].h], writes=[R["ein"].h]))
                each(lambda ti, R: op("dve", lambda e: e.tensor_reduce(out=R["mx1"].t[:], in_=R["ein"].t[:], axis=AX.X, op=ALU.max), reads=[R["ein"].h], writes=[R["mx1"].h]))
                each(lambda ti, R: op("dve", lambda e: e.tensor_scalar(out=R["one1"].t[:], in0=R["ein"].t[:], scalar1=R["mx1"].t[:, 0:1], scalar2=None, op0=ALU.is_equal), reads=[R["ein"].h, R["mx1"].h], writes=[R["one1"].h]))
                each(lambda ti, R: op("dve", lambda e: e.scalar_tensor_tensor(out=R["ein2"].t[:], in0=R["one1"].t[:], scalar=-1e30, in1=R["ein"].t[:], op0=ALU.mult, op1=ALU.add), reads=[R["one1"].h, R["ein"].h], writes=[R["ein2"].h]))
                each(lambda ti, R: op("dve", lambda e: e.tensor_reduce(out=R["mx2"].t[:], in_=R["ein2"].t[:], axis=AX.X, op=ALU.max), reads=[R["ein2"].h], writes=[R["mx2"].h]))
                each(lambda ti, R: op("dve", lambda e: e.tensor_scalar(out=R["one2"].t[:], in0=R["ein2"].t[:], scalar1=R["mx2"].t[:, 0:1], scalar2=None, op0=ALU.is_equal), reads=[R["ein2"].h, R["mx2"].h], writes=[R["one2"].h]))
                each(lambda ti, R: op("dve", lambda e: e.tensor_tensor(out=R["dd"].t[:], in0=R["mx1"].t[:], in1=R["mx2"].t[:], op=ALU.subtract), reads=[R["mx1"].h, R["mx2"].h], writes=[R["dd"].h]))
                each(lambda ti, R: op("dve", lambda e: e.reciprocal(out=R["pgrp"].t[:], in_=R["sume"].t[:]), reads=[R["sume"].h], writes=[R["pgrp"].h]))
                each(lambda ti, R: op("act", lambda e: e.activation(out=R["sg1"].t[:], in_=R["dd"].t[:], func=AF.Sigmoid), reads=[R["dd"].h], writes=[R["sg1"].h]))
                for gg in range(4):
                    each(lambda ti, R, gg=gg: op("dve", lambda e: e.tensor_scalar_mul(out=R["E1"].t[:, gg * 8:(gg + 1) * 8], in0=R["one1"].t[:], scalar1=R["gone"].t[:, gg:gg + 1]), reads=[R["one1"].h, R["gone"].h], writes=[R["E1"].h]))
                    each(lambda ti, R, gg=gg: op("dve", lambda e: e.tensor_scalar_mul(out=R["E2"].t[:, gg * 8:(gg + 1) * 8], in0=R["one2"].t[:], scalar1=R["gone"].t[:, gg:gg + 1]), reads=[R["one2"].h, R["gone"].h], writes=[R["E2"].h]))
                each(lambda ti, R: op("dve", lambda e: e.tensor_tensor(out=R["Ab"].t[:], in0=R["E1"].t[:], in1=R["E2"].t[:], op=ALU.add), reads=[R["E1"].h, R["E2"].h], writes=[R["Ab"].h]))
                each(lambda ti, R: op("dve", lambda e: e.tensor_tensor(out=wts.t[:, ti, 0:1], in0=R["sg1"].t[:], in1=R["pgrp"].t[:], op=ALU.mult), reads=[R["sg1"].h, R["pgrp"].h], writes=[wts.hs[ti % 4]]))
                each(lambda ti, R: op("dve", lambda e: e.tensor_tensor(out=wts.t[:, ti, 1:2], in0=R["pgrp"].t[:], in1=wts.t[:, ti, 0:1], op=ALU.subtract), reads=[R["pgrp"].h, wts.hs[ti % 4]], writes=[wts.hs[ti % 4]]))

            def stageC4(g):
                tis = [g * 4 + tt for tt in range(4)]
                for ti in tis:
                    R = RS[ti % 4]
                    c0 = (ti % 4) * 64
                    mm(pL.t[:, 256 + c0:256 + c0 + 32], triu.t[:], R["Ab"].t[:], True, True, [triu.h, R["Ab"].h], [hpC])
                    mm(pL.t[:, 256 + c0 + 32:256 + c0 + 64], onesb.t[:], R["Ab"].t[:], True, True, [onesb.h, R["Ab"].h], [hpC])
                for ti in tis:
                    R = RS[ti % 4]
                    c0 = (ti % 4) * 64
                    op("dve", lambda e, R=R, c0=c0: e.tensor_tensor(out=R["pos"].t[:], in0=pL.t[:, 256 + c0:256 + c0 + 32], in1=run.t[:], op=ALU.add), reads=[hpC, run.h], writes=[R["pos"].h])
                    op("dve", lambda e, c0=c0: e.tensor_tensor(out=run.t[:], in0=pL.t[:, 256 + c0 + 32:256 + c0 + 64], in1=run.t[:], op=ALU.add), reads=[hpC, run.h], writes=[run.h])

                def each(fn):
                    for ti in tis:
                        fn(ti, RS[ti % 4])
                each(lambda ti, R: op("dve", lambda e: e.tensor_tensor(out=R["pos"].t[:], in0=R["pos"].t[:], in1=offs.t[:], op=ALU.add), reads=[R["pos"].h, offs.h], writes=[R["pos"].h]))
                each(lambda ti, R: op("dve", lambda e: e.tensor_tensor(out=R["tmpa"].t[:], in0=R["pos"].t[:], in1=R["E1"].t[:], op=ALU.mult), reads=[R["pos"].h, R["E1"].h], writes=[R["tmpa"].h]))
                each(lambda ti, R: op("dve", lambda e: e.tensor_tensor(out=R["tmpb"].t[:], in0=R["pos"].t[:], in1=R["E2"].t[:], op=ALU.mult), reads=[R["pos"].h, R["E2"].h], writes=[R["tmpb"].h]))
                each(lambda ti, R: op("dve", lambda e: e.tensor_reduce(out=R["slf"].t[:, 0:1], in_=R["tmpa"].t[:], axis=AX.X, op=ALU.add), reads=[R["tmpa"].h], writes=[R["slf"].h]))
                each(lambda ti, R: op("dve", lambda e: e.tensor_reduce(out=R["slf"].t[:, 1:2], in_=R["tmpb"].t[:], axis=AX.X, op=ALU.add), reads=[R["tmpb"].h, R["slf"].h], writes=[R["slf"].h]))
                each(lambda ti, R: op("dve", lambda e: e.tensor_copy(out=slot_i.t[:, ti, :], in_=R["slf"].t[:]), reads=[R["slf"].h], writes=[slot_i.hs[ti % 4]]))
                for ti in tis:
                    hnb_ = hnb8[ti % 8]
                    for j in range(2):
                        op("pool", lambda e, j=j, ti=ti, hnb_=hnb_: e.indirect_dma_start(
                            out=XS, out_offset=bass.IndirectOffsetOnAxis(ap=slot_i.t[:, ti, j:j + 1], axis=0),
                            in_=hnb_.t[:], in_offset=None), reads=[hnb_.h, slot_i.hs[ti % 4]], writes=[dh("XS")], dma=True)

            ci = 0
            for g in range(NG):
                tok = slice(g * 512, (g + 1) * 512)
                load(hTg.t[:], HTv[:, :, tok], [hTg.h], [dh("HT")])
                load(oag.t[:], OAv[:, :, tok], [oag.h], [dh("OA")])
                load(obg.t[:], OBv[:, :, tok], [obg.h], [dh("OB")])
                it = 0
                for c in range(8):
                    for br_i, (og, wo, gc0) in enumerate(((oag, woa, 0), (obg, wob, 1024))):
                        if it == 3 and g >= 1:
                            stageB4(g - 1)
                        it += 1
                        pa, pg_ = pA[ci % 2], pG[ci % 2]
                        sg, mm1 = sig[ci % 2], m1[ci % 2]
                        ci += 1
                        for k in range(4):
                            mm(pa.t[:], wo.t[:, k, c * 128:(c + 1) * 128], og.t[:, k, :], k == 0, k == 3, [wo.h, og.h], [pa.h])
                        for k in range(8):
                            mm(pg_.t[:], wg.t[:, k, gc0 + c * 128: gc0 + (c + 1) * 128], hTg.t[:, k, :], k == 0, k == 7, [wg.h, hTg.h], [pg_.h])
                        op("act", lambda e, pg_=pg_, sg=sg: e.activation(out=sg.t[:], in_=pg_.t[:], func=AF.Sigmoid), reads=[pg_.h], writes=[sg.h])
                        if br_i == 0:
                            op("dve", lambda e, pa=pa, sg=sg, mm1=mm1: e.tensor_tensor(out=mm1.t[:], in0=sg.t[:], in1=pa.t[:], op=ALU.mult),
                               reads=[sg.h, pa.h], writes=[mm1.h])
                            prev = mm1
                        else:
                            op("dve", lambda e, pa=pa, sg=sg: e.tensor_tensor(out=sg.t[:], in0=sg.t[:], in1=pa.t[:], op=ALU.mult),
                               reads=[sg.h, pa.h], writes=[sg.h])
                            op("dve", lambda e, sg=sg, prev=prev, c=c: e.tensor_tensor(out=mixT.t[:, c, :], in0=sg.t[:], in1=prev.t[:], op=ALU.add),
                               reads=[sg.h, prev.h], writes=[mixT.hs[c]])
                        bg_run(3)
                bg_run(10 ** 6)
                if g >= 1:
                    stageC4(g - 1)
                for tt in range(4):
                    stageA(g * 4 + tt, tt)
            stageB4(NG - 1)
            bg_run(10 ** 6)
            stageC4(NG - 1)
            Sx.barrier()
        if stop_after <= 4:
            return _finish(nc, Sx, out_d, S)

        with ExitStack() as st:
            sb, ps = mk(st)
            identb5x = sb("identb5", [128, 128], BF16)
            load(identb5x.t[:], c_identb, [identb5x.h])
            gf5v = sb("gf5", [128, 8], F32)
            load(gf5v.t[:], ffn_norm_d, [gf5v.h])
            NST = C // 128
            HALF = C // 2
            NSH = HALF // 128
            wgs = [sb(f"wgs{i}", [128, 8, 256], F32) for i in range(2)]
            wus = [sb(f"wus{i}", [128, 8, 256], F32) for i in range(2)]
            wds = [sb(f"wds{i}", [128, 2, D], F32) for i in range(2)]
            wgb = [sb(f"wgb{i}", [128, 8, 256], BF16) for i in range(2)]
            wub = [sb(f"wub{i}", [128, 8, 256], BF16) for i in range(2)]
            wdb = [sb(f"wdb{i}", [128, 2, D], BF16) for i in range(2)]
            xs = [sb(f"xs{i}", [128, D], BF16) for i in range(6)]
            xeT = [sb(f"xeT{i}", [128, 8, C], BF16) for i in range(2)]
            sil = [sb(f"sil{i}", [128, HALF], F32) for i in range(2)]
            actT = [sb(f"actT{i}", [128, 2, HALF], BF16, 2) for i in range(2)]
            ysb = [sb(f"ysb{i}", [128, D], BF16) for i in range(6)]
            ptx = [ps(f"ptx{i}", [128, 1024], BF16) for i in range(2)]
            pgu = [ps(f"pgu{i}", [128, 512], F32) for i in range(4)]
            pyy = [ps(f"pyy{i}", [128, 512], F32) for i in range(2)]
            xi = [0]
            yi = [0]

            def stage_load(ex):
                b = ex % 2
                load(wgs[b].t[:], w_g_d[ex].rearrange("(k p) f -> p k f", p=128), [wgs[b].h])
                load(wus[b].t[:], w_u_d[ex].rearrange("(k p) f -> p k f", p=128), [wus[b].h])
                load(wds[b].t[:], w_d_d[ex].rearrange("(k p) f -> p k f", p=128), [wds[b].h])

            def conv_part(ex, part):
                b = ex % 2
                for k in (2 * part, 2 * part + 1):
                    op("act", lambda e, k=k, b=b: e.activation(out=wgb[b].t[:, k, :], in_=wgs[b].t[:, k, :], func=AF.Copy, scale=gf5v.t[:, k:k + 1]),
                       reads=[wgs[b].h, gf5v.h], writes=[wgb[b].h])
                    op("dve", lambda e, k=k, b=b: e.tensor_scalar_mul(out=wub[b].t[:, k, :], in0=wus[b].t[:, k, :], scalar1=gf5v.t[:, k:k + 1]),
                       reads=[wus[b].h, gf5v.h], writes=[wub[b].h])
                if part == 3:
                    op("dve", lambda e, b=b: e.tensor_copy(out=wdb[b].t[:, 0, :], in_=wds[b].t[:, 0, :]), reads=[wds[b].h], writes=[wdb[b].h])
                    op("dve", lambda e, b=b: e.tensor_copy(out=wdb[b].t[:, 1, :], in_=wds[b].t[:, 1, :]), reads=[wds[b].h], writes=[wdb[b].h])

            def stage_a(ex):
                b = ex % 2
                xe = xeT[b]
                for stl in range(NST):
                    row0 = ex * C + stl * 128
                    xs_ = xs[xi[0] % 6]
                    px = ptx[xi[0] % 2]
                    xi[0] += 1
                    load(xs_.t[:], XS[row0:row0 + 128, :], [xs_.h], [dh("XS")])
                    for k in range(8):
                        op("pe", lambda e, k=k, xs_=xs_, px=px: e.transpose(out=px.t[:, k * 128:(k + 1) * 128], in_=xs_.t[:, k * 128:(k + 1) * 128], identity=identb5x.t[:]),
                           reads=[xs_.h, identb5x.h], writes=[px.h])
                    op("act", lambda e, px=px, xe=xe, stl=stl: e.copy(out=xe.t[:, :, stl * 128:(stl + 1) * 128], in_=px.t[:].rearrange("p (a b) -> p a b", a=8)),
                       reads=[px.h], writes=[xe.h])

            def gu(ex, hf, fc):
                b = ex % 2
                xe = xeT[b]
                cs = slice(hf * HALF, (hf + 1) * HALF)
                at = actT[hf]
                pgt, put = pgu[fc * 2], pgu[fc * 2 + 1]
                for k in range(8):
                    mm(pgt.t[:, 0:HALF], wgb[b].t[:, k, fc * 128:(fc + 1) * 128], xe.t[:, k, cs], k == 0, k == 7, [wgb[b].h, xe.h], [pgt.h])
                for k in range(8):
                    mm(put.t[:, 0:HALF], wub[b].t[:, k, fc * 128:(fc + 1) * 128], xe.t[:, k, cs], k == 0, k == 7, [wub[b].h, xe.h], [put.h])
                sl = sil[fc]
                op("act", lambda e: e.activation(out=sl.t[:], in_=pgt.t[:, 0:HALF], func=AF.Silu), reads=[pgt.h], writes=[sl.h])
                op("dve", lambda e: e.tensor_tensor(out=at.t[:, fc, :], in0=sl.t[:], in1=put.t[:, 0:HALF], op=ALU.mult),
                   reads=[sl.h, put.h], writes=[at.hs[fc]])

            def down(ex, hf):
                b = ex % 2
                at = actT[hf]
                for stl in range(NSH):
                    ys_ = ysb[yi[0] % 6]
                    yi[0] += 1
                    for half in range(2):
                        py = pyy[half]
                        for fc in range(2):
                            mm(py.t[:], at.t[:, fc, stl * 128:(stl + 1) * 128], wdb[b].t[:, fc, half * 512:(half + 1) * 512], fc == 0, fc == 1,
                               [at.hs[fc], wdb[b].h], [py.h])
                        if half == 0:
                            op("act", lambda e, py=py, ys_=ys_: e.copy(out=ys_.t[:, 0:512], in_=py.t[:]), reads=[py.h], writes=[ys_.h])
                        else:
                            op("dve", lambda e, py=py, ys_=ys_: e.tensor_copy(out=ys_.t[:, 512:1024], in_=py.t[:]), reads=[py.h], writes=[ys_.h])
                    row0 = ex * C + hf * HALF + stl * 128
                    store(YS[row0:row0 + 128, :], ys_.t[:], [ys_.h], [dh("YS")])

            stage_load(0)
            stage_load(1)
            for part in range(4):
                conv_part(0, part)
            stage_a(0)
            for ex in range(NE):
                nxt_ex = ex + 1 < NE
                if nxt_ex:
                    stage_a(ex + 1)
                if ex + 2 < NE:
                    stage_load(ex + 2)
                gu(ex, 0, 0)
                if nxt_ex:
                    conv_part(ex + 1, 0)
                gu(ex, 0, 1)
                if nxt_ex:
                    conv_part(ex + 1, 1)
                gu(ex, 1, 0)
                if nxt_ex:
                    conv_part(ex + 1, 2)
                gu(ex, 1, 1)
                if nxt_ex:
                    conv_part(ex + 1, 3)
                down(ex, 0)
                down(ex, 1)
            Sx.barrier()
        if stop_after <= 5:
            return _finish(nc, Sx, out_d, S)

        with ExitStack() as st:
            sb, ps = mk(st)
            onesf6v = sb("onesf6", [128, 128], F32)
            fn_row = sb("fn_row", [1, D], F32)
            GF = sb("GF", [128, D], F32)
            load(onesf6v.t[:], c_onesf, [onesf6v.h])
            load(fn_row.t[:], fnorm_d, [fn_row.h])
            pb = [ps(f"pb{i}", [128, 512], F32) for i in range(2)]
            for half in range(2):
                mm(pb[half].t[:], onesf6v.t[0:1, :], fn_row.t[0:1, half * 512:(half + 1) * 512], True, True, [onesf6v.h, fn_row.h], [pb[half].h])
                op("act", lambda e, half=half: e.copy(out=GF.t[:, half * 512:(half + 1) * 512], in_=pb[half].t[:]), reads=[pb[half].h], writes=[GF.h])
            NB6 = 3
            x1t = [sb(f"x1t{i}", [128, D], F32) for i in range(NB6)]
            y1 = [sb(f"y1_{i}", [128, D], BF16) for i in range(NB6)]
            y2 = [sb(f"y2_{i}", [128, D], BF16) for i in range(NB6)]
            junk6v = sb("junk6", [128, D], F32)
            ssf = [sb(f"ssf{i}", [128, 2], F32) for i in range(2)]
            ot = [sb(f"ot{i}", [128, D], F32) for i in range(2)]
            hout = H()

            def fetch(ti):
                b = ti % NB6
                rows = slice(ti * 128, (ti + 1) * 128)
                load(x1t[b].t[:], X1[rows, :], [x1t[b].h], [dh("X1")])
                for j, yy in enumerate((y1[b], y2[b])):
                    op("pool", lambda e, j=j, yy=yy: e.indirect_dma_start(
                        out=yy.t[:], out_offset=None, in_=YS,
                        in_offset=bass.IndirectOffsetOnAxis(ap=slot_i.t[:, ti, j:j + 1], axis=0)),
                       reads=[dh("YS"), slot_i.h], writes=[yy.h], dma=True)

            def compute(ti):
                b = ti % NB6
                rows = slice(ti * 128, (ti + 1) * 128)
                xx, ya, yb_ = x1t[b], y1[b], y2[b]
                op("dve", lambda e: e.scalar_tensor_tensor(out=xx.t[:], in0=ya.t[:], scalar=wts.t[:, ti, 0:1], in1=xx.t[:], op0=ALU.mult, op1=ALU.add),
                   reads=[ya.h, wts.h, xx.h], writes=[xx.h])
                op("dve", lambda e: e.scalar_tensor_tensor(out=xx.t[:], in0=yb_.t[:], scalar=wts.t[:, ti, 1:2], in1=xx.t[:], op0=ALU.mult, op1=ALU.add),
                   reads=[yb_.h, wts.h, xx.h], writes=[xx.h])
                sf = ssf[ti % 2]
                op("act", lambda e: e.activation(out=junk6v.t[:], in_=xx.t[:], func=AF.Square, accum_out=sf.t[:, 0:1]), reads=[xx.h], writes=[junk6v.h, sf.h])
                op("act", lambda e: e.activation(out=sf.t[:, 1:2], in_=sf.t[:, 0:1], func=AF.Sqrt, scale=1.0 / D, bias=EPS), reads=[sf.h], writes=[sf.h])
                op("dve", lambda e: e.reciprocal(out=sf.t[:, 1:2], in_=sf.t[:, 1:2]), reads=[sf.h], writes=[sf.h])
                o_ = ot[ti % 2]
                op("dve", lambda e: e.scalar_tensor_tensor(out=o_.t[:], in0=xx.t[:], scalar=sf.t[:, 1:2], in1=GF.t[:], op0=ALU.mult, op1=ALU.mult),
                   reads=[xx.h, sf.h, GF.h], writes=[o_.h])
                op("sp", lambda e: e.dma_start(out=out_d[rows, :], in_=o_.t[:]), reads=[o_.h], writes=[hout], dma=True)

            fetch(0)
            if NT > 1:
                fetch(1)
            for ti in range(NT):
                if ti + 2 < NT:
                    fetch(ti + 2)
                compute(ti)
            Sx.barrier()
        return _finish(nc, Sx, out_d, S)


def _finish(nc, Sx, out_d, S):
    Sx.barrier()
    Sx.emit_all()
    return nc


def _consts(S, C):
    bf = ml_dtypes.bfloat16
    c = {}
    c["c_identb"] = np.eye(128, dtype=np.float32).astype(bf)
    c["c_identf"] = np.eye(128, dtype=np.float32)
    c["c_onesb"] = np.ones((128, 128), np.float32).astype(bf)
    c["c_onesf"] = np.ones((128, 128), np.float32)
    pa = np.zeros((96, 96), np.float32)
    for m in range(96):
        if m < 64:
            k = m
        elif m < 80:
            k = m + 16
        else:
            k = m - 16
        pa[k, m] = 1.0
    c["c_perma"] = pa.astype(bf)
    pb = np.zeros((128, 128), np.float32)
    for m in range(128):
        j = m % 64
        if j < 8:
            k = m + 8
        elif j < 16:
            k = m - 8
        else:
            k = m
        pb[k, m] = 1.0
    c["c_permb"] = pb.astype(bf)
    pos = np.arange(S, dtype=np.float64)
    inv = ROPE_THETA ** (-np.arange(16, dtype=np.float64) / 16)
    ang = pos[None, :] * inv[:, None]
    tac = np.ones((96, S), np.float64)
    tas = np.zeros((96, S), np.float64)
    tac[64:80] = np.cos(ang)
    tac[80:96] = np.cos(ang)
    tas[64:80] = -np.sin(ang)
    tas[80:96] = np.sin(ang)
    c["c_tac"] = tac.astype(np.float32)
    c["c_tas"] = tas.astype(np.float32)
    inv = ROPE_THETA ** (-np.arange(8, dtype=np.float64) / 8)
    ang = pos[None, :] * inv[:, None]
    tbc = np.ones((128, S), np.float64)
    tbs = np.zeros((128, S), np.float64)
    for b0 in (0, 64):
        tbc[b0:b0 + 8] = np.cos(ang)
        tbc[b0 + 8:b0 + 16] = np.cos(ang)
        tbs[b0:b0 + 8] = -np.sin(ang)
        tbs[b0 + 8:b0 + 16] = np.sin(ang)
    c["c_tbc"] = tbc.astype(np.float32)
    c["c_tbs"] = tbs.astype(np.float32)
    m = np.zeros((128, 4, 512), np.float32)
    p = np.arange(128)[:, None]
    j = np.arange(512)[None, :]
    for r in range(4):
        m[:, r, :] = (r * 128 + p <= j)
    c["c_mask"] = m.astype(bf)
    oh = np.zeros((32, S), np.float32)
    for n in range(32):
        oh[n, n * 256:(n + 1) * 256] = 1.0
    c["c_oh"] = oh.astype(bf)
    c["c_wneg"] = np.concatenate([np.zeros((1, 32), np.float32), np.full((1, 1), 1e30, np.float32), np.full((1, 31), -1e30, np.float32)], axis=1).astype(bf)
    c["c_offs"] = np.tile((np.arange(32, dtype=np.float32) * C - 1.0)[None, :], (128, 1)).astype(np.float32)
    kk = np.arange(128)[:, None]
    mm_ = np.arange(128)[None, :]
    c["c_triu"] = (kk <= mm_).astype(np.float32).astype(bf)
    return c


def _prep_inputs(inputs, S, C):
    f = lambda a: np.ascontiguousarray(np.asarray(a, dtype=np.float32))
    w_ukv = f(inputs["w_ukv"])[0].reshape(256, 8, 128)
    w_ukv_kv = np.concatenate([w_ukv[:, :, :64].reshape(256, 512), w_ukv[:, :, 64:].reshape(256, 512)], axis=1)
    w_rg = f(inputs["w_router_group"])[0]
    w_re = f(inputs["w_router_expert"])[0]
    w_r = np.concatenate([w_rg] + [w_re[g] for g in range(4)], axis=1)
    b_r = np.concatenate([f(inputs["b_router_group"])[0], f(inputs["b_router_expert"])[0].reshape(32)])[None, :]
    shared = {
        "attn_norm": np.ascontiguousarray(f(inputs["attn_norm"])[0].reshape(8, 128).T),
        "w_in": f(inputs["w_in"])[0],
        "q_norm": np.ascontiguousarray(f(inputs["q_norm"])[0].reshape(6, 128).T),
        "w_uq": f(inputs["w_uq"])[0],
        "kv_norm": np.ascontiguousarray(f(inputs["kv_norm"])[0].reshape(2, 128).T),
        "w_ukv_kv": np.ascontiguousarray(w_ukv_kv),
        "w_o_mla": f(inputs["w_o_mla"])[0],
        "w_o_moba": f(inputs["w_o_moba"])[0],
        "w_out": f(inputs["w_out"])[0],
        "ffn_norm": np.ascontiguousarray(f(inputs["ffn_norm"])[0].reshape(8, 128).T),
        "w_r": np.ascontiguousarray(w_r),
        "b_r": np.ascontiguousarray(b_r),
        "w_exp_gate": f(inputs["w_exp_gate"])[0],
        "w_exp_up": f(inputs["w_exp_up"])[0],
        "w_exp_down": f(inputs["w_exp_down"])[0],
        "final_norm": f(inputs["final_norm"]).reshape(1, D),
    }
    shared.update(_consts(S, C))
    return shared


def kernel(**inputs):
    x = np.asarray(inputs["x"], dtype=np.float32)
    B, S, _ = x.shape
    C = 768 if S >= 8192 else max(256, (S * 2 // 32) * 3 // 128 * 128 + 128)
    nc = build(S=S, C=C)
    shared = _prep_inputs(inputs, S, C)
    in_maps = []
    for b in range(B):
        m = dict(shared)
        m["x"] = np.ascontiguousarray(x[b])
        m["xT"] = np.ascontiguousarray(x[b].T)
        in_maps.append(m)
    res = run_bass_kernel_spmd(nc, in_maps, core_ids=list(range(B)))
    return np.stack([r["out"] for r in res.results], axis=0)
```

```python
import numpy as np
from contextlib import ExitStack
import ml_dtypes
import concourse.bass as bass
import concourse.mybir as mybir
from concourse.bass_utils import run_bass_kernel_spmd

F32 = mybir.dt.float32
BF16 = mybir.dt.bfloat16
I32 = mybir.dt.int32
AF = mybir.ActivationFunctionType
ALU = mybir.AluOpType
AX = mybir.AxisListType

SEM_LIMIT = 30000
D = 1024
NE = 32
EPS = 1e-6
ROPE_THETA = 500000.0


class H:
    __slots__ = ("w", "r")

    def __init__(self):
        self.w = None
        self.r = {}


class Q:
    def __init__(self, name, sems):
        self.name = name
        self.sems = sems
        self.epoch = 0
        self.count = 0
        self.ops = []
        self.waited = {}


class Sched:
    def __init__(self, nc, stack, n_dma_sems=72):
        self.nc = nc
        self.semobj = {}
        self.q = {}
        for name, nep in (("pe", 5), ("act", 3), ("dve", 3), ("pool", 3), ("sp", 1)):
            sems = []
            for e in range(nep):
                s = stack.enter_context(nc.semaphore(f"s_{name}{e}"))
                key = f"{name}{e}"
                self.semobj[key] = s
                sems.append(key)
            self.q[name] = Q(name, sems)
        self.dma_sems = []
        for i in range(n_dma_sems):
            s = stack.enter_context(nc.semaphore(f"s_dma{i}"))
            key = f"dma{i}"
            self.semobj[key] = s
            self.dma_sems.append([key, 0])
        self.dma_rr = 0
        self.dma_rr_q = {}
        self.n_ops = 0

    def _deps(self, reads, writes):
        deps = {}

        def add(k, v):
            if v > deps.get(k, -1):
                deps[k] = v
        for h in reads:
            if h.w is not None:
                add(*h.w)
        for h in writes:
            if h.w is not None:
                add(*h.w)
            for k, v in h.r.items():
                add(k, v)
        return deps

    def op(self, qname, fn, reads=(), writes=(), dma=False):
        q = self.q[qname]
        deps = self._deps(reads, writes)
        if dma:
            third = len(self.dma_sems) // 3
            base = {"sp": 0, "pool": third, "act": 2 * third}[qname]
            rr = self.dma_rr_q.get(qname, 0)
            slot = self.dma_sems[base + rr]
            self.dma_rr_q[qname] = (rr + 1) % third
            if slot[1] > 0 and slot[1] > deps.get(slot[0], -1):
                deps[slot[0]] = slot[1]
            assert slot[1] + 16 < 60000
            slot[1] += 16
            comp = (slot[0], slot[1])
            inc = 16
        else:
            if q.count + 1 > SEM_LIMIT:
                q.epoch += 1
                q.count = 0
            q.count += 1
            comp = (q.sems[q.epoch], q.count)
            inc = 1
        waits = []
        for k, v in deps.items():
            if qname == "pe" and k.startswith("pe"):
                continue
            if q.waited.get(k, -1) >= v:
                continue
            q.waited[k] = v
            waits.append((self.semobj[k], v))
        csem = self.semobj[comp[0]]

        def emit(eng, waits=waits, fn=fn, csem=csem, inc=inc):
            for s, v in waits:
                eng.wait_ge(s, v)
            fn(eng).then_inc(csem, inc)
        q.ops.append(emit)
        for h in writes:
            h.w = comp
            h.r = {}
        for h in reads:
            if comp[1] > h.r.get(comp[0], -1):
                h.r[comp[0]] = comp[1]
        self.n_ops += 1
        return comp

    def barrier(self):
        deps = {}
        for q in self.q.values():
            for e in range(q.epoch + 1):
                cnt = q.count if e == q.epoch else SEM_LIMIT
                if cnt > 0:
                    deps[q.sems[e]] = cnt
        for k, v in self.dma_sems:
            if v > 0:
                deps[k] = v
        for q in self.q.values():
            waits = []
            for k, v in deps.items():
                if q.waited.get(k, -1) >= v:
                    continue
                q.waited[k] = v
                waits.append((self.semobj[k], v))

            def emit(eng, waits=waits):
                for s, v in waits:
                    eng.wait_ge(s, v)
            q.ops.append(emit)

    def emit_all(self):
        nc = self.nc
        with nc.Block() as block:
            @block.tensor
            def _(e):
                for f in self.q["pe"].ops:
                    f(e)

            @block.scalar
            def _(e):
                for f in self.q["act"].ops:
                    f(e)

            @block.vector
            def _(e):
                for f in self.q["dve"].ops:
                    f(e)

            @block.gpsimd
            def _(e):
                for f in self.q["pool"].ops:
                    f(e)

            @block.sync
            def _(e):
                for f in self.q["sp"].ops:
                    f(e)


class T:
    def __init__(self, t, n=1):
        self.t = t
        self.h = H()
        self.hs = [H() for _ in range(n)]


def build(S=8192, C=768, stop_after=99, debug=False):
    NG = S // 512
    NT = S // 128
    NSLOT = NE * C
    nc = bass.Bass("TRN2", target_bir_lowering=False)

    def din(name, shape, dt=F32):
        return nc.dram_tensor(name, shape, dt, kind="ExternalInput").ap()

    def dscr(name, shape, dt):
        return nc.dram_tensor(name, shape, dt, kind=("ExternalOutput" if debug else "Internal")).ap()

    x_d = din("x", [S, D])
    xT_d = din("xT", [D, S])
    attn_norm_d = din("attn_norm", [128, 8])
    w_in_d = din("w_in", [D, 4640])
    q_norm_d = din("q_norm", [128, 6])
    w_uq_d = din("w_uq", [768, 768])
    kv_norm_d = din("kv_norm", [128, 2])
    w_ukv_d = din("w_ukv_kv", [256, 1024])
    w_oa_d = din("w_o_mla", [512, D])
    w_ob_d = din("w_o_moba", [512, D])
    w_out_d = din("w_out", [D, D])
    ffn_norm_d = din("ffn_norm", [128, 8])
    w_r_d = din("w_r", [D, 36])
    b_r_d = din("b_r", [1, 36])
    w_g_d = din("w_exp_gate", [NE, D, 256])
    w_u_d = din("w_exp_up", [NE, D, 256])
    w_d_d = din("w_exp_down", [NE, 256, D])
    fnorm_d = din("final_norm", [1, D])
    c_identb = din("c_identb", [128, 128], BF16)
    c_identf = din("c_identf", [128, 128])
    c_onesb = din("c_onesb", [128, 128], BF16)
    c_onesf = din("c_onesf", [128, 128])
    c_perma = din("c_perma", [96, 96], BF16)
    c_permb = din("c_permb", [128, 128], BF16)
    c_tac = din("c_tac", [96, S])
    c_tas = din("c_tas", [96, S])
    c_tbc = din("c_tbc", [128, S])
    c_tbs = din("c_tbs", [128, S])
    c_mask = din("c_mask", [128, 4, 512], BF16)
    c_oh = din("c_oh", [32, S], BF16)
    c_wneg = din("c_wneg", [1, 64], BF16)
    c_offs = din("c_offs", [128, 32])
    c_triu = din("c_triu", [128, 128], BF16)

    out_d = nc.dram_tensor("out", [S, D], F32, kind="ExternalOutput").ap()

    WIN = dscr("WIN", [D, 4640], BF16)
    WUQ = dscr("WUQ", [768, 768], BF16)
    WUKV = dscr("WUKV", [256, 1024], BF16)
    WOA = dscr("WOA", [512, D], BF16)
    WOB = dscr("WOB", [512, D], BF16)
    WOUT = dscr("WOUT", [D, D], BF16)
    HT = dscr("HT", [D, S], BF16)
    QA = dscr("QA", [768, S], BF16)
    KNA = dscr("KNA", [512, S], BF16)
    KRA = dscr("KRA", [32, S], BF16)
    VA = dscr("VA", [S, 512], BF16)
    QB = dscr("QB", [512, S], BF16)
    KB = dscr("KB", [512, S], BF16)
    VB = dscr("VB", [S, 512], BF16)
    MB = dscr("MB", [256, S], BF16)
    OA = dscr("OA", [512, S], BF16)
    OB = dscr("OB", [512, S], BF16)
    X1 = dscr("X1", [S, D], F32)
    XS = dscr("XS", [NSLOT, D], BF16)
    YS = dscr("YS", [NSLOT, D], BF16)

    with ExitStack() as top:
        Sx = Sched(nc, top)
        op = Sx.op

        def mk(st):
            def sb(name, shape, dt, n=1):
                return T(st.enter_context(nc.sbuf_tensor(name, shape, dt)), n)

            def ps(name, shape, dt):
                return T(st.enter_context(nc.psum_tensor(name, shape, dt)))
            return sb, ps

        def load(dst_ap, src_ap, wr, rd=()):
            op("sp", lambda e: e.dma_start(out=dst_ap, in_=src_ap), reads=rd, writes=wr, dma=True)

        store_q = ["pool"]

        def store(dst_ap, src_ap, rd, wr):
            op(store_q[0], lambda e: e.dma_start(out=dst_ap, in_=src_ap), reads=rd, writes=wr, dma=True)

        def mm(out_ap, lhsT, rhs, start, stop, rd, wr):
            op("pe", lambda e: e.matmul(out_ap, lhsT, rhs, start=start, stop=stop), reads=rd, writes=wr)

        hd = {}

        def dh(name):
            if name not in hd:
                hd[name] = H()
            return hd[name]

        sbp, _ = mk(top)
        slot_i = sbp("slot_i", [128, NT, 2], I32, 4)
        wts = sbp("wts", [128, NT, 2], F32, 4)

        with ExitStack() as st:
            sb, ps = mk(st)
            stg = [sb(f"stg{i}", [128, 8, 512], F32) for i in range(2)]
            obf = [sb(f"obf{i}", [128, 8, 512], BF16) for i in range(2)]
            gains = sb("gains", [128, 3, 8], F32)
            load(gains.t[:, 0, :], attn_norm_d, [gains.hs[0]])
            load(gains.t[:, 1, 0:6], q_norm_d, [gains.hs[0]])
            load(gains.t[:, 2, 0:2], kv_norm_d, [gains.hs[0]])
            cnt = [0]

            def conv(src, dst, dname, nk, ncols, gi, cstart=0):
                srcv = src.rearrange("(k p) c -> p k c", p=128)
                dstv = dst.rearrange("(k p) c -> p k c", p=128)
                for c0 in range(cstart, ncols, 512):
                    cw = min(512, ncols - c0)
                    i = cnt[0] % 2
                    cnt[0] += 1
                    a, b = stg[i], obf[i]
                    load(a.t[:, 0:nk, 0:cw], srcv[:, :, c0:c0 + cw], [a.h])
                    for k in range(nk):
                        if gi is None:
                            if k % 2 == 0:
                                op("act", lambda e, a=a, b=b, k=k, cw=cw: e.copy(out=b.t[:, k, 0:cw], in_=a.t[:, k, 0:cw]),
                                   reads=[a.h], writes=[b.hs[0]] if False else [b.h])
                            else:
                                op("dve", lambda e, a=a, b=b, k=k, cw=cw: e.tensor_copy(out=b.t[:, k, 0:cw], in_=a.t[:, k, 0:cw]),
                                   reads=[a.h], writes=[b.h])
                        else:
                            if k % 2 == 0:
                                op("act", lambda e, a=a, b=b, k=k, cw=cw, gi=gi: e.activation(out=b.t[:, k, 0:cw], in_=a.t[:, k, 0:cw], func=AF.Copy, scale=gains.t[:, gi, k:k + 1]),
                                   reads=[a.h, gains.hs[0]], writes=[b.h])
                            else:
                                op("dve", lambda e, a=a, b=b, k=k, cw=cw, gi=gi: e.tensor_scalar_mul(out=b.t[:, k, 0:cw], in0=a.t[:, k, 0:cw], scalar1=gains.t[:, gi, k:k + 1]),
                                   reads=[a.h, gains.hs[0]], writes=[b.h])
                    store(dstv[:, :, c0:c0 + cw], b.t[:, 0:nk, 0:cw], [b.h], [dh(dname)])

            conv(w_in_d, WIN, "WIN", 8, 2592, 0)
            conv(w_uq_d, WUQ, "WUQ", 6, 768, 1)
            conv(w_ukv_d, WUKV, "WUKV", 2, 1024, 2)
            Sx.barrier()
        if stop_after <= 0:
            return _finish(nc, Sx, out_d, S)

        with ExitStack() as st:
            sb, ps = mk(st)
            onesb = sb("onesb", [128, 128], BF16)
            identb = sb("identb", [128, 128], BF16)
            perma = sb("perma", [96, 96], BF16)
            permb = sb("permb", [128, 128], BF16)
            wneg = sb("wneg", [1, 64], BF16)
            load(onesb.t[:], c_onesb, [onesb.h])
            load(identb.t[:], c_identb, [identb.h])
            load(perma.t[:], c_perma, [perma.h])
            load(permb.t[:], c_permb, [permb.h])
            load(wneg.t[:], c_wneg, [wneg.h])
            store_q[0] = "sp"
            NCW = 2592
            win = sb("win", [128, 8, NCW], BF16)
            wuq = sb("wuq", [128, 6, 768], BF16)
            wukv = sb("wukv", [128, 2, 1024], BF16)
            winv = WIN.rearrange("(k p) c -> p k c", p=128)
            for c0 in range(0, NCW, 648):
                load(win.t[:, :, c0:c0 + 648], winv[:, :, c0:c0 + 648], [win.h], [dh("WIN")])
            load(wuq.t[:], WUQ.rearrange("(k p) c -> p k c", p=128), [wuq.h], [dh("WUQ")])
            load(wukv.t[:], WUKV.rearrange("(k p) c -> p k c", p=128), [wukv.h], [dh("WUKV")])
            km = sb("km", [128, 4, 32], BF16)
            op("dve", lambda e: e.memset(km.t[:], 0.0), writes=[km.h])

            xt = [sb(f"xt{i}", [128, 8, 512], F32) for i in range(2)]
            xsq = sb("xsq", [128, 8, 512], BF16)
            hTs = [sb(f"hT{i}", [128, 8, 512], BF16, 8) for i in range(2)]
            rss = [sb(f"rs{i}", [128, 512], F32) for i in range(2)]
            tac = sb("tac", [96, 512], F32)
            tas = sb("tas", [96, 512], F32)
            tbc = sb("tbc", [128, 512], F32)
            tbs = sb("tbs", [128, 512], F32)
            tac2 = sb("tac2", [96, 512], F32)
            tas2 = sb("tas2", [96, 512], F32)
            tbc2 = sb("tbc2", [128, 512], F32)
            tbs2 = sb("tbs2", [128, 512], F32)
            cq = sb("cq", [128, 6, 512], BF16, 6)
            cqsq = sb("cqsq", [128, 6, 512], BF16, 6)
            cqn = sb("cqn", [128, 6, 512], BF16, 6)
            ckv = sb("ckv", [128, 2, 512], BF16, 2)
            ckvsq = sb("ckvsq", [128, 2, 512], BF16, 2)
            ckvn = sb("ckvn", [128, 2, 512], BF16, 2)
            rsq = sb("rsq", [128, 512], F32)
            rskv = sb("rskv", [128, 512], F32)
            NR = 3
            rsb = [sb(f"rsb{i}", [128, 512], BF16) for i in range(NR)]
            t1 = [sb(f"t1_{i}", [128, 512], F32) for i in range(NR)]
            t2 = [sb(f"t2_{i}", [128, 512], F32) for i in range(NR)]
            ro = [sb(f"ro{i}", [128, 512], BF16) for i in range(NR)]
            qbo = [sb(f"qbo{i}", [128, 512], BF16) for i in range(4)]
            evo = [sb(f"evo{i}", [128, 512], BF16) for i in range(6)]
            kms = sb("kms", [128, 2], F32)
            gsbs = [sb(f"gsb{i}", [128, 256], F32) for i in range(4)]
            t8s = [sb(f"t8_{i}", [128, 8, 8], F32, 8) for i in range(4)]
            thrs = [sb(f"thr{i}", [128, 8], F32) for i in range(4)]
            mkfs = [sb(f"mk_f{i}", [128, 256], F32, 8) for i in range(4)]
            mkbs = [sb(f"mkb{i}", [128, 256], BF16) for i in range(4)]
            mT = sb("mT", [128, 2, 512], BF16, 2)
            pm = [ps(f"pm{i}", [128, 512], F32) for i in range(3)]
            pp = [ps(f"pp{i}", [128, 512], F32) for i in range(1)]
            pstat = ps("pstat", [128, 512], F32)
            pgs = [ps(f"pg{i}", [128, 512], F32) for i in range(2)]
            ptr = ps("ptr", [128, 1024], BF16)
            ctr = {"pm": 0, "pp": 0, "r": 0, "ev": 0}

            def nxt(key, n):
                v = ctr[key] % n
                ctr[key] += 1
                return v

            def rope(src_ps, rows, perm, tc_, ts_, out_t=None, after=None):
                i = nxt("r", NR)
                a, b1, b2, o = rsb[i], t1[i], t2[i], (out_t or ro[i])
                op("act", lambda e: e.copy(out=a.t[0:rows, :], in_=src_ps.t[0:rows, :]), reads=[src_ps.h], writes=[a.h])

                def fin():
                    p2 = pp[nxt("pp", 1)]
                    mm(p2.t[0:rows, :], perm.t[0:rows, 0:rows], a.t[0:rows, :], True, True, [perm.h, a.h], [p2.h])
                    op("dve", lambda e: e.tensor_tensor(out=b1.t[0:rows, :], in0=p2.t[0:rows, :], in1=ts_.t[0:rows, :], op=ALU.mult),
                       reads=[p2.h, ts_.h], writes=[b1.h])
                    op("pool", lambda e: e.tensor_tensor(out=b2.t[0:rows, :], in0=a.t[0:rows, :], in1=tc_.t[0:rows, :], op=ALU.mult),
                       reads=[a.h, tc_.h], writes=[b2.h])
                    op("dve", lambda e: e.tensor_tensor(out=o.t[0:rows, :], in0=b1.t[0:rows, :], in1=b2.t[0:rows, :], op=ALU.add),
                       reads=[b1.h, b2.h], writes=[o.h])
                    if after is not None:
                        after(o)
                return fin

            def rstd_from(pst, dst, n):
                op("act", lambda e: e.activation(out=dst.t[:], in_=pst.t[:], func=AF.Ln, scale=1.0 / n, bias=EPS),
                   reads=[pst.h], writes=[dst.h])
                op("act", lambda e: e.activation(out=dst.t[:], in_=dst.t[:], func=AF.Exp, scale=-0.5), reads=[dst.h], writes=[dst.h])

            xTv = xT_d.rearrange("(k p) s -> p k s", p=128)
            HTv = HT.rearrange("(k p) s -> p k s", p=128)
            dq = []

            def defer(fn):
                dq.append(fn)
                while len(dq) > 1:
                    dq.pop(0)()

            def flush():
                while dq:
                    dq.pop(0)()

            gate_pend = []
            tabs = [(tac, tas, tbc, tbs), (tac2, tas2, tbc2, tbs2)]

            sq_done = {}

            def S1a(g):
                x_ = xt[g % 2]
                sq_done[g] = True
                op("act", lambda e: e.activation(out=xsq.t[:], in_=x_.t[:], func=AF.Square), reads=[x_.h], writes=[xsq.h])

            def S1(g):
                tok = slice(g * 512, (g + 1) * 512)
                x_, hT, rs = xt[g % 2], hTs[g % 2], rss[g % 2]
                ta_c, ta_s, tb_c, tb_s = tabs[g % 2]
                if not sq_done.get(g):
                    S1a(g)
                for k in range(8):
                    mm(pstat.t[:], onesb.t[:], xsq.t[:, k, :], k == 0, k == 7, [onesb.h, xsq.h], [pstat.h])
                rstd_from(pstat, rs, 1024.0)
                for k in range(8):
                    op("dve", lambda e, k=k: e.tensor_tensor(out=hT.t[:, k, :], in0=x_.t[:, k, :], in1=rs.t[:], op=ALU.mult),
                       reads=[x_.h, rs.h], writes=[hT.hs[k]])
                store(HTv[:, :, tok], hT.t[:], list(hT.hs), [dh("HT")])

            def S1_load(g):
                tok = slice(g * 512, (g + 1) * 512)
                x_ = xt[g % 2]
                ta_c, ta_s, tb_c, tb_s = tabs[g % 2]
                for dst, src in ((x_, xTv[:, :, tok]), (ta_c, c_tac[:, tok]), (ta_s, c_tas[:, tok]), (tb_c, c_tbc[:, tok]), (tb_s, c_tbs[:, tok])):
                    op("act", lambda e, dst=dst, src=src: e.dma_start(out=dst.t[:], in_=src), writes=[dst.h], dma=True)

            S1_load(0)
            S1(0)
            for g in range(NG):
                tok = slice(g * 512, (g + 1) * 512)
                if g + 1 < NG:
                    S1_load(g + 1)
                hT = hTs[g % 2]
                tac, tas, tbc, tbs = tabs[g % 2]
                for (dst, dsq, nchunk, col0) in ((cq, cqsq, 6, 0), (ckv, ckvsq, 2, 768)):
                    for c in range(nchunk):
                        p = pm[nxt("pm", 3)]
                        for k in range(8):
                            mm(p.t[:], win.t[:, k, col0 + c * 128: col0 + (c + 1) * 128], hT.t[:, k, :], k == 0, k == 7, [win.h, hT.hs[k]], [p.h])
                        op("act", lambda e, p=p, dst=dst, c=c: e.copy(out=dst.t[:, c, :], in_=p.t[:]), reads=[p.h], writes=[dst.hs[c]])
                        op("dve", lambda e, dst=dst, dsq=dsq, c=c: e.tensor_tensor(out=dsq.t[:, c, :], in0=dst.t[:, c, :], in1=dst.t[:, c, :], op=ALU.mult),
                           reads=[dst.hs[c]], writes=[dsq.hs[c]])

                def after_q(j):
                    def f(o):
                        store(QB[j * 128:(j + 1) * 128, tok], o.t[:], [o.h], [dh("QB")])
                    return f

                def after_k(j, g=g):
                    def f(o):
                        store(KB[j * 128:(j + 1) * 128, tok], o.t[:], [o.h], [dh("KB")])
                        op("dve", lambda e: e.tensor_reduce(out=kms.t[:], in_=o.t[:].rearrange("p (b t) -> p b t", t=256), axis=AX.X, op=ALU.add),
                           reads=[o.h], writes=[kms.h])
                        op("act", lambda e: e.activation(out=km.t[:, j, 2 * g:2 * g + 2], in_=kms.t[:], func=AF.Copy, scale=1.0 / 256),
                           reads=[kms.h], writes=[km.h])
                    return f

                for which, col0 in (("q", 1056), ("k", 1568)):
                    for j in range(4):
                        p = pm[nxt("pm", 3)]
                        for k in range(8):
                            mm(p.t[:], win.t[:, k, col0 + j * 128: col0 + (j + 1) * 128], hT.t[:, k, :], k == 0, k == 7, [win.h, hT.hs[k]], [p.h])
                        if which == "q":
                            defer(rope(p, 128, permb, tbc, tbs, out_t=qbo[j], after=after_q(j)))
                        else:
                            defer(rope(p, 128, permb, tbc, tbs, after=after_k(j)))
                while gate_pend:
                    gate_pend.pop(0)()
                if g + 1 < NG:
                    S1a(g + 1)
                for (dst, dsq, dn, nchunk, rr, nn) in ((cq, cqsq, cqn, 6, rsq, 768.0), (ckv, ckvsq, ckvn, 2, rskv, 256.0)):
                    for c in range(nchunk):
                        mm(pstat.t[:], onesb.t[:], dsq.t[:, c, :], c == 0, c == nchunk - 1, [onesb.h, dsq.hs[c]], [pstat.h])
                    rstd_from(pstat, rr, nn)
                    for c in range(nchunk):
                        op("dve", lambda e, dst=dst, dn=dn, c=c, rr=rr: e.tensor_tensor(out=dn.t[:, c, :], in0=dst.t[:, c, :], in1=rr.t[:], op=ALU.mult),
                           reads=[dst.hs[c], rr.h], writes=[dn.hs[c]])
                p = pm[nxt("pm", 3)]
                for k in range(8):
                    mm(p.t[0:96, :], win.t[:, k, 960:1056], hT.t[:, k, :], k == 0, k == 7, [win.h, hT.hs[k]], [p.h])
                defer(rope(p, 96, perma, tac, tas, after=lambda o: store(KRA[:, tok], o.t[64:96, :], [o.h], [dh("KRA")])))
                for tt in range(4):
                    p = pm[nxt("pm", 3)]
                    for k in range(8):
                        mm(p.t[:], hT.t[:, k, tt * 128:(tt + 1) * 128], win.t[:, k, 2080:2592], k == 0, k == 7, [win.h, hT.hs[k]], [p.h])
                    o = evo[nxt("ev", 6)]
                    op("act", lambda e, p=p, o=o: e.copy(out=o.t[:], in_=p.t[:]), reads=[p.h], writes=[o.h])
                    store(VB[g * 512 + tt * 128: g * 512 + (tt + 1) * 128, :], o.t[:], [o.h], [dh("VB")])
                for h in range(8):
                    p = pm[nxt("pm", 3)]
                    for c in range(6):
                        mm(p.t[0:96, :], wuq.t[:, c, h * 96:(h + 1) * 96], cqn.t[:, c, :], c == 0, c == 5, [wuq.h, cqn.hs[c]], [p.h])
                    defer(rope(p, 96, perma, tac, tas, after=(lambda h: (lambda o: store(QA[h * 96:(h + 1) * 96, tok], o.t[0:96, :], [o.h], [dh("QA")])))(h)))
                for j in range(4):
                    p = pm[nxt("pm", 3)]
                    for c in range(2):
                        mm(p.t[:], wukv.t[:, c, j * 128:(j + 1) * 128], ckvn.t[:, c, :], c == 0, c == 1, [wukv.h, ckvn.hs[c]], [p.h])
                    o = evo[nxt("ev", 6)]
                    op("act", lambda e, p=p, o=o: e.copy(out=o.t[:], in_=p.t[:]), reads=[p.h], writes=[o.h])
                    store(KNA[j * 128:(j + 1) * 128, tok], o.t[:], [o.h], [dh("KNA")])
                for tt in range(4):
                    p = pm[nxt("pm", 3)]
                    for c in range(2):
                        mm(p.t[:], ckvn.t[:, c, tt * 128:(tt + 1) * 128], wukv.t[:, c, 512:1024], c == 0, c == 1, [wukv.h, ckvn.hs[c]], [p.h])
                    o = evo[nxt("ev", 6)]
                    op("act", lambda e, p=p, o=o: e.copy(out=o.t[:], in_=p.t[:]), reads=[p.h], writes=[o.h])
                    store(VA[g * 512 + tt * 128: g * 512 + (tt + 1) * 128, :], o.t[:], [o.h], [dh("VA")])
                if g + 1 < NG:
                    S1(g + 1)
                flush()
                for tt in range(4):
                    qt = g * 4 + tt
                    own = qt // 2
                    pg_ = pgs[tt % 2]
                    gsb_ = gsbs[tt]
                    for h in range(8):
                        j, r0 = h // 2, (h % 2) * 64
                        mm(pg_.t[:, h * 32:(h + 1) * 32], qbo[j].t[r0:r0 + 64, tt * 128:(tt + 1) * 128], km.t[r0:r0 + 64, j, :], True, False,
                           [qbo[j].h, km.h], [pg_.h])
                        mm(pg_.t[:, h * 32:(h + 1) * 32], onesb.t[0:1, :], wneg.t[0:1, 32 - own:64 - own], False, True,
                           [onesb.h, wneg.h], [pg_.h])
                    op("act", lambda e, pg_=pg_, gsb_=gsb_: e.copy(out=gsb_.t[:], in_=pg_.t[:, 0:256]), reads=[pg_.h], writes=[gsb_.h])
                for h in range(8):
                    for tt in range(4):
                        gsb_, t8_ = gsbs[tt], t8s[tt]
                        op("dve", lambda e, h=h, gsb_=gsb_, t8_=t8_: e.max(out=t8_.t[:, h, :], in_=gsb_.t[:, h * 32:(h + 1) * 32]), reads=[gsb_.h], writes=[t8_.hs[h]])
                for tt in range(4):
                    t8_, thr_ = t8s[tt], thrs[tt]
                    op("dve", lambda e, t8_=t8_, thr_=thr_: e.tensor_scalar_max(out=thr_.t[:], in0=t8_.t[:, :, 3], scalar1=-1e29), reads=list(t8_.hs), writes=[thr_.h])
                for h in range(8):
                    for tt in range(4):
                        gsb_, thr_, mkf_ = gsbs[tt], thrs[tt], mkfs[tt]
                        op("dve", lambda e, h=h, gsb_=gsb_, thr_=thr_, mkf_=mkf_: e.tensor_scalar(out=mkf_.t[:, h * 32:(h + 1) * 32], in0=gsb_.t[:, h * 32:(h + 1) * 32],
                                                                 scalar1=thr_.t[:, h:h + 1], scalar2=30000.0, op0=ALU.is_ge, op1=ALU.mult),
                           reads=[gsb_.h, thr_.h], writes=[mkf_.hs[h]])
                for tt in range(4):
                    mkf_, mkb_ = mkfs[tt], mkbs[tt]
                    op("dve", lambda e, mkf_=mkf_, mkb_=mkb_: e.tensor_scalar_add(out=mkb_.t[:], in0=mkf_.t[:], scalar1=-30000.0), reads=list(mkf_.hs), writes=[mkb_.h])

                def gate_fin(g=g, tok=tok):
                    for tt in range(4):
                        mkb_ = mkbs[tt]
                        for half in range(2):
                            op("pe", lambda e, half=half, mkb_=mkb_: e.transpose(out=ptr.t[:, half * 128:(half + 1) * 128], in_=mkb_.t[:, half * 128:(half + 1) * 128], identity=identb.t[:]),
                               reads=[mkb_.h, identb.h], writes=[ptr.h])
                        op("act", lambda e, tt=tt: e.copy(out=mT.t[:, :, tt * 128:(tt + 1) * 128], in_=ptr.t[:, 0:256].rearrange("p (a b) -> p a b", a=2)),
                           reads=[ptr.h], writes=[mT.h])
                    for half in range(2):
                        store(MB[half * 128:(half + 1) * 128, tok], mT.t[:, half, :], [mT.h], [dh("MB")])
                gate_pend.append(gate_fin)
            while gate_pend:
                gate_pend.pop(0)()
            store_q[0] = "pool"
            Sx.barrier()
        if stop_after <= 1:
            return _finish(nc, Sx, out_d, S)

        with ExitStack() as st:
            sb, ps = mk(st)
            maskc = sb("maskc", [128, 2, 1024], BF16)
            load(maskc.t[:], c_mask.rearrange("p (a b) c -> p a (b c)", a=2), [maskc.h])
            QT = [sb(f"QT{i}", [96, S], BF16) for i in range(2)]
            KT = [sb(f"KT{i}", [96, S], BF16) for i in range(2)]
            VV = [sb(f"VV{i}", [128, NT, 128], BF16) for i in range(2)]
            for v in VV:
                op("dve", lambda e, v=v: e.memset(v.t[:, :, 64:128], 1.0), writes=[v.h])
            NP = 4
            PT = [sb(f"PT{i}", [128, 1024], BF16) for i in range(NP)]
            rcp = [sb(f"rcp{i}", [128, 512], F32) for i in range(2)]
            rc0 = [sb(f"rc0{i}", [64, 512], F32) for i in range(2)]
            onb = [sb(f"onb{i}", [64, 512], BF16) for i in range(2)]
            psc = [ps(f"psc{i}", [128, 1024], F32) for i in range(3)]
            pov = [ps(f"pov{i}", [128, 512], F32) for i in range(2)]
            VAv = VA.rearrange("(n p) f -> p n f", p=128)
            VBv = VB.rearrange("(n p) f -> p n f", p=128)
            stg3 = [sb(f"stg3{i}", [128, 8, 512], F32) for i in range(2)]
            obf3 = [sb(f"obf3{i}", [128, 8, 512], BF16) for i in range(2)]
            gain3 = sb("gain3", [128, 8], F32)
            load(gain3.t[:], attn_norm_d, [gain3.h])
            cnt3 = [0]
            c3q = []

            def conv3(src, dst, dname, nk, ncols, use_gain, cstart=0):
                srcv = src.rearrange("(k p) c -> p k c", p=128)
                dstv = dst.rearrange("(k p) c -> p k c", p=128)
                for c0 in range(cstart, ncols, 512):
                    cw = min(512, ncols - c0)
                    i = cnt3[0] % 2
                    cnt3[0] += 1
                    a_, b_ = stg3[i], obf3[i]
                    c3q.append(lambda a_=a_, c0=c0, cw=cw: load(a_.t[:, 0:nk, 0:cw], srcv[:, :, c0:c0 + cw], [a_.h]))
                    for k in range(nk):
                        if use_gain:
                            c3q.append(lambda a_=a_, b_=b_, k=k, cw=cw: op("pool", lambda e: e.tensor_scalar_mul(out=b_.t[:, k, 0:cw], in0=a_.t[:, k, 0:cw], scalar1=gain3.t[:, k:k + 1]),
                                                                      reads=[a_.h, gain3.h], writes=[b_.h]))
                        else:
                            c3q.append(lambda a_=a_, b_=b_, k=k, cw=cw: op("pool", lambda e: e.tensor_copy(out=b_.t[:, k, 0:cw], in_=a_.t[:, k, 0:cw]),
                                                                      reads=[a_.h], writes=[b_.h]))
                    c3q.append(lambda b_=b_, c0=c0, cw=cw: store(dstv[:, :, c0:c0 + cw], b_.t[:, 0:nk, 0:cw], [b_.h], [dh(dname)]))

            LA = 2
            pend = []
            state = {"ui": 0, "gi": 0}

            def emit_pair(q_, k_, v_, sc, g, kp, nkp, odst, on, h):
                ui = state["ui"]
                state["ui"] += 1
                pscore = psc[ui % 3]
                pt = PT[ui % NP]
                r = 2 * kp - 4 * g
                c0s = [((r + j) * 128 if r >= 0 else 0) for j in range(2)]
                for j in range(2):
                    kt = 2 * kp + j
                    c0 = c0s[j]
                    mm(pscore.t[:, j * 512 + c0:(j + 1) * 512], k_.t[0:96, kt * 128:(kt + 1) * 128], q_.t[0:96, g * 512 + c0:(g + 1) * 512], True, True,
                       [k_.h, q_.h], [pscore.h])
                if r == 2:
                    for j in range(2):
                        c0 = c0s[j]
                        op("act", lambda e, j=j, c0=c0: e.activation(out=pt.t[:, j * 512 + c0:(j + 1) * 512], in_=pscore.t[:, j * 512 + c0:(j + 1) * 512], func=AF.Exp, scale=sc),
                           reads=[pscore.h], writes=[pt.h])
                else:
                    op("act", lambda e: e.activation(out=pt.t[:], in_=pscore.t[:], func=AF.Exp, scale=sc), reads=[pscore.h], writes=[pt.h])
                if r >= 0:
                    for j in range(2):
                        c0 = c0s[j]
                        op("dve", lambda e, j=j, c0=c0: e.tensor_tensor(out=pt.t[:, j * 512 + c0:j * 512 + c0 + 128], in0=pt.t[:, j * 512 + c0:j * 512 + c0 + 128],
                                                                      in1=maskc.t[:, r // 2, j * 512 + c0:j * 512 + c0 + 128], op=ALU.mult),
                           reads=[pt.h, maskc.h], writes=[pt.h])
                first, last = (kp == 0), (kp == nkp - 1)
                if first:
                    state["gi"] += 1
                gi = state["gi"]
                po = pov[gi % 2]

                def pv():
                    for j in range(2):
                        kt = 2 * kp + j
                        c0 = c0s[j]
                        mm(po.t[:, c0:512], v_.t[:, kt, :], pt.t[:, j * 512 + c0:(j + 1) * 512], first and j == 0, last and j == 1, [v_.h, pt.h], [po.h])
                    if last:
                        rc, r0, onb_ = rcp[gi % 2], rc0[gi % 2], onb[gi % 2]
                        op("dve", lambda e: e.reciprocal(out=rc.t[64:128, :], in_=po.t[64:128, :]), reads=[po.h], writes=[rc.h])
                        op("dve", lambda e: e.tensor_copy(out=r0.t[0:64, :], in_=rc.t[64:128, :]), reads=[rc.h], writes=[r0.h])
                        op("dve", lambda e: e.tensor_tensor(out=onb_.t[:], in0=po.t[0:64, :], in1=r0.t[0:64, :], op=ALU.mult),
                           reads=[po.h, r0.h], writes=[onb_.h])
                        store(odst[h * 64:(h + 1) * 64, g * 512:(g + 1) * 512], onb_.t[:], [onb_.h], [dh(on)])
                return pv

            for hp in range(16):
                typ, h = hp // 8, hp % 8
                q_, k_, v_ = QT[hp % 2], KT[hp % 2], VV[hp % 2]
                if typ == 0:
                    load(q_.t[0:96, :], QA[h * 96:(h + 1) * 96, :], [q_.h], [dh("QA")])
                    load(k_.t[0:64, :], KNA[h * 64:(h + 1) * 64, :], [k_.h], [dh("KNA")])
                    load(k_.t[64:96, :], KRA[:, :], [k_.h], [dh("KRA")])
                    vsrc, vn, sc, odst, on = VAv, "VA", 96.0 ** -0.5, OA, "OA"
                else:
                    load(q_.t[0:64, :], QB[h * 64:(h + 1) * 64, :], [q_.h], [dh("QB")])
                    load(q_.t[64:96, :], MB[h * 32:(h + 1) * 32, :], [q_.h], [dh("MB")])
                    load(k_.t[0:64, :], KB[h * 64:(h + 1) * 64, :], [k_.h], [dh("KB")])
                    load(k_.t[64:96, :], c_oh[:, :], [k_.h])
                    vsrc, vn, sc, odst, on = VBv, "VB", 64.0 ** -0.5, OB, "OB"
                vstep = max(1, NT // 4)
                for n0 in range(0, NT, vstep):
                    load(v_.t[:, n0:n0 + vstep, 0:64], vsrc[:, n0:n0 + vstep, h * 64:(h + 1) * 64], [v_.h], [dh(vn)])
                if hp == 3:
                    zt = sb("zt", [128, 4, D], BF16)
                    op("pool", lambda e: e.memset(zt.t[:], 0.0), writes=[zt.h])
                    XSv = XS.rearrange("(n p) d -> p n d", p=128)
                    zlist = list(range(0, NSLOT // 128, 4))
                if hp >= 3:
                    nz = -(-len(zlist) // 12) if hp < 15 else len(zlist)
                    for _ in range(min(nz, len(zlist))):
                        n0 = zlist.pop(0)
                        store(XSv[:, n0:n0 + 4, :], zt.t[:], [zt.h], [dh("XS")])
                if hp == 2:
                    conv3(w_in_d, WIN, "WIN", 8, 4640, True, cstart=2592)
                    conv3(w_oa_d, WOA, "WOA", 4, D, False)
                    conv3(w_ob_d, WOB, "WOB", 4, D, False)
                    conv3(w_out_d, WOUT, "WOUT", 8, D, False)
                for g in range(NG):
                    nkp = 2 * g + 2
                    if hp >= 2 and c3q and g >= 4:
                        c3q.pop(0)()
                    for kp in range(nkp):
                        pend.append(emit_pair(q_, k_, v_, sc, g, kp, nkp, odst, on, h))
                        if len(pend) > LA:
                            pend.pop(0)()
            while pend:
                pend.pop(0)()
            while c3q:
                c3q.pop(0)()
            Sx.barrier()
        if stop_after <= 3:
            return _finish(nc, Sx, out_d, S)

        with ExitStack() as st:
            sb, ps = mk(st)
            identf = sb("identf", [128, 128], F32)
            onesf = sb("onesf4", [128, 128], F32)
            onesb = sb("onesb4", [128, 128], BF16)
            triu = sb("triu", [128, 128], BF16)
            offs = sb("offs", [128, 32], F32)
            load(identf.t[:], c_identf, [identf.h])
            load(onesf.t[:], c_onesf, [onesf.h])
            load(onesb.t[:], c_onesb, [onesb.h])
            load(triu.t[:], c_triu, [triu.h])
            load(offs.t[:], c_offs, [offs.h])
            wg = sb("wgate", [128, 8, 2048], BF16)
            woa = sb("woa", [128, 4, D], BF16)
            wob = sb("wob", [128, 4, D], BF16)
            wout = sb("wout", [128, 8, D], BF16)
            winv = WIN.rearrange("(k p) c -> p k c", p=128)
            for c0 in range(0, 2048, 512):
                load(wg.t[:, :, c0:c0 + 512], winv[:, :, 2592 + c0:2592 + c0 + 512], [wg.h], [dh("WIN")])
            load(woa.t[:], WOA.rearrange("(k p) c -> p k c", p=128), [woa.h], [dh("WOA")])
            load(wob.t[:], WOB.rearrange("(k p) c -> p k c", p=128), [wob.h], [dh("WOB")])
            for c0 in range(0, D, 512):
                load(wout.t[:, :, c0:c0 + 512], WOUT.rearrange("(k p) c -> p k c", p=128)[:, :, c0:c0 + 512], [wout.h], [dh("WOUT")])
            gf = sb("gf", [128, 8], F32)
            wr_s = sb("wr_s", [128, 8, 36], F32)
            wr = sb("wr", [128, 8, 36], F32)
            br = sb("br", [1, 36], F32)
            load(gf.t[:], ffn_norm_d, [gf.h])
            load(wr_s.t[:], w_r_d.rearrange("(k p) c -> p k c", p=128), [wr_s.h])
            load(br.t[:], b_r_d, [br.h])
            for k in range(8):
                op("dve", lambda e, k=k: e.tensor_scalar_mul(out=wr.t[:, k, :], in0=wr_s.t[:, k, :], scalar1=gf.t[:, k:k + 1]),
                   reads=[wr_s.h, gf.h], writes=[wr.h])
            run = sb("run", [128, 32], F32)
            op("dve", lambda e: e.memset(run.t[:], 0.0), writes=[run.h])

            hTg = sb("hTg", [128, 8, 512], BF16)
            oag = sb("oag", [128, 4, 512], BF16)
            obg = sb("obg", [128, 4, 512], BF16)
            sig = [sb(f"sig{i}", [128, 512], F32) for i in range(2)]
            m1 = [sb(f"m1_{i}", [128, 512], F32) for i in range(2)]
            mixT = sb("mixT", [128, 8, 512], BF16, 8)
            xtok = [sb(f"xtok{i}", [128, D], F32) for i in range(2)]
            x1 = [sb(f"x1_{i}", [128, D], F32) for i in range(3)]
            junk = sb("junk", [128, D], F32)
            pA = [ps(f"pA{i}", [128, 512], F32) for i in range(2)]
            pG = [ps(f"pG{i}", [128, 512], F32) for i in range(2)]
            pY = [ps(f"pY{i}", [128, 512], F32) for i in range(2)]
            pTr = ps("pTr", [128, 512], F32)
            pL = ps("pL", [128, 512], F32)
            hpC = H()
            HTv = HT.rearrange("(k p) s -> p k s", p=128)
            OAv = OA.rearrange("(k p) s -> p k s", p=128)
            OBv = OB.rearrange("(k p) s -> p k s", p=128)
            RS = []
            for i in range(4):
                RS.append(dict(
                    L=sb(f"L{i}", [128, 36], F32), gmax=sb(f"gmax{i}", [128, 1], F32), ngmax=sb(f"ngmax{i}", [128, 1], F32),
                    sume=sb(f"sume{i}", [128, 1], F32), pgrp=sb(f"pgrp{i}", [128, 1], F32), mx1=sb(f"mx1{i}", [128, 1], F32),
                    mx2=sb(f"mx2{i}", [128, 1], F32), dd=sb(f"dd{i}", [128, 1], F32), sg1=sb(f"sg1{i}", [128, 1], F32),
                    gone=sb(f"gone{i}", [128, 4], F32),
                    ein=sb(f"ein{i}", [128, 8], F32), ein2=sb(f"ein2{i}", [128, 8], F32), one1=sb(f"one1{i}", [128, 8], F32),
                    one2=sb(f"one2{i}", [128, 8], F32), ex4=sb(f"ex4{i}", [128, 4], F32), E1=sb(f"E1{i}", [128, 32], F32),
                    E2=sb(f"E2{i}", [128, 32], F32), Ab=sb(f"Ab{i}", [128, 32], BF16), pos=sb(f"pos{i}", [128, 32], F32),
                    tmpa=sb(f"tmpa{i}", [128, 32], F32), tmpb=sb(f"tmpb{i}", [128, 32], F32), slf=sb(f"slf{i}", [128, 2], F32),
                    ss1=sb(f"ss1{i}", [128, 2], F32), hnT=sb(f"hnT{i}", [128, 8, 128], F32)))
            hn4 = [sb(f"hn4{i}", [128, D], F32) for i in range(4)]
            hnb8 = [sb(f"hnb8{i}", [128, D], BF16) for i in range(8)]

            bgq = []

            def bg_run(n):
                for _ in range(n):
                    if bgq:
                        bgq.pop(0)()

            def stageA1(ti, tt):
                rows = slice(ti * 128, (ti + 1) * 128)
                xk, x1_ = xtok[ti % 2], x1[ti % 3]
                load(xk.t[:], x_d[rows, :], [xk.h])
                for half in range(2):
                    py = pY[half]
                    for k in range(8):
                        mm(py.t[:], mixT.t[:, k, tt * 128:(tt + 1) * 128], wout.t[:, k, half * 512:(half + 1) * 512], k == 0, k == 7,
                           [mixT.hs[k], wout.h], [py.h])
                    op("dve", lambda e, py=py, half=half: e.tensor_tensor(out=x1_.t[:, half * 512:(half + 1) * 512], in0=xk.t[:, half * 512:(half + 1) * 512], in1=py.t[:], op=ALU.add),
                       reads=[py.h, xk.h], writes=[x1_.h])

            def stageA2(ti, tt):
                rows = slice(ti * 128, (ti + 1) * 128)
                x1_, hn_, hnb_ = x1[ti % 3], hn4[ti % 4], hnb8[ti % 8]
                ss1_ = RS[ti % 4]["ss1"]
                op("act", lambda e: e.dma_start(out=X1[rows, :], in_=x1_.t[:]), reads=[x1_.h], writes=[dh("X1")], dma=True)
                op("act", lambda e: e.activation(out=junk.t[:], in_=x1_.t[:], func=AF.Square, accum_out=ss1_.t[:, 0:1]),
                   reads=[x1_.h], writes=[junk.h, ss1_.h])
                op("act", lambda e: e.activation(out=ss1_.t[:, 1:2], in_=ss1_.t[:, 0:1], func=AF.Sqrt, scale=1.0 / D, bias=EPS), reads=[ss1_.h], writes=[ss1_.h])
                op("dve", lambda e: e.reciprocal(out=ss1_.t[:, 1:2], in_=ss1_.t[:, 1:2]), reads=[ss1_.h], writes=[ss1_.h])
                op("dve", lambda e: e.tensor_scalar_mul(out=hn_.t[:], in0=x1_.t[:], scalar1=ss1_.t[:, 1:2]), reads=[x1_.h, ss1_.h], writes=[hn_.h])
                op("act", lambda e: e.copy(out=hnb_.t[:], in_=hn_.t[:]), reads=[hn_.h], writes=[hnb_.h])

            def stageB4(g):
                tis = [g * 4 + tt for tt in range(4)]
                for ti in tis:
                    R = RS[ti % 4]
                    hn_, hnT_, L = hn4[ti % 4], R["hnT"], R["L"]
                    for k in range(8):
                        op("pe", lambda e, k=k, hn_=hn_: e.transpose(out=pTr.t[:, (k % 4) * 128:(k % 4 + 1) * 128], in_=hn_.t[:, k * 128:(k + 1) * 128], identity=identf.t[:]),
                           reads=[hn_.h, identf.h], writes=[pTr.h])
                        if k % 4 == 3:
                            kk = k // 4
                            op("act", lambda e, kk=kk, hnT_=hnT_: e.copy(out=hnT_.t[:, kk * 4:(kk + 1) * 4, :], in_=pTr.t[:].rearrange("p (a b) -> p a b", a=4)),
                               reads=[pTr.h], writes=[hnT_.h])
                for ti in tis:
                    R = RS[ti % 4]
                    hnT_, L = R["hnT"], R["L"]
                    c0 = (ti % 4) * 64
                    for k in range(8):
                        mm(pL.t[:, c0:c0 + 36], hnT_.t[:, k, :], wr.t[:, k, :], k == 0, False, [hnT_.h, wr.h], [pL.h])
                    mm(pL.t[:, c0:c0 + 36], onesf.t[0:1, :], br.t[0:1, :], False, True, [onesf.h, br.h], [pL.h])
                for ti in tis:
                    R = RS[ti % 4]
                    c0 = (ti % 4) * 64
                    op("act", lambda e, R=R, c0=c0: e.copy(out=R["L"].t[:], in_=pL.t[:, c0:c0 + 36]), reads=[pL.h], writes=[R["L"].h])

                def each(fn):
                    def step():
                        for ti in tis:
                            fn(ti, RS[ti % 4])
                    bgq.append(step)
                each(lambda ti, R: op("dve", lambda e: e.tensor_reduce(out=R["gmax"].t[:], in_=R["L"].t[:, 0:4], axis=AX.X, op=ALU.max), reads=[R["L"].h], writes=[R["gmax"].h]))
                each(lambda ti, R: op("dve", lambda e: e.tensor_scalar(out=R["gone"].t[:], in0=R["L"].t[:, 0:4], scalar1=R["gmax"].t[:, 0:1], scalar2=None, op0=ALU.is_equal), reads=[R["L"].h, R["gmax"].h], writes=[R["gone"].h]))
                each(lambda ti, R: op("dve", lambda e: e.tensor_scalar_mul(out=R["ngmax"].t[:], in0=R["gmax"].t[:], scalar1=-1.0), reads=[R["gmax"].h], writes=[R["ngmax"].h]))
                each(lambda ti, R: op("dve", lambda e: e.tensor_scalar_mul(out=R["ein"].t[:], in0=R["L"].t[:, 4:12], scalar1=R["gone"].t[:, 0:1]), reads=[R["L"].h, R["gone"].h], writes=[R["ein"].h]))
                each(lambda ti, R: op("act", lambda e: e.activation(out=R["ex4"].t[:], in_=R["L"].t[:, 0:4], func=AF.Exp, bias=R["ngmax"].t[:, 0:1], accum_out=R["sume"].t[:, 0:1]),
                                     reads=[R["L"].h, R["ngmax"].h], writes=[R["ex4"].h, R["sume"].h]))
                for gg in range(1, 4):
                    each(lambda ti, R, gg=gg: op("dve", lambda e: e.scalar_tensor_tensor(out=R["ein"].t[:], in0=R["L"].t[:, 4 + 8 * gg:12 + 8 * gg], scalar=R["gone"].t[:, gg:gg + 1], in1=R["ein"].t[:], op0=ALU.mult, op1=ALU.add),
                                                 reads=[R["L"].h, R["gone"].h, R["ein"].h], writes=[R["ein"].h]))
                each(lambda ti, R: op("dve", lambda e: e.tensor_reduce(out=R["mx1"].t[:], in_=R["ein"].t[:], axis=AX.X, op=ALU.max), reads=[R["ein"].h], writes=[R["mx1"].h]))
                each(lambda ti, R: op("dve", lambda e: e.tensor_scalar(out=R["one1"].t[:], in0=R["ein"].t[:], scalar1=R["mx1"].t[:, 0:1], scalar2=None, op0=ALU.is_equal), reads=[R["ein"].h, R["mx1"].h], writes=[R["one1"].h]))
                each(lambda ti, R: op("dve", lambda e: e.scalar_tensor_tensor(out=R["ein2"].t[:], in0=R["one1"].t[:], scalar=-1e30, in1=R["ein"].t[:], op0=ALU.mult, op1=ALU.add), reads=[R["one1"].h, R["ein"].h], writes=[R["ein2"].h]))
                each(lambda ti, R: op("dve", lambda e: e.tensor_reduce(out=R["mx2"].t[:], in_=R["ein2"].t[:], axis=AX.X, op=ALU.max), reads=[R["ein2"].h], writes=[R["mx2"].h]))
                each(lambda ti, R: op("dve", lambda e: e.tensor_scalar(out=R["one2"].t[:], in0=R["ein2"].t[:], scalar1=R["mx2"].t[:, 0:1], scalar2=None, op0=ALU.is_equal), reads=[R["ein2"].h, R["mx2"].h], writes=[R["one2"].h]))
                each(lambda ti, R: op("dve", lambda e: e.tensor_tensor(out=R["dd"].t[:], in0=R["mx1"].t[:], in1=R["mx2"].t[:], op=ALU.subtract), reads=[R["mx1"].h, R["mx2"].h], writes=[R["dd"].h]))
                each(lambda ti, R: op("dve", lambda e: e.reciprocal(out=R["pgrp"].t[:], in_=R["sume"].t[:]), reads=[R["sume"].h], writes=[R["pgrp"].h]))
                each(lambda ti, R: op("act", lambda e: e.activation(out=R["sg1"].t[:], in_=R["dd"].t[:], func=AF.Sigmoid), reads=[R["dd"].h], writes=[R["sg1"].h]))
                for gg in range(4):
                    each(lambda ti, R, gg=gg: op("dve", lambda e: e.tensor_scalar_mul(out=R["E1"].t[:, gg * 8:(gg + 1) * 8], in0=R["one1"].t[:], scalar1=R["gone"].t[:, gg:gg + 1]), reads=[R["one1"].h, R["gone"].h], writes=[R["E1"].h]))
                    each(lambda ti, R, gg=gg: op("dve", lambda e: e.tensor_scalar_mul(out=R["E2"].t[:, gg * 8:(gg + 1) * 8], in0=R["one2"].t[:], scalar1=R["gone"].t[:, gg:gg + 1]), reads=[R["one2"].h, R["gone"].h], writes=[R["E2"].h]))
                each(lambda ti, R: op("dve", lambda e: e.tensor_tensor(out=R["Ab"].t[:], in0=R["E1"].t[:], in1=R["E2"].t[:], op=ALU.add), reads=[R["E1"].h, R["E2"].h], writes=[R["Ab"].h]))
                each(lambda ti, R: op("dve", lambda e: e.tensor_tensor(out=wts.t[:, ti, 0:1], in0=R["sg1"].t[:], in1=R["pgrp"].t[:], op=ALU.mult), reads=[R["sg1"].h, R["pgrp"].h], writes=[wts.hs[ti % 4]]))
                each(lambda ti, R: op("dve", lambda e: e.tensor_tensor(out=wts.t[:, ti, 1:2], in0=R["pgrp"].t[:], in1=wts.t[:, ti, 0:1], op=ALU.subtract), reads=[R["pgrp"].h, wts.hs[ti % 4]], writes=[wts.hs[ti % 4]]))

            def stageC4(g):
                tis = [g * 4 + tt for tt in range(4)]
                for ti in tis:
                    R = RS[ti % 4]
                    c0 = (ti % 4) * 64
                    mm(pL.t[:, 256 + c0:256 + c0 + 32], triu.t[:], R["Ab"].t[:], True, True, [triu.h, R["Ab"].h], [hpC])
                    mm(pL.t[:, 256 + c0 + 32:256 + c0 + 64], onesb.t[:], R["Ab"].t[:], True, True, [onesb.h, R["Ab"].h], [hpC])
                for ti in tis:
                    R = RS[ti % 4]
                    c0 = (ti % 4) * 64
                    op("dve", lambda e, R=R, c0=c0: e.tensor_tensor(out=R["pos"].t[:], in0=pL.t[:, 256 + c0:256 + c0 + 32], in1=run.t[:], op=ALU.add), reads=[hpC, run.h], writes=[R["pos"].h])
                    op("dve", lambda e, c0=c0: e.tensor_tensor(out=run.t[:], in0=pL.t[:, 256 + c0 + 32:256 + c0 + 64], in1=run.t[:], op=ALU.add), reads=[hpC, run.h], writes=[run.h])

                def each(fn):
                    for ti in tis:
                        fn(ti, RS[ti % 4])
                each(lambda ti, R: op("dve", lambda e: e.tensor_tensor(out=R["pos"].t[:], in0=R["pos"].t[:], in1=offs.t[:], op=ALU.add), reads=[R["pos"].h, offs.h], writes=[R["pos"].h]))
                each(lambda ti, R: op("dve", lambda e: e.tensor_tensor(out=R["tmpa"].t[:], in0=R["pos"].t[:], in1=R["E1"].t[:], op=ALU.mult), reads=[R["pos"].h, R["E1"].h], writes=[R["tmpa"].h]))
                each(lambda ti, R: op("dve", lambda e: e.tensor_tensor(out=R["tmpb"].t[:], in0=R["pos"].t[:], in1=R["E2"].t[:], op=ALU.mult), reads=[R["pos"].h, R["E2"].h], writes=[R["tmpb"].h]))
                each(lambda ti, R: op("dve", lambda e: e.tensor_reduce(out=R["slf"].t[:, 0:1], in_=R["tmpa"].t[:], axis=AX.X, op=ALU.add), reads=[R["tmpa"].h], writes=[R["slf"].h]))
                each(lambda ti, R: op("dve", lambda e: e.tensor_reduce(out=R["slf"].t[:, 1:2], in_=R["tmpb"].t[:], axis=AX.X, op=ALU.add), reads=[R["tmpb"].h, R["slf"].h], writes=[R["slf"].h]))
                each(lambda ti, R: op("dve", lambda e: e.tensor_copy(out=slot_i.t[:, ti, :], in_=R["slf"].t[:]), reads=[R["slf"].h], writes=[slot_i.hs[ti % 4]]))
                for ti in tis:
                    hnb_ = hnb8[ti % 8]
                    for j in range(2):
                        op("pool", lambda e, j=j, ti=ti, hnb_=hnb_: e.indirect_dma_start(
                            out=XS, out_offset=bass.IndirectOffsetOnAxis(ap=slot_i.t[:, ti, j:j + 1], axis=0),
                            in_=hnb_.t[:], in_offset=None), reads=[hnb_.h, slot_i.hs[ti % 4]], writes=[dh("XS")], dma=True)

            ci = 0
            for g in range(NG):
                tok = slice(g * 512, (g + 1) * 512)
                load(hTg.t[:], HTv[:, :, tok], [hTg.h], [dh("HT")])
                load(oag.t[:], OAv[:, :, tok], [oag.h], [dh("OA")])
                load(obg.t[:], OBv[:, :, tok], [obg.h], [dh("OB")])
                it = 0
                for c in range(8):
                    for br_i, (og, wo, gc0) in enumerate(((oag, woa, 0), (obg, wob, 1024))):
                        if it == 3 and g >= 1:
                            stageB4(g - 1)
                        it += 1
                        pa, pg_ = pA[ci % 2], pG[ci % 2]
                        sg, mm1 = sig[ci % 2], m1[ci % 2]
                        ci += 1
                        for k in range(4):
                            mm(pa.t[:], wo.t[:, k, c * 128:(c + 1) * 128], og.t[:, k, :], k == 0, k == 3, [wo.h, og.h], [pa.h])
                        for k in range(8):
                            mm(pg_.t[:], wg.t[:, k, gc0 + c * 128: gc0 + (c + 1) * 128], hTg.t[:, k, :], k == 0, k == 7, [wg.h, hTg.h], [pg_.h])
                        op("act", lambda e, pg_=pg_, sg=sg: e.activation(out=sg.t[:], in_=pg_.t[:], func=AF.Sigmoid), reads=[pg_.h], writes=[sg.h])
                        if br_i == 0:
                            op("dve", lambda e, pa=pa, sg=sg, mm1=mm1: e.tensor_tensor(out=mm1.t[:], in0=sg.t[:], in1=pa.t[:], op=ALU.mult),
                               reads=[sg.h, pa.h], writes=[mm1.h])
                            prev = mm1
                        else:
                            op("dve", lambda e, pa=pa, sg=sg: e.tensor_tensor(out=sg.t[:], in0=sg.t[:], in1=pa.t[:], op=ALU.mult),
                               reads=[sg.h, pa.h], writes=[sg.h])
                            op("dve", lambda e, sg=sg, prev=prev, c=c: e.tensor_tensor(out=mixT.t[:, c, :], in0=sg.t[:], in1=prev.t[:], op=ALU.add),
                               reads=[sg.h, prev.h], writes=[mixT.hs[c]])
                        bg_run(3)
                bg_run(10 ** 6)
                if g >= 1:
                    stageC4(g - 1)
                for tt in range(4):
                    stageA1(g * 4 + tt, tt)
                    if tt >= 1:
                        stageA2(g * 4 + tt - 1, tt - 1)
                stageA2(g * 4 + 3, 3)
            stageB4(NG - 1)
            bg_run(10 ** 6)
            stageC4(NG - 1)
            Sx.barrier()
        if stop_after <= 4:
            return _finish(nc, Sx, out_d, S)

        with ExitStack() as st:
            sb, ps = mk(st)
            identb5x = sb("identb5", [128, 128], BF16)
            load(identb5x.t[:], c_identb, [identb5x.h])
            gf5v = sb("gf5", [128, 8], F32)
            load(gf5v.t[:], ffn_norm_d, [gf5v.h])
            NST = C // 128
            HALF = C // 2
            NSH = HALF // 128
            wgs = [sb(f"wgs{i}", [128, 8, 256], F32) for i in range(2)]
            wus = [sb(f"wus{i}", [128, 8, 256], F32) for i in range(2)]
            wds = [sb(f"wds{i}", [128, 2, D], F32) for i in range(2)]
            wgb = [sb(f"wgb{i}", [128, 8, 256], BF16) for i in range(2)]
            wub = [sb(f"wub{i}", [128, 8, 256], BF16) for i in range(2)]
            wdb = [sb(f"wdb{i}", [128, 2, D], BF16) for i in range(2)]
            xs = [sb(f"xs{i}", [128, D], BF16) for i in range(6)]
            xeT = [sb(f"xeT{i}", [128, 8, C], BF16) for i in range(2)]
            sil = [sb(f"sil{i}", [128, HALF], F32) for i in range(2)]
            actT = [sb(f"actT{i}", [128, 2, HALF], BF16, 2) for i in range(2)]
            ysb = [sb(f"ysb{i}", [128, D], BF16) for i in range(6)]
            ptx = [ps(f"ptx{i}", [128, 1024], BF16) for i in range(2)]
            pgu = [ps(f"pgu{i}", [128, 512], F32) for i in range(4)]
            pyy = [ps(f"pyy{i}", [128, 512], F32) for i in range(2)]
            xi = [0]
            yi = [0]

            def stage_load(ex):
                b = ex % 2
                load(wgs[b].t[:], w_g_d[ex].rearrange("(k p) f -> p k f", p=128), [wgs[b].h])
                load(wus[b].t[:], w_u_d[ex].rearrange("(k p) f -> p k f", p=128), [wus[b].h])
                load(wds[b].t[:], w_d_d[ex].rearrange("(k p) f -> p k f", p=128), [wds[b].h])

            def conv_part(ex, part):
                b = ex % 2
                for k in (2 * part, 2 * part + 1):
                    op("act", lambda e, k=k, b=b: e.activation(out=wgb[b].t[:, k, :], in_=wgs[b].t[:, k, :], func=AF.Copy, scale=gf5v.t[:, k:k + 1]),
                       reads=[wgs[b].h, gf5v.h], writes=[wgb[b].h])
                    op("dve", lambda e, k=k, b=b: e.tensor_scalar_mul(out=wub[b].t[:, k, :], in0=wus[b].t[:, k, :], scalar1=gf5v.t[:, k:k + 1]),
                       reads=[wus[b].h, gf5v.h], writes=[wub[b].h])
                if part == 3:
                    op("dve", lambda e, b=b: e.tensor_copy(out=wdb[b].t[:, 0, :], in_=wds[b].t[:, 0, :]), reads=[wds[b].h], writes=[wdb[b].h])
                    op("dve", lambda e, b=b: e.tensor_copy(out=wdb[b].t[:, 1, :], in_=wds[b].t[:, 1, :]), reads=[wds[b].h], writes=[wdb[b].h])

            def stage_a(ex):
                b = ex % 2
                xe = xeT[b]
                for stl in range(NST):
                    row0 = ex * C + stl * 128
                    xs_ = xs[xi[0] % 6]
                    px = ptx[xi[0] % 2]
                    xi[0] += 1
                    load(xs_.t[:], XS[row0:row0 + 128, :], [xs_.h], [dh("XS")])
                    for k in range(8):
                        op("pe", lambda e, k=k, xs_=xs_, px=px: e.transpose(out=px.t[:, k * 128:(k + 1) * 128], in_=xs_.t[:, k * 128:(k + 1) * 128], identity=identb5x.t[:]),
                           reads=[xs_.h, identb5x.h], writes=[px.h])
                    op("act", lambda e, px=px, xe=xe, stl=stl: e.copy(out=xe.t[:, :, stl * 128:(stl + 1) * 128], in_=px.t[:].rearrange("p (a b) -> p a b", a=8)),
                       reads=[px.h], writes=[xe.h])

            def gu(ex, hf, fc):
                b = ex % 2
                xe = xeT[b]
                cs = slice(hf * HALF, (hf + 1) * HALF)
                at = actT[hf]
                pgt, put = pgu[fc * 2], pgu[fc * 2 + 1]
                for k in range(8):
                    mm(pgt.t[:, 0:HALF], wgb[b].t[:, k, fc * 128:(fc + 1) * 128], xe.t[:, k, cs], k == 0, k == 7, [wgb[b].h, xe.h], [pgt.h])
                for k in range(8):
                    mm(put.t[:, 0:HALF], wub[b].t[:, k, fc * 128:(fc + 1) * 128], xe.t[:, k, cs], k == 0, k == 7, [wub[b].h, xe.h], [put.h])
                sl = sil[fc]
                op("act", lambda e: e.activation(out=sl.t[:], in_=pgt.t[:, 0:HALF], func=AF.Silu), reads=[pgt.h], writes=[sl.h])
                op("dve", lambda e: e.tensor_tensor(out=at.t[:, fc, :], in0=sl.t[:], in1=put.t[:, 0:HALF], op=ALU.mult),
                   reads=[sl.h, put.h], writes=[at.hs[fc]])

            def down(ex, hf):
                b = ex % 2
                at = actT[hf]
                for stl in range(NSH):
                    ys_ = ysb[yi[0] % 6]
                    yi[0] += 1
                    for half in range(2):
                        py = pyy[half]
                        for fc in range(2):
                            mm(py.t[:], at.t[:, fc, stl * 128:(stl + 1) * 128], wdb[b].t[:, fc, half * 512:(half + 1) * 512], fc == 0, fc == 1,
                               [at.hs[fc], wdb[b].h], [py.h])
                        if half == 0:
                            op("act", lambda e, py=py, ys_=ys_: e.copy(out=ys_.t[:, 0:512], in_=py.t[:]), reads=[py.h], writes=[ys_.h])
                        else:
                            op("dve", lambda e, py=py, ys_=ys_: e.tensor_copy(out=ys_.t[:, 512:1024], in_=py.t[:]), reads=[py.h], writes=[ys_.h])
                    row0 = ex * C + hf * HALF + stl * 128
                    store(YS[row0:row0 + 128, :], ys_.t[:], [ys_.h], [dh("YS")])

            stage_load(0)
            stage_load(1)
            for part in range(4):
                conv_part(0, part)
            stage_a(0)
            for ex in range(NE):
                nxt_ex = ex + 1 < NE
                if nxt_ex:
                    stage_a(ex + 1)
                if ex + 2 < NE:
                    stage_load(ex + 2)
                gu(ex, 0, 0)
                if nxt_ex:
                    conv_part(ex + 1, 0)
                gu(ex, 0, 1)
                if nxt_ex:
                    conv_part(ex + 1, 1)
                gu(ex, 1, 0)
                if nxt_ex:
                    conv_part(ex + 1, 2)
                gu(ex, 1, 1)
                if nxt_ex:
                    conv_part(ex + 1, 3)
                down(ex, 0)
                down(ex, 1)
            Sx.barrier()
        if stop_after <= 5:
            return _finish(nc, Sx, out_d, S)

        with ExitStack() as st:
            sb, ps = mk(st)
            onesf6v = sb("onesf6", [128, 128], F32)
            fn_row = sb("fn_row", [1, D], F32)
            GF = sb("GF", [128, D], F32)
            load(onesf6v.t[:], c_onesf, [onesf6v.h])
            load(fn_row.t[:], fnorm_d, [fn_row.h])
            pb = [ps(f"pb{i}", [128, 512], F32) for i in range(2)]
            for half in range(2):
                mm(pb[half].t[:], onesf6v.t[0:1, :], fn_row.t[0:1, half * 512:(half + 1) * 512], True, True, [onesf6v.h, fn_row.h], [pb[half].h])
                op("act", lambda e, half=half: e.copy(out=GF.t[:, half * 512:(half + 1) * 512], in_=pb[half].t[:]), reads=[pb[half].h], writes=[GF.h])
            NB6 = 3
            x1t = [sb(f"x1t{i}", [128, D], F32) for i in range(NB6)]
            y1 = [sb(f"y1_{i}", [128, D], BF16) for i in range(NB6)]
            y2 = [sb(f"y2_{i}", [128, D], BF16) for i in range(NB6)]
            junk6v = sb("junk6", [128, D], F32)
            ssf = [sb(f"ssf{i}", [128, 2], F32) for i in range(2)]
            ot = [sb(f"ot{i}", [128, D], F32) for i in range(2)]
            hout = H()

            def fetch(ti):
                b = ti % NB6
                rows = slice(ti * 128, (ti + 1) * 128)
                load(x1t[b].t[:], X1[rows, :], [x1t[b].h], [dh("X1")])
                for j, yy in enumerate((y1[b], y2[b])):
                    op("pool", lambda e, j=j, yy=yy: e.indirect_dma_start(
                        out=yy.t[:], out_offset=None, in_=YS,
                        in_offset=bass.IndirectOffsetOnAxis(ap=slot_i.t[:, ti, j:j + 1], axis=0)),
                       reads=[dh("YS"), slot_i.h], writes=[yy.h], dma=True)

            def compute(ti):
                b = ti % NB6
                rows = slice(ti * 128, (ti + 1) * 128)
                xx, ya, yb_ = x1t[b], y1[b], y2[b]
                op("dve", lambda e: e.scalar_tensor_tensor(out=xx.t[:], in0=ya.t[:], scalar=wts.t[:, ti, 0:1], in1=xx.t[:], op0=ALU.mult, op1=ALU.add),
                   reads=[ya.h, wts.h, xx.h], writes=[xx.h])
                op("dve", lambda e: e.scalar_tensor_tensor(out=xx.t[:], in0=yb_.t[:], scalar=wts.t[:, ti, 1:2], in1=xx.t[:], op0=ALU.mult, op1=ALU.add),
                   reads=[yb_.h, wts.h, xx.h], writes=[xx.h])
                sf = ssf[ti % 2]
                op("act", lambda e: e.activation(out=junk6v.t[:], in_=xx.t[:], func=AF.Square, accum_out=sf.t[:, 0:1]), reads=[xx.h], writes=[junk6v.h, sf.h])
                op("act", lambda e: e.activation(out=sf.t[:, 1:2], in_=sf.t[:, 0:1], func=AF.Sqrt, scale=1.0 / D, bias=EPS), reads=[sf.h], writes=[sf.h])
                op("dve", lambda e: e.reciprocal(out=sf.t[:, 1:2], in_=sf.t[:, 1:2]), reads=[sf.h], writes=[sf.h])
                o_ = ot[ti % 2]
                op("dve", lambda e: e.scalar_tensor_tensor(out=o_.t[:], in0=xx.t[:], scalar=sf.t[:, 1:2], in1=GF.t[:], op0=ALU.mult, op1=ALU.mult),
                   reads=[xx.h, sf.h, GF.h], writes=[o_.h])
                op("sp", lambda e: e.dma_start(out=out_d[rows, :], in_=o_.t[:]), reads=[o_.h], writes=[hout], dma=True)

            fetch(0)
            if NT > 1:
                fetch(1)
            for ti in range(NT):
                if ti + 2 < NT:
                    fetch(ti + 2)
                compute(ti)
            Sx.barrier()
        return _finish(nc, Sx, out_d, S)


def _finish(nc, Sx, out_d, S):
    Sx.barrier()
    Sx.emit_all()
    return nc


def _consts(S, C):
    bf = ml_dtypes.bfloat16
    c = {}
    c["c_identb"] = np.eye(128, dtype=np.float32).astype(bf)
    c["c_identf"] = np.eye(128, dtype=np.float32)
    c["c_onesb"] = np.ones((128, 128), np.float32).astype(bf)
    c["c_onesf"] = np.ones((128, 128), np.float32)
    pa = np.zeros((96, 96), np.float32)
    for m in range(96):
        if m < 64:
            k = m
        elif m < 80:
            k = m + 16
        else:
            k = m - 16
        pa[k, m] = 1.0
    c["c_perma"] = pa.astype(bf)
    pb = np.zeros((128, 128), np.float32)
    for m in range(128):
        j = m % 64
        if j < 8:
            k = m + 8
        elif j < 16:
            k = m - 8
        else:
            k = m
        pb[k, m] = 1.0
    c["c_permb"] = pb.astype(bf)
    pos = np.arange(S, dtype=np.float64)
    inv = ROPE_THETA ** (-np.arange(16, dtype=np.float64) / 16)
    ang = pos[None, :] * inv[:, None]
    tac = np.ones((96, S), np.float64)
    tas = np.zeros((96, S), np.float64)
    tac[64:80] = np.cos(ang)
    tac[80:96] = np.cos(ang)
    tas[64:80] = -np.sin(ang)
    tas[80:96] = np.sin(ang)
    c["c_tac"] = tac.astype(np.float32)
    c["c_tas"] = tas.astype(np.float32)
    inv = ROPE_THETA ** (-np.arange(8, dtype=np.float64) / 8)
    ang = pos[None, :] * inv[:, None]
    tbc = np.ones((128, S), np.float64)
    tbs = np.zeros((128, S), np.float64)
    for b0 in (0, 64):
        tbc[b0:b0 + 8] = np.cos(ang)
        tbc[b0 + 8:b0 + 16] = np.cos(ang)
        tbs[b0:b0 + 8] = -np.sin(ang)
        tbs[b0 + 8:b0 + 16] = np.sin(ang)
    c["c_tbc"] = tbc.astype(np.float32)
    c["c_tbs"] = tbs.astype(np.float32)
    m = np.zeros((128, 4, 512), np.float32)
    p = np.arange(128)[:, None]
    j = np.arange(512)[None, :]
    for r in range(4):
        m[:, r, :] = (r * 128 + p <= j)
    c["c_mask"] = m.astype(bf)
    oh = np.zeros((32, S), np.float32)
    for n in range(32):
        oh[n, n * 256:(n + 1) * 256] = 1.0
    c["c_oh"] = oh.astype(bf)
    c["c_wneg"] = np.concatenate([np.zeros((1, 32), np.float32), np.full((1, 1), 1e30, np.float32), np.full((1, 31), -1e30, np.float32)], axis=1).astype(bf)
    c["c_offs"] = np.tile((np.arange(32, dtype=np.float32) * C - 1.0)[None, :], (128, 1)).astype(np.float32)
    kk = np.arange(128)[:, None]
    mm_ = np.arange(128)[None, :]
    c["c_triu"] = (kk <= mm_).astype(np.float32).astype(bf)
    return c


def _prep_inputs(inputs, S, C):
    f = lambda a: np.ascontiguousarray(np.asarray(a, dtype=np.float32))
    w_ukv = f(inputs["w_ukv"])[0].reshape(256, 8, 128)
    w_ukv_kv = np.concatenate([w_ukv[:, :, :64].reshape(256, 512), w_ukv[:, :, 64:].reshape(256, 512)], axis=1)
    w_rg = f(inputs["w_router_group"])[0]
    w_re = f(inputs["w_router_expert"])[0]
    w_r = np.concatenate([w_rg] + [w_re[g] for g in range(4)], axis=1)
    b_r = np.concatenate([f(inputs["b_router_group"])[0], f(inputs["b_router_expert"])[0].reshape(32)])[None, :]
    shared = {
        "attn_norm": np.ascontiguousarray(f(inputs["attn_norm"])[0].reshape(8, 128).T),
        "w_in": f(inputs["w_in"])[0],
        "q_norm": np.ascontiguousarray(f(inputs["q_norm"])[0].reshape(6, 128).T),
        "w_uq": f(inputs["w_uq"])[0],
        "kv_norm": np.ascontiguousarray(f(inputs["kv_norm"])[0].reshape(2, 128).T),
        "w_ukv_kv": np.ascontiguousarray(w_ukv_kv),
        "w_o_mla": f(inputs["w_o_mla"])[0],
        "w_o_moba": f(inputs["w_o_moba"])[0],
        "w_out": f(inputs["w_out"])[0],
        "ffn_norm": np.ascontiguousarray(f(inputs["ffn_norm"])[0].reshape(8, 128).T),
        "w_r": np.ascontiguousarray(w_r),
        "b_r": np.ascontiguousarray(b_r),
        "w_exp_gate": f(inputs["w_exp_gate"])[0],
        "w_exp_up": f(inputs["w_exp_up"])[0],
        "w_exp_down": f(inputs["w_exp_down"])[0],
        "final_norm": f(inputs["final_norm"]).reshape(1, D),
    }
    shared.update(_consts(S, C))
    return shared


def kernel(**inputs):
    x = np.asarray(inputs["x"], dtype=np.float32)
    B, S, _ = x.shape
    C = 768 if S >= 8192 else max(256, (S * 2 // 32) * 3 // 128 * 128 + 128)
    nc = build(S=S, C=C)
    shared = _prep_inputs(inputs, S, C)
    in_maps = []
    for b in range(B):
        m = dict(shared)
        m["x"] = np.ascontiguousarray(x[b])
        m["xT"] = np.ascontiguousarray(x[b].T)
        in_maps.append(m)
    res = run_bass_kernel_spmd(nc, in_maps, core_ids=list(range(B)))
    return np.stack([r["out"] for r in res.results], axis=0)
```

```python
import numpy as np
from contextlib import ExitStack
import ml_dtypes
import concourse.bass as bass
import concourse.mybir as mybir
from concourse.bass_utils import run_bass_kernel_spmd

F32 = mybir.dt.float32
BF16 = mybir.dt.bfloat16
I32 = mybir.dt.int32
AF = mybir.ActivationFunctionType
ALU = mybir.AluOpType
AX = mybir.AxisListType

SEM_LIMIT = 30000
D = 1024
NE = 32
EPS = 1e-6
ROPE_THETA = 500000.0


class H:
    __slots__ = ("w", "r")

    def __init__(self):
        self.w = None
        self.r = {}


class Q:
    def __init__(self, name, sems):
        self.name = name
        self.sems = sems
        self.epoch = 0
        self.count = 0
        self.ops = []
        self.waited = {}


class Sched:
    def __init__(self, nc, stack, n_dma_sems=72):
        self.nc = nc
        self.semobj = {}
        self.q = {}
        for name, nep in (("pe", 5), ("act", 3), ("dve", 3), ("pool", 3), ("sp", 1)):
            sems = []
            for e in range(nep):
                s = stack.enter_context(nc.semaphore(f"s_{name}{e}"))
                key = f"{name}{e}"
                self.semobj[key] = s
                sems.append(key)
            self.q[name] = Q(name, sems)
        self.dma_sems = []
        for i in range(n_dma_sems):
            s = stack.enter_context(nc.semaphore(f"s_dma{i}"))
            key = f"dma{i}"
            self.semobj[key] = s
            self.dma_sems.append([key, 0])
        self.dma_rr = 0
        self.dma_rr_q = {}
        self.n_ops = 0

    def _deps(self, reads, writes):
        deps = {}

        def add(k, v):
            if v > deps.get(k, -1):
                deps[k] = v
        for h in reads:
            if h.w is not None:
                add(*h.w)
        for h in writes:
            if h.w is not None:
                add(*h.w)
            for k, v in h.r.items():
                add(k, v)
        return deps

    def op(self, qname, fn, reads=(), writes=(), dma=False):
        q = self.q[qname]
        deps = self._deps(reads, writes)
        if dma:
            third = len(self.dma_sems) // 3
            base = {"sp": 0, "pool": third, "act": 2 * third}[qname]
            rr = self.dma_rr_q.get(qname, 0)
            slot = self.dma_sems[base + rr]
            self.dma_rr_q[qname] = (rr + 1) % third
            if slot[1] > 0 and slot[1] > deps.get(slot[0], -1):
                deps[slot[0]] = slot[1]
            assert slot[1] + 16 < 60000
            slot[1] += 16
            comp = (slot[0], slot[1])
            inc = 16
        else:
            if q.count + 1 > SEM_LIMIT:
                q.epoch += 1
                q.count = 0
            q.count += 1
            comp = (q.sems[q.epoch], q.count)
            inc = 1
        waits = []
        for k, v in deps.items():
            if qname == "pe" and k.startswith("pe"):
                continue
            if q.waited.get(k, -1) >= v:
                continue
            q.waited[k] = v
            waits.append((self.semobj[k], v))
        csem = self.semobj[comp[0]]

        def emit(eng, waits=waits, fn=fn, csem=csem, inc=inc):
            for s, v in waits:
                eng.wait_ge(s, v)
            fn(eng).then_inc(csem, inc)
        q.ops.append(emit)
        for h in writes:
            h.w = comp
            h.r = {}
        for h in reads:
            if comp[1] > h.r.get(comp[0], -1):
                h.r[comp[0]] = comp[1]
        self.n_ops += 1
        return comp

    def barrier(self):
        deps = {}
        for q in self.q.values():
            for e in range(q.epoch + 1):
                cnt = q.count if e == q.epoch else SEM_LIMIT
                if cnt > 0:
                    deps[q.sems[e]] = cnt
        for k, v in self.dma_sems:
            if v > 0:
                deps[k] = v
        for q in self.q.values():
            waits = []
            for k, v in deps.items():
                if q.waited.get(k, -1) >= v:
                    continue
                q.waited[k] = v
                waits.append((self.semobj[k], v))

            def emit(eng, waits=waits):
                for s, v in waits:
                    eng.wait_ge(s, v)
            q.ops.append(emit)

    def emit_all(self):
        nc = self.nc
        with nc.Block() as block:
            @block.tensor
            def _(e):
                for f in self.q["pe"].ops:
                    f(e)

            @block.scalar
            def _(e):
                for f in self.q["act"].ops:
                    f(e)

            @block.vector
            def _(e):
                for f in self.q["dve"].ops:
                    f(e)

            @block.gpsimd
            def _(e):
                for f in self.q["pool"].ops:
                    f(e)

            @block.sync
            def _(e):
                for f in self.q["sp"].ops:
                    f(e)


class T:
    def __init__(self, t, n=1):
        self.t = t
        self.h = H()
        self.hs = [H() for _ in range(n)]


def build(S=8192, C=768, stop_after=99, debug=False):
    NG = S // 512
    NT = S // 128
    NSLOT = NE * C
    nc = bass.Bass("TRN2", target_bir_lowering=False)

    def din(name, shape, dt=F32):
        return nc.dram_tensor(name, shape, dt, kind="ExternalInput").ap()

    def dscr(name, shape, dt):
        return nc.dram_tensor(name, shape, dt, kind=("ExternalOutput" if debug else "Internal")).ap()

    x_d = din("x", [S, D])
    xT_d = din("xT", [D, S])
    attn_norm_d = din("attn_norm", [128, 8])
    w_in_d = din("w_in", [D, 4640])
    q_norm_d = din("q_norm", [128, 6])
    w_uq_d = din("w_uq", [768, 768])
    kv_norm_d = din("kv_norm", [128, 2])
    w_ukv_d = din("w_ukv_kv", [256, 1024])
    w_oa_d = din("w_o_mla", [512, D])
    w_ob_d = din("w_o_moba", [512, D])
    w_out_d = din("w_out", [D, D])
    ffn_norm_d = din("ffn_norm", [128, 8])
    w_r_d = din("w_r", [D, 36])
    b_r_d = din("b_r", [1, 36])
    w_g_d = din("w_exp_gate", [NE, D, 256])
    w_u_d = din("w_exp_up", [NE, D, 256])
    w_d_d = din("w_exp_down", [NE, 256, D])
    fnorm_d = din("final_norm", [1, D])
    c_identb = din("c_identb", [128, 128], BF16)
    c_identf = din("c_identf", [128, 128])
    c_onesb = din("c_onesb", [128, 128], BF16)
    c_onesf = din("c_onesf", [128, 128])
    c_perma = din("c_perma", [96, 96], BF16)
    c_permb = din("c_permb", [128, 128], BF16)
    c_tac = din("c_tac", [96, S])
    c_tas = din("c_tas", [96, S])
    c_tbc = din("c_tbc", [128, S])
    c_tbs = din("c_tbs", [128, S])
    c_mask = din("c_mask", [128, 4, 512], BF16)
    c_oh = din("c_oh", [32, S], BF16)
    c_wneg = din("c_wneg", [1, 64], BF16)
    c_offs = din("c_offs", [128, 32])
    c_triu = din("c_triu", [128, 128], BF16)

    out_d = nc.dram_tensor("out", [S, D], F32, kind="ExternalOutput").ap()

    WIN = dscr("WIN", [D, 4640], BF16)
    WUQ = dscr("WUQ", [768, 768], BF16)
    WUKV = dscr("WUKV", [256, 1024], BF16)
    WOA = dscr("WOA", [512, D], BF16)
    WOB = dscr("WOB", [512, D], BF16)
    WOUT = dscr("WOUT", [D, D], BF16)
    HT = dscr("HT", [D, S], BF16)
    QA = dscr("QA", [768, S], BF16)
    KNA = dscr("KNA", [512, S], BF16)
    KRA = dscr("KRA", [32, S], BF16)
    VA = dscr("VA", [S, 512], BF16)
    QB = dscr("QB", [512, S], BF16)
    KB = dscr("KB", [512, S], BF16)
    VB = dscr("VB", [S, 512], BF16)
    MB = dscr("MB", [256, S], BF16)
    OA = dscr("OA", [512, S], BF16)
    OB = dscr("OB", [512, S], BF16)
    X1 = dscr("X1", [S, D], F32)
    XS = dscr("XS", [NSLOT, D], BF16)
    YS = dscr("YS", [NSLOT, D], BF16)

    with ExitStack() as top:
        Sx = Sched(nc, top)
        op = Sx.op

        def mk(st):
            def sb(name, shape, dt, n=1):
                return T(st.enter_context(nc.sbuf_tensor(name, shape, dt)), n)

            def ps(name, shape, dt):
                return T(st.enter_context(nc.psum_tensor(name, shape, dt)))
            return sb, ps

        def load(dst_ap, src_ap, wr, rd=()):
            op("sp", lambda e: e.dma_start(out=dst_ap, in_=src_ap), reads=rd, writes=wr, dma=True)

        store_q = ["pool"]

        def store(dst_ap, src_ap, rd, wr):
            op(store_q[0], lambda e: e.dma_start(out=dst_ap, in_=src_ap), reads=rd, writes=wr, dma=True)

        def mm(out_ap, lhsT, rhs, start, stop, rd, wr):
            op("pe", lambda e: e.matmul(out_ap, lhsT, rhs, start=start, stop=stop), reads=rd, writes=wr)

        hd = {}

        def dh(name):
            if name not in hd:
                hd[name] = H()
            return hd[name]

        sbp, _ = mk(top)
        slot_i = sbp("slot_i", [128, NT, 2], I32, 4)
        wts = sbp("wts", [128, NT, 2], F32, 4)

        with ExitStack() as st:
            sb, ps = mk(st)
            stg = [sb(f"stg{i}", [128, 8, 512], F32) for i in range(2)]
            obf = [sb(f"obf{i}", [128, 8, 512], BF16) for i in range(2)]
            gains = sb("gains", [128, 3, 8], F32)
            load(gains.t[:, 0, :], attn_norm_d, [gains.hs[0]])
            load(gains.t[:, 1, 0:6], q_norm_d, [gains.hs[0]])
            load(gains.t[:, 2, 0:2], kv_norm_d, [gains.hs[0]])
            cnt = [0]

            def conv(src, dst, dname, nk, ncols, gi, cstart=0):
                srcv = src.rearrange("(k p) c -> p k c", p=128)
                dstv = dst.rearrange("(k p) c -> p k c", p=128)
                for c0 in range(cstart, ncols, 512):
                    cw = min(512, ncols - c0)
                    i = cnt[0] % 2
                    cnt[0] += 1
                    a, b = stg[i], obf[i]
                    load(a.t[:, 0:nk, 0:cw], srcv[:, :, c0:c0 + cw], [a.h])
                    for k in range(nk):
                        if gi is None:
                            if k % 2 == 0:
                                op("act", lambda e, a=a, b=b, k=k, cw=cw: e.copy(out=b.t[:, k, 0:cw], in_=a.t[:, k, 0:cw]),
                                   reads=[a.h], writes=[b.hs[0]] if False else [b.h])
                            else:
                                op("dve", lambda e, a=a, b=b, k=k, cw=cw: e.tensor_copy(out=b.t[:, k, 0:cw], in_=a.t[:, k, 0:cw]),
                                   reads=[a.h], writes=[b.h])
                        else:
                            if k % 2 == 0:
                                op("act", lambda e, a=a, b=b, k=k, cw=cw, gi=gi: e.activation(out=b.t[:, k, 0:cw], in_=a.t[:, k, 0:cw], func=AF.Copy, scale=gains.t[:, gi, k:k + 1]),
                                   reads=[a.h, gains.hs[0]], writes=[b.h])
                            else:
                                op("dve", lambda e, a=a, b=b, k=k, cw=cw, gi=gi: e.tensor_scalar_mul(out=b.t[:, k, 0:cw], in0=a.t[:, k, 0:cw], scalar1=gains.t[:, gi, k:k + 1]),
                                   reads=[a.h, gains.hs[0]], writes=[b.h])
                    store(dstv[:, :, c0:c0 + cw], b.t[:, 0:nk, 0:cw], [b.h], [dh(dname)])

            conv(w_in_d, WIN, "WIN", 8, 2592, 0)
            conv(w_uq_d, WUQ, "WUQ", 6, 768, 1)
            conv(w_ukv_d, WUKV, "WUKV", 2, 1024, 2)
            Sx.barrier()
        if stop_after <= 0:
            return _finish(nc, Sx, out_d, S)

        with ExitStack() as st:
            sb, ps = mk(st)
            onesb = sb("onesb", [128, 128], BF16)
            identb = sb("identb", [128, 128], BF16)
            perma = sb("perma", [96, 96], BF16)
            permb = sb("permb", [128, 128], BF16)
            wneg = sb("wneg", [1, 64], BF16)
            load(onesb.t[:], c_onesb, [onesb.h])
            load(identb.t[:], c_identb, [identb.h])
            load(perma.t[:], c_perma, [perma.h])
            load(permb.t[:], c_permb, [permb.h])
            load(wneg.t[:], c_wneg, [wneg.h])
            store_q[0] = "sp"
            NCW = 2592
            win = sb("win", [128, 8, NCW], BF16)
            wuq = sb("wuq", [128, 6, 768], BF16)
            wukv = sb("wukv", [128, 2, 1024], BF16)
            winv = WIN.rearrange("(k p) c -> p k c", p=128)
            for c0 in range(0, NCW, 648):
                load(win.t[:, :, c0:c0 + 648], winv[:, :, c0:c0 + 648], [win.h], [dh("WIN")])
            load(wuq.t[:], WUQ.rearrange("(k p) c -> p k c", p=128), [wuq.h], [dh("WUQ")])
            load(wukv.t[:], WUKV.rearrange("(k p) c -> p k c", p=128), [wukv.h], [dh("WUKV")])
            km = sb("km", [128, 4, 32], BF16)
            op("dve", lambda e: e.memset(km.t[:], 0.0), writes=[km.h])

            xt = [sb(f"xt{i}", [128, 8, 512], F32) for i in range(2)]
            xsq = sb("xsq", [128, 8, 512], BF16)
            hTs = [sb(f"hT{i}", [128, 8, 512], BF16, 8) for i in range(2)]
            rss = [sb(f"rs{i}", [128, 512], F32) for i in range(2)]
            tac = sb("tac", [96, 512], F32)
            tas = sb("tas", [96, 512], F32)
            tbc = sb("tbc", [128, 512], F32)
            tbs = sb("tbs", [128, 512], F32)
            tac2 = sb("tac2", [96, 512], F32)
            tas2 = sb("tas2", [96, 512], F32)
            tbc2 = sb("tbc2", [128, 512], F32)
            tbs2 = sb("tbs2", [128, 512], F32)
            cq = sb("cq", [128, 6, 512], BF16, 6)
            cqsq = sb("cqsq", [128, 6, 512], BF16, 6)
            cqn = sb("cqn", [128, 6, 512], BF16, 6)
            ckv = sb("ckv", [128, 2, 512], BF16, 2)
            ckvsq = sb("ckvsq", [128, 2, 512], BF16, 2)
            ckvn = sb("ckvn", [128, 2, 512], BF16, 2)
            rsq = sb("rsq", [128, 512], F32)
            rskv = sb("rskv", [128, 512], F32)
            NR = 3
            rsb = [sb(f"rsb{i}", [128, 512], BF16) for i in range(NR)]
            t1 = [sb(f"t1_{i}", [128, 512], F32) for i in range(NR)]
            t2 = [sb(f"t2_{i}", [128, 512], F32) for i in range(NR)]
            ro = [sb(f"ro{i}", [128, 512], BF16) for i in range(NR)]
            qbo = [sb(f"qbo{i}", [128, 512], BF16) for i in range(4)]
            evo = [sb(f"evo{i}", [128, 512], BF16) for i in range(6)]
            kms = sb("kms", [128, 2], F32)
            gsbs = [sb(f"gsb{i}", [128, 256], F32) for i in range(4)]
            t8s = [sb(f"t8_{i}", [128, 8, 8], F32, 8) for i in range(4)]
            thrs = [sb(f"thr{i}", [128, 8], F32) for i in range(4)]
            mkfs = [sb(f"mk_f{i}", [128, 256], F32, 8) for i in range(4)]
            mkbs = [sb(f"mkb{i}", [128, 256], BF16) for i in range(4)]
            mT = sb("mT", [128, 2, 512], BF16, 2)
            pm = [ps(f"pm{i}", [128, 512], F32) for i in range(3)]
            pp = [ps(f"pp{i}", [128, 512], F32) for i in range(1)]
            pstat = ps("pstat", [128, 512], F32)
            pgs = [ps(f"pg{i}", [128, 512], F32) for i in range(2)]
            ptr = ps("ptr", [128, 1024], BF16)
            ctr = {"pm": 0, "pp": 0, "r": 0, "ev": 0}

            def nxt(key, n):
                v = ctr[key] % n
                ctr[key] += 1
                return v

            def rope(src_ps, rows, perm, tc_, ts_, out_t=None, after=None):
                i = nxt("r", NR)
                a, b1, b2, o = rsb[i], t1[i], t2[i], (out_t or ro[i])
                op("act", lambda e: e.copy(out=a.t[0:rows, :], in_=src_ps.t[0:rows, :]), reads=[src_ps.h], writes=[a.h])

                def fin():
                    p2 = pp[nxt("pp", 1)]
                    mm(p2.t[0:rows, :], perm.t[0:rows, 0:rows], a.t[0:rows, :], True, True, [perm.h, a.h], [p2.h])
                    op("dve", lambda e: e.tensor_tensor(out=b1.t[0:rows, :], in0=p2.t[0:rows, :], in1=ts_.t[0:rows, :], op=ALU.mult),
                       reads=[p2.h, ts_.h], writes=[b1.h])
                    op("pool", lambda e: e.tensor_tensor(out=b2.t[0:rows, :], in0=a.t[0:rows, :], in1=tc_.t[0:rows, :], op=ALU.mult),
                       reads=[a.h, tc_.h], writes=[b2.h])
                    op("dve", lambda e: e.tensor_tensor(out=o.t[0:rows, :], in0=b1.t[0:rows, :], in1=b2.t[0:rows, :], op=ALU.add),
                       reads=[b1.h, b2.h], writes=[o.h])
                    if after is not None:
                        after(o)
                return fin

            def rstd_from(pst, dst, n):
                op("act", lambda e: e.activation(out=dst.t[:], in_=pst.t[:], func=AF.Ln, scale=1.0 / n, bias=EPS),
                   reads=[pst.h], writes=[dst.h])
                op("act", lambda e: e.activation(out=dst.t[:], in_=dst.t[:], func=AF.Exp, scale=-0.5), reads=[dst.h], writes=[dst.h])

            xTv = xT_d.rearrange("(k p) s -> p k s", p=128)
            HTv = HT.rearrange("(k p) s -> p k s", p=128)
            dq = []

            def defer(fn):
                dq.append(fn)
                while len(dq) > 1:
                    dq.pop(0)()

            def flush():
                while dq:
                    dq.pop(0)()

            gate_pend = []
            tabs = [(tac, tas, tbc, tbs), (tac2, tas2, tbc2, tbs2)]

            sq_done = {}

            def S1a(g):
                x_ = xt[g % 2]
                sq_done[g] = True
                op("act", lambda e: e.activation(out=xsq.t[:], in_=x_.t[:], func=AF.Square), reads=[x_.h], writes=[xsq.h])

            def S1(g):
                tok = slice(g * 512, (g + 1) * 512)
                x_, hT, rs = xt[g % 2], hTs[g % 2], rss[g % 2]
                ta_c, ta_s, tb_c, tb_s = tabs[g % 2]
                if not sq_done.get(g):
                    S1a(g)
                for k in range(8):
                    mm(pstat.t[:], onesb.t[:], xsq.t[:, k, :], k == 0, k == 7, [onesb.h, xsq.h], [pstat.h])
                rstd_from(pstat, rs, 1024.0)
                for k in range(8):
                    op("dve", lambda e, k=k: e.tensor_tensor(out=hT.t[:, k, :], in0=x_.t[:, k, :], in1=rs.t[:], op=ALU.mult),
                       reads=[x_.h, rs.h], writes=[hT.hs[k]])
                store(HTv[:, :, tok], hT.t[:], list(hT.hs), [dh("HT")])

            def S1_load(g):
                tok = slice(g * 512, (g + 1) * 512)
                x_ = xt[g % 2]
                ta_c, ta_s, tb_c, tb_s = tabs[g % 2]
                for dst, src in ((x_, xTv[:, :, tok]), (ta_c, c_tac[:, tok]), (ta_s, c_tas[:, tok]), (tb_c, c_tbc[:, tok]), (tb_s, c_tbs[:, tok])):
                    op("act", lambda e, dst=dst, src=src: e.dma_start(out=dst.t[:], in_=src), writes=[dst.h], dma=True)

            S1_load(0)
            S1(0)
            for g in range(NG):
                tok = slice(g * 512, (g + 1) * 512)
                if g + 1 < NG:
                    S1_load(g + 1)
                hT = hTs[g % 2]
                tac, tas, tbc, tbs = tabs[g % 2]
                for (dst, dsq, nchunk, col0) in ((cq, cqsq, 6, 0), (ckv, ckvsq, 2, 768)):
                    for c in range(nchunk):
                        p = pm[nxt("pm", 3)]
                        for k in range(8):
                            mm(p.t[:], win.t[:, k, col0 + c * 128: col0 + (c + 1) * 128], hT.t[:, k, :], k == 0, k == 7, [win.h, hT.hs[k]], [p.h])
                        op("act", lambda e, p=p, dst=dst, c=c: e.copy(out=dst.t[:, c, :], in_=p.t[:]), reads=[p.h], writes=[dst.hs[c]])
                        op("dve", lambda e, dst=dst, dsq=dsq, c=c: e.tensor_tensor(out=dsq.t[:, c, :], in0=dst.t[:, c, :], in1=dst.t[:, c, :], op=ALU.mult),
                           reads=[dst.hs[c]], writes=[dsq.hs[c]])

                def after_q(j):
                    def f(o):
                        store(QB[j * 128:(j + 1) * 128, tok], o.t[:], [o.h], [dh("QB")])
                    return f

                def after_k(j, g=g):
                    def f(o):
                        store(KB[j * 128:(j + 1) * 128, tok], o.t[:], [o.h], [dh("KB")])
                        op("dve", lambda e: e.tensor_reduce(out=kms.t[:], in_=o.t[:].rearrange("p (b t) -> p b t", t=256), axis=AX.X, op=ALU.add),
                           reads=[o.h], writes=[kms.h])
                        op("act", lambda e: e.activation(out=km.t[:, j, 2 * g:2 * g + 2], in_=kms.t[:], func=AF.Copy, scale=1.0 / 256),
                           reads=[kms.h], writes=[km.h])
                    return f

                for which, col0 in (("q", 1056), ("k", 1568)):
                    for j in range(4):
                        p = pm[nxt("pm", 3)]
                        for k in range(8):
                            mm(p.t[:], win.t[:, k, col0 + j * 128: col0 + (j + 1) * 128], hT.t[:, k, :], k == 0, k == 7, [win.h, hT.hs[k]], [p.h])
                        if which == "q":
                            defer(rope(p, 128, permb, tbc, tbs, out_t=qbo[j], after=after_q(j)))
                        else:
                            defer(rope(p, 128, permb, tbc, tbs, after=after_k(j)))
                while gate_pend:
                    gate_pend.pop(0)()
                if g + 1 < NG:
                    S1a(g + 1)
                for (dst, dsq, dn, nchunk, rr, nn) in ((cq, cqsq, cqn, 6, rsq, 768.0), (ckv, ckvsq, ckvn, 2, rskv, 256.0)):
                    for c in range(nchunk):
                        mm(pstat.t[:], onesb.t[:], dsq.t[:, c, :], c == 0, c == nchunk - 1, [onesb.h, dsq.hs[c]], [pstat.h])
                    rstd_from(pstat, rr, nn)
                    for c in range(nchunk):
                        op("dve", lambda e, dst=dst, dn=dn, c=c, rr=rr: e.tensor_tensor(out=dn.t[:, c, :], in0=dst.t[:, c, :], in1=rr.t[:], op=ALU.mult),
                           reads=[dst.hs[c], rr.h], writes=[dn.hs[c]])
                p = pm[nxt("pm", 3)]
                for k in range(8):
                    mm(p.t[0:96, :], win.t[:, k, 960:1056], hT.t[:, k, :], k == 0, k == 7, [win.h, hT.hs[k]], [p.h])
                defer(rope(p, 96, perma, tac, tas, after=lambda o: store(KRA[:, tok], o.t[64:96, :], [o.h], [dh("KRA")])))
                for tt in range(4):
                    p = pm[nxt("pm", 3)]
                    for k in range(8):
                        mm(p.t[:], hT.t[:, k, tt * 128:(tt + 1) * 128], win.t[:, k, 2080:2592], k == 0, k == 7, [win.h, hT.hs[k]], [p.h])
                    o = evo[nxt("ev", 6)]
                    op("act", lambda e, p=p, o=o: e.copy(out=o.t[:], in_=p.t[:]), reads=[p.h], writes=[o.h])
                    store(VB[g * 512 + tt * 128: g * 512 + (tt + 1) * 128, :], o.t[:], [o.h], [dh("VB")])
                for h in range(8):
                    p = pm[nxt("pm", 3)]
                    for c in range(6):
                        mm(p.t[0:96, :], wuq.t[:, c, h * 96:(h + 1) * 96], cqn.t[:, c, :], c == 0, c == 5, [wuq.h, cqn.hs[c]], [p.h])
                    defer(rope(p, 96, perma, tac, tas, after=(lambda h: (lambda o: store(QA[h * 96:(h + 1) * 96, tok], o.t[0:96, :], [o.h], [dh("QA")])))(h)))
                for j in range(4):
                    p = pm[nxt("pm", 3)]
                    for c in range(2):
                        mm(p.t[:], wukv.t[:, c, j * 128:(j + 1) * 128], ckvn.t[:, c, :], c == 0, c == 1, [wukv.h, ckvn.hs[c]], [p.h])
                    o = evo[nxt("ev", 6)]
                    op("act", lambda e, p=p, o=o: e.copy(out=o.t[:], in_=p.t[:]), reads=[p.h], writes=[o.h])
                    store(KNA[j * 128:(j + 1) * 128, tok], o.t[:], [o.h], [dh("KNA")])
                for tt in range(4):
                    p = pm[nxt("pm", 3)]
                    for c in range(2):
                        mm(p.t[:], ckvn.t[:, c, tt * 128:(tt + 1) * 128], wukv.t[:, c, 512:1024], c == 0, c == 1, [wukv.h, ckvn.hs[c]], [p.h])
                    o = evo[nxt("ev", 6)]
                    op("act", lambda e, p=p, o=o: e.copy(out=o.t[:], in_=p.t[:]), reads=[p.h], writes=[o.h])
                    store(VA[g * 512 + tt * 128: g * 512 + (tt + 1) * 128, :], o.t[:], [o.h], [dh("VA")])
                if g + 1 < NG:
                    S1(g + 1)
                flush()
                for tt in range(4):
                    qt = g * 4 + tt
                    own = qt // 2
                    pg_ = pgs[tt % 2]
                    gsb_ = gsbs[tt]
                    for h in range(8):
                        j, r0 = h // 2, (h % 2) * 64
                        mm(pg_.t[:, h * 32:(h + 1) * 32], qbo[j].t[r0:r0 + 64, tt * 128:(tt + 1) * 128], km.t[r0:r0 + 64, j, :], True, False,
                           [qbo[j].h, km.h], [pg_.h])
                        mm(pg_.t[:, h * 32:(h + 1) * 32], onesb.t[0:1, :], wneg.t[0:1, 32 - own:64 - own], False, True,
                           [onesb.h, wneg.h], [pg_.h])
                    op("act", lambda e, pg_=pg_, gsb_=gsb_: e.copy(out=gsb_.t[:], in_=pg_.t[:, 0:256]), reads=[pg_.h], writes=[gsb_.h])
                for h in range(8):
                    for tt in range(4):
                        gsb_, t8_ = gsbs[tt], t8s[tt]
                        op("dve", lambda e, h=h, gsb_=gsb_, t8_=t8_: e.max(out=t8_.t[:, h, :], in_=gsb_.t[:, h * 32:(h + 1) * 32]), reads=[gsb_.h], writes=[t8_.hs[h]])
                for tt in range(4):
                    t8_, thr_ = t8s[tt], thrs[tt]
                    op("dve", lambda e, t8_=t8_, thr_=thr_: e.tensor_scalar_max(out=thr_.t[:], in0=t8_.t[:, :, 3], scalar1=-1e29), reads=list(t8_.hs), writes=[thr_.h])
                for h in range(8):
                    for tt in range(4):
                        gsb_, thr_, mkf_ = gsbs[tt], thrs[tt], mkfs[tt]
                        op("dve", lambda e, h=h, gsb_=gsb_, thr_=thr_, mkf_=mkf_: e.tensor_scalar(out=mkf_.t[:, h * 32:(h + 1) * 32], in0=gsb_.t[:, h * 32:(h + 1) * 32],
                                                                 scalar1=thr_.t[:, h:h + 1], scalar2=30000.0, op0=ALU.is_ge, op1=ALU.mult),
                           reads=[gsb_.h, thr_.h], writes=[mkf_.hs[h]])
                for tt in range(4):
                    mkf_, mkb_ = mkfs[tt], mkbs[tt]
                    op("dve", lambda e, mkf_=mkf_, mkb_=mkb_: e.tensor_scalar_add(out=mkb_.t[:], in0=mkf_.t[:], scalar1=-30000.0), reads=list(mkf_.hs), writes=[mkb_.h])

                def gate_fin(g=g, tok=tok):
                    for tt in range(4):
                        mkb_ = mkbs[tt]
                        for half in range(2):
                            op("pe", lambda e, half=half, mkb_=mkb_: e.transpose(out=ptr.t[:, half * 128:(half + 1) * 128], in_=mkb_.t[:, half * 128:(half + 1) * 128], identity=identb.t[:]),
                               reads=[mkb_.h, identb.h], writes=[ptr.h])
                        op("act", lambda e, tt=tt: e.copy(out=mT.t[:, :, tt * 128:(tt + 1) * 128], in_=ptr.t[:, 0:256].rearrange("p (a b) -> p a b", a=2)),
                           reads=[ptr.h], writes=[mT.h])
                    for half in range(2):
                        store(MB[half * 128:(half + 1) * 128, tok], mT.t[:, half, :], [mT.h], [dh("MB")])
                gate_pend.append(gate_fin)
            while gate_pend:
                gate_pend.pop(0)()
            store_q[0] = "pool"
            Sx.barrier()
        if stop_after <= 1:
            return _finish(nc, Sx, out_d, S)

        with ExitStack() as st:
            sb, ps = mk(st)
            maskc = sb("maskc", [128, 2, 1024], BF16)
            load(maskc.t[:], c_mask.rearrange("p (a b) c -> p a (b c)", a=2), [maskc.h])
            QT = [sb(f"QT{i}", [96, S], BF16) for i in range(2)]
            KT = [sb(f"KT{i}", [96, S], BF16) for i in range(2)]
            VV = [sb(f"VV{i}", [128, NT, 128], BF16) for i in range(2)]
            for v in VV:
                op("dve", lambda e, v=v: e.memset(v.t[:, :, 64:128], 1.0), writes=[v.h])
            NP = 4
            PT = [sb(f"PT{i}", [128, 1024], BF16) for i in range(NP)]
            rcp = [sb(f"rcp{i}", [128, 512], F32) for i in range(2)]
            rc0 = [sb(f"rc0{i}", [64, 512], F32) for i in range(2)]
            onb = [sb(f"onb{i}", [64, 512], BF16) for i in range(2)]
            psc = [ps(f"psc{i}", [128, 1024], F32) for i in range(3)]
            pov = [ps(f"pov{i}", [128, 512], F32) for i in range(2)]
            VAv = VA.rearrange("(n p) f -> p n f", p=128)
            VBv = VB.rearrange("(n p) f -> p n f", p=128)
            stg3 = [sb(f"stg3{i}", [128, 8, 512], F32) for i in range(2)]
            obf3 = [sb(f"obf3{i}", [128, 8, 512], BF16) for i in range(2)]
            gain3 = sb("gain3", [128, 8], F32)
            load(gain3.t[:], attn_norm_d, [gain3.h])
            cnt3 = [0]
            c3q = []

            def conv3(src, dst, dname, nk, ncols, use_gain, cstart=0):
                srcv = src.rearrange("(k p) c -> p k c", p=128)
                dstv = dst.rearrange("(k p) c -> p k c", p=128)
                for c0 in range(cstart, ncols, 512):
                    cw = min(512, ncols - c0)
                    i = cnt3[0] % 2
                    cnt3[0] += 1
                    a_, b_ = stg3[i], obf3[i]
                    c3q.append(lambda a_=a_, c0=c0, cw=cw: load(a_.t[:, 0:nk, 0:cw], srcv[:, :, c0:c0 + cw], [a_.h]))
                    for k in range(nk):
                        if use_gain:
                            c3q.append(lambda a_=a_, b_=b_, k=k, cw=cw: op("pool", lambda e: e.tensor_scalar_mul(out=b_.t[:, k, 0:cw], in0=a_.t[:, k, 0:cw], scalar1=gain3.t[:, k:k + 1]),
                                                                      reads=[a_.h, gain3.h], writes=[b_.h]))
                        else:
                            c3q.append(lambda a_=a_, b_=b_, k=k, cw=cw: op("pool", lambda e: e.tensor_copy(out=b_.t[:, k, 0:cw], in_=a_.t[:, k, 0:cw]),
                                                                      reads=[a_.h], writes=[b_.h]))
                    c3q.append(lambda b_=b_, c0=c0, cw=cw: store(dstv[:, :, c0:c0 + cw], b_.t[:, 0:nk, 0:cw], [b_.h], [dh(dname)]))

            LA = 2
            pend = []
            state = {"ui": 0, "gi": 0}

            def emit_pair(q_, k_, v_, sc, g, kp, nkp, odst, on, h):
                ui = state["ui"]
                state["ui"] += 1
                pscore = psc[ui % 3]
                pt = PT[ui % NP]
                r = 2 * kp - 4 * g
                c0s = [((r + j) * 128 if r >= 0 else 0) for j in range(2)]
                for j in range(2):
                    kt = 2 * kp + j
                    c0 = c0s[j]
                    mm(pscore.t[:, j * 512 + c0:(j + 1) * 512], k_.t[0:96, kt * 128:(kt + 1) * 128], q_.t[0:96, g * 512 + c0:(g + 1) * 512], True, True,
                       [k_.h, q_.h], [pscore.h])
                if r == 2:
                    for j in range(2):
                        c0 = c0s[j]
                        op("act", lambda e, j=j, c0=c0: e.activation(out=pt.t[:, j * 512 + c0:(j + 1) * 512], in_=pscore.t[:, j * 512 + c0:(j + 1) * 512], func=AF.Exp, scale=sc),
                           reads=[pscore.h], writes=[pt.h])
                else:
                    op("act", lambda e: e.activation(out=pt.t[:], in_=pscore.t[:], func=AF.Exp, scale=sc), reads=[pscore.h], writes=[pt.h])
                if r >= 0:
                    for j in range(2):
                        c0 = c0s[j]
                        op("dve", lambda e, j=j, c0=c0: e.tensor_tensor(out=pt.t[:, j * 512 + c0:j * 512 + c0 + 128], in0=pt.t[:, j * 512 + c0:j * 512 + c0 + 128],
                                                                      in1=maskc.t[:, r // 2, j * 512 + c0:j * 512 + c0 + 128], op=ALU.mult),
                           reads=[pt.h, maskc.h], writes=[pt.h])
                first, last = (kp == 0), (kp == nkp - 1)
                if first:
                    state["gi"] += 1
                gi = state["gi"]
                po = pov[gi % 2]

                def pv():
                    for j in range(2):
                        kt = 2 * kp + j
                        c0 = c0s[j]
                        mm(po.t[:, c0:512], v_.t[:, kt, :], pt.t[:, j * 512 + c0:(j + 1) * 512], first and j == 0, last and j == 1, [v_.h, pt.h], [po.h])
                    if last:
                        rc, r0, onb_ = rcp[gi % 2], rc0[gi % 2], onb[gi % 2]
                        op("dve", lambda e: e.reciprocal(out=rc.t[64:128, :], in_=po.t[64:128, :]), reads=[po.h], writes=[rc.h])
                        op("dve", lambda e: e.tensor_copy(out=r0.t[0:64, :], in_=rc.t[64:128, :]), reads=[rc.h], writes=[r0.h])
                        op("dve", lambda e: e.tensor_tensor(out=onb_.t[:], in0=po.t[0:64, :], in1=r0.t[0:64, :], op=ALU.mult),
                           reads=[po.h, r0.h], writes=[onb_.h])
                        store(odst[h * 64:(h + 1) * 64, g * 512:(g + 1) * 512], onb_.t[:], [onb_.h], [dh(on)])
                return pv

            for hp in range(16):
                typ, h = hp // 8, hp % 8
                q_, k_, v_ = QT[hp % 2], KT[hp % 2], VV[hp % 2]
                if typ == 0:
                    load(q_.t[0:96, :], QA[h * 96:(h + 1) * 96, :], [q_.h], [dh("QA")])
                    load(k_.t[0:64, :], KNA[h * 64:(h + 1) * 64, :], [k_.h], [dh("KNA")])
                    load(k_.t[64:96, :], KRA[:, :], [k_.h], [dh("KRA")])
                    vsrc, vn, sc, odst, on = VAv, "VA", 96.0 ** -0.5, OA, "OA"
                else:
                    load(q_.t[0:64, :], QB[h * 64:(h + 1) * 64, :], [q_.h], [dh("QB")])
                    load(q_.t[64:96, :], MB[h * 32:(h + 1) * 32, :], [q_.h], [dh("MB")])
                    load(k_.t[0:64, :], KB[h * 64:(h + 1) * 64, :], [k_.h], [dh("KB")])
                    load(k_.t[64:96, :], c_oh[:, :], [k_.h])
                    vsrc, vn, sc, odst, on = VBv, "VB", 64.0 ** -0.5, OB, "OB"
                vstep = max(1, NT // 4)
                for n0 in range(0, NT, vstep):
                    load(v_.t[:, n0:n0 + vstep, 0:64], vsrc[:, n0:n0 + vstep, h * 64:(h + 1) * 64], [v_.h], [dh(vn)])
                if hp == 3:
                    zt = sb("zt", [128, 4, D], BF16)
                    op("pool", lambda e: e.memset(zt.t[:], 0.0), writes=[zt.h])
                    XSv = XS.rearrange("(n p) d -> p n d", p=128)
                    zlist = list(range(0, NSLOT // 128, 4))
                if hp >= 3:
                    nz = -(-len(zlist) // 12) if hp < 15 else len(zlist)
                    for _ in range(min(nz, len(zlist))):
                        n0 = zlist.pop(0)
                        store(XSv[:, n0:n0 + 4, :], zt.t[:], [zt.h], [dh("XS")])
                if hp == 2:
                    conv3(w_in_d, WIN, "WIN", 8, 4640, True, cstart=2592)
                    conv3(w_oa_d, WOA, "WOA", 4, D, False)
                    conv3(w_ob_d, WOB, "WOB", 4, D, False)
                    conv3(w_out_d, WOUT, "WOUT", 8, D, False)
                for g in range(NG):
                    nkp = 2 * g + 2
                    if hp >= 2 and c3q and g >= 4:
                        c3q.pop(0)()
                    for kp in range(nkp):
                        pend.append(emit_pair(q_, k_, v_, sc, g, kp, nkp, odst, on, h))
                        if len(pend) > LA:
                            pend.pop(0)()
            while pend:
                pend.pop(0)()
            while c3q:
                c3q.pop(0)()
            Sx.barrier()
        if stop_after <= 3:
            return _finish(nc, Sx, out_d, S)

        with ExitStack() as st:
            sb, ps = mk(st)
            identf = sb("identf", [128, 128], F32)
            onesf = sb("onesf4", [128, 128], F32)
            onesb = sb("onesb4", [128, 128], BF16)
            triu = sb("triu", [128, 128], BF16)
            offs = sb("offs", [128, 32], F32)
            load(identf.t[:], c_identf, [identf.h])
            load(onesf.t[:], c_onesf, [onesf.h])
            load(onesb.t[:], c_onesb, [onesb.h])
            load(triu.t[:], c_triu, [triu.h])
            load(offs.t[:], c_offs, [offs.h])
            wg = sb("wgate", [128, 8, 2048], BF16, 4)
            woa = sb("woa", [128, 4, D], BF16)
            wob = sb("wob", [128, 4, D], BF16)
            wout = sb("wout", [128, 8, D], BF16)
            winv = WIN.rearrange("(k p) c -> p k c", p=128)
            load(woa.t[:], WOA.rearrange("(k p) c -> p k c", p=128), [woa.h], [dh("WOA")])
            for c0 in (0, 1024):
                load(wg.t[:, :, c0:c0 + 512], winv[:, :, 2592 + c0:2592 + c0 + 512], [wg.hs[c0 // 512]], [dh("WIN")])
            load(wob.t[:], WOB.rearrange("(k p) c -> p k c", p=128), [wob.h], [dh("WOB")])
            for c0 in (512, 1536):
                load(wg.t[:, :, c0:c0 + 512], winv[:, :, 2592 + c0:2592 + c0 + 512], [wg.hs[c0 // 512]], [dh("WIN")])
            for c0 in range(0, D, 512):
                load(wout.t[:, :, c0:c0 + 512], WOUT.rearrange("(k p) c -> p k c", p=128)[:, :, c0:c0 + 512], [wout.h], [dh("WOUT")])
            gf = sb("gf", [128, 8], F32)
            wr_s = sb("wr_s", [128, 8, 36], F32)
            wr = sb("wr", [128, 8, 36], F32)
            br = sb("br", [1, 36], F32)
            load(gf.t[:], ffn_norm_d, [gf.h])
            load(wr_s.t[:], w_r_d.rearrange("(k p) c -> p k c", p=128), [wr_s.h])
            load(br.t[:], b_r_d, [br.h])
            for k in range(8):
                op("dve", lambda e, k=k: e.tensor_scalar_mul(out=wr.t[:, k, :], in0=wr_s.t[:, k, :], scalar1=gf.t[:, k:k + 1]),
                   reads=[wr_s.h, gf.h], writes=[wr.h])
            run = sb("run", [128, 32], F32)
            op("dve", lambda e: e.memset(run.t[:], 0.0), writes=[run.h])

            hTg2 = [sb(f"hTg{i}", [128, 8, 512], BF16) for i in range(2)]
            oag2 = [sb(f"oag{i}", [128, 4, 512], BF16) for i in range(2)]
            obg2 = [sb(f"obg{i}", [128, 4, 512], BF16) for i in range(2)]
            sig = [sb(f"sig{i}", [128, 512], F32) for i in range(2)]
            m1 = [sb(f"m1_{i}", [128, 512], F32) for i in range(2)]
            mixT = sb("mixT", [128, 8, 512], BF16, 8)
            xtok = [sb(f"xtok{i}", [128, D], F32) for i in range(2)]
            x1 = [sb(f"x1_{i}", [128, D], F32) for i in range(3)]
            junk = sb("junk", [128, D], F32)
            pA = [ps(f"pA{i}", [128, 512], F32) for i in range(2)]
            pG = [ps(f"pG{i}", [128, 512], F32) for i in range(2)]
            pY = [ps(f"pY{i}", [128, 512], F32) for i in range(2)]
            pTr = ps("pTr", [128, 512], F32)
            pL = ps("pL", [128, 512], F32)
            hpC = H()
            HTv = HT.rearrange("(k p) s -> p k s", p=128)
            OAv = OA.rearrange("(k p) s -> p k s", p=128)
            OBv = OB.rearrange("(k p) s -> p k s", p=128)
            RS = []
            for i in range(4):
                RS.append(dict(
                    L=sb(f"L{i}", [128, 36], F32), gmax=sb(f"gmax{i}", [128, 1], F32), ngmax=sb(f"ngmax{i}", [128, 1], F32),
                    sume=sb(f"sume{i}", [128, 1], F32), pgrp=sb(f"pgrp{i}", [128, 1], F32), mx1=sb(f"mx1{i}", [128, 1], F32),
                    mx2=sb(f"mx2{i}", [128, 1], F32), dd=sb(f"dd{i}", [128, 1], F32), sg1=sb(f"sg1{i}", [128, 1], F32),
                    gone=sb(f"gone{i}", [128, 4], F32),
                    ein=sb(f"ein{i}", [128, 8], F32), ein2=sb(f"ein2{i}", [128, 8], F32), one1=sb(f"one1{i}", [128, 8], F32),
                    one2=sb(f"one2{i}", [128, 8], F32), ex4=sb(f"ex4{i}", [128, 4], F32), E1=sb(f"E1{i}", [128, 32], F32),
                    E2=sb(f"E2{i}", [128, 32], F32), Ab=sb(f"Ab{i}", [128, 32], BF16), pos=sb(f"pos{i}", [128, 32], F32),
                    tmpa=sb(f"tmpa{i}", [128, 32], F32), tmpb=sb(f"tmpb{i}", [128, 32], F32), slf=sb(f"slf{i}", [128, 2], F32),
                    ss1=sb(f"ss1{i}", [128, 2], F32), hnT=sb(f"hnT{i}", [128, 8, 128], F32)))
            hn4 = [sb(f"hn4{i}", [128, D], F32) for i in range(4)]
            hnb8 = [sb(f"hnb8{i}", [128, D], BF16) for i in range(8)]

            bgq = []

            def bg_run(n):
                for _ in range(n):
                    if bgq:
                        bgq.pop(0)()

            def stageA1(ti, tt):
                rows = slice(ti * 128, (ti + 1) * 128)
                xk, x1_ = xtok[ti % 2], x1[ti % 3]
                load(xk.t[:], x_d[rows, :], [xk.h])
                for half in range(2):
                    py = pY[half]
                    for k in range(8):
                        mm(py.t[:], mixT.t[:, k, tt * 128:(tt + 1) * 128], wout.t[:, k, half * 512:(half + 1) * 512], k == 0, k == 7,
                           [mixT.hs[k], wout.h], [py.h])
                    op("dve", lambda e, py=py, half=half: e.tensor_tensor(out=x1_.t[:, half * 512:(half + 1) * 512], in0=xk.t[:, half * 512:(half + 1) * 512], in1=py.t[:], op=ALU.add),
                       reads=[py.h, xk.h], writes=[x1_.h])

            def stageA2(ti, tt):
                rows = slice(ti * 128, (ti + 1) * 128)
                x1_, hn_, hnb_ = x1[ti % 3], hn4[ti % 4], hnb8[ti % 8]
                ss1_ = RS[ti % 4]["ss1"]
                op("act", lambda e: e.dma_start(out=X1[rows, :], in_=x1_.t[:]), reads=[x1_.h], writes=[dh("X1")], dma=True)
                op("act", lambda e: e.activation(out=junk.t[:], in_=x1_.t[:], func=AF.Square, accum_out=ss1_.t[:, 0:1]),
                   reads=[x1_.h], writes=[junk.h, ss1_.h])
                op("act", lambda e: e.activation(out=ss1_.t[:, 1:2], in_=ss1_.t[:, 0:1], func=AF.Sqrt, scale=1.0 / D, bias=EPS), reads=[ss1_.h], writes=[ss1_.h])
                op("dve", lambda e: e.reciprocal(out=ss1_.t[:, 1:2], in_=ss1_.t[:, 1:2]), reads=[ss1_.h], writes=[ss1_.h])
                op("dve", lambda e: e.tensor_scalar_mul(out=hn_.t[:], in0=x1_.t[:], scalar1=ss1_.t[:, 1:2]), reads=[x1_.h, ss1_.h], writes=[hn_.h])
                op("act", lambda e: e.copy(out=hnb_.t[:], in_=hn_.t[:]), reads=[hn_.h], writes=[hnb_.h])

            def stageB4(g):
                tis = [g * 4 + tt for tt in range(4)]
                for ti in tis:
                    R = RS[ti % 4]
                    hn_, hnT_, L = hn4[ti % 4], R["hnT"], R["L"]
                    for k in range(8):
                        op("pe", lambda e, k=k, hn_=hn_: e.transpose(out=pTr.t[:, (k % 4) * 128:(k % 4 + 1) * 128], in_=hn_.t[:, k * 128:(k + 1) * 128], identity=identf.t[:]),
                           reads=[hn_.h, identf.h], writes=[pTr.h])
                        if k % 4 == 3:
                            kk = k // 4
                            op("act", lambda e, kk=kk, hnT_=hnT_: e.copy(out=hnT_.t[:, kk * 4:(kk + 1) * 4, :], in_=pTr.t[:].rearrange("p (a b) -> p a b", a=4)),
                               reads=[pTr.h], writes=[hnT_.h])
                for ti in tis:
                    R = RS[ti % 4]
                    hnT_, L = R["hnT"], R["L"]
                    c0 = (ti % 4) * 64
                    for k in range(8):
                        mm(pL.t[:, c0:c0 + 36], hnT_.t[:, k, :], wr.t[:, k, :], k == 0, False, [hnT_.h, wr.h], [pL.h])
                    mm(pL.t[:, c0:c0 + 36], onesf.t[0:1, :], br.t[0:1, :], False, True, [onesf.h, br.h], [pL.h])
                for ti in tis:
                    R = RS[ti % 4]
                    c0 = (ti % 4) * 64
                    op("act", lambda e, R=R, c0=c0: e.copy(out=R["L"].t[:], in_=pL.t[:, c0:c0 + 36]), reads=[pL.h], writes=[R["L"].h])

                def each(fn):
                    def step():
                        for ti in tis:
                            fn(ti, RS[ti % 4])
                    bgq.append(step)
                each(lambda ti, R: op("dve", lambda e: e.tensor_reduce(out=R["gmax"].t[:], in_=R["L"].t[:, 0:4], axis=AX.X, op=ALU.max), reads=[R["L"].h], writes=[R["gmax"].h]))
                each(lambda ti, R: op("dve", lambda e: e.tensor_scalar(out=R["gone"].t[:], in0=R["L"].t[:, 0:4], scalar1=R["gmax"].t[:, 0:1], scalar2=None, op0=ALU.is_equal), reads=[R["L"].h, R["gmax"].h], writes=[R["gone"].h]))
                each(lambda ti, R: op("dve", lambda e: e.tensor_scalar_mul(out=R["ngmax"].t[:], in0=R["gmax"].t[:], scalar1=-1.0), reads=[R["gmax"].h], writes=[R["ngmax"].h]))
                each(lambda ti, R: op("dve", lambda e: e.tensor_scalar_mul(out=R["ein"].t[:], in0=R["L"].t[:, 4:12], scalar1=R["gone"].t[:, 0:1]), reads=[R["L"].h, R["gone"].h], writes=[R["ein"].h]))
                each(lambda ti, R: op("act", lambda e: e.activation(out=R["ex4"].t[:], in_=R["L"].t[:, 0:4], func=AF.Exp, bias=R["ngmax"].t[:, 0:1], accum_out=R["sume"].t[:, 0:1]),
                                     reads=[R["L"].h, R["ngmax"].h], writes=[R["ex4"].h, R["sume"].h]))
                for gg in range(1, 4):
                    each(lambda ti, R, gg=gg: op("dve", lambda e: e.scalar_tensor_tensor(out=R["ein"].t[:], in0=R["L"].t[:, 4 + 8 * gg:12 + 8 * gg], scalar=R["gone"].t[:, gg:gg + 1], in1=R["ein"].t[:], op0=ALU.mult, op1=ALU.add),
                                                 reads=[R["L"].h, R["gone"].h, R["ein"].h], writes=[R["ein"].h]))
                each(lambda ti, R: op("dve", lambda e: e.tensor_reduce(out=R["mx1"].t[:], in_=R["ein"].t[:], axis=AX.X, op=ALU.max), reads=[R["ein"].h], writes=[R["mx1"].h]))
                each(lambda ti, R: op("dve", lambda e: e.tensor_scalar(out=R["one1"].t[:], in0=R["ein"].t[:], scalar1=R["mx1"].t[:, 0:1], scalar2=None, op0=ALU.is_equal), reads=[R["ein"].h, R["mx1"].h], writes=[R["one1"].h]))
                each(lambda ti, R: op("dve", lambda e: e.scalar_tensor_tensor(out=R["ein2"].t[:], in0=R["one1"].t[:], scalar=-1e30, in1=R["ein"].t[:], op0=ALU.mult, op1=ALU.add), reads=[R["one1"].h, R["ein"].h], writes=[R["ein2"].h]))
                each(lambda ti, R: op("dve", lambda e: e.tensor_reduce(out=R["mx2"].t[:], in_=R["ein2"].t[:], axis=AX.X, op=ALU.max), reads=[R["ein2"].h], writes=[R["mx2"].h]))
                each(lambda ti, R: op("dve", lambda e: e.tensor_scalar(out=R["one2"].t[:], in0=R["ein2"].t[:], scalar1=R["mx2"].t[:, 0:1], scalar2=None, op0=ALU.is_equal), reads=[R["ein2"].h, R["mx2"].h], writes=[R["one2"].h]))
                each(lambda ti, R: op("dve", lambda e: e.tensor_tensor(out=R["dd"].t[:], in0=R["mx1"].t[:], in1=R["mx2"].t[:], op=ALU.subtract), reads=[R["mx1"].h, R["mx2"].h], writes=[R["dd"].h]))
                each(lambda ti, R: op("dve", lambda e: e.reciprocal(out=R["pgrp"].t[:], in_=R["sume"].t[:]), reads=[R["sume"].h], writes=[R["pgrp"].h]))
                each(lambda ti, R: op("act", lambda e: e.activation(out=R["sg1"].t[:], in_=R["dd"].t[:], func=AF.Sigmoid), reads=[R["dd"].h], writes=[R["sg1"].h]))
                for gg in range(4):
                    each(lambda ti, R, gg=gg: op("dve", lambda e: e.tensor_scalar_mul(out=R["E1"].t[:, gg * 8:(gg + 1) * 8], in0=R["one1"].t[:], scalar1=R["gone"].t[:, gg:gg + 1]), reads=[R["one1"].h, R["gone"].h], writes=[R["E1"].h]))
                    each(lambda ti, R, gg=gg: op("dve", lambda e: e.tensor_scalar_mul(out=R["E2"].t[:, gg * 8:(gg + 1) * 8], in0=R["one2"].t[:], scalar1=R["gone"].t[:, gg:gg + 1]), reads=[R["one2"].h, R["gone"].h], writes=[R["E2"].h]))
                each(lambda ti, R: op("dve", lambda e: e.tensor_tensor(out=R["Ab"].t[:], in0=R["E1"].t[:], in1=R["E2"].t[:], op=ALU.add), reads=[R["E1"].h, R["E2"].h], writes=[R["Ab"].h]))
                each(lambda ti, R: op("dve", lambda e: e.tensor_tensor(out=wts.t[:, ti, 0:1], in0=R["sg1"].t[:], in1=R["pgrp"].t[:], op=ALU.mult), reads=[R["sg1"].h, R["pgrp"].h], writes=[wts.hs[ti % 4]]))
                each(lambda ti, R: op("dve", lambda e: e.tensor_tensor(out=wts.t[:, ti, 1:2], in0=R["pgrp"].t[:], in1=wts.t[:, ti, 0:1], op=ALU.subtract), reads=[R["pgrp"].h, wts.hs[ti % 4]], writes=[wts.hs[ti % 4]]))

            def stageC4(g):
                tis = [g * 4 + tt for tt in range(4)]
                for ti in tis:
                    R = RS[ti % 4]
                    c0 = (ti % 4) * 64
                    mm(pL.t[:, 256 + c0:256 + c0 + 32], triu.t[:], R["Ab"].t[:], True, True, [triu.h, R["Ab"].h], [hpC])
                    mm(pL.t[:, 256 + c0 + 32:256 + c0 + 64], onesb.t[:], R["Ab"].t[:], True, True, [onesb.h, R["Ab"].h], [hpC])
                for ti in tis:
                    R = RS[ti % 4]
                    c0 = (ti % 4) * 64
                    op("dve", lambda e, R=R, c0=c0: e.tensor_tensor(out=R["pos"].t[:], in0=pL.t[:, 256 + c0:256 + c0 + 32], in1=run.t[:], op=ALU.add), reads=[hpC, run.h], writes=[R["pos"].h])
                    op("dve", lambda e, c0=c0: e.tensor_tensor(out=run.t[:], in0=pL.t[:, 256 + c0 + 32:256 + c0 + 64], in1=run.t[:], op=ALU.add), reads=[hpC, run.h], writes=[run.h])

                def each(fn):
                    for ti in tis:
                        fn(ti, RS[ti % 4])
                each(lambda ti, R: op("dve", lambda e: e.tensor_tensor(out=R["pos"].t[:], in0=R["pos"].t[:], in1=offs.t[:], op=ALU.add), reads=[R["pos"].h, offs.h], writes=[R["pos"].h]))
                each(lambda ti, R: op("dve", lambda e: e.tensor_tensor(out=R["tmpa"].t[:], in0=R["pos"].t[:], in1=R["E1"].t[:], op=ALU.mult), reads=[R["pos"].h, R["E1"].h], writes=[R["tmpa"].h]))
                each(lambda ti, R: op("dve", lambda e: e.tensor_tensor(out=R["tmpb"].t[:], in0=R["pos"].t[:], in1=R["E2"].t[:], op=ALU.mult), reads=[R["pos"].h, R["E2"].h], writes=[R["tmpb"].h]))
                each(lambda ti, R: op("dve", lambda e: e.tensor_reduce(out=R["slf"].t[:, 0:1], in_=R["tmpa"].t[:], axis=AX.X, op=ALU.add), reads=[R["tmpa"].h], writes=[R["slf"].h]))
                each(lambda ti, R: op("dve", lambda e: e.tensor_reduce(out=R["slf"].t[:, 1:2], in_=R["tmpb"].t[:], axis=AX.X, op=ALU.add), reads=[R["tmpb"].h, R["slf"].h], writes=[R["slf"].h]))
                each(lambda ti, R: op("dve", lambda e: e.tensor_copy(out=slot_i.t[:, ti, :], in_=R["slf"].t[:]), reads=[R["slf"].h], writes=[slot_i.hs[ti % 4]]))
                for ti in tis:
                    hnb_ = hnb8[ti % 8]
                    for j in range(2):
                        op("pool", lambda e, j=j, ti=ti, hnb_=hnb_: e.indirect_dma_start(
                            out=XS, out_offset=bass.IndirectOffsetOnAxis(ap=slot_i.t[:, ti, j:j + 1], axis=0),
                            in_=hnb_.t[:], in_offset=None), reads=[hnb_.h, slot_i.hs[ti % 4]], writes=[dh("XS")], dma=True)

            ci = 0
            def p4_loads(g):
                tok = slice(g * 512, (g + 1) * 512)
                load(hTg2[g % 2].t[:], HTv[:, :, tok], [hTg2[g % 2].h], [dh("HT")])
                load(oag2[g % 2].t[:], OAv[:, :, tok], [oag2[g % 2].h], [dh("OA")])
                load(obg2[g % 2].t[:], OBv[:, :, tok], [obg2[g % 2].h], [dh("OB")])

            p4_loads(0)
            for g in range(NG):
                tok = slice(g * 512, (g + 1) * 512)
                if g + 1 < NG:
                    p4_loads(g + 1)
                hTg, oag, obg = hTg2[g % 2], oag2[g % 2], obg2[g % 2]
                it = 0
                for c in range(8):
                    for br_i, (og, wo, gc0) in enumerate(((oag, woa, 0), (obg, wob, 1024))):
                        if it == 3 and g >= 1:
                            stageB4(g - 1)
                        it += 1
                        pa, pg_ = pA[ci % 2], pG[ci % 2]
                        sg, mm1 = sig[ci % 2], m1[ci % 2]
                        ci += 1
                        for k in range(4):
                            mm(pa.t[:], wo.t[:, k, c * 128:(c + 1) * 128], og.t[:, k, :], k == 0, k == 3, [wo.h, og.h], [pa.h])
                        for k in range(8):
                            mm(pg_.t[:], wg.t[:, k, gc0 + c * 128: gc0 + (c + 1) * 128], hTg.t[:, k, :], k == 0, k == 7, [wg.hs[(gc0 + c * 128) // 512], hTg.h], [pg_.h])
                        op("act", lambda e, pg_=pg_, sg=sg: e.activation(out=sg.t[:], in_=pg_.t[:], func=AF.Sigmoid), reads=[pg_.h], writes=[sg.h])
                        if br_i == 0:
                            op("dve", lambda e, pa=pa, sg=sg, mm1=mm1: e.tensor_tensor(out=mm1.t[:], in0=sg.t[:], in1=pa.t[:], op=ALU.mult),
                               reads=[sg.h, pa.h], writes=[mm1.h])
                            prev = mm1
                        else:
                            op("dve", lambda e, pa=pa, sg=sg: e.tensor_tensor(out=sg.t[:], in0=sg.t[:], in1=pa.t[:], op=ALU.mult),
                               reads=[sg.h, pa.h], writes=[sg.h])
                            op("dve", lambda e, sg=sg, prev=prev, c=c: e.tensor_tensor(out=mixT.t[:, c, :], in0=sg.t[:], in1=prev.t[:], op=ALU.add),
                               reads=[sg.h, prev.h], writes=[mixT.hs[c]])
                        bg_run(3)
                bg_run(10 ** 6)
                if g >= 1:
                    stageC4(g - 1)
                for tt in range(4):
                    stageA1(g * 4 + tt, tt)
                    if tt >= 1:
                        stageA2(g * 4 + tt - 1, tt - 1)
                stageA2(g * 4 + 3, 3)
            stageB4(NG - 1)
            bg_run(10 ** 6)
            stageC4(NG - 1)
            Sx.barrier()
        if stop_after <= 4:
            return _finish(nc, Sx, out_d, S)

        with ExitStack() as st:
            sb, ps = mk(st)
            identb5x = sb("identb5", [128, 128], BF16)
            load(identb5x.t[:], c_identb, [identb5x.h])
            gf5v = sb("gf5", [128, 8], F32)
            load(gf5v.t[:], ffn_norm_d, [gf5v.h])
            NST = C // 128
            HALF = C // 2
            NSH = HALF // 128
            wgs = [sb(f"wgs{i}", [128, 8, 256], F32) for i in range(2)]
            wus = [sb(f"wus{i}", [128, 8, 256], F32) for i in range(2)]
            wds = [sb(f"wds{i}", [128, 2, D], F32) for i in range(2)]
            wgb = [sb(f"wgb{i}", [128, 8, 256], BF16) for i in range(2)]
            wub = [sb(f"wub{i}", [128, 8, 256], BF16) for i in range(2)]
            wdb = [sb(f"wdb{i}", [128, 2, D], BF16) for i in range(2)]
            xs = [sb(f"xs{i}", [128, D], BF16) for i in range(6)]
            xeT = [sb(f"xeT{i}", [128, 8, C], BF16) for i in range(2)]
            sil = [sb(f"sil{i}", [128, HALF], F32) for i in range(2)]
            actT = [sb(f"actT{i}", [128, 2, HALF], BF16, 2) for i in range(2)]
            ysb = [sb(f"ysb{i}", [128, D], BF16) for i in range(6)]
            ptx = [ps(f"ptx{i}", [128, 1024], BF16) for i in range(2)]
            pgu = [ps(f"pgu{i}", [128, 512], F32) for i in range(4)]
            pyy = [ps(f"pyy{i}", [128, 512], F32) for i in range(2)]
            xi = [0]
            yi = [0]

            def stage_load(ex):
                b = ex % 2
                load(wgs[b].t[:], w_g_d[ex].rearrange("(k p) f -> p k f", p=128), [wgs[b].h])
                load(wus[b].t[:], w_u_d[ex].rearrange("(k p) f -> p k f", p=128), [wus[b].h])
                load(wds[b].t[:], w_d_d[ex].rearrange("(k p) f -> p k f", p=128), [wds[b].h])

            def conv_part(ex, part):
                b = ex % 2
                for k in (2 * part, 2 * part + 1):
                    op("act", lambda e, k=k, b=b: e.activation(out=wgb[b].t[:, k, :], in_=wgs[b].t[:, k, :], func=AF.Copy, scale=gf5v.t[:, k:k + 1]),
                       reads=[wgs[b].h, gf5v.h], writes=[wgb[b].h])
                    op("dve", lambda e, k=k, b=b: e.tensor_scalar_mul(out=wub[b].t[:, k, :], in0=wus[b].t[:, k, :], scalar1=gf5v.t[:, k:k + 1]),
                       reads=[wus[b].h, gf5v.h], writes=[wub[b].h])
                if part == 3:
                    op("dve", lambda e, b=b: e.tensor_copy(out=wdb[b].t[:, 0, :], in_=wds[b].t[:, 0, :]), reads=[wds[b].h], writes=[wdb[b].h])
                    op("dve", lambda e, b=b: e.tensor_copy(out=wdb[b].t[:, 1, :], in_=wds[b].t[:, 1, :]), reads=[wds[b].h], writes=[wdb[b].h])

            def stage_a(ex):
                b = ex % 2
                xe = xeT[b]
                for stl in range(NST):
                    row0 = ex * C + stl * 128
                    xs_ = xs[xi[0] % 6]
                    px = ptx[xi[0] % 2]
                    xi[0] += 1
                    load(xs_.t[:], XS[row0:row0 + 128, :], [xs_.h], [dh("XS")])
                    for k in range(8):
                        op("pe", lambda e, k=k, xs_=xs_, px=px: e.transpose(out=px.t[:, k * 128:(k + 1) * 128], in_=xs_.t[:, k * 128:(k + 1) * 128], identity=identb5x.t[:]),
                           reads=[xs_.h, identb5x.h], writes=[px.h])
                    op("act", lambda e, px=px, xe=xe, stl=stl: e.copy(out=xe.t[:, :, stl * 128:(stl + 1) * 128], in_=px.t[:].rearrange("p (a b) -> p a b", a=8)),
                       reads=[px.h], writes=[xe.h])

            def gu(ex, hf, fc):
                b = ex % 2
                xe = xeT[b]
                cs = slice(hf * HALF, (hf + 1) * HALF)
                at = actT[hf]
                pgt, put = pgu[fc * 2], pgu[fc * 2 + 1]
                for k in range(8):
                    mm(pgt.t[:, 0:HALF], wgb[b].t[:, k, fc * 128:(fc + 1) * 128], xe.t[:, k, cs], k == 0, k == 7, [wgb[b].h, xe.h], [pgt.h])
                for k in range(8):
                    mm(put.t[:, 0:HALF], wub[b].t[:, k, fc * 128:(fc + 1) * 128], xe.t[:, k, cs], k == 0, k == 7, [wub[b].h, xe.h], [put.h])
                sl = sil[fc]
                op("act", lambda e: e.activation(out=sl.t[:], in_=pgt.t[:, 0:HALF], func=AF.Silu), reads=[pgt.h], writes=[sl.h])
                op("dve", lambda e: e.tensor_tensor(out=at.t[:, fc, :], in0=sl.t[:], in1=put.t[:, 0:HALF], op=ALU.mult),
                   reads=[sl.h, put.h], writes=[at.hs[fc]])

            def down(ex, hf):
                b = ex % 2
                at = actT[hf]
                for stl in range(NSH):
                    ys_ = ysb[yi[0] % 6]
                    yi[0] += 1
                    for half in range(2):
                        py = pyy[half]
                        for fc in range(2):
                            mm(py.t[:], at.t[:, fc, stl * 128:(stl + 1) * 128], wdb[b].t[:, fc, half * 512:(half + 1) * 512], fc == 0, fc == 1,
                               [at.hs[fc], wdb[b].h], [py.h])
                        if half == 0:
                            op("act", lambda e, py=py, ys_=ys_: e.copy(out=ys_.t[:, 0:512], in_=py.t[:]), reads=[py.h], writes=[ys_.h])
                        else:
                            op("dve", lambda e, py=py, ys_=ys_: e.tensor_copy(out=ys_.t[:, 512:1024], in_=py.t[:]), reads=[py.h], writes=[ys_.h])
                    row0 = ex * C + hf * HALF + stl * 128
                    store(YS[row0:row0 + 128, :], ys_.t[:], [ys_.h], [dh("YS")])

            stage_load(0)
            stage_load(1)
            for part in range(4):
                conv_part(0, part)
            stage_a(0)
            for ex in range(NE):
                nxt_ex = ex + 1 < NE
                if nxt_ex:
                    stage_a(ex + 1)
                if ex + 2 < NE:
                    stage_load(ex + 2)
                gu(ex, 0, 0)
                if nxt_ex:
                    conv_part(ex + 1, 0)
                gu(ex, 0, 1)
                if nxt_ex:
                    conv_part(ex + 1, 1)
                gu(ex, 1, 0)
                if nxt_ex:
                    conv_part(ex + 1, 2)
                gu(ex, 1, 1)
                if nxt_ex:
                    conv_part(ex + 1, 3)
                down(ex, 0)
                down(ex, 1)
            Sx.barrier()
        if stop_after <= 5:
            return _finish(nc, Sx, out_d, S)

        with ExitStack() as st:
            sb, ps = mk(st)
            onesf6v = sb("onesf6", [128, 128], F32)
            fn_row = sb("fn_row", [1, D], F32)
            GF = sb("GF", [128, D], F32)
            load(onesf6v.t[:], c_onesf, [onesf6v.h])
            load(fn_row.t[:], fnorm_d, [fn_row.h])
            pb = [ps(f"pb{i}", [128, 512], F32) for i in range(2)]
            for half in range(2):
                mm(pb[half].t[:], onesf6v.t[0:1, :], fn_row.t[0:1, half * 512:(half + 1) * 512], True, True, [onesf6v.h, fn_row.h], [pb[half].h])
                op("act", lambda e, half=half: e.copy(out=GF.t[:, half * 512:(half + 1) * 512], in_=pb[half].t[:]), reads=[pb[half].h], writes=[GF.h])
            NB6 = 3
            x1t = [sb(f"x1t{i}", [128, D], F32) for i in range(NB6)]
            y1 = [sb(f"y1_{i}", [128, D], BF16) for i in range(NB6)]
            y2 = [sb(f"y2_{i}", [128, D], BF16) for i in range(NB6)]
            junk6v = sb("junk6", [128, D], F32)
            ssf = [sb(f"ssf{i}", [128, 2], F32) for i in range(2)]
            ot = [sb(f"ot{i}", [128, D], F32) for i in range(2)]
            hout = H()

            def fetch(ti):
                b = ti % NB6
                rows = slice(ti * 128, (ti + 1) * 128)
                load(x1t[b].t[:], X1[rows, :], [x1t[b].h], [dh("X1")])
                for j, yy in enumerate((y1[b], y2[b])):
                    op("pool", lambda e, j=j, yy=yy: e.indirect_dma_start(
                        out=yy.t[:], out_offset=None, in_=YS,
                        in_offset=bass.IndirectOffsetOnAxis(ap=slot_i.t[:, ti, j:j + 1], axis=0)),
                       reads=[dh("YS"), slot_i.h], writes=[yy.h], dma=True)

            def compute(ti):
                b = ti % NB6
                rows = slice(ti * 128, (ti + 1) * 128)
                xx, ya, yb_ = x1t[b], y1[b], y2[b]
                op("dve", lambda e: e.scalar_tensor_tensor(out=xx.t[:], in0=ya.t[:], scalar=wts.t[:, ti, 0:1], in1=xx.t[:], op0=ALU.mult, op1=ALU.add),
                   reads=[ya.h, wts.h, xx.h], writes=[xx.h])
                op("dve", lambda e: e.scalar_tensor_tensor(out=xx.t[:], in0=yb_.t[:], scalar=wts.t[:, ti, 1:2], in1=xx.t[:], op0=ALU.mult, op1=ALU.add),
                   reads=[yb_.h, wts.h, xx.h], writes=[xx.h])
                sf = ssf[ti % 2]
                op("act", lambda e: e.activation(out=junk6v.t[:], in_=xx.t[:], func=AF.Square, accum_out=sf.t[:, 0:1]), reads=[xx.h], writes=[junk6v.h, sf.h])
                op("act", lambda e: e.activation(out=sf.t[:, 1:2], in_=sf.t[:, 0:1], func=AF.Sqrt, scale=1.0 / D, bias=EPS), reads=[sf.h], writes=[sf.h])
                op("dve", lambda e: e.reciprocal(out=sf.t[:, 1:2], in_=sf.t[:, 1:2]), reads=[sf.h], writes=[sf.h])
                o_ = ot[ti % 2]
                op("dve", lambda e: e.scalar_tensor_tensor(out=o_.t[:], in0=xx.t[:], scalar=sf.t[:, 1:2], in1=GF.t[:], op0=ALU.mult, op1=ALU.mult),
                   reads=[xx.h, sf.h, GF.h], writes=[o_.h])
                op("sp", lambda e: e.dma_start(out=out_d[rows, :], in_=o_.t[:]), reads=[o_.h], writes=[hout], dma=True)

            fetch(0)
            if NT > 1:
                fetch(1)
            for ti in range(NT):
                if ti + 2 < NT:
                    fetch(ti + 2)
                compute(ti)
            Sx.barrier()
        return _finish(nc, Sx, out_d, S)


def _finish(nc, Sx, out_d, S):
    Sx.barrier()
    Sx.emit_all()
    return nc


def _consts(S, C):
    bf = ml_dtypes.bfloat16
    c = {}
    c["c_identb"] = np.eye(128, dtype=np.float32).astype(bf)
    c["c_identf"] = np.eye(128, dtype=np.float32)
    c["c_onesb"] = np.ones((128, 128), np.float32).astype(bf)
    c["c_onesf"] = np.ones((128, 128), np.float32)
    pa = np.zeros((96, 96), np.float32)
    for m in range(96):
        if m < 64:
            k = m
        elif m < 80:
            k = m + 16
        else:
            k = m - 16
        pa[k, m] = 1.0
    c["c_perma"] = pa.astype(bf)
    pb = np.zeros((128, 128), np.float32)
    for m in range(128):
        j = m % 64
        if j < 8:
            k = m + 8
        elif j < 16:
            k = m - 8
        else:
            k = m
        pb[k, m] = 1.0
    c["c_permb"] = pb.astype(bf)
    pos = np.arange(S, dtype=np.float64)
    inv = ROPE_THETA ** (-np.arange(16, dtype=np.float64) / 16)
    ang = pos[None, :] * inv[:, None]
    tac = np.ones((96, S), np.float64)
    tas = np.zeros((96, S), np.float64)
    tac[64:80] = np.cos(ang)
    tac[80:96] = np.cos(ang)
    tas[64:80] = -np.sin(ang)
    tas[80:96] = np.sin(ang)
    c["c_tac"] = tac.astype(np.float32)
    c["c_tas"] = tas.astype(np.float32)
    inv = ROPE_THETA ** (-np.arange(8, dtype=np.float64) / 8)
    ang = pos[None, :] * inv[:, None]
    tbc = np.ones((128, S), np.float64)
    tbs = np.zeros((128, S), np.float64)
    for b0 in (0, 64):
        tbc[b0:b0 + 8] = np.cos(ang)
        tbc[b0 + 8:b0 + 16] = np.cos(ang)
        tbs[b0:b0 + 8] = -np.sin(ang)
        tbs[b0 + 8:b0 + 16] = np.sin(ang)
    c["c_tbc"] = tbc.astype(np.float32)
    c["c_tbs"] = tbs.astype(np.float32)
    m = np.zeros((128, 4, 512), np.float32)
    p = np.arange(128)[:, None]
    j = np.arange(512)[None, :]
    for r in range(4):
        m[:, r, :] = (r * 128 + p <= j)
    c["c_mask"] = m.astype(bf)
    oh = np.zeros((32, S), np.float32)
    for n in range(32):
        oh[n, n * 256:(n + 1) * 256] = 1.0
    c["c_oh"] = oh.astype(bf)
    c["c_wneg"] = np.concatenate([np.zeros((1, 32), np.float32), np.full((1, 1), 1e30, np.float32), np.full((1, 31), -1e30, np.float32)], axis=1).astype(bf)
    c["c_offs"] = np.tile((np.arange(32, dtype=np.float32) * C - 1.0)[None, :], (128, 1)).astype(np.float32)
    kk = np.arange(128)[:, None]
    mm_ = np.arange(128)[None, :]
    c["c_triu"] = (kk <= mm_).astype(np.float32).astype(bf)
    return c


def _prep_inputs(inputs, S, C):
    f = lambda a: np.ascontiguousarray(np.asarray(a, dtype=np.float32))
    w_ukv = f(inputs["w_ukv"])[0].reshape(256, 8, 128)
    w_ukv_kv = np.concatenate([w_ukv[:, :, :64].reshape(256, 512), w_ukv[:, :, 64:].reshape(256, 512)], axis=1)
    w_rg = f(inputs["w_router_group"])[0]
    w_re = f(inputs["w_router_expert"])[0]
    w_r = np.concatenate([w_rg] + [w_re[g] for g in range(4)], axis=1)
    b_r = np.concatenate([f(inputs["b_router_group"])[0], f(inputs["b_router_expert"])[0].reshape(32)])[None, :]
    shared = {
        "attn_norm": np.ascontiguousarray(f(inputs["attn_norm"])[0].reshape(8, 128).T),
        "w_in": f(inputs["w_in"])[0],
        "q_norm": np.ascontiguousarray(f(inputs["q_norm"])[0].reshape(6, 128).T),
        "w_uq": f(inputs["w_uq"])[0],
        "kv_norm": np.ascontiguousarray(f(inputs["kv_norm"])[0].reshape(2, 128).T),
        "w_ukv_kv": np.ascontiguousarray(w_ukv_kv),
        "w_o_mla": f(inputs["w_o_mla"])[0],
        "w_o_moba": f(inputs["w_o_moba"])[0],
        "w_out": f(inputs["w_out"])[0],
        "ffn_norm": np.ascontiguousarray(f(inputs["ffn_norm"])[0].reshape(8, 128).T),
        "w_r": np.ascontiguousarray(w_r),
        "b_r": np.ascontiguousarray(b_r),
        "w_exp_gate": f(inputs["w_exp_gate"])[0],
        "w_exp_up": f(inputs["w_exp_up"])[0],
        "w_exp_down": f(inputs["w_exp_down"])[0],
        "final_norm": f(inputs["final_norm"]).reshape(1, D),
    }
    shared.update(_consts(S, C))
    return shared


def kernel(**inputs):
    x = np.asarray(inputs["x"], dtype=np.float32)
    B, S, _ = x.shape
    C = 768 if S >= 8192 else max(256, (S * 2 // 32) * 3 // 128 * 128 + 128)
    nc = build(S=S, C=C)
    shared = _prep_inputs(inputs, S, C)
    in_maps = []
    for b in range(B):
        m = dict(shared)
        m["x"] = np.ascontiguousarray(x[b])
        m["xT"] = np.ascontiguousarray(x[b].T)
        in_maps.append(m)
    res = run_bass_kernel_spmd(nc, in_maps, core_ids=list(range(B)))
    return np.stack([r["out"] for r in res.results], axis=0)
```

```python
import numpy as np
from contextlib import ExitStack
import ml_dtypes
import concourse.bass as bass
import concourse.mybir as mybir
from concourse.bass_utils import run_bass_kernel_spmd

F32 = mybir.dt.float32
BF16 = mybir.dt.bfloat16
I32 = mybir.dt.int32
AF = mybir.ActivationFunctionType
ALU = mybir.AluOpType
AX = mybir.AxisListType

SEM_LIMIT = 30000
D = 1024
NE = 32
EPS = 1e-6
ROPE_THETA = 500000.0


class H:
    __slots__ = ("w", "r")

    def __init__(self):
        self.w = None
        self.r = {}


class Q:
    def __init__(self, name, sems):
        self.name = name
        self.sems = sems
        self.epoch = 0
        self.count = 0
        self.ops = []
        self.waited = {}


class Sched:
    def __init__(self, nc, stack, n_dma_sems=72):
        self.nc = nc
        self.semobj = {}
        self.q = {}
        for name, nep in (("pe", 5), ("act", 3), ("dve", 3), ("pool", 3), ("sp", 1)):
            sems = []
            for e in range(nep):
                s = stack.enter_context(nc.semaphore(f"s_{name}{e}"))
                key = f"{name}{e}"
                self.semobj[key] = s
                sems.append(key)
            self.q[name] = Q(name, sems)
        self.dma_sems = []
        for i in range(n_dma_sems):
            s = stack.enter_context(nc.semaphore(f"s_dma{i}"))
            key = f"dma{i}"
            self.semobj[key] = s
            self.dma_sems.append([key, 0])
        self.dma_rr = 0
        self.dma_rr_q = {}
        self.n_ops = 0

    def _deps(self, reads, writes):
        deps = {}

        def add(k, v):
            if v > deps.get(k, -1):
                deps[k] = v
        for h in reads:
            if h.w is not None:
                add(*h.w)
        for h in writes:
            if h.w is not None:
                add(*h.w)
            for k, v in h.r.items():
                add(k, v)
        return deps

    def op(self, qname, fn, reads=(), writes=(), dma=False):
        q = self.q[qname]
        deps = self._deps(reads, writes)
        if dma:
            third = len(self.dma_sems) // 3
            base = {"sp": 0, "pool": third, "act": 2 * third}[qname]
            rr = self.dma_rr_q.get(qname, 0)
            slot = self.dma_sems[base + rr]
            self.dma_rr_q[qname] = (rr + 1) % third
            if slot[1] > 0 and slot[1] > deps.get(slot[0], -1):
                deps[slot[0]] = slot[1]
            assert slot[1] + 16 < 60000
            slot[1] += 16
            comp = (slot[0], slot[1])
            inc = 16
        else:
            if q.count + 1 > SEM_LIMIT:
                q.epoch += 1
                q.count = 0
            q.count += 1
            comp = (q.sems[q.epoch], q.count)
            inc = 1
        waits = []
        for k, v in deps.items():
            if qname == "pe" and k.startswith("pe"):
                continue
            if q.waited.get(k, -1) >= v:
                continue
            q.waited[k] = v
            waits.append((self.semobj[k], v))
        csem = self.semobj[comp[0]]

        def emit(eng, waits=waits, fn=fn, csem=csem, inc=inc):
            for s, v in waits:
                eng.wait_ge(s, v)
            fn(eng).then_inc(csem, inc)
        q.ops.append(emit)
        for h in writes:
            h.w = comp
            h.r = {}
        for h in reads:
            if comp[1] > h.r.get(comp[0], -1):
                h.r[comp[0]] = comp[1]
        self.n_ops += 1
        return comp

    def barrier(self):
        deps = {}
        for q in self.q.values():
            for e in range(q.epoch + 1):
                cnt = q.count if e == q.epoch else SEM_LIMIT
                if cnt > 0:
                    deps[q.sems[e]] = cnt
        for k, v in self.dma_sems:
            if v > 0:
                deps[k] = v
        for q in self.q.values():
            waits = []
            for k, v in deps.items():
                if q.waited.get(k, -1) >= v:
                    continue
                q.waited[k] = v
                waits.append((self.semobj[k], v))

            def emit(eng, waits=waits):
                for s, v in waits:
                    eng.wait_ge(s, v)
            q.ops.append(emit)

    def emit_all(self):
        nc = self.nc
        with nc.Block() as block:
            @block.tensor
            def _(e):
                for f in self.q["pe"].ops:
                    f(e)

            @block.scalar
            def _(e):
                for f in self.q["act"].ops:
                    f(e)

            @block.vector
            def _(e):
                for f in self.q["dve"].ops:
                    f(e)

            @block.gpsimd
            def _(e):
                for f in self.q["pool"].ops:
                    f(e)

            @block.sync
            def _(e):
                for f in self.q["sp"].ops:
                    f(e)


class T:
    def __init__(self, t, n=1):
        self.t = t
        self.h = H()
        self.hs = [H() for _ in range(n)]


def build(S=8192, C=768, stop_after=99, debug=False):
    NG = S // 512
    NT = S // 128
    NSLOT = NE * C
    nc = bass.Bass("TRN2", target_bir_lowering=False)

    def din(name, shape, dt=F32):
        return nc.dram_tensor(name, shape, dt, kind="ExternalInput").ap()

    def dscr(name, shape, dt):
        return nc.dram_tensor(name, shape, dt, kind=("ExternalOutput" if debug else "Internal")).ap()

    x_d = din("x", [S, D])
    xT_d = din("xT", [D, S])
    attn_norm_d = din("attn_norm", [128, 8])
    w_in_d = din("w_in", [D, 4640])
    q_norm_d = din("q_norm", [128, 6])
    w_uq_d = din("w_uq", [768, 768])
    kv_norm_d = din("kv_norm", [128, 2])
    w_ukv_d = din("w_ukv_kv", [256, 1024])
    w_oa_d = din("w_o_mla", [512, D])
    w_ob_d = din("w_o_moba", [512, D])
    w_out_d = din("w_out", [D, D])
    ffn_norm_d = din("ffn_norm", [128, 8])
    w_r_d = din("w_r", [D, 36])
    b_r_d = din("b_r", [1, 36])
    w_g_d = din("w_exp_gate", [NE, D, 256])
    w_u_d = din("w_exp_up", [NE, D, 256])
    w_d_d = din("w_exp_down", [NE, 256, D])
    fnorm_d = din("final_norm", [1, D])
    c_identb = din("c_identb", [128, 128], BF16)
    c_identf = din("c_identf", [128, 128])
    c_onesb = din("c_onesb", [128, 128], BF16)
    c_onesf = din("c_onesf", [128, 128])
    c_perma = din("c_perma", [96, 96], BF16)
    c_permb = din("c_permb", [128, 128], BF16)
    c_tac = din("c_tac", [96, S])
    c_tas = din("c_tas", [96, S])
    c_tbc = din("c_tbc", [128, S])
    c_tbs = din("c_tbs", [128, S])
    c_mask = din("c_mask", [128, 4, 512], BF16)
    c_oh = din("c_oh", [32, S], BF16)
    c_wneg = din("c_wneg", [1, 64], BF16)
    c_offs = din("c_offs", [128, 32])
    c_triu = din("c_triu", [128, 128], BF16)

    out_d = nc.dram_tensor("out", [S, D], F32, kind="ExternalOutput").ap()

    WIN = dscr("WIN", [D, 4640], BF16)
    WUQ = dscr("WUQ", [768, 768], BF16)
    WUKV = dscr("WUKV", [256, 1024], BF16)
    WOA = dscr("WOA", [512, D], BF16)
    WOB = dscr("WOB", [512, D], BF16)
    WOUT = dscr("WOUT", [D, D], BF16)
    HT = dscr("HT", [D, S], BF16)
    QA = dscr("QA", [768, S], BF16)
    KNA = dscr("KNA", [512, S], BF16)
    KRA = dscr("KRA", [32, S], BF16)
    VA = dscr("VA", [S, 512], BF16)
    QB = dscr("QB", [512, S], BF16)
    KB = dscr("KB", [512, S], BF16)
    VB = dscr("VB", [S, 512], BF16)
    MB = dscr("MB", [256, S], BF16)
    OA = dscr("OA", [512, S], BF16)
    OB = dscr("OB", [512, S], BF16)
    X1 = dscr("X1", [S, D], F32)
    XS = dscr("XS", [NSLOT, D], BF16)
    YS = dscr("YS", [NSLOT, D], BF16)

    with ExitStack() as top:
        Sx = Sched(nc, top)
        op = Sx.op

        def mk(st):
            def sb(name, shape, dt, n=1):
                return T(st.enter_context(nc.sbuf_tensor(name, shape, dt)), n)

            def ps(name, shape, dt):
                return T(st.enter_context(nc.psum_tensor(name, shape, dt)))
            return sb, ps

        def load(dst_ap, src_ap, wr, rd=()):
            op("sp", lambda e: e.dma_start(out=dst_ap, in_=src_ap), reads=rd, writes=wr, dma=True)

        store_q = ["pool"]

        def store(dst_ap, src_ap, rd, wr):
            op(store_q[0], lambda e: e.dma_start(out=dst_ap, in_=src_ap), reads=rd, writes=wr, dma=True)

        def mm(out_ap, lhsT, rhs, start, stop, rd, wr):
            op("pe", lambda e: e.matmul(out_ap, lhsT, rhs, start=start, stop=stop), reads=rd, writes=wr)

        hd = {}

        def dh(name):
            if name not in hd:
                hd[name] = H()
            return hd[name]

        sbp, _ = mk(top)
        slot_i = sbp("slot_i", [128, NT, 2], I32, 4)
        wts = sbp("wts", [128, NT, 2], F32, 4)

        with ExitStack() as st:
            sb, ps = mk(st)
            stg = [sb(f"stg{i}", [128, 8, 512], F32) for i in range(2)]
            obf = [sb(f"obf{i}", [128, 8, 512], BF16) for i in range(2)]
            gains = sb("gains", [128, 3, 8], F32)
            load(gains.t[:, 0, :], attn_norm_d, [gains.hs[0]])
            load(gains.t[:, 1, 0:6], q_norm_d, [gains.hs[0]])
            load(gains.t[:, 2, 0:2], kv_norm_d, [gains.hs[0]])
            cnt = [0]

            def conv(src, dst, dname, nk, ncols, gi, cstart=0):
                srcv = src.rearrange("(k p) c -> p k c", p=128)
                dstv = dst.rearrange("(k p) c -> p k c", p=128)
                for c0 in range(cstart, ncols, 512):
                    cw = min(512, ncols - c0)
                    i = cnt[0] % 2
                    cnt[0] += 1
                    a, b = stg[i], obf[i]
                    load(a.t[:, 0:nk, 0:cw], srcv[:, :, c0:c0 + cw], [a.h])
                    for k in range(nk):
                        if gi is None:
                            if k % 2 == 0:
                                op("act", lambda e, a=a, b=b, k=k, cw=cw: e.copy(out=b.t[:, k, 0:cw], in_=a.t[:, k, 0:cw]),
                                   reads=[a.h], writes=[b.hs[0]] if False else [b.h])
                            else:
                                op("dve", lambda e, a=a, b=b, k=k, cw=cw: e.tensor_copy(out=b.t[:, k, 0:cw], in_=a.t[:, k, 0:cw]),
                                   reads=[a.h], writes=[b.h])
                        else:
                            if k % 2 == 0:
                                op("act", lambda e, a=a, b=b, k=k, cw=cw, gi=gi: e.activation(out=b.t[:, k, 0:cw], in_=a.t[:, k, 0:cw], func=AF.Copy, scale=gains.t[:, gi, k:k + 1]),
                                   reads=[a.h, gains.hs[0]], writes=[b.h])
                            else:
                                op("dve", lambda e, a=a, b=b, k=k, cw=cw, gi=gi: e.tensor_scalar_mul(out=b.t[:, k, 0:cw], in0=a.t[:, k, 0:cw], scalar1=gains.t[:, gi, k:k + 1]),
                                   reads=[a.h, gains.hs[0]], writes=[b.h])
                    store(dstv[:, :, c0:c0 + cw], b.t[:, 0:nk, 0:cw], [b.h], [dh(dname)])

            conv(w_in_d, WIN, "WIN", 8, 2592, 0)
            conv(w_uq_d, WUQ, "WUQ", 6, 768, 1)
            conv(w_ukv_d, WUKV, "WUKV", 2, 1024, 2)
            Sx.barrier()
        if stop_after <= 0:
            return _finish(nc, Sx, out_d, S)

        with ExitStack() as st:
            sb, ps = mk(st)
            onesb = sb("onesb", [128, 128], BF16)
            identb = sb("identb", [128, 128], BF16)
            perma = sb("perma", [96, 96], BF16)
            permb = sb("permb", [128, 128], BF16)
            wneg = sb("wneg", [1, 64], BF16)
            load(onesb.t[:], c_onesb, [onesb.h])
            load(identb.t[:], c_identb, [identb.h])
            load(perma.t[:], c_perma, [perma.h])
            load(permb.t[:], c_permb, [permb.h])
            load(wneg.t[:], c_wneg, [wneg.h])
            store_q[0] = "sp"
            NCW = 2592
            win = sb("win", [128, 8, NCW], BF16)
            wuq = sb("wuq", [128, 6, 768], BF16)
            wukv = sb("wukv", [128, 2, 1024], BF16)
            winv = WIN.rearrange("(k p) c -> p k c", p=128)
            for c0 in range(0, NCW, 648):
                load(win.t[:, :, c0:c0 + 648], winv[:, :, c0:c0 + 648], [win.h], [dh("WIN")])
            load(wuq.t[:], WUQ.rearrange("(k p) c -> p k c", p=128), [wuq.h], [dh("WUQ")])
            load(wukv.t[:], WUKV.rearrange("(k p) c -> p k c", p=128), [wukv.h], [dh("WUKV")])
            km = sb("km", [128, 4, 32], BF16)
            op("dve", lambda e: e.memset(km.t[:], 0.0), writes=[km.h])

            xt = [sb(f"xt{i}", [128, 8, 512], F32) for i in range(2)]
            xsq = sb("xsq", [128, 8, 512], BF16)
            hTs = [sb(f"hT{i}", [128, 8, 512], BF16, 8) for i in range(2)]
            rss = [sb(f"rs{i}", [128, 512], F32) for i in range(2)]
            tac = sb("tac", [96, 512], F32)
            tas = sb("tas", [96, 512], F32)
            tbc = sb("tbc", [128, 512], F32)
            tbs = sb("tbs", [128, 512], F32)
            tac2 = sb("tac2", [96, 512], F32)
            tas2 = sb("tas2", [96, 512], F32)
            tbc2 = sb("tbc2", [128, 512], F32)
            tbs2 = sb("tbs2", [128, 512], F32)
            cq = sb("cq", [128, 6, 512], BF16, 6)
            cqsq = sb("cqsq", [128, 6, 512], BF16, 6)
            cqn = sb("cqn", [128, 6, 512], BF16, 6)
            ckv = sb("ckv", [128, 2, 512], BF16, 2)
            ckvsq = sb("ckvsq", [128, 2, 512], BF16, 2)
            ckvn = sb("ckvn", [128, 2, 512], BF16, 2)
            rsq = sb("rsq", [128, 512], F32)
            rskv = sb("rskv", [128, 512], F32)
            NR = 3
            rsb = [sb(f"rsb{i}", [128, 512], BF16) for i in range(NR)]
            t1 = [sb(f"t1_{i}", [128, 512], F32) for i in range(NR)]
            t2 = [sb(f"t2_{i}", [128, 512], F32) for i in range(NR)]
            ro = [sb(f"ro{i}", [128, 512], BF16) for i in range(NR)]
            qbo = [sb(f"qbo{i}", [128, 512], BF16) for i in range(4)]
            evo = [sb(f"evo{i}", [128, 512], BF16) for i in range(6)]
            kms = sb("kms", [128, 2], F32)
            gsbs = [sb(f"gsb{i}", [128, 256], F32) for i in range(4)]
            t8s = [sb(f"t8_{i}", [128, 8, 8], F32, 8) for i in range(4)]
            thrs = [sb(f"thr{i}", [128, 8], F32) for i in range(4)]
            mkfs = [sb(f"mk_f{i}", [128, 256], F32, 8) for i in range(4)]
            mkbs = [sb(f"mkb{i}", [128, 256], BF16) for i in range(4)]
            mT = sb("mT", [128, 2, 512], BF16, 2)
            pm = [ps(f"pm{i}", [128, 512], F32) for i in range(3)]
            pp = [ps(f"pp{i}", [128, 512], F32) for i in range(1)]
            pstat = ps("pstat", [128, 512], F32)
            pgs = [ps(f"pg{i}", [128, 512], F32) for i in range(2)]
            ptr = ps("ptr", [128, 1024], BF16)
            ctr = {"pm": 0, "pp": 0, "r": 0, "ev": 0}

            def nxt(key, n):
                v = ctr[key] % n
                ctr[key] += 1
                return v

            def rope(src_ps, rows, perm, tc_, ts_, out_t=None, after=None):
                i = nxt("r", NR)
                a, b1, b2, o = rsb[i], t1[i], t2[i], (out_t or ro[i])
                op("act", lambda e: e.copy(out=a.t[0:rows, :], in_=src_ps.t[0:rows, :]), reads=[src_ps.h], writes=[a.h])

                def fin():
                    p2 = pp[nxt("pp", 1)]
                    mm(p2.t[0:rows, :], perm.t[0:rows, 0:rows], a.t[0:rows, :], True, True, [perm.h, a.h], [p2.h])
                    op("dve", lambda e: e.tensor_tensor(out=b1.t[0:rows, :], in0=p2.t[0:rows, :], in1=ts_.t[0:rows, :], op=ALU.mult),
                       reads=[p2.h, ts_.h], writes=[b1.h])
                    op("pool", lambda e: e.tensor_tensor(out=b2.t[0:rows, :], in0=a.t[0:rows, :], in1=tc_.t[0:rows, :], op=ALU.mult),
                       reads=[a.h, tc_.h], writes=[b2.h])
                    op("dve", lambda e: e.tensor_tensor(out=o.t[0:rows, :], in0=b1.t[0:rows, :], in1=b2.t[0:rows, :], op=ALU.add),
                       reads=[b1.h, b2.h], writes=[o.h])
                    if after is not None:
                        after(o)
                return fin

            def rstd_from(pst, dst, n):
                op("act", lambda e: e.activation(out=dst.t[:], in_=pst.t[:], func=AF.Ln, scale=1.0 / n, bias=EPS),
                   reads=[pst.h], writes=[dst.h])
                op("act", lambda e: e.activation(out=dst.t[:], in_=dst.t[:], func=AF.Exp, scale=-0.5), reads=[dst.h], writes=[dst.h])

            xTv = xT_d.rearrange("(k p) s -> p k s", p=128)
            HTv = HT.rearrange("(k p) s -> p k s", p=128)
            dq = []

            def defer(fn):
                dq.append(fn)
                while len(dq) > 1:
                    dq.pop(0)()

            def flush():
                while dq:
                    dq.pop(0)()

            gate_pend = []
            tabs = [(tac, tas, tbc, tbs), (tac2, tas2, tbc2, tbs2)]

            sq_done = {}

            def S1a(g):
                x_ = xt[g % 2]
                sq_done[g] = True
                op("act", lambda e: e.activation(out=xsq.t[:], in_=x_.t[:], func=AF.Square), reads=[x_.h], writes=[xsq.h])

            def S1(g):
                tok = slice(g * 512, (g + 1) * 512)
                x_, hT, rs = xt[g % 2], hTs[g % 2], rss[g % 2]
                ta_c, ta_s, tb_c, tb_s = tabs[g % 2]
                if not sq_done.get(g):
                    S1a(g)
                for k in range(8):
                    mm(pstat.t[:], onesb.t[:], xsq.t[:, k, :], k == 0, k == 7, [onesb.h, xsq.h], [pstat.h])
                rstd_from(pstat, rs, 1024.0)
                for k in range(8):
                    op("dve", lambda e, k=k: e.tensor_tensor(out=hT.t[:, k, :], in0=x_.t[:, k, :], in1=rs.t[:], op=ALU.mult),
                       reads=[x_.h, rs.h], writes=[hT.hs[k]])
                store(HTv[:, :, tok], hT.t[:], list(hT.hs), [dh("HT")])

            def S1_load(g):
                tok = slice(g * 512, (g + 1) * 512)
                x_ = xt[g % 2]
                ta_c, ta_s, tb_c, tb_s = tabs[g % 2]
                for dst, src in ((x_, xTv[:, :, tok]), (ta_c, c_tac[:, tok]), (ta_s, c_tas[:, tok]), (tb_c, c_tbc[:, tok]), (tb_s, c_tbs[:, tok])):
                    op("act", lambda e, dst=dst, src=src: e.dma_start(out=dst.t[:], in_=src), writes=[dst.h], dma=True)

            S1_load(0)
            S1(0)
            for g in range(NG):
                tok = slice(g * 512, (g + 1) * 512)
                if g + 1 < NG:
                    S1_load(g + 1)
                hT = hTs[g % 2]
                tac, tas, tbc, tbs = tabs[g % 2]
                for (dst, dsq, nchunk, col0) in ((cq, cqsq, 6, 0), (ckv, ckvsq, 2, 768)):
                    for c in range(nchunk):
                        p = pm[nxt("pm", 3)]
                        for k in range(8):
                            mm(p.t[:], win.t[:, k, col0 + c * 128: col0 + (c + 1) * 128], hT.t[:, k, :], k == 0, k == 7, [win.h, hT.hs[k]], [p.h])
                        op("act", lambda e, p=p, dst=dst, c=c: e.copy(out=dst.t[:, c, :], in_=p.t[:]), reads=[p.h], writes=[dst.hs[c]])
                        op("dve", lambda e, dst=dst, dsq=dsq, c=c: e.tensor_tensor(out=dsq.t[:, c, :], in0=dst.t[:, c, :], in1=dst.t[:, c, :], op=ALU.mult),
                           reads=[dst.hs[c]], writes=[dsq.hs[c]])

                def after_q(j):
                    def f(o):
                        store(QB[j * 128:(j + 1) * 128, tok], o.t[:], [o.h], [dh("QB")])
                    return f

                def after_k(j, g=g):
                    def f(o):
                        store(KB[j * 128:(j + 1) * 128, tok], o.t[:], [o.h], [dh("KB")])
                        op("dve", lambda e: e.tensor_reduce(out=kms.t[:], in_=o.t[:].rearrange("p (b t) -> p b t", t=256), axis=AX.X, op=ALU.add),
                           reads=[o.h], writes=[kms.h])
                        op("act", lambda e: e.activation(out=km.t[:, j, 2 * g:2 * g + 2], in_=kms.t[:], func=AF.Copy, scale=1.0 / 256),
                           reads=[kms.h], writes=[km.h])
                    return f

                for which, col0 in (("q", 1056), ("k", 1568)):
                    for j in range(4):
                        p = pm[nxt("pm", 3)]
                        for k in range(8):
                            mm(p.t[:], win.t[:, k, col0 + j * 128: col0 + (j + 1) * 128], hT.t[:, k, :], k == 0, k == 7, [win.h, hT.hs[k]], [p.h])
                        if which == "q":
                            defer(rope(p, 128, permb, tbc, tbs, out_t=qbo[j], after=after_q(j)))
                        else:
                            defer(rope(p, 128, permb, tbc, tbs, after=after_k(j)))
                while gate_pend:
                    gate_pend.pop(0)()
                if g + 1 < NG:
                    S1a(g + 1)
                for (dst, dsq, dn, nchunk, rr, nn) in ((cq, cqsq, cqn, 6, rsq, 768.0), (ckv, ckvsq, ckvn, 2, rskv, 256.0)):
                    for c in range(nchunk):
                        mm(pstat.t[:], onesb.t[:], dsq.t[:, c, :], c == 0, c == nchunk - 1, [onesb.h, dsq.hs[c]], [pstat.h])
                    rstd_from(pstat, rr, nn)
                    for c in range(nchunk):
                        op("dve", lambda e, dst=dst, dn=dn, c=c, rr=rr: e.tensor_tensor(out=dn.t[:, c, :], in0=dst.t[:, c, :], in1=rr.t[:], op=ALU.mult),
                           reads=[dst.hs[c], rr.h], writes=[dn.hs[c]])
                p = pm[nxt("pm", 3)]
                for k in range(8):
                    mm(p.t[0:96, :], win.t[:, k, 960:1056], hT.t[:, k, :], k == 0, k == 7, [win.h, hT.hs[k]], [p.h])
                defer(rope(p, 96, perma, tac, tas, after=lambda o: store(KRA[:, tok], o.t[64:96, :], [o.h], [dh("KRA")])))
                for tt in range(4):
                    p = pm[nxt("pm", 3)]
                    for k in range(8):
                        mm(p.t[:], hT.t[:, k, tt * 128:(tt + 1) * 128], win.t[:, k, 2080:2592], k == 0, k == 7, [win.h, hT.hs[k]], [p.h])
                    o = evo[nxt("ev", 6)]
                    op("act", lambda e, p=p, o=o: e.copy(out=o.t[:], in_=p.t[:]), reads=[p.h], writes=[o.h])
                    store(VB[g * 512 + tt * 128: g * 512 + (tt + 1) * 128, :], o.t[:], [o.h], [dh("VB")])
                for h in range(8):
                    p = pm[nxt("pm", 3)]
                    for c in range(6):
                        mm(p.t[0:96, :], wuq.t[:, c, h * 96:(h + 1) * 96], cqn.t[:, c, :], c == 0, c == 5, [wuq.h, cqn.hs[c]], [p.h])
                    defer(rope(p, 96, perma, tac, tas, after=(lambda h: (lambda o: store(QA[h * 96:(h + 1) * 96, tok], o.t[0:96, :], [o.h], [dh("QA")])))(h)))
                for j in range(4):
                    p = pm[nxt("pm", 3)]
                    for c in range(2):
                        mm(p.t[:], wukv.t[:, c, j * 128:(j + 1) * 128], ckvn.t[:, c, :], c == 0, c == 1, [wukv.h, ckvn.hs[c]], [p.h])
                    o = evo[nxt("ev", 6)]
                    op("act", lambda e, p=p, o=o: e.copy(out=o.t[:], in_=p.t[:]), reads=[p.h], writes=[o.h])
                    store(KNA[j * 128:(j + 1) * 128, tok], o.t[:], [o.h], [dh("KNA")])
                for tt in range(4):
                    p = pm[nxt("pm", 3)]
                    for c in range(2):
                        mm(p.t[:], ckvn.t[:, c, tt * 128:(tt + 1) * 128], wukv.t[:, c, 512:1024], c == 0, c == 1, [wukv.h, ckvn.hs[c]], [p.h])
                    o = evo[nxt("ev", 6)]
                    op("act", lambda e, p=p, o=o: e.copy(out=o.t[:], in_=p.t[:]), reads=[p.h], writes=[o.h])
                    store(VA[g * 512 + tt * 128: g * 512 + (tt + 1) * 128, :], o.t[:], [o.h], [dh("VA")])
                if g + 1 < NG:
                    S1(g + 1)
                flush()
                for tt in range(4):
                    qt = g * 4 + tt
                    own = qt // 2
                    pg_ = pgs[tt % 2]
                    gsb_ = gsbs[tt]
                    for h in range(8):
                        j, r0 = h // 2, (h % 2) * 64
                        mm(pg_.t[:, h * 32:(h + 1) * 32], qbo[j].t[r0:r0 + 64, tt * 128:(tt + 1) * 128], km.t[r0:r0 + 64, j, :], True, False,
                           [qbo[j].h, km.h], [pg_.h])
                        mm(pg_.t[:, h * 32:(h + 1) * 32], onesb.t[0:1, :], wneg.t[0:1, 32 - own:64 - own], False, True,
                           [onesb.h, wneg.h], [pg_.h])
                    op("act", lambda e, pg_=pg_, gsb_=gsb_: e.copy(out=gsb_.t[:], in_=pg_.t[:, 0:256]), reads=[pg_.h], writes=[gsb_.h])
                for h in range(8):
                    for tt in range(4):
                        gsb_, t8_ = gsbs[tt], t8s[tt]
                        op("dve", lambda e, h=h, gsb_=gsb_, t8_=t8_: e.max(out=t8_.t[:, h, :], in_=gsb_.t[:, h * 32:(h + 1) * 32]), reads=[gsb_.h], writes=[t8_.hs[h]])
                for tt in range(4):
                    t8_, thr_ = t8s[tt], thrs[tt]
                    op("dve", lambda e, t8_=t8_, thr_=thr_: e.tensor_scalar_max(out=thr_.t[:], in0=t8_.t[:, :, 3], scalar1=-1e29), reads=list(t8_.hs), writes=[thr_.h])
                for h in range(8):
                    for tt in range(4):
                        gsb_, thr_, mkf_ = gsbs[tt], thrs[tt], mkfs[tt]
                        op("dve", lambda e, h=h, gsb_=gsb_, thr_=thr_, mkf_=mkf_: e.tensor_scalar(out=mkf_.t[:, h * 32:(h + 1) * 32], in0=gsb_.t[:, h * 32:(h + 1) * 32],
                                                                 scalar1=thr_.t[:, h:h + 1], scalar2=30000.0, op0=ALU.is_ge, op1=ALU.mult),
                           reads=[gsb_.h, thr_.h], writes=[mkf_.hs[h]])
                for tt in range(4):
                    mkf_, mkb_ = mkfs[tt], mkbs[tt]
                    op("dve", lambda e, mkf_=mkf_, mkb_=mkb_: e.tensor_scalar_add(out=mkb_.t[:], in0=mkf_.t[:], scalar1=-30000.0), reads=list(mkf_.hs), writes=[mkb_.h])

                def gate_fin(g=g, tok=tok):
                    for tt in range(4):
                        mkb_ = mkbs[tt]
                        for half in range(2):
                            op("pe", lambda e, half=half, mkb_=mkb_: e.transpose(out=ptr.t[:, half * 128:(half + 1) * 128], in_=mkb_.t[:, half * 128:(half + 1) * 128], identity=identb.t[:]),
                               reads=[mkb_.h, identb.h], writes=[ptr.h])
                        op("act", lambda e, tt=tt: e.copy(out=mT.t[:, :, tt * 128:(tt + 1) * 128], in_=ptr.t[:, 0:256].rearrange("p (a b) -> p a b", a=2)),
                           reads=[ptr.h], writes=[mT.h])
                    for half in range(2):
                        store(MB[half * 128:(half + 1) * 128, tok], mT.t[:, half, :], [mT.h], [dh("MB")])
                gate_pend.append(gate_fin)
            while gate_pend:
                gate_pend.pop(0)()
            store_q[0] = "pool"
            Sx.barrier()
        if stop_after <= 1:
            return _finish(nc, Sx, out_d, S)

        with ExitStack() as st:
            sb, ps = mk(st)
            maskc = sb("maskc", [128, 2, 1024], BF16)
            load(maskc.t[:], c_mask.rearrange("p (a b) c -> p a (b c)", a=2), [maskc.h])
            QT = [sb(f"QT{i}", [96, S], BF16) for i in range(2)]
            KT = [sb(f"KT{i}", [96, S], BF16) for i in range(2)]
            VV = [sb(f"VV{i}", [128, NT, 128], BF16) for i in range(2)]
            for v in VV:
                op("dve", lambda e, v=v: e.memset(v.t[:, :, 64:128], 1.0), writes=[v.h])
            NP = 4
            PT = [sb(f"PT{i}", [128, 1024], BF16) for i in range(NP)]
            rcp = [sb(f"rcp{i}", [128, 512], F32) for i in range(2)]
            rc0 = [sb(f"rc0{i}", [64, 512], F32) for i in range(2)]
            onb = [sb(f"onb{i}", [64, 512], BF16) for i in range(2)]
            psc = [ps(f"psc{i}", [128, 1024], F32) for i in range(3)]
            pov = [ps(f"pov{i}", [128, 512], F32) for i in range(2)]
            VAv = VA.rearrange("(n p) f -> p n f", p=128)
            VBv = VB.rearrange("(n p) f -> p n f", p=128)
            stg3 = [sb(f"stg3{i}", [128, 8, 512], F32) for i in range(2)]
            obf3 = [sb(f"obf3{i}", [128, 8, 512], BF16) for i in range(2)]
            gain3 = sb("gain3", [128, 8], F32)
            load(gain3.t[:], attn_norm_d, [gain3.h])
            cnt3 = [0]
            c3q = []

            def conv3(src, dst, dname, nk, ncols, use_gain, cstart=0):
                srcv = src.rearrange("(k p) c -> p k c", p=128)
                dstv = dst.rearrange("(k p) c -> p k c", p=128)
                for c0 in range(cstart, ncols, 512):
                    cw = min(512, ncols - c0)
                    i = cnt3[0] % 2
                    cnt3[0] += 1
                    a_, b_ = stg3[i], obf3[i]
                    c3q.append(lambda a_=a_, c0=c0, cw=cw: load(a_.t[:, 0:nk, 0:cw], srcv[:, :, c0:c0 + cw], [a_.h]))
                    for k in range(nk):
                        if use_gain:
                            c3q.append(lambda a_=a_, b_=b_, k=k, cw=cw: op("pool", lambda e: e.tensor_scalar_mul(out=b_.t[:, k, 0:cw], in0=a_.t[:, k, 0:cw], scalar1=gain3.t[:, k:k + 1]),
                                                                      reads=[a_.h, gain3.h], writes=[b_.h]))
                        else:
                            c3q.append(lambda a_=a_, b_=b_, k=k, cw=cw: op("pool", lambda e: e.tensor_copy(out=b_.t[:, k, 0:cw], in_=a_.t[:, k, 0:cw]),
                                                                      reads=[a_.h], writes=[b_.h]))
                    c3q.append(lambda b_=b_, c0=c0, cw=cw: store(dstv[:, :, c0:c0 + cw], b_.t[:, 0:nk, 0:cw], [b_.h], [dh(dname)]))

            LA = 2
            pend = []
            state = {"ui": 0, "gi": 0}

            def emit_pair(q_, k_, v_, sc, g, kp, nkp, odst, on, h):
                ui = state["ui"]
                state["ui"] += 1
                pscore = psc[ui % 3]
                pt = PT[ui % NP]
                r = 2 * kp - 4 * g
                c0s = [((r + j) * 128 if r >= 0 else 0) for j in range(2)]
                for j in range(2):
                    kt = 2 * kp + j
                    c0 = c0s[j]
                    mm(pscore.t[:, j * 512 + c0:(j + 1) * 512], k_.t[0:96, kt * 128:(kt + 1) * 128], q_.t[0:96, g * 512 + c0:(g + 1) * 512], True, True,
                       [k_.h, q_.h], [pscore.h])
                if r == 2:
                    for j in range(2):
                        c0 = c0s[j]
                        op("act", lambda e, j=j, c0=c0: e.activation(out=pt.t[:, j * 512 + c0:(j + 1) * 512], in_=pscore.t[:, j * 512 + c0:(j + 1) * 512], func=AF.Exp, scale=sc),
                           reads=[pscore.h], writes=[pt.h])
                else:
                    op("act", lambda e: e.activation(out=pt.t[:], in_=pscore.t[:], func=AF.Exp, scale=sc), reads=[pscore.h], writes=[pt.h])
                if r >= 0:
                    for j in range(2):
                        c0 = c0s[j]
                        op("dve", lambda e, j=j, c0=c0: e.tensor_tensor(out=pt.t[:, j * 512 + c0:j * 512 + c0 + 128], in0=pt.t[:, j * 512 + c0:j * 512 + c0 + 128],
                                                                      in1=maskc.t[:, r // 2, j * 512 + c0:j * 512 + c0 + 128], op=ALU.mult),
                           reads=[pt.h, maskc.h], writes=[pt.h])
                first, last = (kp == 0), (kp == nkp - 1)
                if first:
                    state["gi"] += 1
                gi = state["gi"]
                po = pov[gi % 2]

                def pv():
                    for j in range(2):
                        kt = 2 * kp + j
                        c0 = c0s[j]
                        mm(po.t[:, c0:512], v_.t[:, kt, :], pt.t[:, j * 512 + c0:(j + 1) * 512], first and j == 0, last and j == 1, [v_.h, pt.h], [po.h])
                    if last:
                        rc, r0, onb_ = rcp[gi % 2], rc0[gi % 2], onb[gi % 2]
                        op("dve", lambda e: e.reciprocal(out=rc.t[64:128, :], in_=po.t[64:128, :]), reads=[po.h], writes=[rc.h])
                        op("dve", lambda e: e.tensor_copy(out=r0.t[0:64, :], in_=rc.t[64:128, :]), reads=[rc.h], writes=[r0.h])
                        op("dve", lambda e: e.tensor_tensor(out=onb_.t[:], in0=po.t[0:64, :], in1=r0.t[0:64, :], op=ALU.mult),
                           reads=[po.h, r0.h], writes=[onb_.h])
                        store(odst[h * 64:(h + 1) * 64, g * 512:(g + 1) * 512], onb_.t[:], [onb_.h], [dh(on)])
                return pv

            for hp in range(16):
                typ, h = hp // 8, hp % 8
                q_, k_, v_ = QT[hp % 2], KT[hp % 2], VV[hp % 2]
                if typ == 0:
                    load(q_.t[0:96, :], QA[h * 96:(h + 1) * 96, :], [q_.h], [dh("QA")])
                    load(k_.t[0:64, :], KNA[h * 64:(h + 1) * 64, :], [k_.h], [dh("KNA")])
                    load(k_.t[64:96, :], KRA[:, :], [k_.h], [dh("KRA")])
                    vsrc, vn, sc, odst, on = VAv, "VA", 96.0 ** -0.5, OA, "OA"
                else:
                    load(q_.t[0:64, :], QB[h * 64:(h + 1) * 64, :], [q_.h], [dh("QB")])
                    load(q_.t[64:96, :], MB[h * 32:(h + 1) * 32, :], [q_.h], [dh("MB")])
                    load(k_.t[0:64, :], KB[h * 64:(h + 1) * 64, :], [k_.h], [dh("KB")])
                    load(k_.t[64:96, :], c_oh[:, :], [k_.h])
                    vsrc, vn, sc, odst, on = VBv, "VB", 64.0 ** -0.5, OB, "OB"
                vstep = max(1, NT // 4)
                for n0 in range(0, NT, vstep):
                    load(v_.t[:, n0:n0 + vstep, 0:64], vsrc[:, n0:n0 + vstep, h * 64:(h + 1) * 64], [v_.h], [dh(vn)])
                if hp == 3:
                    zt = sb("zt", [128, 4, D], BF16)
                    op("pool", lambda e: e.memset(zt.t[:], 0.0), writes=[zt.h])
                    XSv = XS.rearrange("(n p) d -> p n d", p=128)
                    zlist = list(range(0, NSLOT // 128, 4))
                if hp >= 3:
                    nz = -(-len(zlist) // 12) if hp < 15 else len(zlist)
                    for _ in range(min(nz, len(zlist))):
                        n0 = zlist.pop(0)
                        store(XSv[:, n0:n0 + 4, :], zt.t[:], [zt.h], [dh("XS")])
                if hp == 2:
                    conv3(w_in_d, WIN, "WIN", 8, 4640, True, cstart=2592)
                    conv3(w_oa_d, WOA, "WOA", 4, D, False)
                    conv3(w_ob_d, WOB, "WOB", 4, D, False)
                    conv3(w_out_d, WOUT, "WOUT", 8, D, False)
                for g in range(NG):
                    nkp = 2 * g + 2
                    if hp >= 2 and c3q and g >= 4:
                        c3q.pop(0)()
                    for kp in range(nkp):
                        pend.append(emit_pair(q_, k_, v_, sc, g, kp, nkp, odst, on, h))
                        if len(pend) > LA:
                            pend.pop(0)()
            while pend:
                pend.pop(0)()
            while c3q:
                c3q.pop(0)()
            Sx.barrier()
        if stop_after <= 3:
            return _finish(nc, Sx, out_d, S)

        with ExitStack() as st:
            sb, ps = mk(st)
            identf = sb("identf", [128, 128], F32)
            onesf = sb("onesf4", [128, 128], F32)
            onesb = sb("onesb4", [128, 128], BF16)
            triu = sb("triu", [128, 128], BF16)
            offs = sb("offs", [128, 32], F32)
            load(identf.t[:], c_identf, [identf.h])
            load(onesf.t[:], c_onesf, [onesf.h])
            load(onesb.t[:], c_onesb, [onesb.h])
            load(triu.t[:], c_triu, [triu.h])
            load(offs.t[:], c_offs, [offs.h])
            wg = sb("wgate", [128, 8, 2048], BF16, 4)
            woa = sb("woa", [128, 4, D], BF16)
            wob = sb("wob", [128, 4, D], BF16)
            wout = sb("wout", [128, 8, D], BF16)
            winv = WIN.rearrange("(k p) c -> p k c", p=128)
            load(woa.t[:], WOA.rearrange("(k p) c -> p k c", p=128), [woa.h], [dh("WOA")])
            for c0 in (0, 1024):
                load(wg.t[:, :, c0:c0 + 512], winv[:, :, 2592 + c0:2592 + c0 + 512], [wg.hs[c0 // 512]], [dh("WIN")])
            load(wob.t[:], WOB.rearrange("(k p) c -> p k c", p=128), [wob.h], [dh("WOB")])
            for c0 in (512, 1536):
                load(wg.t[:, :, c0:c0 + 512], winv[:, :, 2592 + c0:2592 + c0 + 512], [wg.hs[c0 // 512]], [dh("WIN")])
            for c0 in range(0, D, 512):
                load(wout.t[:, :, c0:c0 + 512], WOUT.rearrange("(k p) c -> p k c", p=128)[:, :, c0:c0 + 512], [wout.h], [dh("WOUT")])
            gf = sb("gf", [128, 8], F32)
            wr_s = sb("wr_s", [128, 8, 36], F32)
            wr = sb("wr", [128, 8, 36], F32)
            br = sb("br", [1, 36], F32)
            load(gf.t[:], ffn_norm_d, [gf.h])
            load(wr_s.t[:], w_r_d.rearrange("(k p) c -> p k c", p=128), [wr_s.h])
            load(br.t[:], b_r_d, [br.h])
            for k in range(8):
                op("dve", lambda e, k=k: e.tensor_scalar_mul(out=wr.t[:, k, :], in0=wr_s.t[:, k, :], scalar1=gf.t[:, k:k + 1]),
                   reads=[wr_s.h, gf.h], writes=[wr.h])
            run = sb("run", [128, 32], F32)
            op("dve", lambda e: e.memset(run.t[:], 0.0), writes=[run.h])

            hTg2 = [sb(f"hTg{i}", [128, 8, 512], BF16) for i in range(2)]
            oag2 = [sb(f"oag{i}", [128, 4, 512], BF16) for i in range(2)]
            obg2 = [sb(f"obg{i}", [128, 4, 512], BF16) for i in range(2)]
            sig = [sb(f"sig{i}", [128, 512], F32) for i in range(2)]
            m1 = [sb(f"m1_{i}", [128, 512], F32) for i in range(2)]
            mixT = sb("mixT", [128, 8, 512], BF16, 8)
            xtok = [sb(f"xtok{i}", [128, D], F32) for i in range(2)]
            x1 = [sb(f"x1_{i}", [128, D], F32) for i in range(3)]
            junk = sb("junk", [128, D], F32)
            pA = [ps(f"pA{i}", [128, 512], F32) for i in range(2)]
            pG = [ps(f"pG{i}", [128, 512], F32) for i in range(2)]
            pY = [ps(f"pY{i}", [128, 512], F32) for i in range(2)]
            pTr = ps("pTr", [128, 512], F32)
            pL = ps("pL", [128, 512], F32)
            hpC = H()
            HTv = HT.rearrange("(k p) s -> p k s", p=128)
            OAv = OA.rearrange("(k p) s -> p k s", p=128)
            OBv = OB.rearrange("(k p) s -> p k s", p=128)
            RS = []
            for i in range(4):
                RS.append(dict(
                    L=sb(f"L{i}", [128, 36], F32), gmax=sb(f"gmax{i}", [128, 1], F32), ngmax=sb(f"ngmax{i}", [128, 1], F32),
                    sume=sb(f"sume{i}", [128, 1], F32), pgrp=sb(f"pgrp{i}", [128, 1], F32), mx1=sb(f"mx1{i}", [128, 1], F32),
                    mx2=sb(f"mx2{i}", [128, 1], F32), dd=sb(f"dd{i}", [128, 1], F32), sg1=sb(f"sg1{i}", [128, 1], F32),
                    gone=sb(f"gone{i}", [128, 4], F32),
                    ein=sb(f"ein{i}", [128, 8], F32), ein2=sb(f"ein2{i}", [128, 8], F32), one1=sb(f"one1{i}", [128, 8], F32),
                    one2=sb(f"one2{i}", [128, 8], F32), ex4=sb(f"ex4{i}", [128, 4], F32), E1=sb(f"E1{i}", [128, 32], F32),
                    E2=sb(f"E2{i}", [128, 32], F32), Ab=sb(f"Ab{i}", [128, 32], BF16), pos=sb(f"pos{i}", [128, 32], F32),
                    tmpa=sb(f"tmpa{i}", [128, 32], F32), tmpb=sb(f"tmpb{i}", [128, 32], F32), slf=sb(f"slf{i}", [128, 2], F32),
                    ss1=sb(f"ss1{i}", [128, 2], F32), hnT=sb(f"hnT{i}", [128, 8, 128], F32)))
            hn4 = [sb(f"hn4{i}", [128, D], F32) for i in range(4)]
            hnb8 = [sb(f"hnb8{i}", [128, D], BF16) for i in range(8)]

            bgq = []

            def bg_run(n):
                for _ in range(n):
                    if bgq:
                        bgq.pop(0)()

            def stageA1(ti, tt):
                rows = slice(ti * 128, (ti + 1) * 128)
                xk, x1_ = xtok[ti % 2], x1[ti % 3]
                load(xk.t[:], x_d[rows, :], [xk.h])
                for half in range(2):
                    py = pY[half]
                    for k in range(8):
                        mm(py.t[:], mixT.t[:, k, tt * 128:(tt + 1) * 128], wout.t[:, k, half * 512:(half + 1) * 512], k == 0, k == 7,
                           [mixT.hs[k], wout.h], [py.h])
                    op("dve", lambda e, py=py, half=half: e.tensor_tensor(out=x1_.t[:, half * 512:(half + 1) * 512], in0=xk.t[:, half * 512:(half + 1) * 512], in1=py.t[:], op=ALU.add),
                       reads=[py.h, xk.h], writes=[x1_.h])

            def stageA2(ti, tt):
                rows = slice(ti * 128, (ti + 1) * 128)
                x1_, hn_, hnb_ = x1[ti % 3], hn4[ti % 4], hnb8[ti % 8]
                ss1_ = RS[ti % 4]["ss1"]
                op("sp", lambda e: e.dma_start(out=X1[rows, :], in_=x1_.t[:]), reads=[x1_.h], writes=[dh("X1")], dma=True)
                op("act", lambda e: e.activation(out=junk.t[:], in_=x1_.t[:], func=AF.Square, accum_out=ss1_.t[:, 0:1]),
                   reads=[x1_.h], writes=[junk.h, ss1_.h])
                op("act", lambda e: e.activation(out=ss1_.t[:, 1:2], in_=ss1_.t[:, 0:1], func=AF.Sqrt, scale=1.0 / D, bias=EPS), reads=[ss1_.h], writes=[ss1_.h])
                op("dve", lambda e: e.reciprocal(out=ss1_.t[:, 1:2], in_=ss1_.t[:, 1:2]), reads=[ss1_.h], writes=[ss1_.h])
                op("dve", lambda e: e.tensor_scalar_mul(out=hn_.t[:], in0=x1_.t[:], scalar1=ss1_.t[:, 1:2]), reads=[x1_.h, ss1_.h], writes=[hn_.h])
                op("act", lambda e: e.copy(out=hnb_.t[:], in_=hn_.t[:]), reads=[hn_.h], writes=[hnb_.h])

            def stageB4(g):
                tis = [g * 4 + tt for tt in range(4)]
                for ti in tis:
                    R = RS[ti % 4]
                    hn_, hnT_, L = hn4[ti % 4], R["hnT"], R["L"]
                    for k in range(8):
                        op("pe", lambda e, k=k, hn_=hn_: e.transpose(out=pTr.t[:, (k % 4) * 128:(k % 4 + 1) * 128], in_=hn_.t[:, k * 128:(k + 1) * 128], identity=identf.t[:]),
                           reads=[hn_.h, identf.h], writes=[pTr.h])
                        if k % 4 == 3:
                            kk = k // 4
                            op("act", lambda e, kk=kk, hnT_=hnT_: e.copy(out=hnT_.t[:, kk * 4:(kk + 1) * 4, :], in_=pTr.t[:].rearrange("p (a b) -> p a b", a=4)),
                               reads=[pTr.h], writes=[hnT_.h])
                for ti in tis:
                    R = RS[ti % 4]
                    hnT_, L = R["hnT"], R["L"]
                    c0 = (ti % 4) * 64
                    for k in range(8):
                        mm(pL.t[:, c0:c0 + 36], hnT_.t[:, k, :], wr.t[:, k, :], k == 0, False, [hnT_.h, wr.h], [pL.h])
                    mm(pL.t[:, c0:c0 + 36], onesf.t[0:1, :], br.t[0:1, :], False, True, [onesf.h, br.h], [pL.h])
                for ti in tis:
                    R = RS[ti % 4]
                    c0 = (ti % 4) * 64
                    op("act", lambda e, R=R, c0=c0: e.copy(out=R["L"].t[:], in_=pL.t[:, c0:c0 + 36]), reads=[pL.h], writes=[R["L"].h])

                def each(fn):
                    def step():
                        for ti in tis:
                            fn(ti, RS[ti % 4])
                    bgq.append(step)
                each(lambda ti, R: op("dve", lambda e: e.tensor_reduce(out=R["gmax"].t[:], in_=R["L"].t[:, 0:4], axis=AX.X, op=ALU.max), reads=[R["L"].h], writes=[R["gmax"].h]))
                each(lambda ti, R: op("dve", lambda e: e.tensor_scalar(out=R["gone"].t[:], in0=R["L"].t[:, 0:4], scalar1=R["gmax"].t[:, 0:1], scalar2=None, op0=ALU.is_equal), reads=[R["L"].h, R["gmax"].h], writes=[R["gone"].h]))
                each(lambda ti, R: op("dve", lambda e: e.tensor_scalar_mul(out=R["ngmax"].t[:], in0=R["gmax"].t[:], scalar1=-1.0), reads=[R["gmax"].h], writes=[R["ngmax"].h]))
                each(lambda ti, R: op("dve", lambda e: e.tensor_scalar_mul(out=R["ein"].t[:], in0=R["L"].t[:, 4:12], scalar1=R["gone"].t[:, 0:1]), reads=[R["L"].h, R["gone"].h], writes=[R["ein"].h]))
                each(lambda ti, R: op("act", lambda e: e.activation(out=R["ex4"].t[:], in_=R["L"].t[:, 0:4], func=AF.Exp, bias=R["ngmax"].t[:, 0:1], accum_out=R["sume"].t[:, 0:1]),
                                     reads=[R["L"].h, R["ngmax"].h], writes=[R["ex4"].h, R["sume"].h]))
                for gg in range(1, 4):
                    each(lambda ti, R, gg=gg: op("dve", lambda e: e.scalar_tensor_tensor(out=R["ein"].t[:], in0=R["L"].t[:, 4 + 8 * gg:12 + 8 * gg], scalar=R["gone"].t[:, gg:gg + 1], in1=R["ein"].t[:], op0=ALU.mult, op1=ALU.add),
                                                 reads=[R["L"].h, R["gone"].h, R["ein"].h], writes=[R["ein"].h]))
                each(lambda ti, R: op("dve", lambda e: e.tensor_reduce(out=R["mx1"].t[:], in_=R["ein"].t[:], axis=AX.X, op=ALU.max), reads=[R["ein"].h], writes=[R["mx1"].h]))
                each(lambda ti, R: op("dve", lambda e: e.tensor_scalar(out=R["one1"].t[:], in0=R["ein"].t[:], scalar1=R["mx1"].t[:, 0:1], scalar2=None, op0=ALU.is_equal), reads=[R["ein"].h, R["mx1"].h], writes=[R["one1"].h]))
                each(lambda ti, R: op("dve", lambda e: e.scalar_tensor_tensor(out=R["ein2"].t[:], in0=R["one1"].t[:], scalar=-1e30, in1=R["ein"].t[:], op0=ALU.mult, op1=ALU.add), reads=[R["one1"].h, R["ein"].h], writes=[R["ein2"].h]))
                each(lambda ti, R: op("dve", lambda e: e.tensor_reduce(out=R["mx2"].t[:], in_=R["ein2"].t[:], axis=AX.X, op=ALU.max), reads=[R["ein2"].h], writes=[R["mx2"].h]))
                each(lambda ti, R: op("dve", lambda e: e.tensor_scalar(out=R["one2"].t[:], in0=R["ein2"].t[:], scalar1=R["mx2"].t[:, 0:1], scalar2=None, op0=ALU.is_equal), reads=[R["ein2"].h, R["mx2"].h], writes=[R["one2"].h]))
                each(lambda ti, R: op("dve", lambda e: e.tensor_tensor(out=R["dd"].t[:], in0=R["mx1"].t[:], in1=R["mx2"].t[:], op=ALU.subtract), reads=[R["mx1"].h, R["mx2"].h], writes=[R["dd"].h]))
                each(lambda ti, R: op("dve", lambda e: e.reciprocal(out=R["pgrp"].t[:], in_=R["sume"].t[:]), reads=[R["sume"].h], writes=[R["pgrp"].h]))
                each(lambda ti, R: op("act", lambda e: e.activation(out=R["sg1"].t[:], in_=R["dd"].t[:], func=AF.Sigmoid), reads=[R["dd"].h], writes=[R["sg1"].h]))
                for gg in range(4):
                    each(lambda ti, R, gg=gg: op("dve", lambda e: e.tensor_scalar_mul(out=R["E1"].t[:, gg * 8:(gg + 1) * 8], in0=R["one1"].t[:], scalar1=R["gone"].t[:, gg:gg + 1]), reads=[R["one1"].h, R["gone"].h], writes=[R["E1"].h]))
                    each(lambda ti, R, gg=gg: op("dve", lambda e: e.tensor_scalar_mul(out=R["E2"].t[:, gg * 8:(gg + 1) * 8], in0=R["one2"].t[:], scalar1=R["gone"].t[:, gg:gg + 1]), reads=[R["one2"].h, R["gone"].h], writes=[R["E2"].h]))
                each(lambda ti, R: op("dve", lambda e: e.tensor_tensor(out=R["Ab"].t[:], in0=R["E1"].t[:], in1=R["E2"].t[:], op=ALU.add), reads=[R["E1"].h, R["E2"].h], writes=[R["Ab"].h]))
                each(lambda ti, R: op("dve", lambda e: e.tensor_tensor(out=wts.t[:, ti, 0:1], in0=R["sg1"].t[:], in1=R["pgrp"].t[:], op=ALU.mult), reads=[R["sg1"].h, R["pgrp"].h], writes=[wts.hs[ti % 4]]))
                each(lambda ti, R: op("dve", lambda e: e.tensor_tensor(out=wts.t[:, ti, 1:2], in0=R["pgrp"].t[:], in1=wts.t[:, ti, 0:1], op=ALU.subtract), reads=[R["pgrp"].h, wts.hs[ti % 4]], writes=[wts.hs[ti % 4]]))

            def stageC4(g):
                tis = [g * 4 + tt for tt in range(4)]
                for ti in tis:
                    R = RS[ti % 4]
                    c0 = (ti % 4) * 64
                    mm(pL.t[:, 256 + c0:256 + c0 + 32], triu.t[:], R["Ab"].t[:], True, True, [triu.h, R["Ab"].h], [hpC])
                    mm(pL.t[:, 256 + c0 + 32:256 + c0 + 64], onesb.t[:], R["Ab"].t[:], True, True, [onesb.h, R["Ab"].h], [hpC])
                for ti in tis:
                    R = RS[ti % 4]
                    c0 = (ti % 4) * 64
                    op("dve", lambda e, R=R, c0=c0: e.tensor_tensor(out=R["pos"].t[:], in0=pL.t[:, 256 + c0:256 + c0 + 32], in1=run.t[:], op=ALU.add), reads=[hpC, run.h], writes=[R["pos"].h])
                    op("dve", lambda e, c0=c0: e.tensor_tensor(out=run.t[:], in0=pL.t[:, 256 + c0 + 32:256 + c0 + 64], in1=run.t[:], op=ALU.add), reads=[hpC, run.h], writes=[run.h])

                def each(fn):
                    for ti in tis:
                        fn(ti, RS[ti % 4])
                each(lambda ti, R: op("dve", lambda e: e.tensor_tensor(out=R["pos"].t[:], in0=R["pos"].t[:], in1=offs.t[:], op=ALU.add), reads=[R["pos"].h, offs.h], writes=[R["pos"].h]))
                each(lambda ti, R: op("dve", lambda e: e.tensor_tensor(out=R["tmpa"].t[:], in0=R["pos"].t[:], in1=R["E1"].t[:], op=ALU.mult), reads=[R["pos"].h, R["E1"].h], writes=[R["tmpa"].h]))
                each(lambda ti, R: op("dve", lambda e: e.tensor_tensor(out=R["tmpb"].t[:], in0=R["pos"].t[:], in1=R["E2"].t[:], op=ALU.mult), reads=[R["pos"].h, R["E2"].h], writes=[R["tmpb"].h]))
                each(lambda ti, R: op("dve", lambda e: e.tensor_reduce(out=R["slf"].t[:, 0:1], in_=R["tmpa"].t[:], axis=AX.X, op=ALU.add), reads=[R["tmpa"].h], writes=[R["slf"].h]))
                each(lambda ti, R: op("dve", lambda e: e.tensor_reduce(out=R["slf"].t[:, 1:2], in_=R["tmpb"].t[:], axis=AX.X, op=ALU.add), reads=[R["tmpb"].h, R["slf"].h], writes=[R["slf"].h]))
                each(lambda ti, R: op("dve", lambda e: e.tensor_copy(out=slot_i.t[:, ti, :], in_=R["slf"].t[:]), reads=[R["slf"].h], writes=[slot_i.hs[ti % 4]]))
                for ti in tis:
                    hnb_ = hnb8[ti % 8]
                    for j in range(2):
                        op("pool", lambda e, j=j, ti=ti, hnb_=hnb_: e.indirect_dma_start(
                            out=XS, out_offset=bass.IndirectOffsetOnAxis(ap=slot_i.t[:, ti, j:j + 1], axis=0),
                            in_=hnb_.t[:], in_offset=None), reads=[hnb_.h, slot_i.hs[ti % 4]], writes=[dh("XS")], dma=True)

            ci = 0
            def p4_loads(g):
                tok = slice(g * 512, (g + 1) * 512)
                load(hTg2[g % 2].t[:], HTv[:, :, tok], [hTg2[g % 2].h], [dh("HT")])
                load(oag2[g % 2].t[:], OAv[:, :, tok], [oag2[g % 2].h], [dh("OA")])
                load(obg2[g % 2].t[:], OBv[:, :, tok], [obg2[g % 2].h], [dh("OB")])

            p4_loads(0)
            for g in range(NG):
                tok = slice(g * 512, (g + 1) * 512)
                if g + 1 < NG:
                    p4_loads(g + 1)
                hTg, oag, obg = hTg2[g % 2], oag2[g % 2], obg2[g % 2]
                it = 0
                for c in range(8):
                    for br_i, (og, wo, gc0) in enumerate(((oag, woa, 0), (obg, wob, 1024))):
                        if it == 3 and g >= 1:
                            stageB4(g - 1)
                        it += 1
                        pa, pg_ = pA[ci % 2], pG[ci % 2]
                        sg, mm1 = sig[ci % 2], m1[ci % 2]
                        ci += 1
                        for k in range(4):
                            mm(pa.t[:], wo.t[:, k, c * 128:(c + 1) * 128], og.t[:, k, :], k == 0, k == 3, [wo.h, og.h], [pa.h])
                        for k in range(8):
                            mm(pg_.t[:], wg.t[:, k, gc0 + c * 128: gc0 + (c + 1) * 128], hTg.t[:, k, :], k == 0, k == 7, [wg.hs[(gc0 + c * 128) // 512], hTg.h], [pg_.h])
                        op("act", lambda e, pg_=pg_, sg=sg: e.activation(out=sg.t[:], in_=pg_.t[:], func=AF.Sigmoid), reads=[pg_.h], writes=[sg.h])
                        if br_i == 0:
                            op("dve", lambda e, pa=pa, sg=sg, mm1=mm1: e.tensor_tensor(out=mm1.t[:], in0=sg.t[:], in1=pa.t[:], op=ALU.mult),
                               reads=[sg.h, pa.h], writes=[mm1.h])
                            prev = mm1
                        else:
                            op("dve", lambda e, pa=pa, sg=sg: e.tensor_tensor(out=sg.t[:], in0=sg.t[:], in1=pa.t[:], op=ALU.mult),
                               reads=[sg.h, pa.h], writes=[sg.h])
                            op("dve", lambda e, sg=sg, prev=prev, c=c: e.tensor_tensor(out=mixT.t[:, c, :], in0=sg.t[:], in1=prev.t[:], op=ALU.add),
                               reads=[sg.h, prev.h], writes=[mixT.hs[c]])
                        bg_run(3)
                bg_run(10 ** 6)
                if g >= 1:
                    stageC4(g - 1)
                for tt in range(4):
                    stageA1(g * 4 + tt, tt)
                    if tt >= 1:
                        stageA2(g * 4 + tt - 1, tt - 1)
                stageA2(g * 4 + 3, 3)
            stageB4(NG - 1)
            bg_run(10 ** 6)
            stageC4(NG - 1)
            Sx.barrier()
        if stop_after <= 4:
            return _finish(nc, Sx, out_d, S)

        with ExitStack() as st:
            sb, ps = mk(st)
            identb5x = sb("identb5", [128, 128], BF16)
            load(identb5x.t[:], c_identb, [identb5x.h])
            gf5v = sb("gf5", [128, 8], F32)
            load(gf5v.t[:], ffn_norm_d, [gf5v.h])
            NST = C // 128
            HALF = C // 2
            NSH = HALF // 128
            wgs = [sb(f"wgs{i}", [128, 8, 256], F32) for i in range(2)]
            wus = [sb(f"wus{i}", [128, 8, 256], F32) for i in range(2)]
            wds = [sb(f"wds{i}", [128, 2, D], F32) for i in range(2)]
            wgb = [sb(f"wgb{i}", [128, 8, 256], BF16) for i in range(2)]
            wub = [sb(f"wub{i}", [128, 8, 256], BF16) for i in range(2)]
            wdb = [sb(f"wdb{i}", [128, 2, D], BF16) for i in range(2)]
            xs = [sb(f"xs{i}", [128, D], BF16) for i in range(6)]
            xeT = [sb(f"xeT{i}", [128, 8, C], BF16) for i in range(2)]
            sil = [sb(f"sil{i}", [128, HALF], F32) for i in range(2)]
            actT = [sb(f"actT{i}", [128, 2, HALF], BF16, 2) for i in range(2)]
            ysb = [sb(f"ysb{i}", [128, D], BF16) for i in range(6)]
            ptx = [ps(f"ptx{i}", [128, 1024], BF16) for i in range(2)]
            pgu = [ps(f"pgu{i}", [128, 512], F32) for i in range(4)]
            pyy = [ps(f"pyy{i}", [128, 512], F32) for i in range(2)]
            xi = [0]
            yi = [0]

            def stage_load(ex):
                b = ex % 2
                load(wgs[b].t[:], w_g_d[ex].rearrange("(k p) f -> p k f", p=128), [wgs[b].h])
                load(wus[b].t[:], w_u_d[ex].rearrange("(k p) f -> p k f", p=128), [wus[b].h])
                load(wds[b].t[:], w_d_d[ex].rearrange("(k p) f -> p k f", p=128), [wds[b].h])

            def conv_part(ex, part):
                b = ex % 2
                for k in (2 * part, 2 * part + 1):
                    op("act", lambda e, k=k, b=b: e.activation(out=wgb[b].t[:, k, :], in_=wgs[b].t[:, k, :], func=AF.Copy, scale=gf5v.t[:, k:k + 1]),
                       reads=[wgs[b].h, gf5v.h], writes=[wgb[b].h])
                    op("dve", lambda e, k=k, b=b: e.tensor_scalar_mul(out=wub[b].t[:, k, :], in0=wus[b].t[:, k, :], scalar1=gf5v.t[:, k:k + 1]),
                       reads=[wus[b].h, gf5v.h], writes=[wub[b].h])
                if part == 3:
                    op("dve", lambda e, b=b: e.tensor_copy(out=wdb[b].t[:, 0, :], in_=wds[b].t[:, 0, :]), reads=[wds[b].h], writes=[wdb[b].h])
                    op("dve", lambda e, b=b: e.tensor_copy(out=wdb[b].t[:, 1, :], in_=wds[b].t[:, 1, :]), reads=[wds[b].h], writes=[wdb[b].h])

            def stage_a(ex):
                b = ex % 2
                xe = xeT[b]
                for stl in range(NST):
                    row0 = ex * C + stl * 128
                    xs_ = xs[xi[0] % 6]
                    px = ptx[xi[0] % 2]
                    xi[0] += 1
                    load(xs_.t[:], XS[row0:row0 + 128, :], [xs_.h], [dh("XS")])
                    for k in range(8):
                        op("pe", lambda e, k=k, xs_=xs_, px=px: e.transpose(out=px.t[:, k * 128:(k + 1) * 128], in_=xs_.t[:, k * 128:(k + 1) * 128], identity=identb5x.t[:]),
                           reads=[xs_.h, identb5x.h], writes=[px.h])
                    op("act", lambda e, px=px, xe=xe, stl=stl: e.copy(out=xe.t[:, :, stl * 128:(stl + 1) * 128], in_=px.t[:].rearrange("p (a b) -> p a b", a=8)),
                       reads=[px.h], writes=[xe.h])

            def gu(ex, hf, fc):
                b = ex % 2
                xe = xeT[b]
                cs = slice(hf * HALF, (hf + 1) * HALF)
                at = actT[hf]
                pgt, put = pgu[fc * 2], pgu[fc * 2 + 1]
                for k in range(8):
                    mm(pgt.t[:, 0:HALF], wgb[b].t[:, k, fc * 128:(fc + 1) * 128], xe.t[:, k, cs], k == 0, k == 7, [wgb[b].h, xe.h], [pgt.h])
                for k in range(8):
                    mm(put.t[:, 0:HALF], wub[b].t[:, k, fc * 128:(fc + 1) * 128], xe.t[:, k, cs], k == 0, k == 7, [wub[b].h, xe.h], [put.h])
                sl = sil[fc]
                op("act", lambda e: e.activation(out=sl.t[:], in_=pgt.t[:, 0:HALF], func=AF.Silu), reads=[pgt.h], writes=[sl.h])
                op("dve", lambda e: e.tensor_tensor(out=at.t[:, fc, :], in0=sl.t[:], in1=put.t[:, 0:HALF], op=ALU.mult),
                   reads=[sl.h, put.h], writes=[at.hs[fc]])

            def down(ex, hf):
                b = ex % 2
                at = actT[hf]
                for stl in range(NSH):
                    ys_ = ysb[yi[0] % 6]
                    yi[0] += 1
                    for half in range(2):
                        py = pyy[half]
                        for fc in range(2):
                            mm(py.t[:], at.t[:, fc, stl * 128:(stl + 1) * 128], wdb[b].t[:, fc, half * 512:(half + 1) * 512], fc == 0, fc == 1,
                               [at.hs[fc], wdb[b].h], [py.h])
                        if half == 0:
                            op("act", lambda e, py=py, ys_=ys_: e.copy(out=ys_.t[:, 0:512], in_=py.t[:]), reads=[py.h], writes=[ys_.h])
                        else:
                            op("dve", lambda e, py=py, ys_=ys_: e.tensor_copy(out=ys_.t[:, 512:1024], in_=py.t[:]), reads=[py.h], writes=[ys_.h])
                    row0 = ex * C + hf * HALF + stl * 128
                    store(YS[row0:row0 + 128, :], ys_.t[:], [ys_.h], [dh("YS")])

            stage_load(0)
            stage_load(1)
            for part in range(4):
                conv_part(0, part)
            stage_a(0)
            for ex in range(NE):
                nxt_ex = ex + 1 < NE
                if nxt_ex:
                    stage_a(ex + 1)
                if ex + 2 < NE:
                    stage_load(ex + 2)
                gu(ex, 0, 0)
                if nxt_ex:
                    conv_part(ex + 1, 0)
                gu(ex, 0, 1)
                if nxt_ex:
                    conv_part(ex + 1, 1)
                gu(ex, 1, 0)
                if nxt_ex:
                    conv_part(ex + 1, 2)
                gu(ex, 1, 1)
                if nxt_ex:
                    conv_part(ex + 1, 3)
                down(ex, 0)
                down(ex, 1)
            Sx.barrier()
        if stop_after <= 5:
            return _finish(nc, Sx, out_d, S)

        with ExitStack() as st:
            sb, ps = mk(st)
            onesf6v = sb("onesf6", [128, 128], F32)
            fn_row = sb("fn_row", [1, D], F32)
            GF = sb("GF", [128, D], F32)
            load(onesf6v.t[:], c_onesf, [onesf6v.h])
            load(fn_row.t[:], fnorm_d, [fn_row.h])
            pb = [ps(f"pb{i}", [128, 512], F32) for i in range(2)]
            for half in range(2):
                mm(pb[half].t[:], onesf6v.t[0:1, :], fn_row.t[0:1, half * 512:(half + 1) * 512], True, True, [onesf6v.h, fn_row.h], [pb[half].h])
                op("act", lambda e, half=half: e.copy(out=GF.t[:, half * 512:(half + 1) * 512], in_=pb[half].t[:]), reads=[pb[half].h], writes=[GF.h])
            NB6 = 3
            x1t = [sb(f"x1t{i}", [128, D], F32) for i in range(NB6)]
            y1 = [sb(f"y1_{i}", [128, D], BF16) for i in range(NB6)]
            y2 = [sb(f"y2_{i}", [128, D], BF16) for i in range(NB6)]
            junk6v = sb("junk6", [128, D], F32)
            ssf = [sb(f"ssf{i}", [128, 2], F32) for i in range(2)]
            ot = [sb(f"ot{i}", [128, D], F32) for i in range(2)]
            hout = H()

            def fetch(ti):
                b = ti % NB6
                rows = slice(ti * 128, (ti + 1) * 128)
                load(x1t[b].t[:], X1[rows, :], [x1t[b].h], [dh("X1")])
                for j, yy in enumerate((y1[b], y2[b])):
                    op("pool", lambda e, j=j, yy=yy: e.indirect_dma_start(
                        out=yy.t[:], out_offset=None, in_=YS,
                        in_offset=bass.IndirectOffsetOnAxis(ap=slot_i.t[:, ti, j:j + 1], axis=0)),
                       reads=[dh("YS"), slot_i.h], writes=[yy.h], dma=True)

            def compute(ti):
                b = ti % NB6
                rows = slice(ti * 128, (ti + 1) * 128)
                xx, ya, yb_ = x1t[b], y1[b], y2[b]
                op("dve", lambda e: e.scalar_tensor_tensor(out=xx.t[:], in0=ya.t[:], scalar=wts.t[:, ti, 0:1], in1=xx.t[:], op0=ALU.mult, op1=ALU.add),
                   reads=[ya.h, wts.h, xx.h], writes=[xx.h])
                op("dve", lambda e: e.scalar_tensor_tensor(out=xx.t[:], in0=yb_.t[:], scalar=wts.t[:, ti, 1:2], in1=xx.t[:], op0=ALU.mult, op1=ALU.add),
                   reads=[yb_.h, wts.h, xx.h], writes=[xx.h])
                sf = ssf[ti % 2]
                op("act", lambda e: e.activation(out=junk6v.t[:], in_=xx.t[:], func=AF.Square, accum_out=sf.t[:, 0:1]), reads=[xx.h], writes=[junk6v.h, sf.h])
                op("act", lambda e: e.activation(out=sf.t[:, 1:2], in_=sf.t[:, 0:1], func=AF.Sqrt, scale=1.0 / D, bias=EPS), reads=[sf.h], writes=[sf.h])
                op("dve", lambda e: e.reciprocal(out=sf.t[:, 1:2], in_=sf.t[:, 1:2]), reads=[sf.h], writes=[sf.h])
                o_ = ot[ti % 2]
                op("dve", lambda e: e.scalar_tensor_tensor(out=o_.t[:], in0=xx.t[:], scalar=sf.t[:, 1:2], in1=GF.t[:], op0=ALU.mult, op1=ALU.mult),
                   reads=[xx.h, sf.h, GF.h], writes=[o_.h])
                op("sp", lambda e: e.dma_start(out=out_d[rows, :], in_=o_.t[:]), reads=[o_.h], writes=[hout], dma=True)

            fetch(0)
            if NT > 1:
                fetch(1)
            for ti in range(NT):
                if ti + 2 < NT:
                    fetch(ti + 2)
                compute(ti)
            Sx.barrier()
        return _finish(nc, Sx, out_d, S)


def _finish(nc, Sx, out_d, S):
    Sx.barrier()
    Sx.emit_all()
    return nc


def _consts(S, C):
    bf = ml_dtypes.bfloat16
    c = {}
    c["c_identb"] = np.eye(128, dtype=np.float32).astype(bf)
    c["c_identf"] = np.eye(128, dtype=np.float32)
    c["c_onesb"] = np.ones((128, 128), np.float32).astype(bf)
    c["c_onesf"] = np.ones((128, 128), np.float32)
    pa = np.zeros((96, 96), np.float32)
    for m in range(96):
        if m < 64:
            k = m
        elif m < 80:
            k = m + 16
        else:
            k = m - 16
        pa[k, m] = 1.0
    c["c_perma"] = pa.astype(bf)
    pb = np.zeros((128, 128), np.float32)
    for m in range(128):
        j = m % 64
        if j < 8:
            k = m + 8
        elif j < 16:
            k = m - 8
        else:
            k = m
        pb[k, m] = 1.0
    c["c_permb"] = pb.astype(bf)
    pos = np.arange(S, dtype=np.float64)
    inv = ROPE_THETA ** (-np.arange(16, dtype=np.float64) / 16)
    ang = pos[None, :] * inv[:, None]
    tac = np.ones((96, S), np.float64)
    tas = np.zeros((96, S), np.float64)
    tac[64:80] = np.cos(ang)
    tac[80:96] = np.cos(ang)
    tas[64:80] = -np.sin(ang)
    tas[80:96] = np.sin(ang)
    c["c_tac"] = tac.astype(np.float32)
    c["c_tas"] = tas.astype(np.float32)
    inv = ROPE_THETA ** (-np.arange(8, dtype=np.float64) / 8)
    ang = pos[None, :] * inv[:, None]
    tbc = np.ones((128, S), np.float64)
    tbs = np.zeros((128, S), np.float64)
    for b0 in (0, 64):
        tbc[b0:b0 + 8] = np.cos(ang)
        tbc[b0 + 8:b0 + 16] = np.cos(ang)
        tbs[b0:b0 + 8] = -np.sin(ang)
        tbs[b0 + 8:b0 + 16] = np.sin(ang)
    c["c_tbc"] = tbc.astype(np.float32)
    c["c_tbs"] = tbs.astype(np.float32)
    m = np.zeros((128, 4, 512), np.float32)
    p = np.arange(128)[:, None]
    j = np.arange(512)[None, :]
    for r in range(4):
        m[:, r, :] = (r * 128 + p <= j)
    c["c_mask"] = m.astype(bf)
    oh = np.zeros((32, S), np.float32)
    for n in range(32):
        oh[n, n * 256:(n + 1) * 256] = 1.0
    c["c_oh"] = oh.astype(bf)
    c["c_wneg"] = np.concatenate([np.zeros((1, 32), np.float32), np.full((1, 1), 1e30, np.float32), np.full((1, 31), -1e30, np.float32)], axis=1).astype(bf)
    c["c_offs"] = np.tile((np.arange(32, dtype=np.float32) * C - 1.0)[None, :], (128, 1)).astype(np.float32)
    kk = np.arange(128)[:, None]
    mm_ = np.arange(128)[None, :]
    c["c_triu"] = (kk <= mm_).astype(np.float32).astype(bf)
    return c


def _prep_inputs(inputs, S, C):
    f = lambda a: np.ascontiguousarray(np.asarray(a, dtype=np.float32))
    w_ukv = f(inputs["w_ukv"])[0].reshape(256, 8, 128)
    w_ukv_kv = np.concatenate([w_ukv[:, :, :64].reshape(256, 512), w_ukv[:, :, 64:].reshape(256, 512)], axis=1)
    w_rg = f(inputs["w_router_group"])[0]
    w_re = f(inputs["w_router_expert"])[0]
    w_r = np.concatenate([w_rg] + [w_re[g] for g in range(4)], axis=1)
    b_r = np.concatenate([f(inputs["b_router_group"])[0], f(inputs["b_router_expert"])[0].reshape(32)])[None, :]
    shared = {
        "attn_norm": np.ascontiguousarray(f(inputs["attn_norm"])[0].reshape(8, 128).T),
        "w_in": f(inputs["w_in"])[0],
        "q_norm": np.ascontiguousarray(f(inputs["q_norm"])[0].reshape(6, 128).T),
        "w_uq": f(inputs["w_uq"])[0],
        "kv_norm": np.ascontiguousarray(f(inputs["kv_norm"])[0].reshape(2, 128).T),
        "w_ukv_kv": np.ascontiguousarray(w_ukv_kv),
        "w_o_mla": f(inputs["w_o_mla"])[0],
        "w_o_moba": f(inputs["w_o_moba"])[0],
        "w_out": f(inputs["w_out"])[0],
        "ffn_norm": np.ascontiguousarray(f(inputs["ffn_norm"])[0].reshape(8, 128).T),
        "w_r": np.ascontiguousarray(w_r),
        "b_r": np.ascontiguousarray(b_r),
        "w_exp_gate": f(inputs["w_exp_gate"])[0],
        "w_exp_up": f(inputs["w_exp_up"])[0],
        "w_exp_down": f(inputs["w_exp_down"])[0],
        "final_norm": f(inputs["final_norm"]).reshape(1, D),
    }
    shared.update(_consts(S, C))
    return shared


def kernel(**inputs):
    x = np.asarray(inputs["x"], dtype=np.float32)
    B, S, _ = x.shape
    C = 768 if S >= 8192 else max(256, (S * 2 // 32) * 3 // 128 * 128 + 128)
    nc = build(S=S, C=C)
    shared = _prep_inputs(inputs, S, C)
    in_maps = []
    for b in range(B):
        m = dict(shared)
        m["x"] = np.ascontiguousarray(x[b])
        m["xT"] = np.ascontiguousarray(x[b].T)
        in_maps.append(m)
    res = run_bass_kernel_spmd(nc, in_maps, core_ids=list(range(B)))
    return np.stack([r["out"] for r in res.results], axis=0)
```

```python
import numpy as np
from contextlib import ExitStack
import ml_dtypes
import concourse.bass as bass
import concourse.mybir as mybir
from concourse.bass_utils import run_bass_kernel_spmd

F32 = mybir.dt.float32
BF16 = mybir.dt.bfloat16
I32 = mybir.dt.int32
AF = mybir.ActivationFunctionType
ALU = mybir.AluOpType
AX = mybir.AxisListType

SEM_LIMIT = 30000
D = 1024
NE = 32
EPS = 1e-6
ROPE_THETA = 500000.0


class H:
    __slots__ = ("w", "r")

    def __init__(self):
        self.w = None
        self.r = {}


class Q:
    def __init__(self, name, sems):
        self.name = name
        self.sems = sems
        self.epoch = 0
        self.count = 0
        self.ops = []
        self.waited = {}


class Sched:
    def __init__(self, nc, stack, n_dma_sems=72):
        self.nc = nc
        self.semobj = {}
        self.q = {}
        for name, nep in (("pe", 5), ("act", 3), ("dve", 3), ("pool", 3), ("sp", 1)):
            sems = []
            for e in range(nep):
                s = stack.enter_context(nc.semaphore(f"s_{name}{e}"))
                key = f"{name}{e}"
                self.semobj[key] = s
                sems.append(key)
            self.q[name] = Q(name, sems)
        self.dma_sems = []
        for i in range(n_dma_sems):
            s = stack.enter_context(nc.semaphore(f"s_dma{i}"))
            key = f"dma{i}"
            self.semobj[key] = s
            self.dma_sems.append([key, 0])
        self.dma_rr = 0
        self.dma_rr_q = {}
        self.n_ops = 0

    def _deps(self, reads, writes):
        deps = {}

        def add(k, v):
            if v > deps.get(k, -1):
                deps[k] = v
        for h in reads:
            if h.w is not None:
                add(*h.w)
        for h in writes:
            if h.w is not None:
                add(*h.w)
            for k, v in h.r.items():
                add(k, v)
        return deps

    def op(self, qname, fn, reads=(), writes=(), dma=False):
        q = self.q[qname]
        deps = self._deps(reads, writes)
        if dma:
            third = len(self.dma_sems) // 3
            base = {"sp": 0, "pool": third, "act": 2 * third}[qname]
            rr = self.dma_rr_q.get(qname, 0)
            slot = self.dma_sems[base + rr]
            self.dma_rr_q[qname] = (rr + 1) % third
            if slot[1] > 0 and slot[1] > deps.get(slot[0], -1):
                deps[slot[0]] = slot[1]
            assert slot[1] + 16 < 60000
            slot[1] += 16
            comp = (slot[0], slot[1])
            inc = 16
        else:
            if q.count + 1 > SEM_LIMIT:
                q.epoch += 1
                q.count = 0
            q.count += 1
            comp = (q.sems[q.epoch], q.count)
            inc = 1
        waits = []
        for k, v in deps.items():
            if qname == "pe" and k.startswith("pe"):
                continue
            if q.waited.get(k, -1) >= v:
                continue
            q.waited[k] = v
            waits.append((self.semobj[k], v))
        csem = self.semobj[comp[0]]

        def emit(eng, waits=waits, fn=fn, csem=csem, inc=inc):
            for s, v in waits:
                eng.wait_ge(s, v)
            fn(eng).then_inc(csem, inc)
        q.ops.append(emit)
        for h in writes:
            h.w = comp
            h.r = {}
        for h in reads:
            if comp[1] > h.r.get(comp[0], -1):
                h.r[comp[0]] = comp[1]
        self.n_ops += 1
        return comp

    def barrier(self):
        deps = {}
        for q in self.q.values():
            for e in range(q.epoch + 1):
                cnt = q.count if e == q.epoch else SEM_LIMIT
                if cnt > 0:
                    deps[q.sems[e]] = cnt
        for k, v in self.dma_sems:
            if v > 0:
                deps[k] = v
        for q in self.q.values():
            waits = []
            for k, v in deps.items():
                if q.waited.get(k, -1) >= v:
                    continue
                q.waited[k] = v
                waits.append((self.semobj[k], v))

            def emit(eng, waits=waits):
                for s, v in waits:
                    eng.wait_ge(s, v)
            q.ops.append(emit)

    def emit_all(self):
        nc = self.nc
        with nc.Block() as block:
            @block.tensor
            def _(e):
                for f in self.q["pe"].ops:
                    f(e)

            @block.scalar
            def _(e):
                for f in self.q["act"].ops:
                    f(e)

            @block.vector
            def _(e):
                for f in self.q["dve"].ops:
                    f(e)

            @block.gpsimd
            def _(e):
                for f in self.q["pool"].ops:
                    f(e)

            @block.sync
            def _(e):
                for f in self.q["sp"].ops:
                    f(e)


class T:
    def __init__(self, t, n=1):
        self.t = t
        self.h = H()
        self.hs = [H() for _ in range(n)]


def build(S=8192, C=768, stop_after=99, debug=False):
    NG = S // 512
    NT = S // 128
    NSLOT = NE * C
    nc = bass.Bass("TRN2", target_bir_lowering=False)

    def din(name, shape, dt=F32):
        return nc.dram_tensor(name, shape, dt, kind="ExternalInput").ap()

    def dscr(name, shape, dt):
        return nc.dram_tensor(name, shape, dt, kind=("ExternalOutput" if debug else "Internal")).ap()

    x_d = din("x", [S, D])
    xT_d = din("xT", [D, S])
    attn_norm_d = din("attn_norm", [128, 8])
    w_in_d = din("w_in", [D, 4640])
    q_norm_d = din("q_norm", [128, 6])
    w_uq_d = din("w_uq", [768, 768])
    kv_norm_d = din("kv_norm", [128, 2])
    w_ukv_d = din("w_ukv_kv", [256, 1024])
    w_oa_d = din("w_o_mla", [512, D])
    w_ob_d = din("w_o_moba", [512, D])
    w_out_d = din("w_out", [D, D])
    ffn_norm_d = din("ffn_norm", [128, 8])
    w_r_d = din("w_r", [D, 36])
    b_r_d = din("b_r", [1, 36])
    w_g_d = din("w_exp_gate", [NE, D, 256])
    w_u_d = din("w_exp_up", [NE, D, 256])
    w_d_d = din("w_exp_down", [NE, 256, D])
    fnorm_d = din("final_norm", [1, D])
    c_identb = din("c_identb", [128, 128], BF16)
    c_identf = din("c_identf", [128, 128])
    c_onesb = din("c_onesb", [128, 128], BF16)
    c_onesf = din("c_onesf", [128, 128])
    c_perma = din("c_perma", [96, 96], BF16)
    c_permb = din("c_permb", [128, 128], BF16)
    c_tac = din("c_tac", [96, S])
    c_tas = din("c_tas", [96, S])
    c_tbc = din("c_tbc", [128, S])
    c_tbs = din("c_tbs", [128, S])
    c_mask = din("c_mask", [128, 4, 512], BF16)
    c_oh = din("c_oh", [32, S], BF16)
    c_wneg = din("c_wneg", [1, 64], BF16)
    c_offs = din("c_offs", [128, 32])
    c_triu = din("c_triu", [128, 128], BF16)

    out_d = nc.dram_tensor("out", [S, D], F32, kind="ExternalOutput").ap()

    WIN = dscr("WIN", [D, 4640], BF16)
    WUQ = dscr("WUQ", [768, 768], BF16)
    WUKV = dscr("WUKV", [256, 1024], BF16)
    WOA = dscr("WOA", [512, D], BF16)
    WOB = dscr("WOB", [512, D], BF16)
    WOUT = dscr("WOUT", [D, D], BF16)
    HT = dscr("HT", [D, S], BF16)
    QA = dscr("QA", [768, S], BF16)
    KNA = dscr("KNA", [512, S], BF16)
    KRA = dscr("KRA", [32, S], BF16)
    VA = dscr("VA", [S, 512], BF16)
    QB = dscr("QB", [512, S], BF16)
    KB = dscr("KB", [512, S], BF16)
    VB = dscr("VB", [S, 512], BF16)
    MB = dscr("MB", [256, S], BF16)
    OA = dscr("OA", [512, S], BF16)
    OB = dscr("OB", [512, S], BF16)
    X1 = dscr("X1", [S, D], F32)
    XS = dscr("XS", [NSLOT, D], BF16)
    YS = dscr("YS", [NSLOT, D], BF16)

    with ExitStack() as top:
        Sx = Sched(nc, top)
        op = Sx.op

        def mk(st):
            def sb(name, shape, dt, n=1):
                return T(st.enter_context(nc.sbuf_tensor(name, shape, dt)), n)

            def ps(name, shape, dt):
                return T(st.enter_context(nc.psum_tensor(name, shape, dt)))
            return sb, ps

        def load(dst_ap, src_ap, wr, rd=()):
            op("sp", lambda e: e.dma_start(out=dst_ap, in_=src_ap), reads=rd, writes=wr, dma=True)

        store_q = ["pool"]

        def store(dst_ap, src_ap, rd, wr):
            op(store_q[0], lambda e: e.dma_start(out=dst_ap, in_=src_ap), reads=rd, writes=wr, dma=True)

        def mm(out_ap, lhsT, rhs, start, stop, rd, wr):
            op("pe", lambda e: e.matmul(out_ap, lhsT, rhs, start=start, stop=stop), reads=rd, writes=wr)

        hd = {}

        def dh(name):
            if name not in hd:
                hd[name] = H()
            return hd[name]

        sbp, _ = mk(top)
        slot_i = sbp("slot_i", [128, NT, 2], I32, 4)
        wts = sbp("wts", [128, NT, 2], F32, 4)

        with ExitStack() as st:
            sb, ps = mk(st)
            stg = [sb(f"stg{i}", [128, 8, 512], F32) for i in range(4)]
            obf = [sb(f"obf{i}", [128, 8, 512], BF16) for i in range(4)]
            gains = sb("gains", [128, 3, 8], F32)
            load(gains.t[:, 0, :], attn_norm_d, [gains.hs[0]])
            load(gains.t[:, 1, 0:6], q_norm_d, [gains.hs[0]])
            load(gains.t[:, 2, 0:2], kv_norm_d, [gains.hs[0]])
            cnt = [0]

            def conv(src, dst, dname, nk, ncols, gi, cstart=0):
                srcv = src.rearrange("(k p) c -> p k c", p=128)
                dstv = dst.rearrange("(k p) c -> p k c", p=128)
                for c0 in range(cstart, ncols, 512):
                    cw = min(512, ncols - c0)
                    i = cnt[0] % 4
                    cnt[0] += 1
                    a, b = stg[i], obf[i]
                    load(a.t[:, 0:nk, 0:cw], srcv[:, :, c0:c0 + cw], [a.h])
                    for k in range(nk):
                        if gi is None:
                            if k % 2 == 0:
                                op("act", lambda e, a=a, b=b, k=k, cw=cw: e.copy(out=b.t[:, k, 0:cw], in_=a.t[:, k, 0:cw]),
                                   reads=[a.h], writes=[b.hs[0]] if False else [b.h])
                            else:
                                op("dve", lambda e, a=a, b=b, k=k, cw=cw: e.tensor_copy(out=b.t[:, k, 0:cw], in_=a.t[:, k, 0:cw]),
                                   reads=[a.h], writes=[b.h])
                        else:
                            if k % 2 == 0:
                                op("act", lambda e, a=a, b=b, k=k, cw=cw, gi=gi: e.activation(out=b.t[:, k, 0:cw], in_=a.t[:, k, 0:cw], func=AF.Copy, scale=gains.t[:, gi, k:k + 1]),
                                   reads=[a.h, gains.hs[0]], writes=[b.h])
                            else:
                                op("dve", lambda e, a=a, b=b, k=k, cw=cw, gi=gi: e.tensor_scalar_mul(out=b.t[:, k, 0:cw], in0=a.t[:, k, 0:cw], scalar1=gains.t[:, gi, k:k + 1]),
                                   reads=[a.h, gains.hs[0]], writes=[b.h])
                    store(dstv[:, :, c0:c0 + cw], b.t[:, 0:nk, 0:cw], [b.h], [dh(dname)])

            conv(w_in_d, WIN, "WIN", 8, 2592, 0)
            conv(w_uq_d, WUQ, "WUQ", 6, 768, 1)
            conv(w_ukv_d, WUKV, "WUKV", 2, 1024, 2)
            Sx.barrier()
        if stop_after <= 0:
            return _finish(nc, Sx, out_d, S)

        with ExitStack() as st:
            sb, ps = mk(st)
            onesb = sb("onesb", [128, 128], BF16)
            identb = sb("identb", [128, 128], BF16)
            perma = sb("perma", [96, 96], BF16)
            permb = sb("permb", [128, 128], BF16)
            wneg = sb("wneg", [1, 64], BF16)
            load(onesb.t[:], c_onesb, [onesb.h])
            load(identb.t[:], c_identb, [identb.h])
            load(perma.t[:], c_perma, [perma.h])
            load(permb.t[:], c_permb, [permb.h])
            load(wneg.t[:], c_wneg, [wneg.h])
            store_q[0] = "sp"
            NCW = 2592
            win = sb("win", [128, 8, NCW], BF16)
            wuq = sb("wuq", [128, 6, 768], BF16)
            wukv = sb("wukv", [128, 2, 1024], BF16)
            winv = WIN.rearrange("(k p) c -> p k c", p=128)
            for c0 in range(0, NCW, 648):
                load(win.t[:, :, c0:c0 + 648], winv[:, :, c0:c0 + 648], [win.h], [dh("WIN")])
            load(wuq.t[:], WUQ.rearrange("(k p) c -> p k c", p=128), [wuq.h], [dh("WUQ")])
            load(wukv.t[:], WUKV.rearrange("(k p) c -> p k c", p=128), [wukv.h], [dh("WUKV")])
            km = sb("km", [128, 4, 32], BF16)
            op("dve", lambda e: e.memset(km.t[:], 0.0), writes=[km.h])

            xt = [sb(f"xt{i}", [128, 8, 512], F32) for i in range(2)]
            xsq = sb("xsq", [128, 8, 512], BF16)
            hTs = [sb(f"hT{i}", [128, 8, 512], BF16, 8) for i in range(2)]
            rss = [sb(f"rs{i}", [128, 512], F32) for i in range(2)]
            tac = sb("tac", [96, 512], F32)
            tas = sb("tas", [96, 512], F32)
            tbc = sb("tbc", [128, 512], F32)
            tbs = sb("tbs", [128, 512], F32)
            tac2 = sb("tac2", [96, 512], F32)
            tas2 = sb("tas2", [96, 512], F32)
            tbc2 = sb("tbc2", [128, 512], F32)
            tbs2 = sb("tbs2", [128, 512], F32)
            cq = sb("cq", [128, 6, 512], BF16, 6)
            cqsq = sb("cqsq", [128, 6, 512], BF16, 6)
            cqn = sb("cqn", [128, 6, 512], BF16, 6)
            ckv = sb("ckv", [128, 2, 512], BF16, 2)
            ckvsq = sb("ckvsq", [128, 2, 512], BF16, 2)
            ckvn = sb("ckvn", [128, 2, 512], BF16, 2)
            rsq = sb("rsq", [128, 512], F32)
            rskv = sb("rskv", [128, 512], F32)
            NR = 3
            rsb = [sb(f"rsb{i}", [128, 512], BF16) for i in range(NR)]
            t1 = [sb(f"t1_{i}", [128, 512], F32) for i in range(NR)]
            t2 = [sb(f"t2_{i}", [128, 512], F32) for i in range(NR)]
            ro = [sb(f"ro{i}", [128, 512], BF16) for i in range(NR)]
            qbo = [sb(f"qbo{i}", [128, 512], BF16) for i in range(4)]
            evo = [sb(f"evo{i}", [128, 512], BF16) for i in range(6)]
            kms = sb("kms", [128, 2], F32)
            gsbs = [sb(f"gsb{i}", [128, 256], F32) for i in range(4)]
            t8s = [sb(f"t8_{i}", [128, 8, 8], F32, 8) for i in range(4)]
            thrs = [sb(f"thr{i}", [128, 8], F32) for i in range(4)]
            mkfs = [sb(f"mk_f{i}", [128, 256], F32, 8) for i in range(4)]
            mkbs = [sb(f"mkb{i}", [128, 256], BF16) for i in range(4)]
            mT = sb("mT", [128, 2, 512], BF16, 2)
            pm = [ps(f"pm{i}", [128, 512], F32) for i in range(3)]
            pp = [ps(f"pp{i}", [128, 512], F32) for i in range(1)]
            pstat = ps("pstat", [128, 512], F32)
            pgs = [ps(f"pg{i}", [128, 512], F32) for i in range(2)]
            ptr = ps("ptr", [128, 1024], BF16)
            ctr = {"pm": 0, "pp": 0, "r": 0, "ev": 0}

            def nxt(key, n):
                v = ctr[key] % n
                ctr[key] += 1
                return v

            def rope(src_ps, rows, perm, tc_, ts_, out_t=None, after=None):
                i = nxt("r", NR)
                a, b1, b2, o = rsb[i], t1[i], t2[i], (out_t or ro[i])
                op("act", lambda e: e.copy(out=a.t[0:rows, :], in_=src_ps.t[0:rows, :]), reads=[src_ps.h], writes=[a.h])

                def fin():
                    p2 = pp[nxt("pp", 1)]
                    mm(p2.t[0:rows, :], perm.t[0:rows, 0:rows], a.t[0:rows, :], True, True, [perm.h, a.h], [p2.h])
                    op("dve", lambda e: e.tensor_tensor(out=b1.t[0:rows, :], in0=p2.t[0:rows, :], in1=ts_.t[0:rows, :], op=ALU.mult),
                       reads=[p2.h, ts_.h], writes=[b1.h])
                    op("pool", lambda e: e.tensor_tensor(out=b2.t[0:rows, :], in0=a.t[0:rows, :], in1=tc_.t[0:rows, :], op=ALU.mult),
                       reads=[a.h, tc_.h], writes=[b2.h])
                    op("dve", lambda e: e.tensor_tensor(out=o.t[0:rows, :], in0=b1.t[0:rows, :], in1=b2.t[0:rows, :], op=ALU.add),
                       reads=[b1.h, b2.h], writes=[o.h])
                    if after is not None:
                        after(o)
                return fin

            def rstd_from(pst, dst, n):
                op("act", lambda e: e.activation(out=dst.t[:], in_=pst.t[:], func=AF.Ln, scale=1.0 / n, bias=EPS),
                   reads=[pst.h], writes=[dst.h])
                op("act", lambda e: e.activation(out=dst.t[:], in_=dst.t[:], func=AF.Exp, scale=-0.5), reads=[dst.h], writes=[dst.h])

            xTv = xT_d.rearrange("(k p) s -> p k s", p=128)
            HTv = HT.rearrange("(k p) s -> p k s", p=128)
            dq = []

            def defer(fn):
                dq.append(fn)
                while len(dq) > 1:
                    dq.pop(0)()

            def flush():
                while dq:
                    dq.pop(0)()

            gate_pend = []
            tabs = [(tac, tas, tbc, tbs), (tac2, tas2, tbc2, tbs2)]

            sq_done = {}

            def S1a(g):
                x_ = xt[g % 2]
                sq_done[g] = True
                op("act", lambda e: e.activation(out=xsq.t[:], in_=x_.t[:], func=AF.Square), reads=[x_.h], writes=[xsq.h])

            def S1(g):
                tok = slice(g * 512, (g + 1) * 512)
                x_, hT, rs = xt[g % 2], hTs[g % 2], rss[g % 2]
                ta_c, ta_s, tb_c, tb_s = tabs[g % 2]
                if not sq_done.get(g):
                    S1a(g)
                for k in range(8):
                    mm(pstat.t[:], onesb.t[:], xsq.t[:, k, :], k == 0, k == 7, [onesb.h, xsq.h], [pstat.h])
                rstd_from(pstat, rs, 1024.0)
                for k in range(8):
                    op("dve", lambda e, k=k: e.tensor_tensor(out=hT.t[:, k, :], in0=x_.t[:, k, :], in1=rs.t[:], op=ALU.mult),
                       reads=[x_.h, rs.h], writes=[hT.hs[k]])
                store(HTv[:, :, tok], hT.t[:], list(hT.hs), [dh("HT")])

            def S1_load(g):
                tok = slice(g * 512, (g + 1) * 512)
                x_ = xt[g % 2]
                ta_c, ta_s, tb_c, tb_s = tabs[g % 2]
                for dst, src in ((x_, xTv[:, :, tok]), (ta_c, c_tac[:, tok]), (ta_s, c_tas[:, tok]), (tb_c, c_tbc[:, tok]), (tb_s, c_tbs[:, tok])):
                    op("act", lambda e, dst=dst, src=src: e.dma_start(out=dst.t[:], in_=src), writes=[dst.h], dma=True)

            S1_load(0)
            S1(0)
            for g in range(NG):
                tok = slice(g * 512, (g + 1) * 512)
                if g + 1 < NG:
                    S1_load(g + 1)
                hT = hTs[g % 2]
                tac, tas, tbc, tbs = tabs[g % 2]
                for (dst, dsq, nchunk, col0) in ((cq, cqsq, 6, 0), (ckv, ckvsq, 2, 768)):
                    for c in range(nchunk):
                        p = pm[nxt("pm", 3)]
                        for k in range(8):
                            mm(p.t[:], win.t[:, k, col0 + c * 128: col0 + (c + 1) * 128], hT.t[:, k, :], k == 0, k == 7, [win.h, hT.hs[k]], [p.h])
                        op("act", lambda e, p=p, dst=dst, c=c: e.copy(out=dst.t[:, c, :], in_=p.t[:]), reads=[p.h], writes=[dst.hs[c]])
                        op("dve", lambda e, dst=dst, dsq=dsq, c=c: e.tensor_tensor(out=dsq.t[:, c, :], in0=dst.t[:, c, :], in1=dst.t[:, c, :], op=ALU.mult),
                           reads=[dst.hs[c]], writes=[dsq.hs[c]])

                def after_q(j):
                    def f(o):
                        store(QB[j * 128:(j + 1) * 128, tok], o.t[:], [o.h], [dh("QB")])
                    return f

                def after_k(j, g=g):
                    def f(o):
                        store(KB[j * 128:(j + 1) * 128, tok], o.t[:], [o.h], [dh("KB")])
                        op("dve", lambda e: e.tensor_reduce(out=kms.t[:], in_=o.t[:].rearrange("p (b t) -> p b t", t=256), axis=AX.X, op=ALU.add),
                           reads=[o.h], writes=[kms.h])
                        op("act", lambda e: e.activation(out=km.t[:, j, 2 * g:2 * g + 2], in_=kms.t[:], func=AF.Copy, scale=1.0 / 256),
                           reads=[kms.h], writes=[km.h])
                    return f

                for which, col0 in (("q", 1056), ("k", 1568)):
                    for j in range(4):
                        p = pm[nxt("pm", 3)]
                        for k in range(8):
                            mm(p.t[:], win.t[:, k, col0 + j * 128: col0 + (j + 1) * 128], hT.t[:, k, :], k == 0, k == 7, [win.h, hT.hs[k]], [p.h])
                        if which == "q":
                            defer(rope(p, 128, permb, tbc, tbs, out_t=qbo[j], after=after_q(j)))
                        else:
                            defer(rope(p, 128, permb, tbc, tbs, after=after_k(j)))
                while gate_pend:
                    gate_pend.pop(0)()
                if g + 1 < NG:
                    S1a(g + 1)
                for (dst, dsq, dn, nchunk, rr, nn) in ((cq, cqsq, cqn, 6, rsq, 768.0), (ckv, ckvsq, ckvn, 2, rskv, 256.0)):
                    for c in range(nchunk):
                        mm(pstat.t[:], onesb.t[:], dsq.t[:, c, :], c == 0, c == nchunk - 1, [onesb.h, dsq.hs[c]], [pstat.h])
                    rstd_from(pstat, rr, nn)
                    for c in range(nchunk):
                        op("dve", lambda e, dst=dst, dn=dn, c=c, rr=rr: e.tensor_tensor(out=dn.t[:, c, :], in0=dst.t[:, c, :], in1=rr.t[:], op=ALU.mult),
                           reads=[dst.hs[c], rr.h], writes=[dn.hs[c]])
                p = pm[nxt("pm", 3)]
                for k in range(8):
                    mm(p.t[0:96, :], win.t[:, k, 960:1056], hT.t[:, k, :], k == 0, k == 7, [win.h, hT.hs[k]], [p.h])
                defer(rope(p, 96, perma, tac, tas, after=lambda o: store(KRA[:, tok], o.t[64:96, :], [o.h], [dh("KRA")])))
                for tt in range(4):
                    p = pm[nxt("pm", 3)]
                    for k in range(8):
                        mm(p.t[:], hT.t[:, k, tt * 128:(tt + 1) * 128], win.t[:, k, 2080:2592], k == 0, k == 7, [win.h, hT.hs[k]], [p.h])
                    o = evo[nxt("ev", 6)]
                    op("act", lambda e, p=p, o=o: e.copy(out=o.t[:], in_=p.t[:]), reads=[p.h], writes=[o.h])
                    store(VB[g * 512 + tt * 128: g * 512 + (tt + 1) * 128, :], o.t[:], [o.h], [dh("VB")])
                for h in range(8):
                    p = pm[nxt("pm", 3)]
                    for c in range(6):
                        mm(p.t[0:96, :], wuq.t[:, c, h * 96:(h + 1) * 96], cqn.t[:, c, :], c == 0, c == 5, [wuq.h, cqn.hs[c]], [p.h])
                    defer(rope(p, 96, perma, tac, tas, after=(lambda h: (lambda o: store(QA[h * 96:(h + 1) * 96, tok], o.t[0:96, :], [o.h], [dh("QA")])))(h)))
                for j in range(4):
                    p = pm[nxt("pm", 3)]
                    for c in range(2):
                        mm(p.t[:], wukv.t[:, c, j * 128:(j + 1) * 128], ckvn.t[:, c, :], c == 0, c == 1, [wukv.h, ckvn.hs[c]], [p.h])
                    o = evo[nxt("ev", 6)]
                    op("act", lambda e, p=p, o=o: e.copy(out=o.t[:], in_=p.t[:]), reads=[p.h], writes=[o.h])
                    store(KNA[j * 128:(j + 1) * 128, tok], o.t[:], [o.h], [dh("KNA")])
                for tt in range(4):
                    p = pm[nxt("pm", 3)]
                    for c in range(2):
                        mm(p.t[:], ckvn.t[:, c, tt * 128:(tt + 1) * 128], wukv.t[:, c, 512:1024], c == 0, c == 1, [wukv.h, ckvn.hs[c]], [p.h])
                    o = evo[nxt("ev", 6)]
                    op("act", lambda e, p=p, o=o: e.copy(out=o.t[:], in_=p.t[:]), reads=[p.h], writes=[o.h])
                    store(VA[g * 512 + tt * 128: g * 512 + (tt + 1) * 128, :], o.t[:], [o.h], [dh("VA")])
                if g + 1 < NG:
                    S1(g + 1)
                flush()
                for tt in range(4):
                    qt = g * 4 + tt
                    own = qt // 2
                    pg_ = pgs[tt % 2]
                    gsb_ = gsbs[tt]
                    for h in range(8):
                        j, r0 = h // 2, (h % 2) * 64
                        mm(pg_.t[:, h * 32:(h + 1) * 32], qbo[j].t[r0:r0 + 64, tt * 128:(tt + 1) * 128], km.t[r0:r0 + 64, j, :], True, False,
                           [qbo[j].h, km.h], [pg_.h])
                        mm(pg_.t[:, h * 32:(h + 1) * 32], onesb.t[0:1, :], wneg.t[0:1, 32 - own:64 - own], False, True,
                           [onesb.h, wneg.h], [pg_.h])
                    op("act", lambda e, pg_=pg_, gsb_=gsb_: e.copy(out=gsb_.t[:], in_=pg_.t[:, 0:256]), reads=[pg_.h], writes=[gsb_.h])
                for h in range(8):
                    for tt in range(4):
                        gsb_, t8_ = gsbs[tt], t8s[tt]
                        op("dve", lambda e, h=h, gsb_=gsb_, t8_=t8_: e.max(out=t8_.t[:, h, :], in_=gsb_.t[:, h * 32:(h + 1) * 32]), reads=[gsb_.h], writes=[t8_.hs[h]])
                for tt in range(4):
                    t8_, thr_ = t8s[tt], thrs[tt]
                    op("dve", lambda e, t8_=t8_, thr_=thr_: e.tensor_scalar_max(out=thr_.t[:], in0=t8_.t[:, :, 3], scalar1=-1e29), reads=list(t8_.hs), writes=[thr_.h])
                for h in range(8):
                    for tt in range(4):
                        gsb_, thr_, mkf_ = gsbs[tt], thrs[tt], mkfs[tt]
                        op("dve", lambda e, h=h, gsb_=gsb_, thr_=thr_, mkf_=mkf_: e.tensor_scalar(out=mkf_.t[:, h * 32:(h + 1) * 32], in0=gsb_.t[:, h * 32:(h + 1) * 32],
                                                                 scalar1=thr_.t[:, h:h + 1], scalar2=30000.0, op0=ALU.is_ge, op1=ALU.mult),
                           reads=[gsb_.h, thr_.h], writes=[mkf_.hs[h]])
                for tt in range(4):
                    mkf_, mkb_ = mkfs[tt], mkbs[tt]
                    op("dve", lambda e, mkf_=mkf_, mkb_=mkb_: e.tensor_scalar_add(out=mkb_.t[:], in0=mkf_.t[:], scalar1=-30000.0), reads=list(mkf_.hs), writes=[mkb_.h])

                def gate_fin(g=g, tok=tok):
                    for tt in range(4):
                        mkb_ = mkbs[tt]
                        for half in range(2):
                            op("pe", lambda e, half=half, mkb_=mkb_: e.transpose(out=ptr.t[:, half * 128:(half + 1) * 128], in_=mkb_.t[:, half * 128:(half + 1) * 128], identity=identb.t[:]),
                               reads=[mkb_.h, identb.h], writes=[ptr.h])
                        op("act", lambda e, tt=tt: e.copy(out=mT.t[:, :, tt * 128:(tt + 1) * 128], in_=ptr.t[:, 0:256].rearrange("p (a b) -> p a b", a=2)),
                           reads=[ptr.h], writes=[mT.h])
                    for half in range(2):
                        store(MB[half * 128:(half + 1) * 128, tok], mT.t[:, half, :], [mT.h], [dh("MB")])
                gate_pend.append(gate_fin)
            while gate_pend:
                gate_pend.pop(0)()
            store_q[0] = "pool"
            Sx.barrier()
        if stop_after <= 1:
            return _finish(nc, Sx, out_d, S)

        with ExitStack() as st:
            sb, ps = mk(st)
            maskc = sb("maskc", [128, 2, 1024], BF16)
            load(maskc.t[:], c_mask.rearrange("p (a b) c -> p a (b c)", a=2), [maskc.h])
            QT = [sb(f"QT{i}", [96, S], BF16) for i in range(2)]
            KT = [sb(f"KT{i}", [96, S], BF16) for i in range(2)]
            VV = [sb(f"VV{i}", [128, NT, 128], BF16) for i in range(2)]
            for v in VV:
                op("dve", lambda e, v=v: e.memset(v.t[:, :, 64:128], 1.0), writes=[v.h])
            NP = 4
            PT = [sb(f"PT{i}", [128, 1024], BF16) for i in range(NP)]
            rcp = [sb(f"rcp{i}", [128, 512], F32) for i in range(2)]
            rc0 = [sb(f"rc0{i}", [64, 512], F32) for i in range(2)]
            onb = [sb(f"onb{i}", [64, 512], BF16) for i in range(2)]
            psc = [ps(f"psc{i}", [128, 1024], F32) for i in range(3)]
            pov = [ps(f"pov{i}", [128, 512], F32) for i in range(2)]
            VAv = VA.rearrange("(n p) f -> p n f", p=128)
            VBv = VB.rearrange("(n p) f -> p n f", p=128)
            stg3 = [sb(f"stg3{i}", [128, 8, 512], F32) for i in range(2)]
            obf3 = [sb(f"obf3{i}", [128, 8, 512], BF16) for i in range(2)]
            gain3 = sb("gain3", [128, 8], F32)
            load(gain3.t[:], attn_norm_d, [gain3.h])
            cnt3 = [0]
            c3q = []

            def conv3(src, dst, dname, nk, ncols, use_gain, cstart=0):
                srcv = src.rearrange("(k p) c -> p k c", p=128)
                dstv = dst.rearrange("(k p) c -> p k c", p=128)
                for c0 in range(cstart, ncols, 512):
                    cw = min(512, ncols - c0)
                    i = cnt3[0] % 2
                    cnt3[0] += 1
                    a_, b_ = stg3[i], obf3[i]
                    c3q.append(lambda a_=a_, c0=c0, cw=cw: load(a_.t[:, 0:nk, 0:cw], srcv[:, :, c0:c0 + cw], [a_.h]))
                    for k in range(nk):
                        if use_gain:
                            c3q.append(lambda a_=a_, b_=b_, k=k, cw=cw: op("pool", lambda e: e.tensor_scalar_mul(out=b_.t[:, k, 0:cw], in0=a_.t[:, k, 0:cw], scalar1=gain3.t[:, k:k + 1]),
                                                                      reads=[a_.h, gain3.h], writes=[b_.h]))
                        else:
                            c3q.append(lambda a_=a_, b_=b_, k=k, cw=cw: op("pool", lambda e: e.tensor_copy(out=b_.t[:, k, 0:cw], in_=a_.t[:, k, 0:cw]),
                                                                      reads=[a_.h], writes=[b_.h]))
                    c3q.append(lambda b_=b_, c0=c0, cw=cw: store(dstv[:, :, c0:c0 + cw], b_.t[:, 0:nk, 0:cw], [b_.h], [dh(dname)]))

            LA = 2
            pend = []
            state = {"ui": 0, "gi": 0}

            def emit_pair(q_, k_, v_, sc, g, kp, nkp, odst, on, h):
                ui = state["ui"]
                state["ui"] += 1
                pscore = psc[ui % 3]
                pt = PT[ui % NP]
                r = 2 * kp - 4 * g
                c0s = [((r + j) * 128 if r >= 0 else 0) for j in range(2)]
                for j in range(2):
                    kt = 2 * kp + j
                    c0 = c0s[j]
                    mm(pscore.t[:, j * 512 + c0:(j + 1) * 512], k_.t[0:96, kt * 128:(kt + 1) * 128], q_.t[0:96, g * 512 + c0:(g + 1) * 512], True, True,
                       [k_.h, q_.h], [pscore.h])
                if r == 2:
                    for j in range(2):
                        c0 = c0s[j]
                        op("act", lambda e, j=j, c0=c0: e.activation(out=pt.t[:, j * 512 + c0:(j + 1) * 512], in_=pscore.t[:, j * 512 + c0:(j + 1) * 512], func=AF.Exp, scale=sc),
                           reads=[pscore.h], writes=[pt.h])
                else:
                    op("act", lambda e: e.activation(out=pt.t[:], in_=pscore.t[:], func=AF.Exp, scale=sc), reads=[pscore.h], writes=[pt.h])
                if r >= 0:
                    for j in range(2):
                        c0 = c0s[j]
                        op("dve", lambda e, j=j, c0=c0: e.tensor_tensor(out=pt.t[:, j * 512 + c0:j * 512 + c0 + 128], in0=pt.t[:, j * 512 + c0:j * 512 + c0 + 128],
                                                                      in1=maskc.t[:, r // 2, j * 512 + c0:j * 512 + c0 + 128], op=ALU.mult),
                           reads=[pt.h, maskc.h], writes=[pt.h])
                first, last = (kp == 0), (kp == nkp - 1)
                if first:
                    state["gi"] += 1
                gi = state["gi"]
                po = pov[gi % 2]

                def pv():
                    for j in range(2):
                        kt = 2 * kp + j
                        c0 = c0s[j]
                        mm(po.t[:, c0:512], v_.t[:, kt, :], pt.t[:, j * 512 + c0:(j + 1) * 512], first and j == 0, last and j == 1, [v_.h, pt.h], [po.h])
                    if last:
                        rc, r0, onb_ = rcp[gi % 2], rc0[gi % 2], onb[gi % 2]
                        op("dve", lambda e: e.reciprocal(out=rc.t[64:128, :], in_=po.t[64:128, :]), reads=[po.h], writes=[rc.h])
                        op("dve", lambda e: e.tensor_copy(out=r0.t[0:64, :], in_=rc.t[64:128, :]), reads=[rc.h], writes=[r0.h])
                        op("dve", lambda e: e.tensor_tensor(out=onb_.t[:], in0=po.t[0:64, :], in1=r0.t[0:64, :], op=ALU.mult),
                           reads=[po.h, r0.h], writes=[onb_.h])
                        store(odst[h * 64:(h + 1) * 64, g * 512:(g + 1) * 512], onb_.t[:], [onb_.h], [dh(on)])
                return pv

            for hp in range(16):
                typ, h = hp // 8, hp % 8
                q_, k_, v_ = QT[hp % 2], KT[hp % 2], VV[hp % 2]
                if typ == 0:
                    load(q_.t[0:96, :], QA[h * 96:(h + 1) * 96, :], [q_.h], [dh("QA")])
                    load(k_.t[0:64, :], KNA[h * 64:(h + 1) * 64, :], [k_.h], [dh("KNA")])
                    load(k_.t[64:96, :], KRA[:, :], [k_.h], [dh("KRA")])
                    vsrc, vn, sc, odst, on = VAv, "VA", 96.0 ** -0.5, OA, "OA"
                else:
                    load(q_.t[0:64, :], QB[h * 64:(h + 1) * 64, :], [q_.h], [dh("QB")])
                    load(q_.t[64:96, :], MB[h * 32:(h + 1) * 32, :], [q_.h], [dh("MB")])
                    load(k_.t[0:64, :], KB[h * 64:(h + 1) * 64, :], [k_.h], [dh("KB")])
                    load(k_.t[64:96, :], c_oh[:, :], [k_.h])
                    vsrc, vn, sc, odst, on = VBv, "VB", 64.0 ** -0.5, OB, "OB"
                vstep = max(1, NT // 4)
                for n0 in range(0, NT, vstep):
                    load(v_.t[:, n0:n0 + vstep, 0:64], vsrc[:, n0:n0 + vstep, h * 64:(h + 1) * 64], [v_.h], [dh(vn)])
                if hp == 3:
                    zt = sb("zt", [128, 4, D], BF16)
                    op("pool", lambda e: e.memset(zt.t[:], 0.0), writes=[zt.h])
                    XSv = XS.rearrange("(n p) d -> p n d", p=128)
                    zlist = list(range(0, NSLOT // 128, 4))
                if hp >= 3:
                    nz = -(-len(zlist) // 12) if hp < 15 else len(zlist)
                    for _ in range(min(nz, len(zlist))):
                        n0 = zlist.pop(0)
                        store(XSv[:, n0:n0 + 4, :], zt.t[:], [zt.h], [dh("XS")])
                if hp == 2:
                    conv3(w_in_d, WIN, "WIN", 8, 4640, True, cstart=2592)
                    conv3(w_oa_d, WOA, "WOA", 4, D, False)
                    conv3(w_ob_d, WOB, "WOB", 4, D, False)
                    conv3(w_out_d, WOUT, "WOUT", 8, D, False)
                for g in range(NG):
                    nkp = 2 * g + 2
                    if hp >= 2 and c3q and g >= 4:
                        c3q.pop(0)()
                    for kp in range(nkp):
                        pend.append(emit_pair(q_, k_, v_, sc, g, kp, nkp, odst, on, h))
                        if len(pend) > LA:
                            pend.pop(0)()
            while pend:
                pend.pop(0)()
            while c3q:
                c3q.pop(0)()
            Sx.barrier()
        if stop_after <= 3:
            return _finish(nc, Sx, out_d, S)

        with ExitStack() as st:
            sb, ps = mk(st)
            identf = sb("identf", [128, 128], F32)
            onesf = sb("onesf4", [128, 128], F32)
            onesb = sb("onesb4", [128, 128], BF16)
            triu = sb("triu", [128, 128], BF16)
            offs = sb("offs", [128, 32], F32)
            load(identf.t[:], c_identf, [identf.h])
            load(onesf.t[:], c_onesf, [onesf.h])
            load(onesb.t[:], c_onesb, [onesb.h])
            load(triu.t[:], c_triu, [triu.h])
            load(offs.t[:], c_offs, [offs.h])
            wg = sb("wgate", [128, 8, 2048], BF16, 4)
            woa = sb("woa", [128, 4, D], BF16)
            wob = sb("wob", [128, 4, D], BF16)
            wout = sb("wout", [128, 8, D], BF16)
            winv = WIN.rearrange("(k p) c -> p k c", p=128)
            load(woa.t[:], WOA.rearrange("(k p) c -> p k c", p=128), [woa.h], [dh("WOA")])
            for c0 in (0, 1024):
                load(wg.t[:, :, c0:c0 + 512], winv[:, :, 2592 + c0:2592 + c0 + 512], [wg.hs[c0 // 512]], [dh("WIN")])
            load(wob.t[:], WOB.rearrange("(k p) c -> p k c", p=128), [wob.h], [dh("WOB")])
            for c0 in (512, 1536):
                load(wg.t[:, :, c0:c0 + 512], winv[:, :, 2592 + c0:2592 + c0 + 512], [wg.hs[c0 // 512]], [dh("WIN")])
            for c0 in range(0, D, 512):
                load(wout.t[:, :, c0:c0 + 512], WOUT.rearrange("(k p) c -> p k c", p=128)[:, :, c0:c0 + 512], [wout.h], [dh("WOUT")])
            gf = sb("gf", [128, 8], F32)
            wr_s = sb("wr_s", [128, 8, 36], F32)
            wr = sb("wr", [128, 8, 36], F32)
            br = sb("br", [1, 36], F32)
            load(gf.t[:], ffn_norm_d, [gf.h])
            load(wr_s.t[:], w_r_d.rearrange("(k p) c -> p k c", p=128), [wr_s.h])
            load(br.t[:], b_r_d, [br.h])
            for k in range(8):
                op("dve", lambda e, k=k: e.tensor_scalar_mul(out=wr.t[:, k, :], in0=wr_s.t[:, k, :], scalar1=gf.t[:, k:k + 1]),
                   reads=[wr_s.h, gf.h], writes=[wr.h])
            run = sb("run", [128, 32], F32)
            op("dve", lambda e: e.memset(run.t[:], 0.0), writes=[run.h])

            hTg2 = [sb(f"hTg{i}", [128, 8, 512], BF16) for i in range(2)]
            oag2 = [sb(f"oag{i}", [128, 4, 512], BF16) for i in range(2)]
            obg2 = [sb(f"obg{i}", [128, 4, 512], BF16) for i in range(2)]
            sig = [sb(f"sig{i}", [128, 512], F32) for i in range(2)]
            m1 = [sb(f"m1_{i}", [128, 512], F32) for i in range(2)]
            mixT = sb("mixT", [128, 8, 512], BF16, 8)
            xtok = [sb(f"xtok{i}", [128, D], F32) for i in range(2)]
            x1 = [sb(f"x1_{i}", [128, D], F32) for i in range(3)]
            junk = sb("junk", [128, D], F32)
            pA = [ps(f"pA{i}", [128, 512], F32) for i in range(2)]
            pG = [ps(f"pG{i}", [128, 512], F32) for i in range(2)]
            pY = [ps(f"pY{i}", [128, 512], F32) for i in range(2)]
            pTr = ps("pTr", [128, 512], F32)
            pL = ps("pL", [128, 512], F32)
            hpC = H()
            HTv = HT.rearrange("(k p) s -> p k s", p=128)
            OAv = OA.rearrange("(k p) s -> p k s", p=128)
            OBv = OB.rearrange("(k p) s -> p k s", p=128)
            RS = []
            for i in range(4):
                RS.append(dict(
                    L=sb(f"L{i}", [128, 36], F32), gmax=sb(f"gmax{i}", [128, 1], F32), ngmax=sb(f"ngmax{i}", [128, 1], F32),
                    sume=sb(f"sume{i}", [128, 1], F32), pgrp=sb(f"pgrp{i}", [128, 1], F32), mx1=sb(f"mx1{i}", [128, 1], F32),
                    mx2=sb(f"mx2{i}", [128, 1], F32), dd=sb(f"dd{i}", [128, 1], F32), sg1=sb(f"sg1{i}", [128, 1], F32),
                    gone=sb(f"gone{i}", [128, 4], F32),
                    ein=sb(f"ein{i}", [128, 8], F32), ein2=sb(f"ein2{i}", [128, 8], F32), one1=sb(f"one1{i}", [128, 8], F32),
                    one2=sb(f"one2{i}", [128, 8], F32), ex4=sb(f"ex4{i}", [128, 4], F32), E1=sb(f"E1{i}", [128, 32], F32),
                    E2=sb(f"E2{i}", [128, 32], F32), Ab=sb(f"Ab{i}", [128, 32], BF16), pos=sb(f"pos{i}", [128, 32], F32),
                    tmpa=sb(f"tmpa{i}", [128, 32], F32), tmpb=sb(f"tmpb{i}", [128, 32], F32), slf=sb(f"slf{i}", [128, 2], F32),
                    ss1=sb(f"ss1{i}", [128, 2], F32), hnT=sb(f"hnT{i}", [128, 8, 128], F32)))
            hn4 = [sb(f"hn4{i}", [128, D], F32) for i in range(4)]
            hnb8 = [sb(f"hnb8{i}", [128, D], BF16) for i in range(8)]

            bgq = []

            def bg_run(n):
                for _ in range(n):
                    if bgq:
                        bgq.pop(0)()

            def stageA1(ti, tt):
                rows = slice(ti * 128, (ti + 1) * 128)
                xk, x1_ = xtok[ti % 2], x1[ti % 3]
                load(xk.t[:], x_d[rows, :], [xk.h])
                for half in range(2):
                    py = pY[half]
                    for k in range(8):
                        mm(py.t[:], mixT.t[:, k, tt * 128:(tt + 1) * 128], wout.t[:, k, half * 512:(half + 1) * 512], k == 0, k == 7,
                           [mixT.hs[k], wout.h], [py.h])
                    op("dve", lambda e, py=py, half=half: e.tensor_tensor(out=x1_.t[:, half * 512:(half + 1) * 512], in0=xk.t[:, half * 512:(half + 1) * 512], in1=py.t[:], op=ALU.add),
                       reads=[py.h, xk.h], writes=[x1_.h])

            def stageA2(ti, tt):
                rows = slice(ti * 128, (ti + 1) * 128)
                x1_, hn_, hnb_ = x1[ti % 3], hn4[ti % 4], hnb8[ti % 8]
                ss1_ = RS[ti % 4]["ss1"]
                op("sp", lambda e: e.dma_start(out=X1[rows, :], in_=x1_.t[:]), reads=[x1_.h], writes=[dh("X1")], dma=True)
                op("act", lambda e: e.activation(out=junk.t[:], in_=x1_.t[:], func=AF.Square, accum_out=ss1_.t[:, 0:1]),
                   reads=[x1_.h], writes=[junk.h, ss1_.h])
                op("act", lambda e: e.activation(out=ss1_.t[:, 1:2], in_=ss1_.t[:, 0:1], func=AF.Sqrt, scale=1.0 / D, bias=EPS), reads=[ss1_.h], writes=[ss1_.h])
                op("dve", lambda e: e.reciprocal(out=ss1_.t[:, 1:2], in_=ss1_.t[:, 1:2]), reads=[ss1_.h], writes=[ss1_.h])
                op("dve", lambda e: e.tensor_scalar_mul(out=hn_.t[:], in0=x1_.t[:], scalar1=ss1_.t[:, 1:2]), reads=[x1_.h, ss1_.h], writes=[hn_.h])
                op("act", lambda e: e.copy(out=hnb_.t[:], in_=hn_.t[:]), reads=[hn_.h], writes=[hnb_.h])

            def stageB4(g):
                tis = [g * 4 + tt for tt in range(4)]
                for ti in tis:
                    R = RS[ti % 4]
                    hn_, hnT_, L = hn4[ti % 4], R["hnT"], R["L"]
                    for k in range(8):
                        op("pe", lambda e, k=k, hn_=hn_: e.transpose(out=pTr.t[:, (k % 4) * 128:(k % 4 + 1) * 128], in_=hn_.t[:, k * 128:(k + 1) * 128], identity=identf.t[:]),
                           reads=[hn_.h, identf.h], writes=[pTr.h])
                        if k % 4 == 3:
                            kk = k // 4
                            op("act", lambda e, kk=kk, hnT_=hnT_: e.copy(out=hnT_.t[:, kk * 4:(kk + 1) * 4, :], in_=pTr.t[:].rearrange("p (a b) -> p a b", a=4)),
                               reads=[pTr.h], writes=[hnT_.h])
                for ti in tis:
                    R = RS[ti % 4]
                    hnT_, L = R["hnT"], R["L"]
                    c0 = (ti % 4) * 64
                    for k in range(8):
                        mm(pL.t[:, c0:c0 + 36], hnT_.t[:, k, :], wr.t[:, k, :], k == 0, False, [hnT_.h, wr.h], [pL.h])
                    mm(pL.t[:, c0:c0 + 36], onesf.t[0:1, :], br.t[0:1, :], False, True, [onesf.h, br.h], [pL.h])
                for ti in tis:
                    R = RS[ti % 4]
                    c0 = (ti % 4) * 64
                    op("act", lambda e, R=R, c0=c0: e.copy(out=R["L"].t[:], in_=pL.t[:, c0:c0 + 36]), reads=[pL.h], writes=[R["L"].h])

                def each(fn):
                    def step():
                        for ti in tis:
                            fn(ti, RS[ti % 4])
                    bgq.append(step)
                each(lambda ti, R: op("dve", lambda e: e.tensor_reduce(out=R["gmax"].t[:], in_=R["L"].t[:, 0:4], axis=AX.X, op=ALU.max), reads=[R["L"].h], writes=[R["gmax"].h]))
                each(lambda ti, R: op("dve", lambda e: e.tensor_scalar(out=R["gone"].t[:], in0=R["L"].t[:, 0:4], scalar1=R["gmax"].t[:, 0:1], scalar2=None, op0=ALU.is_equal), reads=[R["L"].h, R["gmax"].h], writes=[R["gone"].h]))
                each(lambda ti, R: op("dve", lambda e: e.tensor_scalar_mul(out=R["ngmax"].t[:], in0=R["gmax"].t[:], scalar1=-1.0), reads=[R["gmax"].h], writes=[R["ngmax"].h]))
                each(lambda ti, R: op("dve", lambda e: e.tensor_scalar_mul(out=R["ein"].t[:], in0=R["L"].t[:, 4:12], scalar1=R["gone"].t[:, 0:1]), reads=[R["L"].h, R["gone"].h], writes=[R["ein"].h]))
                each(lambda ti, R: op("act", lambda e: e.activation(out=R["ex4"].t[:], in_=R["L"].t[:, 0:4], func=AF.Exp, bias=R["ngmax"].t[:, 0:1], accum_out=R["sume"].t[:, 0:1]),
                                     reads=[R["L"].h, R["ngmax"].h], writes=[R["ex4"].h, R["sume"].h]))
                for gg in range(1, 4):
                    each(lambda ti, R, gg=gg: op("dve", lambda e: e.scalar_tensor_tensor(out=R["ein"].t[:], in0=R["L"].t[:, 4 + 8 * gg:12 + 8 * gg], scalar=R["gone"].t[:, gg:gg + 1], in1=R["ein"].t[:], op0=ALU.mult, op1=ALU.add),
                                                 reads=[R["L"].h, R["gone"].h, R["ein"].h], writes=[R["ein"].h]))
                each(lambda ti, R: op("dve", lambda e: e.tensor_reduce(out=R["mx1"].t[:], in_=R["ein"].t[:], axis=AX.X, op=ALU.max), reads=[R["ein"].h], writes=[R["mx1"].h]))
                each(lambda ti, R: op("dve", lambda e: e.tensor_scalar(out=R["one1"].t[:], in0=R["ein"].t[:], scalar1=R["mx1"].t[:, 0:1], scalar2=None, op0=ALU.is_equal), reads=[R["ein"].h, R["mx1"].h], writes=[R["one1"].h]))
                each(lambda ti, R: op("dve", lambda e: e.scalar_tensor_tensor(out=R["ein2"].t[:], in0=R["one1"].t[:], scalar=-1e30, in1=R["ein"].t[:], op0=ALU.mult, op1=ALU.add), reads=[R["one1"].h, R["ein"].h], writes=[R["ein2"].h]))
                each(lambda ti, R: op("dve", lambda e: e.tensor_reduce(out=R["mx2"].t[:], in_=R["ein2"].t[:], axis=AX.X, op=ALU.max), reads=[R["ein2"].h], writes=[R["mx2"].h]))
                each(lambda ti, R: op("dve", lambda e: e.tensor_scalar(out=R["one2"].t[:], in0=R["ein2"].t[:], scalar1=R["mx2"].t[:, 0:1], scalar2=None, op0=ALU.is_equal), reads=[R["ein2"].h, R["mx2"].h], writes=[R["one2"].h]))
                each(lambda ti, R: op("dve", lambda e: e.tensor_tensor(out=R["dd"].t[:], in0=R["mx1"].t[:], in1=R["mx2"].t[:], op=ALU.subtract), reads=[R["mx1"].h, R["mx2"].h], writes=[R["dd"].h]))
                each(lambda ti, R: op("dve", lambda e: e.reciprocal(out=R["pgrp"].t[:], in_=R["sume"].t[:]), reads=[R["sume"].h], writes=[R["pgrp"].h]))
                each(lambda ti, R: op("act", lambda e: e.activation(out=R["sg1"].t[:], in_=R["dd"].t[:], func=AF.Sigmoid), reads=[R["dd"].h], writes=[R["sg1"].h]))
                for gg in range(4):
                    each(lambda ti, R, gg=gg: op("dve", lambda e: e.tensor_scalar_mul(out=R["E1"].t[:, gg * 8:(gg + 1) * 8], in0=R["one1"].t[:], scalar1=R["gone"].t[:, gg:gg + 1]), reads=[R["one1"].h, R["gone"].h], writes=[R["E1"].h]))
                    each(lambda ti, R, gg=gg: op("dve", lambda e: e.tensor_scalar_mul(out=R["E2"].t[:, gg * 8:(gg + 1) * 8], in0=R["one2"].t[:], scalar1=R["gone"].t[:, gg:gg + 1]), reads=[R["one2"].h, R["gone"].h], writes=[R["E2"].h]))
                each(lambda ti, R: op("dve", lambda e: e.tensor_tensor(out=R["Ab"].t[:], in0=R["E1"].t[:], in1=R["E2"].t[:], op=ALU.add), reads=[R["E1"].h, R["E2"].h], writes=[R["Ab"].h]))
                each(lambda ti, R: op("dve", lambda e: e.tensor_tensor(out=wts.t[:, ti, 0:1], in0=R["sg1"].t[:], in1=R["pgrp"].t[:], op=ALU.mult), reads=[R["sg1"].h, R["pgrp"].h], writes=[wts.hs[ti % 4]]))
                each(lambda ti, R: op("dve", lambda e: e.tensor_tensor(out=wts.t[:, ti, 1:2], in0=R["pgrp"].t[:], in1=wts.t[:, ti, 0:1], op=ALU.subtract), reads=[R["pgrp"].h, wts.hs[ti % 4]], writes=[wts.hs[ti % 4]]))

            def stageC4(g):
                tis = [g * 4 + tt for tt in range(4)]
                for ti in tis:
                    R = RS[ti % 4]
                    c0 = (ti % 4) * 64
                    mm(pL.t[:, 256 + c0:256 + c0 + 32], triu.t[:], R["Ab"].t[:], True, True, [triu.h, R["Ab"].h], [hpC])
                    mm(pL.t[:, 256 + c0 + 32:256 + c0 + 64], onesb.t[:], R["Ab"].t[:], True, True, [onesb.h, R["Ab"].h], [hpC])
                for ti in tis:
                    R = RS[ti % 4]
                    c0 = (ti % 4) * 64
                    op("dve", lambda e, R=R, c0=c0: e.tensor_tensor(out=R["pos"].t[:], in0=pL.t[:, 256 + c0:256 + c0 + 32], in1=run.t[:], op=ALU.add), reads=[hpC, run.h], writes=[R["pos"].h])
                    op("dve", lambda e, c0=c0: e.tensor_tensor(out=run.t[:], in0=pL.t[:, 256 + c0 + 32:256 + c0 + 64], in1=run.t[:], op=ALU.add), reads=[hpC, run.h], writes=[run.h])

                def each(fn):
                    for ti in tis:
                        fn(ti, RS[ti % 4])
                each(lambda ti, R: op("dve", lambda e: e.tensor_tensor(out=R["pos"].t[:], in0=R["pos"].t[:], in1=offs.t[:], op=ALU.add), reads=[R["pos"].h, offs.h], writes=[R["pos"].h]))
                each(lambda ti, R: op("dve", lambda e: e.tensor_tensor(out=R["tmpa"].t[:], in0=R["pos"].t[:], in1=R["E1"].t[:], op=ALU.mult), reads=[R["pos"].h, R["E1"].h], writes=[R["tmpa"].h]))
                each(lambda ti, R: op("dve", lambda e: e.tensor_tensor(out=R["tmpb"].t[:], in0=R["pos"].t[:], in1=R["E2"].t[:], op=ALU.mult), reads=[R["pos"].h, R["E2"].h], writes=[R["tmpb"].h]))
                each(lambda ti, R: op("dve", lambda e: e.tensor_reduce(out=R["slf"].t[:, 0:1], in_=R["tmpa"].t[:], axis=AX.X, op=ALU.add), reads=[R["tmpa"].h], writes=[R["slf"].h]))
                each(lambda ti, R: op("dve", lambda e: e.tensor_reduce(out=R["slf"].t[:, 1:2], in_=R["tmpb"].t[:], axis=AX.X, op=ALU.add), reads=[R["tmpb"].h, R["slf"].h], writes=[R["slf"].h]))
                each(lambda ti, R: op("dve", lambda e: e.tensor_copy(out=slot_i.t[:, ti, :], in_=R["slf"].t[:]), reads=[R["slf"].h], writes=[slot_i.hs[ti % 4]]))
                for ti in tis:
                    hnb_ = hnb8[ti % 8]
                    for j in range(2):
                        op("pool", lambda e, j=j, ti=ti, hnb_=hnb_: e.indirect_dma_start(
                            out=XS, out_offset=bass.IndirectOffsetOnAxis(ap=slot_i.t[:, ti, j:j + 1], axis=0),
                            in_=hnb_.t[:], in_offset=None), reads=[hnb_.h, slot_i.hs[ti % 4]], writes=[dh("XS")], dma=True)

            ci = 0
            def p4_loads(g):
                tok = slice(g * 512, (g + 1) * 512)
                load(hTg2[g % 2].t[:], HTv[:, :, tok], [hTg2[g % 2].h], [dh("HT")])
                load(oag2[g % 2].t[:], OAv[:, :, tok], [oag2[g % 2].h], [dh("OA")])
                load(obg2[g % 2].t[:], OBv[:, :, tok], [obg2[g % 2].h], [dh("OB")])

            p4_loads(0)
            for g in range(NG):
                tok = slice(g * 512, (g + 1) * 512)
                if g + 1 < NG:
                    p4_loads(g + 1)
                hTg, oag, obg = hTg2[g % 2], oag2[g % 2], obg2[g % 2]
                it = 0
                for c in range(8):
                    for br_i, (og, wo, gc0) in enumerate(((oag, woa, 0), (obg, wob, 1024))):
                        if it == 3 and g >= 1:
                            stageB4(g - 1)
                        it += 1
                        pa, pg_ = pA[ci % 2], pG[ci % 2]
                        sg, mm1 = sig[ci % 2], m1[ci % 2]
                        ci += 1
                        for k in range(4):
                            mm(pa.t[:], wo.t[:, k, c * 128:(c + 1) * 128], og.t[:, k, :], k == 0, k == 3, [wo.h, og.h], [pa.h])
                        for k in range(8):
                            mm(pg_.t[:], wg.t[:, k, gc0 + c * 128: gc0 + (c + 1) * 128], hTg.t[:, k, :], k == 0, k == 7, [wg.hs[(gc0 + c * 128) // 512], hTg.h], [pg_.h])
                        op("act", lambda e, pg_=pg_, sg=sg: e.activation(out=sg.t[:], in_=pg_.t[:], func=AF.Sigmoid), reads=[pg_.h], writes=[sg.h])
                        if br_i == 0:
                            op("dve", lambda e, pa=pa, sg=sg, mm1=mm1: e.tensor_tensor(out=mm1.t[:], in0=sg.t[:], in1=pa.t[:], op=ALU.mult),
                               reads=[sg.h, pa.h], writes=[mm1.h])
                            prev = mm1
                        else:
                            op("dve", lambda e, pa=pa, sg=sg: e.tensor_tensor(out=sg.t[:], in0=sg.t[:], in1=pa.t[:], op=ALU.mult),
                               reads=[sg.h, pa.h], writes=[sg.h])
                            op("dve", lambda e, sg=sg, prev=prev, c=c: e.tensor_tensor(out=mixT.t[:, c, :], in0=sg.t[:], in1=prev.t[:], op=ALU.add),
                               reads=[sg.h, prev.h], writes=[mixT.hs[c]])
                        bg_run(3)
                bg_run(10 ** 6)
                if g >= 1:
                    stageC4(g - 1)
                for tt in range(4):
                    stageA1(g * 4 + tt, tt)
                    if tt >= 1:
                        stageA2(g * 4 + tt - 1, tt - 1)
                stageA2(g * 4 + 3, 3)
            stageB4(NG - 1)
            bg_run(10 ** 6)
            stageC4(NG - 1)
            Sx.barrier()
        if stop_after <= 4:
            return _finish(nc, Sx, out_d, S)

        with ExitStack() as st:
            sb, ps = mk(st)
            identb5x = sb("identb5", [128, 128], BF16)
            load(identb5x.t[:], c_identb, [identb5x.h])
            gf5v = sb("gf5", [128, 8], F32)
            load(gf5v.t[:], ffn_norm_d, [gf5v.h])
            NST = C // 128
            HALF = C // 2
            NSH = HALF // 128
            wgs = [sb(f"wgs{i}", [128, 8, 256], F32) for i in range(2)]
            wus = [sb(f"wus{i}", [128, 8, 256], F32) for i in range(2)]
            wds = [sb(f"wds{i}", [128, 2, D], F32) for i in range(2)]
            wgb = [sb(f"wgb{i}", [128, 8, 256], BF16) for i in range(2)]
            wub = [sb(f"wub{i}", [128, 8, 256], BF16) for i in range(2)]
            wdb = [sb(f"wdb{i}", [128, 2, D], BF16) for i in range(2)]
            xs = [sb(f"xs{i}", [128, D], BF16) for i in range(6)]
            xeT = [sb(f"xeT{i}", [128, 8, C], BF16) for i in range(2)]
            sil = [sb(f"sil{i}", [128, HALF], F32) for i in range(2)]
            actT = [sb(f"actT{i}", [128, 2, HALF], BF16, 2) for i in range(2)]
            ysb = [sb(f"ysb{i}", [128, D], BF16) for i in range(6)]
            ptx = [ps(f"ptx{i}", [128, 1024], BF16) for i in range(2)]
            pgu = [ps(f"pgu{i}", [128, 512], F32) for i in range(4)]
            pyy = [ps(f"pyy{i}", [128, 512], F32) for i in range(2)]
            xi = [0]
            yi = [0]

            def stage_load(ex):
                b = ex % 2
                load(wgs[b].t[:], w_g_d[ex].rearrange("(k p) f -> p k f", p=128), [wgs[b].h])
                load(wus[b].t[:], w_u_d[ex].rearrange("(k p) f -> p k f", p=128), [wus[b].h])
                load(wds[b].t[:], w_d_d[ex].rearrange("(k p) f -> p k f", p=128), [wds[b].h])

            def conv_part(ex, part):
                b = ex % 2
                for k in (2 * part, 2 * part + 1):
                    op("act", lambda e, k=k, b=b: e.activation(out=wgb[b].t[:, k, :], in_=wgs[b].t[:, k, :], func=AF.Copy, scale=gf5v.t[:, k:k + 1]),
                       reads=[wgs[b].h, gf5v.h], writes=[wgb[b].h])
                    op("dve", lambda e, k=k, b=b: e.tensor_scalar_mul(out=wub[b].t[:, k, :], in0=wus[b].t[:, k, :], scalar1=gf5v.t[:, k:k + 1]),
                       reads=[wus[b].h, gf5v.h], writes=[wub[b].h])
                if part == 3:
                    op("dve", lambda e, b=b: e.tensor_copy(out=wdb[b].t[:, 0, :], in_=wds[b].t[:, 0, :]), reads=[wds[b].h], writes=[wdb[b].h])
                    op("dve", lambda e, b=b: e.tensor_copy(out=wdb[b].t[:, 1, :], in_=wds[b].t[:, 1, :]), reads=[wds[b].h], writes=[wdb[b].h])

            def stage_a(ex):
                b = ex % 2
                xe = xeT[b]
                for stl in range(NST):
                    row0 = ex * C + stl * 128
                    xs_ = xs[xi[0] % 6]
                    px = ptx[xi[0] % 2]
                    xi[0] += 1
                    load(xs_.t[:], XS[row0:row0 + 128, :], [xs_.h], [dh("XS")])
                    for k in range(8):
                        op("pe", lambda e, k=k, xs_=xs_, px=px: e.transpose(out=px.t[:, k * 128:(k + 1) * 128], in_=xs_.t[:, k * 128:(k + 1) * 128], identity=identb5x.t[:]),
                           reads=[xs_.h, identb5x.h], writes=[px.h])
                    op("act", lambda e, px=px, xe=xe, stl=stl: e.copy(out=xe.t[:, :, stl * 128:(stl + 1) * 128], in_=px.t[:].rearrange("p (a b) -> p a b", a=8)),
                       reads=[px.h], writes=[xe.h])

            def gu(ex, hf, fc):
                b = ex % 2
                xe = xeT[b]
                cs = slice(hf * HALF, (hf + 1) * HALF)
                at = actT[hf]
                pgt, put = pgu[fc * 2], pgu[fc * 2 + 1]
                for k in range(8):
                    mm(pgt.t[:, 0:HALF], wgb[b].t[:, k, fc * 128:(fc + 1) * 128], xe.t[:, k, cs], k == 0, k == 7, [wgb[b].h, xe.h], [pgt.h])
                for k in range(8):
                    mm(put.t[:, 0:HALF], wub[b].t[:, k, fc * 128:(fc + 1) * 128], xe.t[:, k, cs], k == 0, k == 7, [wub[b].h, xe.h], [put.h])
                sl = sil[fc]
                op("act", lambda e: e.activation(out=sl.t[:], in_=pgt.t[:, 0:HALF], func=AF.Silu), reads=[pgt.h], writes=[sl.h])
                op("dve", lambda e: e.tensor_tensor(out=at.t[:, fc, :], in0=sl.t[:], in1=put.t[:, 0:HALF], op=ALU.mult),
                   reads=[sl.h, put.h], writes=[at.hs[fc]])

            def down(ex, hf):
                b = ex % 2
                at = actT[hf]
                for stl in range(NSH):
                    ys_ = ysb[yi[0] % 6]
                    yi[0] += 1
                    for half in range(2):
                        py = pyy[half]
                        for fc in range(2):
                            mm(py.t[:], at.t[:, fc, stl * 128:(stl + 1) * 128], wdb[b].t[:, fc, half * 512:(half + 1) * 512], fc == 0, fc == 1,
                               [at.hs[fc], wdb[b].h], [py.h])
                        if half == 0:
                            op("act", lambda e, py=py, ys_=ys_: e.copy(out=ys_.t[:, 0:512], in_=py.t[:]), reads=[py.h], writes=[ys_.h])
                        else:
                            op("dve", lambda e, py=py, ys_=ys_: e.tensor_copy(out=ys_.t[:, 512:1024], in_=py.t[:]), reads=[py.h], writes=[ys_.h])
                    row0 = ex * C + hf * HALF + stl * 128
                    store(YS[row0:row0 + 128, :], ys_.t[:], [ys_.h], [dh("YS")])

            stage_load(0)
            stage_load(1)
            for part in range(4):
                conv_part(0, part)
            stage_a(0)
            for ex in range(NE):
                nxt_ex = ex + 1 < NE
                if nxt_ex:
                    stage_a(ex + 1)
                if ex + 2 < NE:
                    stage_load(ex + 2)
                gu(ex, 0, 0)
                if nxt_ex:
                    conv_part(ex + 1, 0)
                gu(ex, 0, 1)
                if nxt_ex:
                    conv_part(ex + 1, 1)
                gu(ex, 1, 0)
                if nxt_ex:
                    conv_part(ex + 1, 2)
                gu(ex, 1, 1)
                if nxt_ex:
                    conv_part(ex + 1, 3)
                down(ex, 0)
                down(ex, 1)
            Sx.barrier()
        if stop_after <= 5:
            return _finish(nc, Sx, out_d, S)

        with ExitStack() as st:
            sb, ps = mk(st)
            onesf6v = sb("onesf6", [128, 128], F32)
            fn_row = sb("fn_row", [1, D], F32)
            GF = sb("GF", [128, D], F32)
            load(onesf6v.t[:], c_onesf, [onesf6v.h])
            load(fn_row.t[:], fnorm_d, [fn_row.h])
            pb = [ps(f"pb{i}", [128, 512], F32) for i in range(2)]
            for half in range(2):
                mm(pb[half].t[:], onesf6v.t[0:1, :], fn_row.t[0:1, half * 512:(half + 1) * 512], True, True, [onesf6v.h, fn_row.h], [pb[half].h])
                op("act", lambda e, half=half: e.copy(out=GF.t[:, half * 512:(half + 1) * 512], in_=pb[half].t[:]), reads=[pb[half].h], writes=[GF.h])
            NB6 = 3
            x1t = [sb(f"x1t{i}", [128, D], F32) for i in range(NB6)]
            y1 = [sb(f"y1_{i}", [128, D], BF16) for i in range(NB6)]
            y2 = [sb(f"y2_{i}", [128, D], BF16) for i in range(NB6)]
            junk6v = sb("junk6", [128, D], F32)
            ssf = [sb(f"ssf{i}", [128, 2], F32) for i in range(2)]
            ot = [sb(f"ot{i}", [128, D], F32) for i in range(2)]
            hout = H()

            def fetch(ti):
                b = ti % NB6
                rows = slice(ti * 128, (ti + 1) * 128)
                load(x1t[b].t[:], X1[rows, :], [x1t[b].h], [dh("X1")])
                for j, yy in enumerate((y1[b], y2[b])):
                    op("pool", lambda e, j=j, yy=yy: e.indirect_dma_start(
                        out=yy.t[:], out_offset=None, in_=YS,
                        in_offset=bass.IndirectOffsetOnAxis(ap=slot_i.t[:, ti, j:j + 1], axis=0)),
                       reads=[dh("YS"), slot_i.h], writes=[yy.h], dma=True)

            def compute(ti):
                b = ti % NB6
                rows = slice(ti * 128, (ti + 1) * 128)
                xx, ya, yb_ = x1t[b], y1[b], y2[b]
                op("dve", lambda e: e.scalar_tensor_tensor(out=xx.t[:], in0=ya.t[:], scalar=wts.t[:, ti, 0:1], in1=xx.t[:], op0=ALU.mult, op1=ALU.add),
                   reads=[ya.h, wts.h, xx.h], writes=[xx.h])
                op("dve", lambda e: e.scalar_tensor_tensor(out=xx.t[:], in0=yb_.t[:], scalar=wts.t[:, ti, 1:2], in1=xx.t[:], op0=ALU.mult, op1=ALU.add),
                   reads=[yb_.h, wts.h, xx.h], writes=[xx.h])
                sf = ssf[ti % 2]
                op("act", lambda e: e.activation(out=junk6v.t[:], in_=xx.t[:], func=AF.Square, accum_out=sf.t[:, 0:1]), reads=[xx.h], writes=[junk6v.h, sf.h])
                op("act", lambda e: e.activation(out=sf.t[:, 1:2], in_=sf.t[:, 0:1], func=AF.Sqrt, scale=1.0 / D, bias=EPS), reads=[sf.h], writes=[sf.h])
                op("dve", lambda e: e.reciprocal(out=sf.t[:, 1:2], in_=sf.t[:, 1:2]), reads=[sf.h], writes=[sf.h])
                o_ = ot[ti % 2]
                op("dve", lambda e: e.scalar_tensor_tensor(out=o_.t[:], in0=xx.t[:], scalar=sf.t[:, 1:2], in1=GF.t[:], op0=ALU.mult, op1=ALU.mult),
                   reads=[xx.h, sf.h, GF.h], writes=[o_.h])
                op("sp", lambda e: e.dma_start(out=out_d[rows, :], in_=o_.t[:]), reads=[o_.h], writes=[hout], dma=True)

            fetch(0)
            if NT > 1:
                fetch(1)
            for ti in range(NT):
                if ti + 2 < NT:
                    fetch(ti + 2)
                compute(ti)
            Sx.barrier()
        return _finish(nc, Sx, out_d, S)


def _finish(nc, Sx, out_d, S):
    Sx.barrier()
    Sx.emit_all()
    return nc


def _consts(S, C):
    bf = ml_dtypes.bfloat16
    c = {}
    c["c_identb"] = np.eye(128, dtype=np.float32).astype(bf)
    c["c_identf"] = np.eye(128, dtype=np.float32)
    c["c_onesb"] = np.ones((128, 128), np.float32).astype(bf)
    c["c_onesf"] = np.ones((128, 128), np.float32)
    pa = np.zeros((96, 96), np.float32)
    for m in range(96):
        if m < 64:
            k = m
        elif m < 80:
            k = m + 16
        else:
            k = m - 16
        pa[k, m] = 1.0
    c["c_perma"] = pa.astype(bf)
    pb = np.zeros((128, 128), np.float32)
    for m in range(128):
        j = m % 64
        if j < 8:
            k = m + 8
        elif j < 16:
            k = m - 8
        else:
            k = m
        pb[k, m] = 1.0
    c["c_permb"] = pb.astype(bf)
    pos = np.arange(S, dtype=np.float64)
    inv = ROPE_THETA ** (-np.arange(16, dtype=np.float64) / 16)
    ang = pos[None, :] * inv[:, None]
    tac = np.ones((96, S), np.float64)
    tas = np.zeros((96, S), np.float64)
    tac[64:80] = np.cos(ang)
    tac[80:96] = np.cos(ang)
    tas[64:80] = -np.sin(ang)
    tas[80:96] = np.sin(ang)
    c["c_tac"] = tac.astype(np.float32)
    c["c_tas"] = tas.astype(np.float32)
    inv = ROPE_THETA ** (-np.arange(8, dtype=np.float64) / 8)
    ang = pos[None, :] * inv[:, None]
    tbc = np.ones((128, S), np.float64)
    tbs = np.zeros((128, S), np.float64)
    for b0 in (0, 64):
        tbc[b0:b0 + 8] = np.cos(ang)
        tbc[b0 + 8:b0 + 16] = np.cos(ang)
        tbs[b0:b0 + 8] = -np.sin(ang)
        tbs[b0 + 8:b0 + 16] = np.sin(ang)
    c["c_tbc"] = tbc.astype(np.float32)
    c["c_tbs"] = tbs.astype(np.float32)
    m = np.zeros((128, 4, 512), np.float32)
    p = np.arange(128)[:, None]
    j = np.arange(512)[None, :]
    for r in range(4):
        m[:, r, :] = (r * 128 + p <= j)
    c["c_mask"] = m.astype(bf)
    oh = np.zeros((32, S), np.float32)
    for n in range(32):
        oh[n, n * 256:(n + 1) * 256] = 1.0
    c["c_oh"] = oh.astype(bf)
    c["c_wneg"] = np.concatenate([np.zeros((1, 32), np.float32), np.full((1, 1), 1e30, np.float32), np.full((1, 31), -1e30, np.float32)], axis=1).astype(bf)
    c["c_offs"] = np.tile((np.arange(32, dtype=np.float32) * C - 1.0)[None, :], (128, 1)).astype(np.float32)
    kk = np.arange(128)[:, None]
    mm_ = np.arange(128)[None, :]
    c["c_triu"] = (kk <= mm_).astype(np.float32).astype(bf)
    return c


def _prep_inputs(inputs, S, C):
    f = lambda a: np.ascontiguousarray(np.asarray(a, dtype=np.float32))
    w_ukv = f(inputs["w_ukv"])[0].reshape(256, 8, 128)
    w_ukv_kv = np.concatenate([w_ukv[:, :, :64].reshape(256, 512), w_ukv[:, :, 64:].reshape(256, 512)], axis=1)
    w_rg = f(inputs["w_router_group"])[0]
    w_re = f(inputs["w_router_expert"])[0]
    w_r = np.concatenate([w_rg] + [w_re[g] for g in range(4)], axis=1)
    b_r = np.concatenate([f(inputs["b_router_group"])[0], f(inputs["b_router_expert"])[0].reshape(32)])[None, :]
    shared = {
        "attn_norm": np.ascontiguousarray(f(inputs["attn_norm"])[0].reshape(8, 128).T),
        "w_in": f(inputs["w_in"])[0],
        "q_norm": np.ascontiguousarray(f(inputs["q_norm"])[0].reshape(6, 128).T),
        "w_uq": f(inputs["w_uq"])[0],
        "kv_norm": np.ascontiguousarray(f(inputs["kv_norm"])[0].reshape(2, 128).T),
        "w_ukv_kv": np.ascontiguousarray(w_ukv_kv),
        "w_o_mla": f(inputs["w_o_mla"])[0],
        "w_o_moba": f(inputs["w_o_moba"])[0],
        "w_out": f(inputs["w_out"])[0],
        "ffn_norm": np.ascontiguousarray(f(inputs["ffn_norm"])[0].reshape(8, 128).T),
        "w_r": np.ascontiguousarray(w_r),
        "b_r": np.ascontiguousarray(b_r),
        "w_exp_gate": f(inputs["w_exp_gate"])[0],
        "w_exp_up": f(inputs["w_exp_up"])[0],
        "w_exp_down": f(inputs["w_exp_down"])[0],
        "final_norm": f(inputs["final_norm"]).reshape(1, D),
    }
    shared.update(_consts(S, C))
    return shared


def kernel(**inputs):
    x = np.asarray(inputs["x"], dtype=np.float32)
    B, S, _ = x.shape
    C = 768 if S >= 8192 else max(256, (S * 2 // 32) * 3 // 128 * 128 + 128)
    nc = build(S=S, C=C)
    shared = _prep_inputs(inputs, S, C)
    in_maps = []
    for b in range(B):
        m = dict(shared)
        m["x"] = np.ascontiguousarray(x[b])
        m["xT"] = np.ascontiguousarray(x[b].T)
        in_maps.append(m)
    res = run_bass_kernel_spmd(nc, in_maps, core_ids=list(range(B)))
    return np.stack([r["out"] for r in res.results], axis=0)
```
